# Optimizing a Trainium2 kernel written in Bass

```python
import math
import jax, jax.numpy as jnp
from jax import lax
import numpy as np


D_MODEL = 1024
BATCH = 8
SEQ = 2048
DEPTH = 2

GRID_W = 64
CTX_LEN = 256
D_MIX = D_MODEL
MLSTM_HEADS = 4
MLSTM_WIDTH = D_MIX // 2
MLSTM_HD = MLSTM_WIDTH // MLSTM_HEADS
MLSTM_CHUNK = 64
SHORT_CONV = 3
N_GATES = 4 * MLSTM_HEADS
FOURIER_HEADS = 4
FOURIER_WIDTH = D_MIX // 4
FOURIER_HD = FOURIER_WIDTH // FOURIER_HEADS
CONV_WIDTH = D_MIX - MLSTM_WIDTH - FOURIER_WIDTH
CONV_K = 31
D_FF = 4 * D_MODEL
Q_OFF = 0
K_OFF = MLSTM_WIDTH
V_OFF = 2 * MLSTM_WIDTH
O_OFF = 3 * MLSTM_WIDTH
G_OFF = 4 * MLSTM_WIDTH
MLSTM_COLS = 4 * MLSTM_WIDTH + N_GATES
F_OFF = MLSTM_COLS
C_OFF = F_OFF + FOURIER_WIDTH
P_IN = C_OFF + 2 * CONV_WIDTH
EPS = 1e-6
POS_BASE = 10000.0

kernel_name = 'hybrid_mlstm_fourier_conv_prefix_block'


def rmsnorm(x, g):
    xf = x.astype(jnp.float32)
    y = xf * lax.rsqrt(jnp.mean(xf * xf, axis=-1, keepdims=True) + EPS)
    return (y * g.astype(jnp.float32)).astype(x.dtype)


def layernorm(x, g, b):
    xf = x.astype(jnp.float32)
    mu = jnp.mean(xf, axis=-1, keepdims=True)
    var = jnp.mean(jnp.square(xf - mu), axis=-1, keepdims=True)
    y = (xf - mu) * lax.rsqrt(var + EPS)
    return (y * g.astype(jnp.float32) + b.astype(jnp.float32)).astype(x.dtype)


def modulate(h, shift, scale):
    return h * (1 + scale) + shift


def grid_sincos(rows, d):
    quarter = d // 4
    freq = jnp.exp(-math.log(POS_BASE) * jnp.arange(quarter, dtype=jnp.float32) / quarter)
    r = jnp.broadcast_to(jnp.arange(rows, dtype=jnp.float32)[:, None], (rows, GRID_W)).reshape(-1)
    col = jnp.broadcast_to(jnp.arange(GRID_W, dtype=jnp.float32)[None, :], (rows, GRID_W)).reshape(-1)
    ar = r[:, None] * freq
    ac = col[:, None] * freq
    return jnp.concatenate([jnp.sin(ar), jnp.cos(ar), jnp.sin(ac), jnp.cos(ac)], axis=-1)


def dwconv(u, w):
    k = w.shape[0]
    ch = u.shape[-1]
    return lax.conv_general_dilated(u, w[:, None, :], window_strides=(1,), padding=[(k // 2, k // 2)],
                                    dimension_numbers=('NWC', 'WIO', 'NWC'), feature_group_count=ch)


def mlstm_chunked(q, k, v, li, lf, state):
    b_, h_, t_, hd = q.shape
    nc = t_ // MLSTM_CHUNK
    L = MLSTM_CHUNK

    def to_chunks(a):
        return jnp.moveaxis(a.reshape(a.shape[:2] + (nc, L) + a.shape[3:]), 2, 0)

    tri = jnp.tril(jnp.ones((L, L), dtype=bool))

    def step(carry, inp):
        C, n, m = carry
        qc, kc, vc, lic, lfc = inp
        bcum = jnp.cumsum(lfc, axis=-1)
        a = bcum + m[..., None]
        D = bcum[..., :, None] - bcum[..., None, :] + lic[..., None, :]
        D = jnp.where(tri, D, -jnp.inf)
        m_t = jnp.maximum(a, jnp.max(D, axis=-1))
        w_inter = jnp.exp(a - m_t)
        S = jnp.einsum('bhld,bhsd->bhls', qc, kc) * jnp.exp(D - m_t[..., None])
        num = w_inter[..., None] * jnp.einsum('bhed,bhld->bhle', C, qc) + jnp.einsum('bhls,bhse->bhle', S, vc)
        den = w_inter * jnp.einsum('bhd,bhld->bhl', n, qc) + jnp.sum(S, axis=-1)
        h = num / jnp.maximum(jnp.abs(den), jnp.exp(-m_t))[..., None]
        bl = bcum[..., -1]
        g = bl[..., None] - bcum + lic
        m_new = jnp.maximum(bl + m, jnp.max(g, axis=-1))
        decay = jnp.exp(bl + m - m_new)
        ws = jnp.exp(g - m_new[..., None])
        C_new = decay[..., None, None] * C + jnp.einsum('bhs,bhse,bhsd->bhed', ws, vc, kc)
        n_new = decay[..., None] * n + jnp.einsum('bhs,bhsd->bhd', ws, kc)
        return (C_new, n_new, m_new), h

    final, hs = lax.scan(step, state, (to_chunks(q), to_chunks(k), to_chunks(v), to_chunks(li), to_chunks(lf)))
    h = jnp.moveaxis(hs, 0, 2).reshape(b_, h_, t_, hd)
    return h, final


def mlstm_heads(z, w_qk_conv, b_gate):
    b_, t_, _ = z.shape
    qk = dwconv(z[..., Q_OFF:V_OFF], w_qk_conv)

    def heads(a):
        return a.reshape(b_, t_, MLSTM_HEADS, MLSTM_HD).transpose(0, 2, 1, 3).astype(jnp.float32)

    q = heads(qk[..., :MLSTM_WIDTH])
    k = heads(qk[..., MLSTM_WIDTH:]) * (MLSTM_HD ** -0.5)
    v = heads(z[..., V_OFF:O_OFF])
    gates = (z[..., G_OFF:G_OFF + N_GATES].reshape(b_, t_, 4, MLSTM_HEADS) + b_gate).astype(jnp.float32)
    gates = gates.transpose(2, 0, 3, 1)
    fwd = (gates[0], jax.nn.log_sigmoid(gates[1]))
    bwd = (gates[2], jax.nn.log_sigmoid(gates[3]))
    return q, k, v, fwd, bwd


def mlstm_out(h, o_pre, g):
    b_, h_, t_, hd = h.shape
    mu = jnp.mean(h, axis=-1, keepdims=True)
    var = jnp.mean(jnp.square(h - mu), axis=-1, keepdims=True)
    hn = ((h - mu) * lax.rsqrt(var + EPS)).transpose(0, 2, 1, 3).reshape(b_, t_, h_ * hd)
    return (hn * g.astype(jnp.float32) * jax.nn.sigmoid(o_pre.astype(jnp.float32))).astype(o_pre.dtype)


def mlstm_mixer(zx, zc, w_qk_conv, b_gate, g_norm, need_ctx):
    qx, kx, vx, fx, bx = mlstm_heads(zx, w_qk_conv, b_gate)
    qc, kc, vc, fc, bc = mlstm_heads(zc, w_qk_conv, b_gate)
    bsz = zx.shape[0]
    zero = (jnp.zeros((bsz, MLSTM_HEADS, MLSTM_HD, MLSTM_HD), jnp.float32),
            jnp.zeros((bsz, MLSTM_HEADS, MLSTM_HD), jnp.float32),
            jnp.zeros((bsz, MLSTM_HEADS), jnp.float32))

    def fl(a):
        return jnp.flip(a, axis=2)

    hc_f, st_f = mlstm_chunked(qc, kc, vc, fc[0], fc[1], zero)
    hx_f, _ = mlstm_chunked(qx, kx, vx, fx[0], fx[1], st_f)
    hc_b, st_b = mlstm_chunked(fl(qc), fl(kc), fl(vc), fl(bc[0]), fl(bc[1]), zero)
    hx_b, _ = mlstm_chunked(fl(qx), fl(kx), fl(vx), fl(bx[0]), fl(bx[1]), st_b)
    yx = mlstm_out(hx_f + fl(hx_b), zx[..., O_OFF:G_OFF], g_norm)
    yc = mlstm_out(hc_f + fl(hc_b), zc[..., O_OFF:G_OFF], g_norm) if need_ctx else None
    return yx, yc


def fourier_mix(u):
    b_, t_, _ = u.shape
    uh = u.reshape(b_, t_, FOURIER_HEADS, FOURIER_HD).astype(jnp.float32)
    y = jnp.real(jnp.fft.fftn(uh, axes=(1, 3), norm='ortho'))
    return y.reshape(b_, t_, FOURIER_WIDTH).astype(u.dtype)


def conv_module(z, w_dw, b_dw, g_ln, b_ln):
    a, gt = z[..., :CONV_WIDTH], z[..., CONV_WIDTH:]
    u = a * jax.nn.sigmoid(gt)
    u = dwconv(u, w_dw) + b_dw
    return jax.nn.silu(layernorm(u, g_ln, b_ln))


def token_mixers(hx, hc, w_in, w_out, b_gate, w_qk_conv, g_mlstm_norm, w_dw, b_dw, g_conv_ln, b_conv_ln, need_ctx):
    zx = hx @ w_in
    zc = hc @ (w_in if need_ctx else w_in[:, :MLSTM_COLS])
    ym_x, ym_c = mlstm_mixer(zx, zc, w_qk_conv, b_gate, g_mlstm_norm, need_ctx)

    def merge(z, ym):
        yf = fourier_mix(z[..., F_OFF:C_OFF])
        yv = conv_module(z[..., C_OFF:P_IN], w_dw, b_dw, g_conv_ln, b_conv_ln)
        return jnp.concatenate([ym, yf, yv], axis=-1) @ w_out

    out_x = merge(zx, ym_x)
    out_c = merge(zc, ym_c) if need_ctx else None
    return out_x, out_c


def sq_relu_mlp(h, w1, w2):
    return jnp.square(jax.nn.relu(h @ w1)) @ w2


def setup_inputs(seed: int = 0) -> dict:
    key = jax.random.key(seed)
    ks = jax.random.split(key, 24)
    f32 = jnp.float32
    L = DEPTH

    def nrm(k, shape, scale):
        return jax.random.normal(k, shape, f32) * scale

    def gain(k, shape):
        return 1.0 + nrm(k, shape, 0.05)

    return {
        'x': nrm(ks[0], (BATCH, SEQ, D_MODEL), 1.0),
        'c': nrm(ks[1], (BATCH, D_MODEL), 1.0),
        'ctx': nrm(ks[2], (BATCH, CTX_LEN, D_MODEL), 1.0),
        'c_ctx': nrm(ks[3], (D_MODEL,), 1.0),
        'w_ada': nrm(ks[4], (L, D_MODEL, 6 * D_MODEL), D_MODEL ** -0.5),
        'b_ada': nrm(ks[5], (L, 6 * D_MODEL), 0.02),
        'g_pre_mix': gain(ks[6], (L, D_MODEL)),
        'g_post_mix': gain(ks[7], (L, D_MODEL)),
        'g_pre_mlp': gain(ks[8], (L, D_MODEL)),
        'g_post_mlp': gain(ks[9], (L, D_MODEL)),
        'w_in': nrm(ks[10], (L, D_MODEL, P_IN), D_MODEL ** -0.5),
        'b_gate': jnp.array([0.0, 3.0, 0.0, 3.0], f32)[None, :, None] + nrm(ks[11], (L, 4, MLSTM_HEADS), 0.1),
        'w_qk_conv': nrm(ks[12], (L, SHORT_CONV, 2 * MLSTM_WIDTH), SHORT_CONV ** -0.5),
        'g_mlstm_norm': gain(ks[13], (L, MLSTM_WIDTH)),
        'w_dw': nrm(ks[14], (L, CONV_K, CONV_WIDTH), CONV_K ** -0.5),
        'b_dw': nrm(ks[15], (L, CONV_WIDTH), 0.02),
        'g_conv_ln': gain(ks[16], (L, CONV_WIDTH)),
        'b_conv_ln': nrm(ks[17], (L, CONV_WIDTH), 0.02),
        'w_out': nrm(ks[18], (L, D_MIX, D_MODEL), D_MIX ** -0.5),
        'w_mlp1': nrm(ks[19], (L, D_MODEL, D_FF), D_MODEL ** -0.5),
        'w_mlp2': nrm(ks[20], (L, D_FF, D_MODEL), D_FF ** -0.5),
    }


def reference(x, c, ctx, c_ctx, w_ada, b_ada, g_pre_mix, g_post_mix, g_pre_mlp, g_post_mlp, w_in, b_gate,
              w_qk_conv, g_mlstm_norm, w_dw, b_dw, g_conv_ln, b_conv_ln, w_out, w_mlp1, w_mlp2):
    n_tok = x.shape[1]
    rows = n_tok // GRID_W
    x = x + grid_sincos(rows, x.shape[-1]).astype(x.dtype)[None]
    for l in range(DEPTH):
        need_ctx = l < DEPTH - 1
        mx = jnp.split((jax.nn.silu(c) @ w_ada[l] + b_ada[l])[:, None, :], 6, axis=-1)
        mc = jnp.split((jax.nn.silu(c_ctx) @ w_ada[l] + b_ada[l])[None, None, :], 6, axis=-1)
        hx = modulate(rmsnorm(x, g_pre_mix[l]), mx[0], mx[1])
        hc = modulate(rmsnorm(ctx, g_pre_mix[l]), mc[0], mc[1])
        out_x, out_c = token_mixers(hx, hc, w_in[l], w_out[l], b_gate[l], w_qk_conv[l], g_mlstm_norm[l],
                                    w_dw[l], b_dw[l], g_conv_ln[l], b_conv_ln[l], need_ctx)
        x = x + mx[2] * rmsnorm(out_x, g_post_mix[l])
        hx = modulate(rmsnorm(x, g_pre_mlp[l]), mx[3], mx[4])
        x = x + mx[5] * rmsnorm(sq_relu_mlp(hx, w_mlp1[l], w_mlp2[l]), g_post_mlp[l])
        if need_ctx:
            ctx = ctx + mc[2] * rmsnorm(out_c, g_post_mix[l])
            hc = modulate(rmsnorm(ctx, g_pre_mlp[l]), mc[3], mc[4])
            ctx = ctx + mc[5] * rmsnorm(sq_relu_mlp(hc, w_mlp1[l], w_mlp2[l]), g_post_mlp[l])
    return x
```

```python
import math
from contextlib import ExitStack

import numpy as np
import ml_dtypes

import concourse.bass as bass
import concourse.mybir as mybir
from concourse.bass_utils import run_bass_kernel_spmd

F32 = mybir.dt.float32
BF16 = mybir.dt.bfloat16
AF = mybir.ActivationFunctionType
ALU = mybir.AluOpType
AX = mybir.AxisListType

ENGS = ("pe", "act", "dve", "pool", "sp")
SEM_LIM = 2000
N_DMA_SEMS = 16


class Res:
    __slots__ = ("name", "w", "r", "excl")

    def __init__(self, name, excl=False):
        self.name = name
        self.w = None
        self.r = []
        self.excl = excl


class Op:
    __slots__ = ("eng", "idx", "fn", "deps", "dma", "signal", "semref", "dma_slot", "dma_val")

    def __init__(self, eng, idx, fn, dma):
        self.eng = eng
        self.idx = idx
        self.fn = fn
        self.deps = []
        self.dma = dma
        self.signal = False
        self.semref = None
        self.dma_slot = None
        self.dma_val = None


class Prog:
    def __init__(self, nc, same_engine_sync=True):
        self.nc = nc
        self.ops = {e: [] for e in ENGS}
        self.n_dma_q = {}
        self.same_engine_sync = same_engine_sync

    def res(self, name="", excl=False):
        return Res(name, excl)

    def op(self, eng, fn, reads=(), writes=(), dma=False):
        o = Op(eng, len(self.ops[eng]), fn, dma)
        if dma:
            half = N_DMA_SEMS // 2
            k = self.n_dma_q.get(eng, 0)
            self.n_dma_q[eng] = k + 1
            o.dma_slot = (k % half) + (half if eng == "pool" else 0)
            o.dma_val = 16 * (k // half + 1)
        deps = []
        for r in reads:
            if r.w is not None:
                deps.append(r.w)
            if r.excl:
                deps.extend(x for x in r.r if x.eng != eng)
        for r in writes:
            if r.w is not None:
                deps.append(r.w)
            deps.extend(r.r)
        for r in reads:
            r.r.append(o)
        for r in writes:
            r.w = o
            r.r = []
        seen = set()
        for d in deps:
            if d is o or id(d) in seen:
                continue
            seen.add(id(d))
            if (not d.dma) and (not dma) and d.eng == eng:
                if eng == "pe" or not self.same_engine_sync:
                    continue
            o.deps.append(d)
        self.ops[eng].append(o)
        return o

    def barrier(self):
        targets = []
        for e in ENGS:
            cs = [o for o in self.ops[e] if not o.dma and o.fn is not None]
            if cs:
                targets.append(cs[-1])
        by_slot = {}
        for e in ENGS:
            for o in self.ops[e]:
                if o.dma and (o.dma_slot not in by_slot or o.dma_val > by_slot[o.dma_slot].dma_val):
                    by_slot[o.dma_slot] = o
        targets += list(by_slot.values())
        for e in ENGS:
            o = Op(e, len(self.ops[e]), None, False)
            o.deps = list(targets) if e != "pe" else [t for t in targets if t.dma or t.eng != e]
            self.ops[e].append(o)

    def emit(self):
        nc = self.nc
        for e in ENGS:
            for o in self.ops[e]:
                for d in o.deps:
                    d.signal = True
        with ExitStack() as st:
            dma_sems = [st.enter_context(nc.semaphore(f"dq{i}")) for i in range(N_DMA_SEMS)]
            for e in ENGS:
                n = sum(1 for o in self.ops[e] if o.signal and not o.dma)
                k = max((n + SEM_LIM - 1) // SEM_LIM, 1)
                sems = [st.enter_context(nc.semaphore(f"s_{e}{i}")) for i in range(k)]
                c = 0
                for o in self.ops[e]:
                    if o.signal and not o.dma:
                        o.semref = (sems[c // SEM_LIM], c % SEM_LIM + 1, c)
                        c += 1
            block = st.enter_context(nc.Block())

            def run(e, eng):
                waited_c = {p: -1 for p in ENGS}
                waited_d = [0] * N_DMA_SEMS
                for o in self.ops[e]:
                    for d in o.deps:
                        if d.dma:
                            if waited_d[d.dma_slot] >= d.dma_val:
                                continue
                            waited_d[d.dma_slot] = d.dma_val
                            eng.wait_ge(dma_sems[d.dma_slot], d.dma_val)
                        else:
                            sem, val, gc = d.semref
                            if waited_c[d.eng] >= gc:
                                continue
                            waited_c[d.eng] = gc
                            eng.wait_ge(sem, val)
                    if o.fn is None:
                        continue
                    ins = o.fn(eng)
                    if o.dma:
                        ins.then_inc(dma_sems[o.dma_slot], 16)
                    elif o.signal:
                        ins.then_inc(o.semref[0], 1)

            fin_dma = {}
            for e in ENGS:
                for o in self.ops[e]:
                    if o.dma:
                        fin_dma[o.dma_slot] = max(fin_dma.get(o.dma_slot, 0), o.dma_val)

            @block.tensor
            def _(eng):
                run("pe", eng)

            @block.scalar
            def _(eng):
                run("act", eng)

            @block.vector
            def _(eng):
                run("dve", eng)

            @block.gpsimd
            def _(eng):
                run("pool", eng)

            @block.sync
            def _(eng):
                run("sp", eng)
                for slot, val in sorted(fin_dma.items()):
                    eng.wait_ge(dma_sems[slot], val)


D = 1024
NB = 8
SEQ = 2048
CTX = 256
NT = CTX + SEQ
DEPTH = 2
KC = 8
NJ = NT // 128
HD = 128
NH = 4
DFF = 4096
EPS = 1e-6
CONV_K = 31
PADC = 15
Q_OFF, K_OFF, V_OFF, O_OFF, G_OFF = 0, 512, 1024, 1536, 2048
F_OFF = 2064
CA_OFF = 2320
CG_OFF = 2576
UPW = (CTX + 2 * PADC) + (SEQ + 2 * PADC)


def tiles_for(need_ctx):
    t = [(256, 768, 0), (768, 1280, 0), (1280, 1792, 0), (1792, 2304, 0)]
    if need_ctx:
        t = [(0, 256, 1)] + t
    return t


class _Stop(Exception):
    pass


def build_program(dbg=False, stop=None):
    nc = bass.Bass("TRN2", target_bir_lowering=False)
    P = Prog(nc)

    def din(name, shape, dt=F32):
        return nc.dram_tensor(name, list(shape), dt, kind="ExternalInput").ap()

    def dout(name, shape, dt=F32):
        return nc.dram_tensor(name, list(shape), dt, kind="ExternalOutput").ap()

    xT = din("xT", [KC, 128, SEQ])
    ctxT = din("ctxT", [KC, 128, CTX])
    posT = din("posT", [KC, 128, SEQ])
    cvec = din("cvec", [128, KC, 2])
    wada = din("wada", [DEPTH, 12, 128, KC, 512])
    bada = din("bada", [DEPTH, 128, 48])
    gvec = din("gvec", [DEPTH, 128, 4, KC])
    winc = din("winc", [DEPTH, 14, 128, KC, 128])
    wvo = din("wvo", [DEPTH, NH, 128, KC, 256])
    wgate = din("wgate", [DEPTH, 128, KC, 16])
    bgate = din("bgate", [DEPTH, 16])
    wqkc = din("wqkc", [DEPTH, 8, 3, 128])
    gmn = din("gmn", [DEPTH, 512])
    wdw = din("wdw", [DEPTH, 128, 2, CONV_K])
    cvp = din("cvp", [DEPTH, 128, 3, 2])
    woutc = din("woutc", [DEPTH, 128, KC, KC, 128])
    w1c = din("w1c", [DEPTH, 16, 128, 2, KC, 128])
    w2c = din("w2c", [DEPTH, KC, 128, 32, 128])
    ident_d = din("ident", [128, 128])
    umask_d = din("umask", [128, 128])
    lmask_d = din("lmask", [128, 128])
    cs64_d = din("cs64", [128, 256])
    dftx_d = din("dftx", [4, 4, 128, 2, 4, 512], BF16)
    dftc_d = din("dftc", [128, 2, 2, 256], BF16)
    outT = dout("outT", [KC, 128, SEQ])
    dbg_out = {}
    if dbg:
        for nm in ("d_hx", "d_ym", "d_x1", "d_x2"):
            dbg_out[nm] = dout(nm, [KC, 128, NT])

    with ExitStack() as st:
        def sb(name, shape, dt):
            return st.enter_context(nc.sbuf_tensor(name, list(shape), dt))

        xs = sb("xs", [128, KC, NT], F32)
        hx = sb("hx", [128, KC, NT], BF16)
        SCR_ARENA = 25088
        SCR_N = SCR_ARENA + KC * NT
        scr = sb("scr", [128, SCR_N], BF16)
        ym_flat = scr[:, SCR_ARENA:SCR_N]
        ym = ym_flat.rearrange("p (c t) -> p c t", t=NT)
        ident_f = sb("ident_f", [128, 128], F32)
        ident_b = sb("ident_b", [128, 128], BF16)
        umask_f = sb("umask_f", [128, 128], F32)
        lmask_f = sb("lmask_f", [128, 128], F32)
        negm_f = sb("negm_f", [128, 128], BF16)
        negm_b = sb("negm_b", [128, 128], BF16)
        ones_f = sb("ones_f", [128, 128], F32)
        ones_b = sb("ones_b", [128, 128], BF16)
        cs64 = sb("cs64b", [128, 256], BF16)
        cst = sb("cst", [128, 4], F32)
        cv_f = sb("cv_f", [128, KC, 2], F32)
        sc_b = sb("sc_b", [128, KC, 2], BF16)
        mod = sb("mod", [128, DEPTH, 48, 2], F32)
        bada_s = sb("bada_s", [128, DEPTH, 48], F32)
        gv = sb("gv", [128, DEPTH, 4, KC], F32)
        A1 = sb("A1", [128, KC, 2], F32)
        A2 = sb("A2", [128, KC, 2], F32)
        G1 = sb("G1", [128, KC, 2], F32)
        G2 = sb("G2", [128, KC, 2], F32)
        rs = sb("rs", [128, 512], F32)
        sqb = [sb(f"sqb{i}", [128, 512], BF16) for i in range(2)]
        tmpf = [sb(f"tmpf{i}", [128, 512], F32) for i in range(2)]
        psb = [st.enter_context(nc.psum_tensor(f"ps{i}", [128, 512], F32)) for i in range(8)]

        R = P.res
        r_xs = [[R() for _ in range(NJ)] for _ in range(KC)]
        r_hx = [[R() for _ in range(NJ)] for _ in range(KC)]
        r_ym = [[R() for _ in range(NJ)] for _ in range(KC)]
        r_ps = [P.res("ps", excl=True) for _ in range(8)]
        r_const = R()
        r_mod = R()
        r_lay = R()
        r_rs = R()
        r_sqb = [R(), R()]
        r_tmpf = [R(), R()]
        r_arena = R()

        def trs(rl, kc, a, b):
            return [rl[kc][j] for j in range(a // 128, (b + 127) // 128)]

        def trs_all(rl, a, b):
            out = []
            for kc in range(KC):
                out += trs(rl, kc, a, b)
            return out

        cnt = {"ps": 0, "sq": 0, "tf": 0}

        ps_reserved = set()

        def next_ps():
            while True:
                i = cnt["ps"] % 8
                cnt["ps"] += 1
                if i not in ps_reserved:
                    return i

        def next_sq():
            i = cnt["sq"] % 2
            cnt["sq"] += 1
            return i

        def next_tf():
            i = cnt["tf"] % 2
            cnt["tf"] += 1
            return i

        def MM(out, lhsT, rhs, start, stop, reads, writes):
            P.op("pe", lambda e: e.matmul(out, lhsT=lhsT, rhs=rhs, start=start, stop=stop), reads, writes)

        def TR(out, in_, ident, reads, writes):
            P.op("pe", lambda e: e.transpose(out, in_, ident), reads, writes)

        def ACT(out, in_, func, reads, writes, bias=None, scale=None, accum=None):
            kw = {}
            if bias is not None:
                kw["bias"] = bias
            if scale is not None:
                kw["scale"] = scale
            if accum is not None:
                kw["accum_out"] = accum
            P.op("act", lambda e: e.activation(out=out, in_=in_, func=func, **kw), reads, writes)

        def TT(eng, out, in0, in1, op, reads, writes):
            P.op(eng, lambda e: e.tensor_tensor(out=out, in0=in0, in1=in1, op=op), reads, writes)

        def TS(eng, out, in0, s1, s2, op0, op1, reads, writes):
            if s2 is None:
                P.op(eng, lambda e: e.tensor_scalar(out=out, in0=in0, scalar1=s1, scalar2=None, op0=op0), reads, writes)
            else:
                P.op(eng, lambda e: e.tensor_scalar(out=out, in0=in0, scalar1=s1, scalar2=s2, op0=op0, op1=op1), reads, writes)

        def STT(eng, out, in0, scalar, in1, op0, op1, reads, writes):
            P.op(eng, lambda e: e.scalar_tensor_tensor(out=out, in0=in0, scalar=scalar, in1=in1, op0=op0, op1=op1), reads, writes)

        def CP(eng, out, in_, reads, writes):
            P.op(eng, lambda e: e.tensor_copy(out=out, in_=in_), reads, writes)

        def MS(eng, ap, val, writes):
            P.op(eng, lambda e: e.memset(ap, val), (), writes)

        def RECIP(out, in_, reads, writes):
            P.op("dve", lambda e: e.reciprocal(out=out, in_=in_), reads, writes)

        def DMA(q, out, in_, reads, writes):
            P.op(q, lambda e: e.dma_start(out=out, in_=in_), reads, writes, dma=True)

        def arena(off, n, dt=BF16):
            if dt == BF16:
                return scr[:, off:off + n]
            assert off % 2 == 0
            return scr[:, off:off + 2 * n].bitcast(F32)

        DMA("sp", ident_f[:], ident_d, [], [r_const])
        DMA("sp", umask_f[:], umask_d, [], [r_const])
        DMA("sp", lmask_f[:], lmask_d, [], [r_const])
        DMA("pool", cs64[:], cs64_d, [], [r_const])
        DMA("sp", cv_f[:], cvec, [], [r_const])
        DMA("sp", bada_s[:], bada.rearrange("l p n -> p l n"), [], [r_const])
        DMA("sp", gv[:], gvec.rearrange("l p a k -> p l a k"), [], [r_const])
        CP("dve", ident_b[:], ident_f[:], [r_const], [r_const])
        TS("dve", negm_f[:], umask_f[:], -1.0, 30000.0, ALU.add, ALU.mult, [r_const], [r_const])
        TS("dve", negm_b[:], lmask_f[:], -1.0, 30000.0, ALU.add, ALU.mult, [r_const], [r_const])
        MS("dve", ones_f[:], 1.0, [r_const])
        MS("dve", ones_b[:], 1.0, [r_const])
        MS("dve", cst[:, 0:1], 1024.0 * EPS, [r_const])
        MS("dve", cst[:, 1:2], EPS, [r_const])
        MS("dve", cst[:, 2:3], 1.0, [r_const])
        MS("dve", cst[:, 3:4], 0.0, [r_const])

        for kc in range(KC):
            DMA("sp", xs[:, kc, CTX:NT], xT[kc], [], trs(r_xs, kc, CTX, NT))
            DMA("sp", xs[:, kc, 0:CTX], ctxT[kc], [], trs(r_xs, kc, 0, CTX))
        pos_buf = [arena(0, SEQ, F32), arena(2 * SEQ, SEQ, F32)]
        r_pos = [R(), R()]
        for kc in range(KC):
            i = kc % 2
            DMA("sp", pos_buf[i], posT[kc], [], [r_pos[i]])
            TT("dve", xs[:, kc, CTX:NT], xs[:, kc, CTX:NT], pos_buf[i], ALU.add,
               [r_pos[i]] + trs(r_xs, kc, CTX, NT), trs(r_xs, kc, CTX, NT))

        ACT(sc_b[:], cv_f[:], AF.Silu, [r_const], [r_const])
        wa_off = 4 * SEQ
        wa_buf = [arena(wa_off + i * KC * 512, KC * 512).rearrange("p (k c) -> p k c", c=512) for i in range(2)]
        r_wa = [R(), R()]
        for l in range(DEPTH):
            pi = next_ps()
            pv = psb[pi][:, 0:96].rearrange("p (n w) -> p n w", w=2)
            for blk in range(12):
                i = (l * 12 + blk) % 2
                DMA("pool", wa_buf[i], wada[l, blk], [], [r_wa[i]])
                for n4 in range(4):
                    n = blk * 4 + n4
                    for kc in range(KC):
                        MM(pv[:, n, :], wa_buf[i][:, kc, n4 * 128:(n4 + 1) * 128], sc_b[:, kc, :], kc == 0, kc == KC - 1,
                           [r_wa[i], r_const], [r_ps[pi]])
            TT("dve", mod[:, l], pv, bada_s[:, l].unsqueeze(2).to_broadcast([128, 48, 2]), ALU.add,
               [r_ps[pi], r_const], [r_mod])

        def rstd_of(src_fn, n, reads):
            pi = next_ps()
            for kc in range(KC):
                si = next_sq()
                ACT(sqb[si][:, 0:n], src_fn(kc), AF.Square, reads(kc), [r_sqb[si]])
                MM(psb[pi][:, 0:n], ones_b[:], sqb[si][:, 0:n], kc == 0, kc == KC - 1, [r_sqb[si], r_const], [r_ps[pi]])
            ACT(rs[:, 0:n], psb[pi][:, 0:n], AF.Sqrt, [r_ps[pi], r_const], [r_rs], bias=cst[:, 0:1], scale=1.0)
            RECIP(rs[:, 0:n], rs[:, 0:n], [r_rs], [r_rs])

        def prenorm(tl, Amod, shift_base, l):
            for (a, b, w) in tl:
                n = b - a
                rstd_of(lambda kc: xs[:, kc, a:b], n, lambda kc: trs(r_xs, kc, a, b))
                for kc in range(KC):
                    ti = next_tf()
                    TT("dve", tmpf[ti][:, 0:n], xs[:, kc, a:b], rs[:, 0:n], ALU.mult,
                       trs(r_xs, kc, a, b) + [r_rs], [r_tmpf[ti]])
                    ACT(hx[:, kc, a:b], tmpf[ti][:, 0:n], AF.Identity, [r_tmpf[ti], r_lay, r_mod], trs(r_hx, kc, a, b),
                        bias=mod[:, l, shift_base + kc, w:w + 1], scale=Amod[:, kc, w:w + 1])

        def resid_update(tl, src_fn, src_reads, Gm):
            for (a, b, w) in tl:
                n = b - a
                rstd_of(lambda kc: src_fn(kc, a, b), n, lambda kc: src_reads(kc, a, b))
                for kc in range(KC):
                    ti = next_tf()
                    TT("dve", tmpf[ti][:, 0:n], src_fn(kc, a, b), rs[:, 0:n], ALU.mult,
                       src_reads(kc, a, b) + [r_rs], [r_tmpf[ti]])
                    STT("dve", xs[:, kc, a:b], tmpf[ti][:, 0:n], Gm[:, kc, w:w + 1], xs[:, kc, a:b], ALU.mult, ALU.add,
                        [r_tmpf[ti], r_lay] + trs(r_xs, kc, a, b), trs(r_xs, kc, a, b))

        def dump(name, src_fn, reads_fn):
            if not dbg:
                return
            for kc in range(KC):
                DMA("pool", dbg_out[name][kc], src_fn(kc), reads_fn(kc), [])

        def maybe_stop(k):
            if stop is not None and stop == k:
                raise _Stop()

        try:
            P.barrier()
            for l in range(DEPTH):
                need_ctx = l < DEPTH - 1
                TL_all = tiles_for(True)
                TL = tiles_for(need_ctx)
                j0 = 0 if need_ctx else 2

                def mk(dst, gidx, mbase, plus1):
                    for w in range(2):
                        if plus1:
                            STT("dve", dst[:, :, w], mod[:, l, mbase:mbase + KC, w], 1.0, gv[:, l, gidx, :], ALU.add, ALU.mult,
                                [r_mod, r_const, r_lay], [r_lay])
                        else:
                            TT("dve", dst[:, :, w], mod[:, l, mbase:mbase + KC, w], gv[:, l, gidx, :], ALU.mult,
                               [r_mod, r_const, r_lay], [r_lay])
                    TS("dve", dst[:], dst[:], 32.0, None, ALU.mult, None, [r_lay], [r_lay])

                mk(A1, 0, 8, True)
                mk(G1, 1, 16, False)
                mk(A2, 2, 32, True)
                mk(G2, 3, 40, False)

                prenorm(TL_all, A1, 0, l)
                maybe_stop(10 * l + 1)
                if l == 0:
                    dump("d_hx", lambda kc: hx[:, kc, :], lambda kc: trs(r_hx, kc, 0, NT))

                o_w = 0
                wbuf = [arena(o_w + i * 1024, 1024).rearrange("p (k c) -> p k c", c=128) for i in range(4)]
                r_wbuf = [R() for _ in range(4)]
                wcnt = {"n": 0}

                def load_w(src):
                    i = wcnt["n"] % 4
                    wcnt["n"] += 1
                    DMA("pool", wbuf[i], src, [], [r_wbuf[i]])
                    return i

                o_up = 4096
                upad = arena(o_up, 2 * UPW).rearrange("p (c t) -> p c t", t=UPW)
                r_up = R()
                o_vb = o_up + 2 * UPW
                vbuf = arena(o_vb, 2 * NT, F32).rearrange("p (c t) -> p c t", t=NT)
                r_vb = [R(), R()]
                o_cs = o_vb + 4 * NT
                wdw_s = arena(o_cs, 2 * CONV_K, F32).rearrange("p (c k) -> p c k", k=CONV_K)
                cvp_s = arena(o_cs + 4 * CONV_K, 6, F32).rearrange("p (a c) -> p a c", c=2)
                r_cs = R()
                lnt = [ym_flat[:, i * 1024:(i + 1) * 1024].bitcast(F32) for i in range(5)]
                r_lnt = [R() for _ in range(5)]
                dgm = ym_flat[:, 5120:5120 + 2 * CONV_K * 128].rearrange("p (c k m) -> p c k m", k=CONV_K, m=128)
                r_dg = R()

                DMA("sp", wdw_s, wdw[l], [], [r_cs])
                DMA("sp", cvp_s, cvp[l], [], [r_cs])
                MS("dve", upad, 0.0, [r_up])
                for cc in range(2):
                    for k in range(CONV_K):
                        TS("dve", dgm[:, cc, k, :], ident_f[:], wdw_s[:, cc, k:k + 1], None, ALU.mult, None,
                           [r_const, r_cs], [r_dg])

                def upos(a):
                    return a + PADC if a < CTX else (CTX + 2 * PADC) + (a - CTX) + PADC

                for cc in range(2):
                    ia = load_w(winc[l, 10 + cc])
                    ig = load_w(winc[l, 12 + cc])
                    for (a, b, w) in TL:
                        n = b - a
                        pa = next_ps()
                        for kc in range(KC):
                            MM(psb[pa][:, 0:n], wbuf[ia][:, kc, :], hx[:, kc, a:b], kc == 0, kc == KC - 1,
                               [r_wbuf[ia]] + trs(r_hx, kc, a, b), [r_ps[pa]])
                        pg = next_ps()
                        for kc in range(KC):
                            MM(psb[pg][:, 0:n], wbuf[ig][:, kc, :], hx[:, kc, a:b], kc == 0, kc == KC - 1,
                               [r_wbuf[ig]] + trs(r_hx, kc, a, b), [r_ps[pg]])
                        ti = next_tf()
                        ACT(tmpf[ti][:, 0:n], psb[pg][:, 0:n], AF.Sigmoid, [r_ps[pg]], [r_tmpf[ti]])
                        TT("dve", upad[:, cc, upos(a):upos(a) + n], psb[pa][:, 0:n], tmpf[ti][:, 0:n], ALU.mult,
                           [r_ps[pa], r_tmpf[ti]], [r_up])
                for cc in range(2):
                    for (a, b, w) in TL:
                        n = b - a
                        pi = next_ps()
                        p0 = upos(a) - PADC
                        for k in range(CONV_K):
                            MM(psb[pi][:, 0:n], dgm[:, cc, k, :], upad[:, cc, p0 + k:p0 + k + n], k == 0, k == CONV_K - 1,
                               [r_dg, r_up], [r_ps[pi]])
                        ACT(vbuf[:, cc, a:b], psb[pi][:, 0:n], AF.Identity, [r_ps[pi], r_cs], [r_vb[cc]],
                            bias=cvp_s[:, 0, cc:cc + 1], scale=1.0)
                for (a, b, w) in TL:
                    n = b - a
                    p1 = next_ps()
                    p2 = next_ps()
                    for cc in range(2):
                        MM(psb[p1][:, 0:n], ones_f[:], vbuf[:, cc, a:b], cc == 0, cc == 1, [r_const, r_vb[cc]], [r_ps[p1]])
                    for cc in range(2):
                        TT("dve", lnt[0][:, 0:n], vbuf[:, cc, a:b], vbuf[:, cc, a:b], ALU.mult, [r_vb[cc]], [r_lnt[0]])
                        MM(psb[p2][:, 0:n], ones_f[:], lnt[0][:, 0:n], cc == 0, cc == 1, [r_const, r_lnt[0]], [r_ps[p2]])
                    mean = lnt[1]
                    TS("dve", mean[:, 0:n], psb[p1][:, 0:n], 1.0 / 256.0, None, ALU.mult, None, [r_ps[p1]], [r_lnt[1]])
                    TT("dve", lnt[2][:, 0:n], mean[:, 0:n], mean[:, 0:n], ALU.mult, [r_lnt[1]], [r_lnt[2]])
                    STT("dve", lnt[2][:, 0:n], psb[p2][:, 0:n], 1.0 / 256.0, lnt[2][:, 0:n], ALU.mult, ALU.subtract,
                        [r_ps[p2], r_lnt[2]], [r_lnt[2]])
                    ACT(lnt[2][:, 0:n], lnt[2][:, 0:n], AF.Sqrt, [r_lnt[2], r_const], [r_lnt[2]], bias=cst[:, 1:2], scale=1.0)
                    RECIP(lnt[2][:, 0:n], lnt[2][:, 0:n], [r_lnt[2]], [r_lnt[2]])
                    for cc in range(2):
                        TT("dve", lnt[3 + cc][:, 0:n], vbuf[:, cc, a:b], mean[:, 0:n], ALU.subtract,
                           [r_vb[cc], r_lnt[1]], [r_lnt[3 + cc]])
                        TT("dve", lnt[3 + cc][:, 0:n], lnt[3 + cc][:, 0:n], lnt[2][:, 0:n], ALU.mult,
                           [r_lnt[3 + cc], r_lnt[2]], [r_lnt[3 + cc]])
                        ACT(ym[:, 6 + cc, a:b], lnt[3 + cc][:, 0:n], AF.Silu, [r_lnt[3 + cc], r_cs], trs(r_ym, 6 + cc, a, b),
                            bias=cvp_s[:, 2, cc:cc + 1], scale=cvp_s[:, 1, cc:cc + 1])
                P.barrier()

                maybe_stop(10 * l + 2)
                o_uf = 4096
                uF = arena(o_uf, 2 * NT).rearrange("p (c t) -> p c t", t=NT)
                r_uf = [R(), R()]
                o_dp = o_uf + 2 * NT
                dpc = [arena(o_dp + i * 4096, 4096).rearrange("p (s k t) -> p s k t", s=2, k=4) for i in range(3)]
                r_dpc = [R() for _ in range(3)]
                o_dc = o_dp + 3 * 4096
                dcc = arena(o_dc, 1024).rearrange("p (s k t) -> p s k t", s=2, k=2)
                r_dcc = R()
                AB = ym_flat[:, 0:NJ * 512].rearrange("p (j c m) -> p j c m", c=2, m=256)
                r_ab = [R() for _ in range(NJ)]

                for cc in range(2):
                    iw = load_w(winc[l, 8 + cc])
                    for (a, b, w) in TL:
                        n = b - a
                        pi = next_ps()
                        for kc in range(KC):
                            MM(psb[pi][:, 0:n], wbuf[iw][:, kc, :], hx[:, kc, a:b], kc == 0, kc == KC - 1,
                               [r_wbuf[iw]] + trs(r_hx, kc, a, b), [r_ps[pi]])
                        ACT(uF[:, cc, a:b], psb[pi][:, 0:n], AF.Copy, [r_ps[pi]], [r_uf[cc]])
                for j in range(j0, NJ):
                    pi = next_ps()
                    for cc in range(2):
                        MM(psb[pi][:, cc * 256:(cc + 1) * 256], uF[:, cc, j * 128:(j + 1) * 128], cs64[:], True, True,
                           [r_uf[cc], r_const], [r_ps[pi]])
                    CP("dve", AB[:, j].rearrange("p c m -> p (c m)"), psb[pi][:, :], [r_ps[pi]], [r_ab[j]])
                npc = 0
                for tq in range(4):
                    pp = [next_ps(), next_ps()]
                    for kg in range(4):
                        i = npc % 3
                        npc += 1
                        DMA("sp", dpc[i].rearrange("p s k t -> p (s k t)"),
                            dftx_d[tq, kg].rearrange("p s k t -> p (s k t)"), [], [r_dpc[i]])
                        for ki in range(4):
                            j = 2 + kg * 4 + ki
                            for s in range(2):
                                first = (kg == 0 and ki == 0 and s == 0)
                                last = (kg == 3 and ki == 3 and s == 1)
                                for cc in range(2):
                                    MM(psb[pp[cc]][:, :], AB[:, j, cc, s * 128:(s + 1) * 128], dpc[i][:, s, ki, :], first, last,
                                       [r_ab[j], r_dpc[i]], [r_ps[pp[cc]]])
                    for cc in range(2):
                        a = CTX + tq * 512
                        ACT(ym[:, 4 + cc, a:a + 512], psb[pp[cc]][:, :], AF.Copy, [r_ps[pp[cc]]], trs(r_ym, 4 + cc, a, a + 512))
                if need_ctx:
                    DMA("sp", dcc.rearrange("p s k t -> p (s k t)"), dftc_d.rearrange("p s k t -> p (s k t)"), [], [r_dcc])
                    pp = [next_ps(), next_ps()]
                    for ki in range(2):
                        for s in range(2):
                            for cc in range(2):
                                MM(psb[pp[cc]][:, 0:256], AB[:, ki, cc, s * 128:(s + 1) * 128], dcc[:, s, ki, :],
                                   ki == 0 and s == 0, ki == 1 and s == 1, [r_ab[ki], r_dcc], [r_ps[pp[cc]]])
                    for cc in range(2):
                        ACT(ym[:, 4 + cc, 0:CTX], psb[pp[cc]][:, 0:256], AF.Copy, [r_ps[pp[cc]]], trs(r_ym, 4 + cc, 0, CTX))

                P.barrier()
                maybe_stop(10 * l + 3)
                o_s = 0
                dgq = [arena(o_s + i * 256, 128, F32) for i in range(2)]
                qs = [arena(o_s + 512 + i * 128, 128) for i in range(2)]
                sTt = [arena(o_s + 768 + i * 128, 128) for i in range(2)]
                kts = [arena(o_s + 1024 + i * 128, 128) for i in range(2)]
                Et = [arena(o_s + 1280 + i * 128, 128) for i in range(2)]
                Cst = [arena(o_s + 1536 + i * 260, 130, F32) for i in range(2)]
                Cbf = [arena(o_s + 2056 + i * 130, 130) for i in range(2)]
                rden = [arena(o_s + 2316 + i * 4, 2, F32) for i in range(2)]
                st1 = arena(o_s + 2324, 2 * NJ, F32)
                st2 = arena(o_s + 2396, 2 * NJ, F32)
                r_dgq, r_qs, r_sT, r_kts = [R(), R()], [R(), R()], [R(), R()], [R(), R()]
                r_dgb, r_Et = [R(), R()], [R(), R()]
                r_C, r_Cb, r_rden = [R(), R()], [R(), R()], [R(), R()]
                r_st = R()
                wqk_raw = arena(2468, 1024).rearrange("p (k c) -> p k c", c=128)
                r_wqk = R()
                w3 = arena(3492, 3072).rearrange("p (t k c) -> p t k c", t=3, k=KC)
                wtap = arena(6564, 384, F32).rearrange("p (t c) -> p t c", c=128)
                r_w3, r_wtap = R(), R()
                o_g = 7332
                Gtok = arena(o_g, NJ * 16, F32).rearrange("p (j g) -> p j g", g=16)
                dgb = [arena(o_g + i * 256, 128, F32) for i in range(2)]
                LF = arena(o_g + 576, NJ * 8, F32).rearrange("p (j g) -> p j g", g=8)
                CM = arena(o_g + 864, NJ * 8, F32).rearrange("p (j g) -> p j g", g=8)
                Bc = arena(o_g + 1152, NJ * 8, F32).rearrange("p (j g) -> p j g", g=8)
                EB = arena(o_g + 1440, NJ * 8, F32).rearrange("p (j g) -> p j g", g=8)
                EBL = arena(o_g + 1728, NJ * 8, F32).rearrange("p (j g) -> p j g", g=8)
                ECL = arena(o_g + 2016, NJ * 8, F32).rearrange("p (j g) -> p j g", g=8)
                bg_s = arena(o_g + 2304, 16, F32)
                wg_s = arena(o_g + 2336, KC * 16).rearrange("p (k g) -> p k g", g=16)
                gmn_s = arena(o_g + 2464, 512, F32)
                r_gate = R()
                o_h = o_g + 3488
                qT = arena(o_h, NT)
                kT = arena(o_h + NT, NT)
                vaug = arena(o_h + 2 * NT, NJ * 130).rearrange("p (j e) -> p j e", e=130)
                sgo = arena(o_h + 2 * NT + 2340, NT).rearrange("p (j e) -> p j e", e=128)
                hsum = arena(o_h + 3 * NT + 2340, NT, F32).rearrange("p (j e) -> p j e", e=128)
                assert o_h + 5 * NT + 2340 <= SCR_ARENA, (o_h + 5 * NT + 2340)
                r_q, r_k, r_v, r_sg, r_hs = R(), R(), R(), R(), R()
                wvo_s = ym_flat[:, 3 * NT:3 * NT + KC * 256].rearrange("p (k c) -> p k c", c=256)
                r_wvo = R()

                DMA("pool", wg_s, wgate[l], [], [r_gate])
                DMA("sp", bg_s, bgate[l].partition_broadcast(128), [], [r_gate])
                DMA("sp", gmn_s, gmn[l].partition_broadcast(128), [], [r_gate])
                for jg in range(0, NJ, 6):
                    pi = next_ps()
                    for jj in range(6):
                        j = jg + jj
                        for kc in range(KC):
                            MM(psb[pi][:, jj * 16:(jj + 1) * 16], hx[:, kc, j * 128:(j + 1) * 128], wg_s[:, kc, :], kc == 0, kc == KC - 1,
                               [r_gate] + trs(r_hx, kc, j * 128, (j + 1) * 128), [r_ps[pi]])
                    TT("dve", Gtok[:, jg:jg + 6, :], psb[pi][:, 0:96].rearrange("p (j g) -> p j g", g=16),
                       bg_s.unsqueeze(1).to_broadcast([128, 6, 16]), ALU.add, [r_ps[pi], r_gate], [r_gate])
                CP("dve", CM[:, :, 0:4], Gtok[:, :, 0:4], [r_gate], [r_gate])
                CP("dve", CM[:, :, 4:8], Gtok[:, :, 8:12], [r_gate], [r_gate])
                ACT(LF[:, :, 0:4], Gtok[:, :, 4:8], AF.Abs, [r_gate], [r_gate])
                ACT(LF[:, :, 4:8], Gtok[:, :, 12:16], AF.Abs, [r_gate], [r_gate])
                ACT(LF[:], LF[:], AF.Exp, [r_gate], [r_gate], scale=-1.0)
                ACT(LF[:], LF[:], AF.Ln, [r_gate, r_const], [r_gate], bias=cst[:, 2:3], scale=1.0)
                TS("dve", EB[:, :, 0:4], Gtok[:, :, 4:8], 0.0, None, ALU.min, None, [r_gate], [r_gate])
                TS("dve", EB[:, :, 4:8], Gtok[:, :, 12:16], 0.0, None, ALU.min, None, [r_gate], [r_gate])
                TT("dve", LF[:], EB[:], LF[:], ALU.subtract, [r_gate], [r_gate])
                pi = next_ps()
                pbv = psb[pi][:, 0:NJ * 8].rearrange("p (j g) -> p j g", g=8)
                for j in range(NJ):
                    MM(pbv[:, j, 0:4], umask_f[:], LF[:, j, 0:4], True, True, [r_const, r_gate], [r_ps[pi]])
                    MM(pbv[:, j, 4:8], lmask_f[:], LF[:, j, 4:8], True, True, [r_const, r_gate], [r_ps[pi]])
                CP("dve", Bc[:], pbv, [r_ps[pi]], [r_gate])
                pi = next_ps()
                ptv = psb[pi][:, 0:NJ * 8].rearrange("p (j g) -> p j g", g=8)
                MM(psb[pi][:, 0:NJ * 8], ones_f[:], LF[:].rearrange("p j g -> p (j g)"), True, True, [r_const, r_gate], [r_ps[pi]])
                ACT(EBL[:], ptv, AF.Exp, [r_ps[pi]], [r_gate])
                ACT(EB[:], Bc[:], AF.Exp, [r_gate], [r_gate])
                TT("dve", CM[:], CM[:], Bc[:], ALU.subtract, [r_gate], [r_gate])
                TT("dve", ECL[:], CM[:], ptv, ALU.add, [r_gate, r_ps[pi]], [r_gate])
                ACT(ECL[:], ECL[:], AF.Exp, [r_gate], [r_gate])
                P.barrier()

                MS("dve", vaug[:, :, 128:129], 1.0, [r_v])

                for h in range(NH):
                    for qk in range(2):
                        dst, r_dst = (qT, r_q) if qk == 0 else (kT, r_k)
                        ci = qk * 4 + h
                        DMA("pool", wqk_raw, winc[l, ci], [], [r_wqk])
                        DMA("sp", wtap, wqkc[l, ci].partition_broadcast(128), [], [r_wtap])
                        if qk == 1:
                            TS("dve", wtap, wtap, HD ** -0.5, None, ALU.mult, None, [r_wtap], [r_wtap])
                        for t in range(3):
                            TT("dve", w3[:, t], wqk_raw, wtap[:, t, :].unsqueeze(1).to_broadcast([128, KC, 128]), ALU.mult,
                               [r_wqk, r_wtap], [r_w3])
                        for (a, b, w) in TL_all:
                            n = b - a
                            s0, s1 = (0, CTX) if a < CTX else (CTX, NT)
                            pi = next_ps()
                            for kc in range(KC):
                                MM(psb[pi][:, 0:n], w3[:, 1, kc, :], hx[:, kc, a:b], kc == 0, False,
                                   [r_w3] + trs(r_hx, kc, a, b), [r_ps[pi]])
                            lo = max(a, s0 + 1)
                            for kc in range(KC):
                                MM(psb[pi][:, lo - a:n], w3[:, 0, kc, :], hx[:, kc, lo - 1:b - 1], False, False,
                                   [r_w3] + trs(r_hx, kc, lo - 1, b - 1), [r_ps[pi]])
                            hi = min(b, s1 - 1)
                            for kc in range(KC):
                                MM(psb[pi][:, 0:hi - a], w3[:, 2, kc, :], hx[:, kc, a + 1:hi + 1], False, kc == KC - 1,
                                   [r_w3] + trs(r_hx, kc, a + 1, hi + 1), [r_ps[pi]])
                            ACT(dst[:, a:b], psb[pi][:, 0:n], AF.Copy, [r_ps[pi]], [r_dst])
                    DMA("pool", wvo_s, wvo[l, h], [], [r_wvo])
                    for j in range(NJ):
                        pi = next_ps()
                        for kc in range(KC):
                            MM(psb[pi][:, 0:256], hx[:, kc, j * 128:(j + 1) * 128], wvo_s[:, kc, :], kc == 0, kc == KC - 1,
                               [r_wvo] + trs(r_hx, kc, j * 128, (j + 1) * 128), [r_ps[pi]])
                        CP("dve", vaug[:, j, 0:128], psb[pi][:, 0:128], [r_ps[pi]], [r_v])
                        ACT(sgo[:, j, :], psb[pi][:, 128:256], AF.Sigmoid, [r_ps[pi]], [r_sg])
                        TT("dve", sgo[:, j, :], sgo[:, j, :], gmn_s[:, h * 128:(h + 1) * 128], ALU.mult, [r_sg, r_gate], [r_sg])
                    MS("dve", hsum, 0.0, [r_hs])
                    for d_ in range(2):
                        MS("dve", Cst[d_], 0.0, [r_C[d_]])
                        MS("dve", Cbf[d_], 0.0, [r_Cb[d_]])
                    order = [list(range(NJ)), [1, 0] + list(range(NJ - 1, 1, -1))]
                    t0f = tmpf[0]
                    t1b = tmpf[1][:, :].bitcast(BF16)
                    dgb2 = [dgb, [dgq[0], dgq[1]]]
                    sT2 = [sTt, [t1b[:, 256:384], t1b[:, 384:512]]]
                    kts2 = [kts, [t1b[:, 512:640], t1b[:, 640:768]]]
                    Et2 = [Et, [t1b[:, 768:896], t1b[:, 896:1024]]]
                    ndi = [t0f[:, 0:130], t0f[:, 130:260]]
                    r_ndi = [R(), R()]
                    t0b = t0f[:, 260:390].bitcast(BF16)
                    Cbf2 = [Cbf, [t0b[:, 0:130], t0b[:, 130:260]]]
                    r_Cb2 = [r_Cb, [R(), R()]]
                    rr = lambda: [[R(), R()], [R(), R()]]
                    r_dgb2, r_sT2, r_kts2, r_Et2 = rr(), rr(), rr(), rr()

                    def stage_a(step, d_):
                        j = order[d_][step]
                        bs = step % 2
                        col = d_ * 4 + h
                        c0, c1 = j * 128, (j + 1) * 128
                        need_out = need_ctx or j >= 2
                        is_last = step == NJ - 1
                        if need_out:
                            TS("dve", dgb2[bs][d_], ident_f[:], Bc[:, j, col:col + 1], None, ALU.mult, None,
                               [r_const, r_gate], [r_dgb2[bs][d_]])
                            pd_ = next_ps()
                            MM(psb[pd_][:, 0:128], ones_f[:], dgb2[bs][d_], True, False, [r_const, r_dgb2[bs][d_]], [r_ps[pd_]])
                            MM(psb[pd_][:, 0:128], ident_b[:], (negm_f if d_ == 0 else negm_b)[:], False, True,
                               [r_const], [r_ps[pd_]])
                            ACT(Et2[bs][d_], psb[pd_][:, 0:128], AF.Exp, [r_ps[pd_], r_gate], [r_Et2[bs][d_]],
                                bias=CM[:, j, col:col + 1], scale=1.0)
                            p_s = next_ps()
                            MM(psb[p_s][:, 0:128], kT[:, c0:c1], qT[:, c0:c1], True, True, [r_k, r_q], [r_ps[p_s]])
                            TT("dve", sT2[bs][d_], psb[p_s][:, 0:128], Et2[bs][d_], ALU.mult,
                               [r_ps[p_s], r_Et2[bs][d_]], [r_sT2[bs][d_]])
                        if not is_last:
                            p_t = next_ps()
                            ptb = psb[p_t][:, 0:64].bitcast(BF16)
                            TR(ptb, kT[:, c0:c1], ident_b[:], [r_k, r_const], [r_ps[p_t]])
                            ACT(kts2[bs][d_], ptb, AF.Copy, [r_ps[p_t], r_gate], [r_kts2[bs][d_]], scale=ECL[:, j, col:col + 1])

                    def stage_b(step, d_):
                        j = order[d_][step]
                        bs = step % 2
                        col = d_ * 4 + h
                        need_out = need_ctx or j >= 2
                        is_last = step == NJ - 1
                        cur, nxt = Cbf2[step % 2][d_], Cbf2[(step + 1) % 2][d_]
                        r_cur, r_nxt = r_Cb2[step % 2][d_], r_Cb2[(step + 1) % 2][d_]
                        if not is_last:
                            p_u = next_ps()
                            MM(psb[p_u][:, 0:129], kts2[bs][d_], vaug[:, j, 0:129], True, True, [r_kts2[bs][d_], r_v], [r_ps[p_u]])
                            STT("dve", Cst[d_][:, 0:129], Cst[d_][:, 0:129], EBL[:, j, col:col + 1], psb[p_u][:, 0:129],
                                ALU.mult, ALU.add, [r_C[d_], r_gate, r_ps[p_u]], [r_C[d_]])
                            ACT(nxt[:, 0:129], Cst[d_][:, 0:129], AF.Copy, [r_C[d_]], [r_nxt])
                        if need_out:
                            c0, c1 = j * 128, (j + 1) * 128
                            p_i = next_ps()
                            MM(psb[p_i][:, 0:129], qT[:, c0:c1], cur[:, 0:129], True, True, [r_q, r_cur], [r_ps[p_i]])
                            ACT(ndi[d_][:, 0:129], psb[p_i][:, 0:129], AF.Copy, [r_ps[p_i], r_gate], [r_ndi[d_]], scale=EB[:, j, col:col + 1])
                            p_n = next_ps()
                            MM(psb[p_n][:, 0:129], sT2[bs][d_], vaug[:, j, 0:129], True, True, [r_sT2[bs][d_], r_v], [r_ps[p_n]])
                            TT("dve", ndi[d_][:, 0:129], ndi[d_][:, 0:129], psb[p_n][:, 0:129], ALU.add, [r_ndi[d_], r_ps[p_n]], [r_ndi[d_]])
                            STT("dve", rden[d_][:, 0:1], ndi[d_][:, 128:129], -1.0, ndi[d_][:, 128:129], ALU.mult, ALU.max,
                                [r_ndi[d_]], [r_rden[d_]])
                            TS("dve", rden[d_][:, 0:1], rden[d_][:, 0:1], 1.0, None, ALU.max, None,
                               [r_rden[d_]], [r_rden[d_]])
                            RECIP(rden[d_][:, 0:1], rden[d_][:, 0:1], [r_rden[d_]], [r_rden[d_]])
                            STT("dve", hsum[:, j, :], ndi[d_][:, 0:128], rden[d_][:, 0:1], hsum[:, j, :], ALU.mult, ALU.add,
                                [r_ndi[d_], r_rden[d_], r_hs], [r_hs])

                    for d_ in range(2):
                        stage_a(0, d_)
                    for step in range(NJ):
                        if step + 1 < NJ:
                            for d_ in range(2):
                                stage_a(step + 1, d_)
                        for d_ in range(2):
                            stage_b(step, d_)
                    nj = NJ - j0
                    hv = hsum[:, j0:NJ, :]
                    sqv = ym[:, h, j0 * 128:NT].rearrange("p (j e) -> p j e", e=128)
                    ymh_res = trs(r_ym, h, j0 * 128, NT) + ([r_wvo] if h == 3 else [])
                    P.op("dve", (lambda hv=hv, nj=nj: lambda e: e.tensor_reduce(out=st1[:, 0:nj], in_=hv, axis=AX.X, op=ALU.add))(),
                         [r_hs], [r_st])
                    TT("dve", sqv, hv, hv, ALU.mult, [r_hs], ymh_res)
                    P.op("dve", (lambda sqv=sqv, nj=nj: lambda e: e.tensor_reduce(out=st1[:, NJ:NJ + nj], in_=sqv, axis=AX.X, op=ALU.add))(),
                         ymh_res, [r_st])
                    mean_ = st2[:, 0:nj]
                    var_ = st2[:, NJ:NJ + nj]
                    TS("dve", mean_, st1[:, 0:nj], 1.0 / HD, None, ALU.mult, None, [r_st], [r_st])
                    TT("dve", var_, mean_, mean_, ALU.mult, [r_st], [r_st])
                    STT("dve", var_, st1[:, NJ:NJ + nj], 1.0 / HD, var_, ALU.mult, ALU.subtract, [r_st], [r_st])
                    ACT(var_, var_, AF.Sqrt, [r_st, r_const], [r_st], bias=cst[:, 1:2], scale=1.0)
                    RECIP(var_, var_, [r_st], [r_st])
                    TT("dve", hv, hv, mean_.unsqueeze(2).to_broadcast([128, nj, 128]), ALU.subtract, [r_hs, r_st], [r_hs])
                    TT("dve", hv, hv, var_.unsqueeze(2).to_broadcast([128, nj, 128]), ALU.mult, [r_hs, r_st], [r_hs])
                    TT("dve", sgo[:, j0:NJ, :], hv, sgo[:, j0:NJ, :], ALU.mult, [r_hs, r_sg], [r_sg])
                    for jg in range(j0, NJ, 4):
                        p_t = next_ps()
                        ptb = psb[p_t][:, 0:256].bitcast(BF16)
                        jn = min(4, NJ - jg)
                        for jj in range(jn):
                            TR(ptb[:, jj * 128:(jj + 1) * 128], sgo[:, jg + jj, :], ident_b[:], [r_sg, r_const], [r_ps[p_t]])
                        ACT(ym[:, h, jg * 128:(jg + jn) * 128], ptb[:, 0:jn * 128], AF.Copy, [r_ps[p_t]],
                            trs(r_ym, h, jg * 128, (jg + jn) * 128) + ([r_wvo] if h == 3 else []))

                if l == 0:
                    dump("d_ym", lambda kc: ym[:, kc, :], lambda kc: trs(r_ym, kc, 0, NT))

                P.barrier()
                maybe_stop(10 * l + 4)
                o_wo = 4096
                wo_s = arena(o_wo, KC * KC * 128).rearrange("p (o k c) -> p o k c", o=KC, k=KC)
                r_wo = R()
                obuf = arena(o_wo + 8192, KC * 512, F32).rearrange("p (o t) -> p o t", t=512)
                r_ob = [R() for _ in range(KC)]
                DMA("pool", wo_s.rearrange("p o k c -> p (o k c)"), woutc[l].rearrange("p o k c -> p (o k c)"), [],
                    [r_wo])
                for (a, b, w) in TL:
                    n = b - a
                    for oc in range(KC):
                        pi = next_ps()
                        for kc in range(KC):
                            MM(psb[pi][:, 0:n], wo_s[:, oc, kc, :], ym[:, kc, a:b], kc == 0, kc == KC - 1,
                               [r_wo] + trs(r_ym, kc, a, b), [r_ps[pi]])
                        ACT(obuf[:, oc, 0:n], psb[pi][:, 0:n], AF.Copy, [r_ps[pi]], [r_ob[oc]])
                    resid_update([(a, b, w)], lambda kc, a_, b_: obuf[:, kc, 0:b_ - a_], lambda kc, a_, b_: [r_ob[kc]], G1)
                if l == 0:
                    dump("d_x1", lambda kc: xs[:, kc, :], lambda kc: trs(r_xs, kc, 0, NT))

                P.barrier()
                maybe_stop(10 * l + 5)
                hT = scr[:, 0:24576].rearrange("p (f t) -> p f t", t=768)
                r_hT = [R() for _ in range(32)]
                ob2 = scr[:, 24576:30720].rearrange("p (o t) -> p o t", t=768)
                r_ob2 = [R() for _ in range(KC)]
                w1b = [scr[:, 30720 + i * 2048:30720 + (i + 1) * 2048].rearrange("p (g k c) -> p g k c", g=2, k=KC) for i in range(2)]
                w2b = [scr[:, 34816 + i * 4096:34816 + (i + 1) * 4096].rearrange("p (f c) -> p f c", c=128) for i in range(2)]
                r_w1b = [R(), R()]
                r_w2b = [R(), R()]
                if need_ctx:
                    supers = [[(0, 256, 1), (256, 768, 0)], [(768, 1280, 0), (1280, 1536, 0)], [(1536, 2048, 0), (2048, 2304, 0)]]
                else:
                    supers = [[(256, 768, 0), (768, 1024, 0)], [(1024, 1536, 0), (1536, 1792, 0)], [(1792, 2304, 0)]]
                n1 = 0
                n2 = 0
                for sup in supers:
                    a0 = sup[0][0]
                    prenorm(sup, A2, 24, l)
                    for g in range(16):
                        i = n1 % 2
                        n1 += 1
                        DMA("pool", w1b[i].rearrange("p g k c -> p (g k c)"), w1c[l, g].rearrange("p g k c -> p (g k c)"), [],
                            [r_w1b[i]])
                        for f2 in range(2):
                            f = g * 2 + f2
                            for (a, b, w) in sup:
                                n = b - a
                                pi = next_ps()
                                for kc in range(KC):
                                    MM(psb[pi][:, 0:n], w1b[i][:, f2, kc, :], hx[:, kc, a:b], kc == 0, kc == KC - 1,
                                       [r_w1b[i]] + trs(r_hx, kc, a, b), [r_ps[pi]])
                                ti = next_tf()
                                ACT(tmpf[ti][:, 0:n], psb[pi][:, 0:n], AF.Relu, [r_ps[pi]], [r_tmpf[ti]])
                                TT("dve", hT[:, f, a - a0:b - a0], tmpf[ti][:, 0:n], tmpf[ti][:, 0:n], ALU.mult, [r_tmpf[ti]], [r_hT[f]])
                    ssb = []
                    for _ in sup:
                        pss = next_ps()
                        ps_reserved.add(pss)
                        ssb.append(pss)
                    for oc in range(KC):
                        i = n2 % 2
                        n2 += 1
                        DMA("pool", w2b[i].rearrange("p f c -> p (f c)"), w2c[l, oc].rearrange("p f c -> p (f c)"), [],
                            [r_w2b[i]])
                        for si_, (a, b, w) in enumerate(sup):
                            n = b - a
                            pi = next_ps()
                            for f in range(32):
                                MM(psb[pi][:, 0:n], w2b[i][:, f, :], hT[:, f, a - a0:b - a0], f == 0, f == 31,
                                   [r_w2b[i], r_hT[f]], [r_ps[pi]])
                            ACT(ob2[:, oc, a - a0:b - a0], psb[pi][:, 0:n], AF.Copy, [r_ps[pi]], [r_ob2[oc]])
                            sq_i = next_sq()
                            ACT(sqb[sq_i][:, 0:n], psb[pi][:, 0:n], AF.Square, [r_ps[pi]], [r_sqb[sq_i]])
                            MM(psb[ssb[si_]][:, 0:n], ones_b[:], sqb[sq_i][:, 0:n], oc == 0, oc == KC - 1,
                               [r_sqb[sq_i], r_const], [r_ps[ssb[si_]]])
                    for si_, (a, b, w) in enumerate(sup):
                        n = b - a
                        ACT(rs[:, 0:n], psb[ssb[si_]][:, 0:n], AF.Sqrt, [r_ps[ssb[si_]], r_const], [r_rs], bias=cst[:, 0:1], scale=1.0)
                        RECIP(rs[:, 0:n], rs[:, 0:n], [r_rs], [r_rs])
                        for kc in range(KC):
                            ti = next_tf()
                            TT("dve", tmpf[ti][:, 0:n], ob2[:, kc, a - a0:b - a0], rs[:, 0:n], ALU.mult,
                               [r_ob2[kc], r_rs], [r_tmpf[ti]])
                            STT("dve", xs[:, kc, a:b], tmpf[ti][:, 0:n], G2[:, kc, w:w + 1], xs[:, kc, a:b], ALU.mult, ALU.add,
                                [r_tmpf[ti], r_lay] + trs(r_xs, kc, a, b), trs(r_xs, kc, a, b))
                    for pss in ssb:
                        ps_reserved.discard(pss)
                if l == 0:
                    dump("d_x2", lambda kc: xs[:, kc, :], lambda kc: trs(r_xs, kc, 0, NT))
                P.barrier()

        except _Stop:
            pass
        for kc in range(KC):
            DMA("sp", outT[kc], xs[:, kc, CTX:NT], trs(r_xs, kc, CTX, NT), [])
        P.emit()
    return nc


_CACHE = {}


def _consts():
    if "c" in _CACHE:
        return _CACHE["c"]
    c = {}
    c["ident"] = np.eye(128, dtype=np.float32)
    s = np.arange(128)
    c["umask"] = (s[:, None] <= s[None, :]).astype(np.float32)
    c["lmask"] = (s[:, None] >= s[None, :]).astype(np.float32)
    k = np.arange(64)
    ang = 2.0 * np.pi * np.outer(k, k) / 64.0
    cs = np.zeros((128, 256), np.float64)
    for hh in range(2):
        cs[hh * 64:(hh + 1) * 64, hh * 64:(hh + 1) * 64] = np.cos(ang) / 8.0
        cs[hh * 64:(hh + 1) * 64, 128 + hh * 64:128 + (hh + 1) * 64] = np.sin(ang) / 8.0
    c["cs64"] = cs.astype(np.float32)
    t = np.arange(SEQ, dtype=np.int64)
    ph = (np.outer(t, t) % SEQ).astype(np.float64) * (2.0 * np.pi / SEQ)
    dc = (np.cos(ph) / math.sqrt(SEQ)).astype(np.float32)
    ds = (-np.sin(ph) / math.sqrt(SEQ)).astype(np.float32)
    both = np.stack([dc, ds], 0)
    both = both.reshape(2, 4, 4, 128, 4, 512)
    c["dftx"] = np.ascontiguousarray(both.transpose(4, 1, 3, 0, 2, 5)).astype(ml_dtypes.bfloat16)
    t = np.arange(CTX, dtype=np.int64)
    ph = (np.outer(t, t) % CTX).astype(np.float64) * (2.0 * np.pi / CTX)
    both = np.stack([np.cos(ph), -np.sin(ph)], 0) / math.sqrt(CTX)
    both = both.reshape(2, 2, 128, CTX)
    c["dftc"] = np.ascontiguousarray(both.transpose(2, 0, 1, 3)).astype(ml_dtypes.bfloat16)
    rows = SEQ // 64
    quarter = D // 4
    freq = np.exp(-math.log(10000.0) * np.arange(quarter, dtype=np.float32) / quarter).astype(np.float32)
    r = np.broadcast_to(np.arange(rows, dtype=np.float32)[:, None], (rows, 64)).reshape(-1)
    col = np.broadcast_to(np.arange(64, dtype=np.float32)[None, :], (rows, 64)).reshape(-1)
    ar = r[:, None] * freq
    ac = col[:, None] * freq
    pos = np.concatenate([np.sin(ar), np.cos(ar), np.sin(ac), np.cos(ac)], axis=-1).astype(np.float32)
    c["posT"] = np.ascontiguousarray(pos.T).reshape(KC, 128, SEQ)
    _CACHE["c"] = c
    return c


def _chunk_w(w, cols):
    return np.ascontiguousarray(w[:, cols].reshape(KC, 128, -1).transpose(1, 0, 2))


def _prep_shared(inp):
    f = np.float32
    L = DEPTH
    w_in = np.asarray(inp["w_in"], f)
    sh = {}
    wada = np.asarray(inp["w_ada"], f)
    sh["wada"] = np.ascontiguousarray(wada.reshape(L, KC, 128, 12, 512).transpose(0, 3, 2, 1, 4))
    sh["bada"] = np.ascontiguousarray(np.asarray(inp["b_ada"], f).reshape(L, 48, 128).transpose(0, 2, 1))
    gs = np.stack([np.asarray(inp[k], f) for k in ("g_pre_mix", "g_post_mix", "g_pre_mlp", "g_post_mlp")], 1)
    sh["gvec"] = np.ascontiguousarray(gs.reshape(L, 4, KC, 128).transpose(0, 3, 1, 2))
    offs = [Q_OFF + 128 * i for i in range(4)] + [K_OFF + 128 * i for i in range(4)] + \
           [F_OFF, F_OFF + 128, CA_OFF, CA_OFF + 128, CG_OFF, CG_OFF + 128]
    sh["winc"] = np.stack([np.stack([_chunk_w(w_in[l], np.arange(o, o + 128)) for o in offs]) for l in range(L)])
    sh["wvo"] = np.stack([np.stack([_chunk_w(w_in[l], np.concatenate([np.arange(V_OFF + 128 * h, V_OFF + 128 * h + 128),
                                                                      np.arange(O_OFF + 128 * h, O_OFF + 128 * h + 128)]))
                                    for h in range(NH)]) for l in range(L)])
    sh["wgate"] = np.stack([_chunk_w(w_in[l], np.arange(G_OFF, G_OFF + 16)) for l in range(L)])
    sh["bgate"] = np.ascontiguousarray(np.asarray(inp["b_gate"], f).reshape(L, 16))
    wqk = np.asarray(inp["w_qk_conv"], f)
    sh["wqkc"] = np.ascontiguousarray(wqk.reshape(L, 3, 8, 128).transpose(0, 2, 1, 3))
    sh["gmn"] = np.ascontiguousarray(np.asarray(inp["g_mlstm_norm"], f))
    wdw = np.asarray(inp["w_dw"], f)
    sh["wdw"] = np.ascontiguousarray(wdw.reshape(L, CONV_K, 2, 128).transpose(0, 3, 2, 1))
    cv = np.stack([np.asarray(inp[k], f) for k in ("b_dw", "g_conv_ln", "b_conv_ln")], 1)
    sh["cvp"] = np.ascontiguousarray(cv.reshape(L, 3, 2, 128).transpose(0, 3, 1, 2))
    wout = np.asarray(inp["w_out"], f)
    sh["woutc"] = np.ascontiguousarray(wout.reshape(L, KC, 128, KC, 128).transpose(0, 2, 3, 1, 4))
    w1 = np.asarray(inp["w_mlp1"], f)
    sh["w1c"] = np.ascontiguousarray(w1.reshape(L, KC, 128, 16, 2, 128).transpose(0, 3, 2, 4, 1, 5))
    w2 = np.asarray(inp["w_mlp2"], f)
    sh["w2c"] = np.ascontiguousarray(w2.reshape(L, 32, 128, KC, 128).transpose(0, 3, 2, 1, 4))
    c = _consts()
    for k in ("ident", "umask", "lmask", "cs64", "dftx", "dftc", "posT"):
        sh[k] = c[k]
    return sh


def make_in_maps(inp, cores):
    sh = _prep_shared(inp)
    x = np.asarray(inp["x"], np.float32)
    ctx = np.asarray(inp["ctx"], np.float32)
    c = np.asarray(inp["c"], np.float32)
    c_ctx = np.asarray(inp["c_ctx"], np.float32)
    maps = []
    for b in cores:
        m = dict(sh)
        m["xT"] = np.ascontiguousarray(x[b].T).reshape(KC, 128, SEQ)
        m["ctxT"] = np.ascontiguousarray(ctx[b].T).reshape(KC, 128, CTX)
        cv = np.stack([c[b], c_ctx], -1)
        m["cvec"] = np.ascontiguousarray(cv.reshape(KC, 128, 2).transpose(1, 0, 2))
        maps.append(m)
    return maps


def kernel(**inputs):
    if "nc" not in _CACHE:
        _CACHE["nc"] = build_program(dbg=False)
    nc = _CACHE["nc"]
    in_maps = make_in_maps(inputs, list(range(NB)))
    res = run_bass_kernel_spmd(nc, in_maps, core_ids=list(range(NB)))
    out = np.empty((NB, SEQ, D), np.float32)
    for b in range(NB):
        oT = np.asarray(res.results[b]["outT"], np.float32).reshape(D, SEQ)
        out[b] = oT.T
    return out
```

```python
import math
from contextlib import ExitStack

import numpy as np
import ml_dtypes

import concourse.bass as bass
import concourse.mybir as mybir
from concourse.bass_utils import run_bass_kernel_spmd

F32 = mybir.dt.float32
BF16 = mybir.dt.bfloat16
AF = mybir.ActivationFunctionType
ALU = mybir.AluOpType
AX = mybir.AxisListType

ENGS = ("pe", "act", "dve", "pool", "sp")
SEM_LIM = 2000
N_DMA_SEMS = 16


class Res:
    __slots__ = ("name", "w", "r", "excl")

    def __init__(self, name, excl=False):
        self.name = name
        self.w = None
        self.r = []
        self.excl = excl


class Op:
    __slots__ = ("eng", "idx", "fn", "deps", "dma", "signal", "semref", "dma_slot", "dma_val")

    def __init__(self, eng, idx, fn, dma):
        self.eng = eng
        self.idx = idx
        self.fn = fn
        self.deps = []
        self.dma = dma
        self.signal = False
        self.semref = None
        self.dma_slot = None
        self.dma_val = None


class Prog:
    def __init__(self, nc, same_engine_sync=True):
        self.nc = nc
        self.ops = {e: [] for e in ENGS}
        self.n_dma_q = {}
        self.same_engine_sync = same_engine_sync

    def res(self, name="", excl=False):
        return Res(name, excl)

    def op(self, eng, fn, reads=(), writes=(), dma=False):
        o = Op(eng, len(self.ops[eng]), fn, dma)
        if dma:
            half = N_DMA_SEMS // 2
            k = self.n_dma_q.get(eng, 0)
            self.n_dma_q[eng] = k + 1
            o.dma_slot = (k % half) + (half if eng == "pool" else 0)
            o.dma_val = 16 * (k // half + 1)
        deps = []
        for r in reads:
            if r.w is not None:
                deps.append(r.w)
            if r.excl:
                deps.extend(x for x in r.r if x.eng != eng)
        for r in writes:
            if r.w is not None:
                deps.append(r.w)
            deps.extend(r.r)
        for r in reads:
            r.r.append(o)
        for r in writes:
            r.w = o
            r.r = []
        seen = set()
        for d in deps:
            if d is o or id(d) in seen:
                continue
            seen.add(id(d))
            if (not d.dma) and (not dma) and d.eng == eng:
                if eng == "pe" or not self.same_engine_sync:
                    continue
            o.deps.append(d)
        self.ops[eng].append(o)
        return o

    def barrier(self):
        targets = []
        for e in ENGS:
            cs = [o for o in self.ops[e] if not o.dma and o.fn is not None]
            if cs:
                targets.append(cs[-1])
        by_slot = {}
        for e in ENGS:
            for o in self.ops[e]:
                if o.dma and (o.dma_slot not in by_slot or o.dma_val > by_slot[o.dma_slot].dma_val):
                    by_slot[o.dma_slot] = o
        targets += list(by_slot.values())
        for e in ENGS:
            o = Op(e, len(self.ops[e]), None, False)
            o.deps = list(targets) if e != "pe" else [t for t in targets if t.dma or t.eng != e]
            self.ops[e].append(o)

    def emit(self):
        nc = self.nc
        for e in ENGS:
            for o in self.ops[e]:
                for d in o.deps:
                    d.signal = True
        with ExitStack() as st:
            dma_sems = [st.enter_context(nc.semaphore(f"dq{i}")) for i in range(N_DMA_SEMS)]
            for e in ENGS:
                n = sum(1 for o in self.ops[e] if o.signal and not o.dma)
                k = max((n + SEM_LIM - 1) // SEM_LIM, 1)
                sems = [st.enter_context(nc.semaphore(f"s_{e}{i}")) for i in range(k)]
                c = 0
                for o in self.ops[e]:
                    if o.signal and not o.dma:
                        o.semref = (sems[c // SEM_LIM], c % SEM_LIM + 1, c)
                        c += 1
            block = st.enter_context(nc.Block())

            def run(e, eng):
                waited_c = {p: -1 for p in ENGS}
                waited_d = [0] * N_DMA_SEMS
                for o in self.ops[e]:
                    for d in o.deps:
                        if d.dma:
                            if waited_d[d.dma_slot] >= d.dma_val:
                                continue
                            waited_d[d.dma_slot] = d.dma_val
                            eng.wait_ge(dma_sems[d.dma_slot], d.dma_val)
                        else:
                            sem, val, gc = d.semref
                            if waited_c[d.eng] >= gc:
                                continue
                            waited_c[d.eng] = gc
                            eng.wait_ge(sem, val)
                    if o.fn is None:
                        continue
                    ins = o.fn(eng)
                    if o.dma:
                        ins.then_inc(dma_sems[o.dma_slot], 16)
                    elif o.signal:
                        ins.then_inc(o.semref[0], 1)

            fin_dma = {}
            for e in ENGS:
                for o in self.ops[e]:
                    if o.dma:
                        fin_dma[o.dma_slot] = max(fin_dma.get(o.dma_slot, 0), o.dma_val)

            @block.tensor
            def _(eng):
                run("pe", eng)

            @block.scalar
            def _(eng):
                run("act", eng)

            @block.vector
            def _(eng):
                run("dve", eng)

            @block.gpsimd
            def _(eng):
                run("pool", eng)

            @block.sync
            def _(eng):
                run("sp", eng)
                for slot, val in sorted(fin_dma.items()):
                    eng.wait_ge(dma_sems[slot], val)


D = 1024
NB = 8
SEQ = 2048
CTX = 256
NT = CTX + SEQ
DEPTH = 2
KC = 8
NJ = NT // 128
HD = 128
NH = 4
DFF = 4096
EPS = 1e-6
CONV_K = 31
PADC = 15
Q_OFF, K_OFF, V_OFF, O_OFF, G_OFF = 0, 512, 1024, 1536, 2048
F_OFF = 2064
CA_OFF = 2320
CG_OFF = 2576
UPW = (CTX + 2 * PADC) + (SEQ + 2 * PADC)


def tiles_for(need_ctx):
    t = [(256, 768, 0), (768, 1280, 0), (1280, 1792, 0), (1792, 2304, 0)]
    if need_ctx:
        t = [(0, 256, 1)] + t
    return t


class _Stop(Exception):
    pass


def build_program(dbg=False, stop=None):
    nc = bass.Bass("TRN2", target_bir_lowering=False)
    P = Prog(nc)

    def din(name, shape, dt=F32):
        return nc.dram_tensor(name, list(shape), dt, kind="ExternalInput").ap()

    def dout(name, shape, dt=F32):
        return nc.dram_tensor(name, list(shape), dt, kind="ExternalOutput").ap()

    xT = din("xT", [KC, 128, SEQ])
    ctxT = din("ctxT", [KC, 128, CTX])
    posT = din("posT", [KC, 128, SEQ])
    cvec = din("cvec", [128, KC, 2])
    wada = din("wada", [DEPTH, 12, 128, KC, 512])
    bada = din("bada", [DEPTH, 128, 48])
    gvec = din("gvec", [DEPTH, 128, 4, KC])
    winc = din("winc", [DEPTH, 14, 128, KC, 128])
    wvo = din("wvo", [DEPTH, NH, 128, KC, 256])
    wgate = din("wgate", [DEPTH, 128, KC, 16])
    bgate = din("bgate", [DEPTH, 16])
    wqkc = din("wqkc", [DEPTH, 8, 3, 128])
    gmn = din("gmn", [DEPTH, 512])
    wdw = din("wdw", [DEPTH, 128, 2, CONV_K])
    cvp = din("cvp", [DEPTH, 128, 3, 2])
    woutc = din("woutc", [DEPTH, 128, KC, KC, 128])
    w1c = din("w1c", [DEPTH, 16, 128, 2, KC, 128])
    w2c = din("w2c", [DEPTH, KC, 128, 32, 128])
    ident_d = din("ident", [128, 128])
    umask_d = din("umask", [128, 128])
    lmask_d = din("lmask", [128, 128])
    cs64_d = din("cs64", [128, 256])
    dftx_d = din("dftx", [4, 4, 128, 2, 4, 512], BF16)
    dftc_d = din("dftc", [128, 2, 2, 256], BF16)
    outT = dout("outT", [KC, 128, SEQ])
    dbg_out = {}
    if dbg:
        for nm in ("d_hx", "d_ym", "d_x1", "d_x2"):
            dbg_out[nm] = dout(nm, [KC, 128, NT])

    with ExitStack() as st:
        def sb(name, shape, dt):
            return st.enter_context(nc.sbuf_tensor(name, list(shape), dt))

        xs = sb("xs", [128, KC, NT], F32)
        hx = sb("hx", [128, KC, NT], BF16)
        SCR_ARENA = 25088
        SCR_N = SCR_ARENA + KC * NT
        scr = sb("scr", [128, SCR_N], BF16)
        ym_flat = scr[:, SCR_ARENA:SCR_N]
        ym = ym_flat.rearrange("p (c t) -> p c t", t=NT)
        ident_f = sb("ident_f", [128, 128], F32)
        ident_b = sb("ident_b", [128, 128], BF16)
        umask_f = sb("umask_f", [128, 128], F32)
        lmask_f = sb("lmask_f", [128, 128], F32)
        negm_f = sb("negm_f", [128, 128], BF16)
        negm_b = sb("negm_b", [128, 128], BF16)
        ones_f = sb("ones_f", [128, 128], F32)
        ones_b = sb("ones_b", [128, 128], BF16)
        cs64 = sb("cs64b", [128, 256], BF16)
        cst = sb("cst", [128, 4], F32)
        cv_f = sb("cv_f", [128, KC, 2], F32)
        sc_b = sb("sc_b", [128, KC, 2], BF16)
        mod = sb("mod", [128, DEPTH, 48, 2], F32)
        bada_s = sb("bada_s", [128, DEPTH, 48], F32)
        gv = sb("gv", [128, DEPTH, 4, KC], F32)
        A1 = sb("A1", [128, KC, 2], F32)
        A2 = sb("A2", [128, KC, 2], F32)
        G1 = sb("G1", [128, KC, 2], F32)
        G2 = sb("G2", [128, KC, 2], F32)
        rs = sb("rs", [128, 512], F32)
        sqb = [sb(f"sqb{i}", [128, 512], BF16) for i in range(2)]
        tmpf = [sb(f"tmpf{i}", [128, 512], F32) for i in range(2)]
        psb = [st.enter_context(nc.psum_tensor(f"ps{i}", [128, 512], F32)) for i in range(8)]

        R = P.res
        r_xs = [[R() for _ in range(NJ)] for _ in range(KC)]
        r_hx = [[R() for _ in range(NJ)] for _ in range(KC)]
        r_ym = [[R() for _ in range(NJ)] for _ in range(KC)]
        r_ps = [P.res("ps", excl=True) for _ in range(8)]
        r_const = R()
        r_mod = R()
        r_lay = R()
        r_rs = R()
        r_sqb = [R(), R()]
        r_tmpf = [R(), R()]
        r_arena = R()

        def trs(rl, kc, a, b):
            return [rl[kc][j] for j in range(a // 128, (b + 127) // 128)]

        def trs_all(rl, a, b):
            out = []
            for kc in range(KC):
                out += trs(rl, kc, a, b)
            return out

        cnt = {"ps": 0, "sq": 0, "tf": 0}

        ps_reserved = set()

        def next_ps():
            while True:
                i = cnt["ps"] % 8
                cnt["ps"] += 1
                if i not in ps_reserved:
                    return i

        def next_sq():
            i = cnt["sq"] % 2
            cnt["sq"] += 1
            return i

        def next_tf():
            i = cnt["tf"] % 2
            cnt["tf"] += 1
            return i

        def MM(out, lhsT, rhs, start, stop, reads, writes):
            P.op("pe", lambda e: e.matmul(out, lhsT=lhsT, rhs=rhs, start=start, stop=stop), reads, writes)

        def TR(out, in_, ident, reads, writes):
            P.op("pe", lambda e: e.transpose(out, in_, ident), reads, writes)

        def ACT(out, in_, func, reads, writes, bias=None, scale=None, accum=None):
            kw = {}
            if bias is not None:
                kw["bias"] = bias
            if scale is not None:
                kw["scale"] = scale
            if accum is not None:
                kw["accum_out"] = accum
            P.op("act", lambda e: e.activation(out=out, in_=in_, func=func, **kw), reads, writes)

        def TT(eng, out, in0, in1, op, reads, writes):
            P.op(eng, lambda e: e.tensor_tensor(out=out, in0=in0, in1=in1, op=op), reads, writes)

        def TS(eng, out, in0, s1, s2, op0, op1, reads, writes):
            if s2 is None:
                P.op(eng, lambda e: e.tensor_scalar(out=out, in0=in0, scalar1=s1, scalar2=None, op0=op0), reads, writes)
            else:
                P.op(eng, lambda e: e.tensor_scalar(out=out, in0=in0, scalar1=s1, scalar2=s2, op0=op0, op1=op1), reads, writes)

        def STT(eng, out, in0, scalar, in1, op0, op1, reads, writes):
            P.op(eng, lambda e: e.scalar_tensor_tensor(out=out, in0=in0, scalar=scalar, in1=in1, op0=op0, op1=op1), reads, writes)

        def CP(eng, out, in_, reads, writes):
            P.op(eng, lambda e: e.tensor_copy(out=out, in_=in_), reads, writes)

        def MS(eng, ap, val, writes):
            P.op(eng, lambda e: e.memset(ap, val), (), writes)

        def RECIP(out, in_, reads, writes):
            P.op("dve", lambda e: e.reciprocal(out=out, in_=in_), reads, writes)

        def DMA(q, out, in_, reads, writes):
            P.op(q, lambda e: e.dma_start(out=out, in_=in_), reads, writes, dma=True)

        def arena(off, n, dt=BF16):
            if dt == BF16:
                return scr[:, off:off + n]
            assert off % 2 == 0
            return scr[:, off:off + 2 * n].bitcast(F32)

        DMA("sp", ident_f[:], ident_d, [], [r_const])
        DMA("sp", umask_f[:], umask_d, [], [r_const])
        DMA("sp", lmask_f[:], lmask_d, [], [r_const])
        DMA("pool", cs64[:], cs64_d, [], [r_const])
        DMA("sp", cv_f[:], cvec, [], [r_const])
        DMA("sp", bada_s[:], bada.rearrange("l p n -> p l n"), [], [r_const])
        DMA("sp", gv[:], gvec.rearrange("l p a k -> p l a k"), [], [r_const])
        CP("dve", ident_b[:], ident_f[:], [r_const], [r_const])
        TS("dve", negm_f[:], umask_f[:], -1.0, 30000.0, ALU.add, ALU.mult, [r_const], [r_const])
        TS("dve", negm_b[:], lmask_f[:], -1.0, 30000.0, ALU.add, ALU.mult, [r_const], [r_const])
        MS("dve", ones_f[:], 1.0, [r_const])
        MS("dve", ones_b[:], 1.0, [r_const])
        MS("dve", cst[:, 0:1], 1024.0 * EPS, [r_const])
        MS("dve", cst[:, 1:2], EPS, [r_const])
        MS("dve", cst[:, 2:3], 1.0, [r_const])
        MS("dve", cst[:, 3:4], 0.0, [r_const])

        for kc in range(KC):
            DMA("sp", xs[:, kc, CTX:NT], xT[kc], [], trs(r_xs, kc, CTX, NT))
            DMA("sp", xs[:, kc, 0:CTX], ctxT[kc], [], trs(r_xs, kc, 0, CTX))
        pos_buf = [arena(0, SEQ, F32), arena(2 * SEQ, SEQ, F32)]
        r_pos = [R(), R()]
        for kc in range(KC):
            i = kc % 2
            DMA("sp", pos_buf[i], posT[kc], [], [r_pos[i]])
            TT("dve", xs[:, kc, CTX:NT], xs[:, kc, CTX:NT], pos_buf[i], ALU.add,
               [r_pos[i]] + trs(r_xs, kc, CTX, NT), trs(r_xs, kc, CTX, NT))

        ACT(sc_b[:], cv_f[:], AF.Silu, [r_const], [r_const])
        wa_off = 4 * SEQ
        wa_buf = [arena(wa_off + i * KC * 512, KC * 512).rearrange("p (k c) -> p k c", c=512) for i in range(2)]
        r_wa = [R(), R()]
        for l in range(DEPTH):
            pi = next_ps()
            pv = psb[pi][:, 0:96].rearrange("p (n w) -> p n w", w=2)
            for blk in range(12):
                i = (l * 12 + blk) % 2
                DMA("pool", wa_buf[i], wada[l, blk], [], [r_wa[i]])
                for n4 in range(4):
                    n = blk * 4 + n4
                    for kc in range(KC):
                        MM(pv[:, n, :], wa_buf[i][:, kc, n4 * 128:(n4 + 1) * 128], sc_b[:, kc, :], kc == 0, kc == KC - 1,
                           [r_wa[i], r_const], [r_ps[pi]])
            TT("dve", mod[:, l], pv, bada_s[:, l].unsqueeze(2).to_broadcast([128, 48, 2]), ALU.add,
               [r_ps[pi], r_const], [r_mod])

        def rstd_of(src_fn, n, reads):
            pi = next_ps()
            for kc in range(KC):
                si = next_sq()
                ACT(sqb[si][:, 0:n], src_fn(kc), AF.Square, reads(kc), [r_sqb[si]])
                MM(psb[pi][:, 0:n], ones_b[:], sqb[si][:, 0:n], kc == 0, kc == KC - 1, [r_sqb[si], r_const], [r_ps[pi]])
            ACT(rs[:, 0:n], psb[pi][:, 0:n], AF.Sqrt, [r_ps[pi], r_const], [r_rs], bias=cst[:, 0:1], scale=1.0)
            RECIP(rs[:, 0:n], rs[:, 0:n], [r_rs], [r_rs])

        def prenorm(tl, Amod, shift_base, l):
            for (a, b, w) in tl:
                n = b - a
                rstd_of(lambda kc: xs[:, kc, a:b], n, lambda kc: trs(r_xs, kc, a, b))
                for kc in range(KC):
                    ti = next_tf()
                    TT("dve", tmpf[ti][:, 0:n], xs[:, kc, a:b], rs[:, 0:n], ALU.mult,
                       trs(r_xs, kc, a, b) + [r_rs], [r_tmpf[ti]])
                    ACT(hx[:, kc, a:b], tmpf[ti][:, 0:n], AF.Identity, [r_tmpf[ti], r_lay, r_mod], trs(r_hx, kc, a, b),
                        bias=mod[:, l, shift_base + kc, w:w + 1], scale=Amod[:, kc, w:w + 1])

        def resid_update(tl, src_fn, src_reads, Gm):
            for (a, b, w) in tl:
                n = b - a
                rstd_of(lambda kc: src_fn(kc, a, b), n, lambda kc: src_reads(kc, a, b))
                for kc in range(KC):
                    ti = next_tf()
                    TT("dve", tmpf[ti][:, 0:n], src_fn(kc, a, b), rs[:, 0:n], ALU.mult,
                       src_reads(kc, a, b) + [r_rs], [r_tmpf[ti]])
                    STT("dve", xs[:, kc, a:b], tmpf[ti][:, 0:n], Gm[:, kc, w:w + 1], xs[:, kc, a:b], ALU.mult, ALU.add,
                        [r_tmpf[ti], r_lay] + trs(r_xs, kc, a, b), trs(r_xs, kc, a, b))

        def dump(name, src_fn, reads_fn):
            if not dbg:
                return
            for kc in range(KC):
                DMA("pool", dbg_out[name][kc], src_fn(kc), reads_fn(kc), [])

        def maybe_stop(k):
            if stop is not None and stop == k:
                raise _Stop()

        try:
            P.barrier()
            for l in range(DEPTH):
                need_ctx = l < DEPTH - 1
                TL_all = tiles_for(True)
                TL = tiles_for(need_ctx)
                j0 = 0 if need_ctx else 2

                def mk(dst, gidx, mbase, plus1):
                    for w in range(2):
                        if plus1:
                            STT("dve", dst[:, :, w], mod[:, l, mbase:mbase + KC, w], 1.0, gv[:, l, gidx, :], ALU.add, ALU.mult,
                                [r_mod, r_const, r_lay], [r_lay])
                        else:
                            TT("dve", dst[:, :, w], mod[:, l, mbase:mbase + KC, w], gv[:, l, gidx, :], ALU.mult,
                               [r_mod, r_const, r_lay], [r_lay])
                    TS("dve", dst[:], dst[:], 32.0, None, ALU.mult, None, [r_lay], [r_lay])

                mk(A1, 0, 8, True)
                mk(G1, 1, 16, False)
                mk(A2, 2, 32, True)
                mk(G2, 3, 40, False)

                prenorm(TL_all, A1, 0, l)
                maybe_stop(10 * l + 1)
                if l == 0:
                    dump("d_hx", lambda kc: hx[:, kc, :], lambda kc: trs(r_hx, kc, 0, NT))

                o_w = 0
                wbuf = [arena(o_w + i * 1024, 1024).rearrange("p (k c) -> p k c", c=128) for i in range(4)]
                r_wbuf = [R() for _ in range(4)]
                wcnt = {"n": 0}

                def load_w(src):
                    i = wcnt["n"] % 4
                    wcnt["n"] += 1
                    DMA("pool", wbuf[i], src, [], [r_wbuf[i]])
                    return i

                o_up = 4096
                upad = arena(o_up, 2 * UPW).rearrange("p (c t) -> p c t", t=UPW)
                r_up = R()
                o_vb = o_up + 2 * UPW
                vbuf = arena(o_vb, 2 * NT, F32).rearrange("p (c t) -> p c t", t=NT)
                r_vb = [R(), R()]
                o_cs = o_vb + 4 * NT
                wdw_s = arena(o_cs, 2 * CONV_K, F32).rearrange("p (c k) -> p c k", k=CONV_K)
                cvp_s = arena(o_cs + 4 * CONV_K, 6, F32).rearrange("p (a c) -> p a c", c=2)
                r_cs = R()
                lnt = [ym_flat[:, i * 1024:(i + 1) * 1024].bitcast(F32) for i in range(5)]
                r_lnt = [R() for _ in range(5)]
                dgm = ym_flat[:, 5120:5120 + 2 * CONV_K * 128].rearrange("p (c k m) -> p c k m", k=CONV_K, m=128)
                r_dg = R()

                DMA("sp", wdw_s, wdw[l], [], [r_cs])
                DMA("sp", cvp_s, cvp[l], [], [r_cs])
                MS("dve", upad, 0.0, [r_up])
                for cc in range(2):
                    for k in range(CONV_K):
                        TS("dve", dgm[:, cc, k, :], ident_f[:], wdw_s[:, cc, k:k + 1], None, ALU.mult, None,
                           [r_const, r_cs], [r_dg])

                def upos(a):
                    return a + PADC if a < CTX else (CTX + 2 * PADC) + (a - CTX) + PADC

                for cc in range(2):
                    ia = load_w(winc[l, 10 + cc])
                    ig = load_w(winc[l, 12 + cc])
                    for (a, b, w) in TL:
                        n = b - a
                        pa = next_ps()
                        for kc in range(KC):
                            MM(psb[pa][:, 0:n], wbuf[ia][:, kc, :], hx[:, kc, a:b], kc == 0, kc == KC - 1,
                               [r_wbuf[ia]] + trs(r_hx, kc, a, b), [r_ps[pa]])
                        pg = next_ps()
                        for kc in range(KC):
                            MM(psb[pg][:, 0:n], wbuf[ig][:, kc, :], hx[:, kc, a:b], kc == 0, kc == KC - 1,
                               [r_wbuf[ig]] + trs(r_hx, kc, a, b), [r_ps[pg]])
                        ti = next_tf()
                        ACT(tmpf[ti][:, 0:n], psb[pg][:, 0:n], AF.Sigmoid, [r_ps[pg]], [r_tmpf[ti]])
                        TT("dve", upad[:, cc, upos(a):upos(a) + n], psb[pa][:, 0:n], tmpf[ti][:, 0:n], ALU.mult,
                           [r_ps[pa], r_tmpf[ti]], [r_up])
                for cc in range(2):
                    for (a, b, w) in TL:
                        n = b - a
                        pi = next_ps()
                        p0 = upos(a) - PADC
                        for k in range(CONV_K):
                            MM(psb[pi][:, 0:n], dgm[:, cc, k, :], upad[:, cc, p0 + k:p0 + k + n], k == 0, k == CONV_K - 1,
                               [r_dg, r_up], [r_ps[pi]])
                        ACT(vbuf[:, cc, a:b], psb[pi][:, 0:n], AF.Identity, [r_ps[pi], r_cs], [r_vb[cc]],
                            bias=cvp_s[:, 0, cc:cc + 1], scale=1.0)
                for (a, b, w) in TL:
                    n = b - a
                    p1 = next_ps()
                    p2 = next_ps()
                    for cc in range(2):
                        MM(psb[p1][:, 0:n], ones_f[:], vbuf[:, cc, a:b], cc == 0, cc == 1, [r_const, r_vb[cc]], [r_ps[p1]])
                    for cc in range(2):
                        TT("dve", lnt[0][:, 0:n], vbuf[:, cc, a:b], vbuf[:, cc, a:b], ALU.mult, [r_vb[cc]], [r_lnt[0]])
                        MM(psb[p2][:, 0:n], ones_f[:], lnt[0][:, 0:n], cc == 0, cc == 1, [r_const, r_lnt[0]], [r_ps[p2]])
                    mean = lnt[1]
                    TS("dve", mean[:, 0:n], psb[p1][:, 0:n], 1.0 / 256.0, None, ALU.mult, None, [r_ps[p1]], [r_lnt[1]])
                    TT("dve", lnt[2][:, 0:n], mean[:, 0:n], mean[:, 0:n], ALU.mult, [r_lnt[1]], [r_lnt[2]])
                    STT("dve", lnt[2][:, 0:n], psb[p2][:, 0:n], 1.0 / 256.0, lnt[2][:, 0:n], ALU.mult, ALU.subtract,
                        [r_ps[p2], r_lnt[2]], [r_lnt[2]])
                    ACT(lnt[2][:, 0:n], lnt[2][:, 0:n], AF.Sqrt, [r_lnt[2], r_const], [r_lnt[2]], bias=cst[:, 1:2], scale=1.0)
                    RECIP(lnt[2][:, 0:n], lnt[2][:, 0:n], [r_lnt[2]], [r_lnt[2]])
                    for cc in range(2):
                        TT("dve", lnt[3 + cc][:, 0:n], vbuf[:, cc, a:b], mean[:, 0:n], ALU.subtract,
                           [r_vb[cc], r_lnt[1]], [r_lnt[3 + cc]])
                        TT("dve", lnt[3 + cc][:, 0:n], lnt[3 + cc][:, 0:n], lnt[2][:, 0:n], ALU.mult,
                           [r_lnt[3 + cc], r_lnt[2]], [r_lnt[3 + cc]])
                        ACT(ym[:, 6 + cc, a:b], lnt[3 + cc][:, 0:n], AF.Silu, [r_lnt[3 + cc], r_cs], trs(r_ym, 6 + cc, a, b),
                            bias=cvp_s[:, 2, cc:cc + 1], scale=cvp_s[:, 1, cc:cc + 1])
                P.barrier()

                maybe_stop(10 * l + 2)
                o_uf = 4096
                uF = arena(o_uf, 2 * NT).rearrange("p (c t) -> p c t", t=NT)
                r_uf = [R(), R()]
                o_dp = o_uf + 2 * NT
                dpc = [arena(o_dp + i * 4096, 4096).rearrange("p (s k t) -> p s k t", s=2, k=4) for i in range(3)]
                r_dpc = [R() for _ in range(3)]
                o_dc = o_dp + 3 * 4096
                dcc = arena(o_dc, 1024).rearrange("p (s k t) -> p s k t", s=2, k=2)
                r_dcc = R()
                AB = ym_flat[:, 0:NJ * 512].rearrange("p (j c m) -> p j c m", c=2, m=256)
                r_ab = [R() for _ in range(NJ)]

                for cc in range(2):
                    iw = load_w(winc[l, 8 + cc])
                    for (a, b, w) in TL:
                        n = b - a
                        pi = next_ps()
                        for kc in range(KC):
                            MM(psb[pi][:, 0:n], wbuf[iw][:, kc, :], hx[:, kc, a:b], kc == 0, kc == KC - 1,
                               [r_wbuf[iw]] + trs(r_hx, kc, a, b), [r_ps[pi]])
                        ACT(uF[:, cc, a:b], psb[pi][:, 0:n], AF.Copy, [r_ps[pi]], [r_uf[cc]])
                for j in range(j0, NJ):
                    pi = next_ps()
                    for cc in range(2):
                        MM(psb[pi][:, cc * 256:(cc + 1) * 256], uF[:, cc, j * 128:(j + 1) * 128], cs64[:], True, True,
                           [r_uf[cc], r_const], [r_ps[pi]])
                    CP("dve", AB[:, j].rearrange("p c m -> p (c m)"), psb[pi][:, :], [r_ps[pi]], [r_ab[j]])
                npc = 0
                for tq in range(4):
                    pp = [next_ps(), next_ps()]
                    for kg in range(4):
                        i = npc % 3
                        npc += 1
                        DMA("sp", dpc[i].rearrange("p s k t -> p (s k t)"),
                            dftx_d[tq, kg].rearrange("p s k t -> p (s k t)"), [], [r_dpc[i]])
                        for ki in range(4):
                            j = 2 + kg * 4 + ki
                            for s in range(2):
                                first = (kg == 0 and ki == 0 and s == 0)
                                last = (kg == 3 and ki == 3 and s == 1)
                                for cc in range(2):
                                    MM(psb[pp[cc]][:, :], AB[:, j, cc, s * 128:(s + 1) * 128], dpc[i][:, s, ki, :], first, last,
                                       [r_ab[j], r_dpc[i]], [r_ps[pp[cc]]])
                    for cc in range(2):
                        a = CTX + tq * 512
                        ACT(ym[:, 4 + cc, a:a + 512], psb[pp[cc]][:, :], AF.Copy, [r_ps[pp[cc]]], trs(r_ym, 4 + cc, a, a + 512))
                if need_ctx:
                    DMA("sp", dcc.rearrange("p s k t -> p (s k t)"), dftc_d.rearrange("p s k t -> p (s k t)"), [], [r_dcc])
                    pp = [next_ps(), next_ps()]
                    for ki in range(2):
                        for s in range(2):
                            for cc in range(2):
                                MM(psb[pp[cc]][:, 0:256], AB[:, ki, cc, s * 128:(s + 1) * 128], dcc[:, s, ki, :],
                                   ki == 0 and s == 0, ki == 1 and s == 1, [r_ab[ki], r_dcc], [r_ps[pp[cc]]])
                    for cc in range(2):
                        ACT(ym[:, 4 + cc, 0:CTX], psb[pp[cc]][:, 0:256], AF.Copy, [r_ps[pp[cc]]], trs(r_ym, 4 + cc, 0, CTX))

                P.barrier()
                maybe_stop(10 * l + 3)
                o_s = 0
                dgq = [arena(o_s + i * 256, 128, F32) for i in range(2)]
                qs = [arena(o_s + 512 + i * 128, 128) for i in range(2)]
                sTt = [arena(o_s + 768 + i * 128, 128) for i in range(2)]
                kts = [arena(o_s + 1024 + i * 128, 128) for i in range(2)]
                Et = [arena(o_s + 1280 + i * 128, 128) for i in range(2)]
                Cst = [arena(o_s + 1536 + i * 260, 130, F32) for i in range(2)]
                Cbf = [arena(o_s + 2056 + i * 130, 130) for i in range(2)]
                rden = [arena(o_s + 2316 + i * 4, 2, F32) for i in range(2)]
                st1 = arena(o_s + 2324, 2 * NJ, F32)
                st2 = arena(o_s + 2396, 2 * NJ, F32)
                r_dgq, r_qs, r_sT, r_kts = [R(), R()], [R(), R()], [R(), R()], [R(), R()]
                r_dgb, r_Et = [R(), R()], [R(), R()]
                r_C, r_Cb, r_rden = [R(), R()], [R(), R()], [R(), R()]
                r_st = R()
                wqk_raw = arena(2468, 1024).rearrange("p (k c) -> p k c", c=128)
                r_wqk = R()
                w3 = arena(3492, 3072).rearrange("p (t k c) -> p t k c", t=3, k=KC)
                wtap = arena(6564, 384, F32).rearrange("p (t c) -> p t c", c=128)
                r_w3, r_wtap = R(), R()
                o_g = 7332
                Gtok = arena(o_g, NJ * 16, F32).rearrange("p (j g) -> p j g", g=16)
                dgb = [arena(o_g + i * 256, 128, F32) for i in range(2)]
                LF = arena(o_g + 576, NJ * 8, F32).rearrange("p (j g) -> p j g", g=8)
                CM = arena(o_g + 864, NJ * 8, F32).rearrange("p (j g) -> p j g", g=8)
                Bc = arena(o_g + 1152, NJ * 8, F32).rearrange("p (j g) -> p j g", g=8)
                EB = arena(o_g + 1440, NJ * 8, F32).rearrange("p (j g) -> p j g", g=8)
                EBL = arena(o_g + 1728, NJ * 8, F32).rearrange("p (j g) -> p j g", g=8)
                ECL = arena(o_g + 2016, NJ * 8, F32).rearrange("p (j g) -> p j g", g=8)
                bg_s = arena(o_g + 2304, 16, F32)
                wg_s = arena(o_g + 2336, KC * 16).rearrange("p (k g) -> p k g", g=16)
                gmn_s = arena(o_g + 2464, 512, F32)
                r_gate = R()
                o_h = o_g + 3488
                qT = arena(o_h, NT)
                kT = arena(o_h + NT, NT)
                vaug = arena(o_h + 2 * NT, NJ * 130).rearrange("p (j e) -> p j e", e=130)
                sgo = arena(o_h + 2 * NT + 2340, NT).rearrange("p (j e) -> p j e", e=128)
                hsum = arena(o_h + 3 * NT + 2340, NT, F32).rearrange("p (j e) -> p j e", e=128)
                assert o_h + 5 * NT + 2340 <= SCR_ARENA, (o_h + 5 * NT + 2340)
                r_q, r_k, r_v, r_sg, r_hs = R(), R(), R(), R(), R()
                wvo_s = ym_flat[:, 3 * NT:3 * NT + KC * 256].rearrange("p (k c) -> p k c", c=256)
                r_wvo = R()

                DMA("pool", wg_s, wgate[l], [], [r_gate])
                DMA("sp", bg_s, bgate[l].partition_broadcast(128), [], [r_gate])
                DMA("sp", gmn_s, gmn[l].partition_broadcast(128), [], [r_gate])
                for jg in range(0, NJ, 6):
                    pi = next_ps()
                    for jj in range(6):
                        j = jg + jj
                        for kc in range(KC):
                            MM(psb[pi][:, jj * 16:(jj + 1) * 16], hx[:, kc, j * 128:(j + 1) * 128], wg_s[:, kc, :], kc == 0, kc == KC - 1,
                               [r_gate] + trs(r_hx, kc, j * 128, (j + 1) * 128), [r_ps[pi]])
                    TT("dve", Gtok[:, jg:jg + 6, :], psb[pi][:, 0:96].rearrange("p (j g) -> p j g", g=16),
                       bg_s.unsqueeze(1).to_broadcast([128, 6, 16]), ALU.add, [r_ps[pi], r_gate], [r_gate])
                CP("dve", CM[:, :, 0:4], Gtok[:, :, 0:4], [r_gate], [r_gate])
                CP("dve", CM[:, :, 4:8], Gtok[:, :, 8:12], [r_gate], [r_gate])
                ACT(LF[:, :, 0:4], Gtok[:, :, 4:8], AF.Abs, [r_gate], [r_gate])
                ACT(LF[:, :, 4:8], Gtok[:, :, 12:16], AF.Abs, [r_gate], [r_gate])
                ACT(LF[:], LF[:], AF.Exp, [r_gate], [r_gate], scale=-1.0)
                ACT(LF[:], LF[:], AF.Ln, [r_gate, r_const], [r_gate], bias=cst[:, 2:3], scale=1.0)
                TS("dve", EB[:, :, 0:4], Gtok[:, :, 4:8], 0.0, None, ALU.min, None, [r_gate], [r_gate])
                TS("dve", EB[:, :, 4:8], Gtok[:, :, 12:16], 0.0, None, ALU.min, None, [r_gate], [r_gate])
                TT("dve", LF[:], EB[:], LF[:], ALU.subtract, [r_gate], [r_gate])
                pi = next_ps()
                pbv = psb[pi][:, 0:NJ * 8].rearrange("p (j g) -> p j g", g=8)
                for j in range(NJ):
                    MM(pbv[:, j, 0:4], umask_f[:], LF[:, j, 0:4], True, True, [r_const, r_gate], [r_ps[pi]])
                    MM(pbv[:, j, 4:8], lmask_f[:], LF[:, j, 4:8], True, True, [r_const, r_gate], [r_ps[pi]])
                CP("dve", Bc[:], pbv, [r_ps[pi]], [r_gate])
                pi = next_ps()
                ptv = psb[pi][:, 0:NJ * 8].rearrange("p (j g) -> p j g", g=8)
                MM(psb[pi][:, 0:NJ * 8], ones_f[:], LF[:].rearrange("p j g -> p (j g)"), True, True, [r_const, r_gate], [r_ps[pi]])
                ACT(EBL[:], ptv, AF.Exp, [r_ps[pi]], [r_gate])
                ACT(EB[:], Bc[:], AF.Exp, [r_gate], [r_gate])
                TT("dve", CM[:], CM[:], Bc[:], ALU.subtract, [r_gate], [r_gate])
                TT("dve", ECL[:], CM[:], ptv, ALU.add, [r_gate, r_ps[pi]], [r_gate])
                ACT(ECL[:], ECL[:], AF.Exp, [r_gate], [r_gate])
                P.barrier()

                MS("dve", vaug[:, :, 128:129], 1.0, [r_v])

                for h in range(NH):
                    for qk in range(2):
                        dst, r_dst = (qT, r_q) if qk == 0 else (kT, r_k)
                        ci = qk * 4 + h
                        DMA("pool", wqk_raw, winc[l, ci], [], [r_wqk])
                        DMA("sp", wtap, wqkc[l, ci].partition_broadcast(128), [], [r_wtap])
                        if qk == 1:
                            TS("dve", wtap, wtap, HD ** -0.5, None, ALU.mult, None, [r_wtap], [r_wtap])
                        for t in range(3):
                            TT("dve", w3[:, t], wqk_raw, wtap[:, t, :].unsqueeze(1).to_broadcast([128, KC, 128]), ALU.mult,
                               [r_wqk, r_wtap], [r_w3])
                        for (a, b, w) in TL_all:
                            n = b - a
                            s0, s1 = (0, CTX) if a < CTX else (CTX, NT)
                            pi = next_ps()
                            for kc in range(KC):
                                MM(psb[pi][:, 0:n], w3[:, 1, kc, :], hx[:, kc, a:b], kc == 0, False,
                                   [r_w3] + trs(r_hx, kc, a, b), [r_ps[pi]])
                            lo = max(a, s0 + 1)
                            for kc in range(KC):
                                MM(psb[pi][:, lo - a:n], w3[:, 0, kc, :], hx[:, kc, lo - 1:b - 1], False, False,
                                   [r_w3] + trs(r_hx, kc, lo - 1, b - 1), [r_ps[pi]])
                            hi = min(b, s1 - 1)
                            for kc in range(KC):
                                MM(psb[pi][:, 0:hi - a], w3[:, 2, kc, :], hx[:, kc, a + 1:hi + 1], False, kc == KC - 1,
                                   [r_w3] + trs(r_hx, kc, a + 1, hi + 1), [r_ps[pi]])
                            ACT(dst[:, a:b], psb[pi][:, 0:n], AF.Copy, [r_ps[pi]], [r_dst])
                    DMA("pool", wvo_s, wvo[l, h], [], [r_wvo])
                    for j in range(NJ):
                        pi = next_ps()
                        for kc in range(KC):
                            MM(psb[pi][:, 0:256], hx[:, kc, j * 128:(j + 1) * 128], wvo_s[:, kc, :], kc == 0, kc == KC - 1,
                               [r_wvo] + trs(r_hx, kc, j * 128, (j + 1) * 128), [r_ps[pi]])
                        CP("dve", vaug[:, j, 0:128], psb[pi][:, 0:128], [r_ps[pi]], [r_v])
                        ACT(sgo[:, j, :], psb[pi][:, 128:256], AF.Sigmoid, [r_ps[pi]], [r_sg])
                        TT("dve", sgo[:, j, :], sgo[:, j, :], gmn_s[:, h * 128:(h + 1) * 128], ALU.mult, [r_sg, r_gate], [r_sg])
                    MS("dve", hsum, 0.0, [r_hs])
                    for d_ in range(2):
                        MS("dve", Cst[d_], 0.0, [r_C[d_]])
                        MS("dve", Cbf[d_], 0.0, [r_Cb[d_]])
                    order = [list(range(NJ)), [1, 0] + list(range(NJ - 1, 1, -1))]
                    t0f = tmpf[0]
                    t1b = tmpf[1][:, :].bitcast(BF16)
                    dgb2 = [dgb, [dgq[0], dgq[1]]]
                    sT2 = [sTt, [t1b[:, 256:384], t1b[:, 384:512]]]
                    kts2 = [kts, [t1b[:, 512:640], t1b[:, 640:768]]]
                    Et2 = [Et, [t1b[:, 768:896], t1b[:, 896:1024]]]
                    ndi = [t0f[:, 0:130], t0f[:, 130:260]]
                    r_ndi = [R(), R()]
                    t0b = t0f[:, 260:390].bitcast(BF16)
                    Cbf2 = [Cbf, [t0b[:, 0:130], t0b[:, 130:260]]]
                    r_Cb2 = [r_Cb, [R(), R()]]
                    rr = lambda: [[R(), R()], [R(), R()]]
                    r_dgb2, r_sT2, r_kts2, r_Et2 = rr(), rr(), rr(), rr()

                    def info(step, d_):
                        j = order[d_][step]
                        return j, step % 2, d_ * 4 + h, (need_ctx or j >= 2), step == NJ - 1

                    ps_hold = {}

                    def a1(step, d_):
                        j, bs, col, need_out, is_last = info(step, d_)
                        if need_out:
                            TS("dve", dgb2[bs][d_], ident_f[:], Bc[:, j, col:col + 1], None, ALU.mult, None,
                               [r_const, r_gate], [r_dgb2[bs][d_]])
                        yield

                    def a2a(step, d_):
                        j, bs, col, need_out, is_last = info(step, d_)
                        c0, c1 = j * 128, (j + 1) * 128
                        if need_out:
                            pd_ = d_
                            MM(psb[pd_][:, 0:128], ones_f[:], dgb2[bs][d_], True, False, [r_const, r_dgb2[bs][d_]], [r_ps[pd_]])
                            MM(psb[pd_][:, 0:128], ident_b[:], (negm_f if d_ == 0 else negm_b)[:], False, True,
                               [r_const], [r_ps[pd_]])
                            yield
                            ACT(Et2[bs][d_], psb[pd_][:, 0:128], AF.Exp, [r_ps[pd_], r_gate], [r_Et2[bs][d_]],
                                bias=CM[:, j, col:col + 1], scale=1.0)
                            yield
                            p_s = 2 + d_
                            MM(psb[p_s][:, 0:128], kT[:, c0:c1], qT[:, c0:c1], True, True, [r_k, r_q], [r_ps[p_s]])
                            yield
                        if not is_last:
                            p_t = next_ps()
                            ptb = psb[p_t][:, 0:64].bitcast(BF16)
                            TR(ptb, kT[:, c0:c1], ident_b[:], [r_k, r_const], [r_ps[p_t]])
                            yield
                            ACT(kts2[bs][d_], ptb, AF.Copy, [r_ps[p_t], r_gate], [r_kts2[bs][d_]], scale=ECL[:, j, col:col + 1])
                            yield

                    def a2b(step, d_):
                        j, bs, col, need_out, is_last = info(step, d_)
                        if need_out:
                            p_s = 2 + d_
                            TT("dve", sT2[bs][d_], psb[p_s][:, 0:128], Et2[bs][d_], ALU.mult,
                               [r_ps[p_s], r_Et2[bs][d_]], [r_sT2[bs][d_]])
                        yield

                    def stage_b(step, d_):
                        j, bs, col, need_out, is_last = info(step, d_)
                        cur, nxt = Cbf2[step % 2][d_], Cbf2[(step + 1) % 2][d_]
                        r_cur, r_nxt = r_Cb2[step % 2][d_], r_Cb2[(step + 1) % 2][d_]
                        if not is_last:
                            p_u = next_ps()
                            MM(psb[p_u][:, 0:129], kts2[bs][d_], vaug[:, j, 0:129], True, True, [r_kts2[bs][d_], r_v], [r_ps[p_u]])
                            yield
                            STT("dve", Cst[d_][:, 0:129], Cst[d_][:, 0:129], EBL[:, j, col:col + 1], psb[p_u][:, 0:129],
                                ALU.mult, ALU.add, [r_C[d_], r_gate, r_ps[p_u]], [r_C[d_]])
                            yield
                            ACT(nxt[:, 0:129], Cst[d_][:, 0:129], AF.Copy, [r_C[d_]], [r_nxt])
                            yield
                        if need_out:
                            c0, c1 = j * 128, (j + 1) * 128
                            p_i = next_ps()
                            MM(psb[p_i][:, 0:129], qT[:, c0:c1], cur[:, 0:129], True, True, [r_q, r_cur], [r_ps[p_i]])
                            p_n = next_ps()
                            MM(psb[p_n][:, 0:129], sT2[bs][d_], vaug[:, j, 0:129], True, True, [r_sT2[bs][d_], r_v], [r_ps[p_n]])
                            yield
                            ACT(ndi[d_][:, 0:129], psb[p_i][:, 0:129], AF.Copy, [r_ps[p_i], r_gate], [r_ndi[d_]], scale=EB[:, j, col:col + 1])
                            yield
                            TT("dve", ndi[d_][:, 0:129], ndi[d_][:, 0:129], psb[p_n][:, 0:129], ALU.add, [r_ndi[d_], r_ps[p_n]], [r_ndi[d_]])
                            yield
                            STT("dve", rden[d_][:, 0:1], ndi[d_][:, 128:129], -1.0, ndi[d_][:, 128:129], ALU.mult, ALU.max,
                                [r_ndi[d_]], [r_rden[d_]])
                            yield
                            TS("dve", rden[d_][:, 0:1], rden[d_][:, 0:1], 1.0, None, ALU.max, None,
                               [r_rden[d_]], [r_rden[d_]])
                            yield
                            RECIP(rden[d_][:, 0:1], rden[d_][:, 0:1], [r_rden[d_]], [r_rden[d_]])
                            yield
                            STT("dve", hsum[:, j, :], ndi[d_][:, 0:128], rden[d_][:, 0:1], hsum[:, j, :], ALU.mult, ALU.add,
                                [r_ndi[d_], r_rden[d_], r_hs], [r_hs])
                            yield

                    def interleave(*gens):
                        gens = list(gens)
                        while gens:
                            for g in list(gens):
                                try:
                                    next(g)
                                except StopIteration:
                                    gens.remove(g)

                    for bnk in range(4):
                        ps_reserved.add(bnk)
                    interleave(a1(0, 0), a1(0, 1))
                    interleave(a2a(0, 0), a2a(0, 1))
                    interleave(a2b(0, 0), a2b(0, 1))
                    interleave(a1(1, 0), a1(1, 1))
                    for step in range(NJ):
                        if step + 1 < NJ:
                            interleave(a2a(step + 1, 0), a2a(step + 1, 1))
                        interleave(stage_b(step, 0), stage_b(step, 1))
                        if step + 1 < NJ:
                            interleave(a2b(step + 1, 0), a2b(step + 1, 1))
                        if step + 2 < NJ:
                            interleave(a1(step + 2, 0), a1(step + 2, 1))
                    for bnk in range(4):
                        ps_reserved.discard(bnk)
                    nj = NJ - j0
                    hv = hsum[:, j0:NJ, :]
                    sqv = ym[:, h, j0 * 128:NT].rearrange("p (j e) -> p j e", e=128)
                    ymh_res = trs(r_ym, h, j0 * 128, NT) + ([r_wvo] if h == 3 else [])
                    P.op("dve", (lambda hv=hv, nj=nj: lambda e: e.tensor_reduce(out=st1[:, 0:nj], in_=hv, axis=AX.X, op=ALU.add))(),
                         [r_hs], [r_st])
                    TT("dve", sqv, hv, hv, ALU.mult, [r_hs], ymh_res)
                    P.op("dve", (lambda sqv=sqv, nj=nj: lambda e: e.tensor_reduce(out=st1[:, NJ:NJ + nj], in_=sqv, axis=AX.X, op=ALU.add))(),
                         ymh_res, [r_st])
                    mean_ = st2[:, 0:nj]
                    var_ = st2[:, NJ:NJ + nj]
                    TS("dve", mean_, st1[:, 0:nj], 1.0 / HD, None, ALU.mult, None, [r_st], [r_st])
                    TT("dve", var_, mean_, mean_, ALU.mult, [r_st], [r_st])
                    STT("dve", var_, st1[:, NJ:NJ + nj], 1.0 / HD, var_, ALU.mult, ALU.subtract, [r_st], [r_st])
                    ACT(var_, var_, AF.Sqrt, [r_st, r_const], [r_st], bias=cst[:, 1:2], scale=1.0)
                    RECIP(var_, var_, [r_st], [r_st])
                    TT("dve", hv, hv, mean_.unsqueeze(2).to_broadcast([128, nj, 128]), ALU.subtract, [r_hs, r_st], [r_hs])
                    TT("dve", hv, hv, var_.unsqueeze(2).to_broadcast([128, nj, 128]), ALU.mult, [r_hs, r_st], [r_hs])
                    TT("dve", sgo[:, j0:NJ, :], hv, sgo[:, j0:NJ, :], ALU.mult, [r_hs, r_sg], [r_sg])
                    for jg in range(j0, NJ, 4):
                        p_t = next_ps()
                        ptb = psb[p_t][:, 0:256].bitcast(BF16)
                        jn = min(4, NJ - jg)
                        for jj in range(jn):
                            TR(ptb[:, jj * 128:(jj + 1) * 128], sgo[:, jg + jj, :], ident_b[:], [r_sg, r_const], [r_ps[p_t]])
                        ACT(ym[:, h, jg * 128:(jg + jn) * 128], ptb[:, 0:jn * 128], AF.Copy, [r_ps[p_t]],
                            trs(r_ym, h, jg * 128, (jg + jn) * 128) + ([r_wvo] if h == 3 else []))

                if l == 0:
                    dump("d_ym", lambda kc: ym[:, kc, :], lambda kc: trs(r_ym, kc, 0, NT))

                P.barrier()
                maybe_stop(10 * l + 4)
                o_wo = 4096
                wo_s = arena(o_wo, KC * KC * 128).rearrange("p (o k c) -> p o k c", o=KC, k=KC)
                r_wo = R()
                obuf = arena(o_wo + 8192, KC * 512, F32).rearrange("p (o t) -> p o t", t=512)
                r_ob = [R() for _ in range(KC)]
                DMA("pool", wo_s.rearrange("p o k c -> p (o k c)"), woutc[l].rearrange("p o k c -> p (o k c)"), [],
                    [r_wo])
                for (a, b, w) in TL:
                    n = b - a
                    for oc in range(KC):
                        pi = next_ps()
                        for kc in range(KC):
                            MM(psb[pi][:, 0:n], wo_s[:, oc, kc, :], ym[:, kc, a:b], kc == 0, kc == KC - 1,
                               [r_wo] + trs(r_ym, kc, a, b), [r_ps[pi]])
                        ACT(obuf[:, oc, 0:n], psb[pi][:, 0:n], AF.Copy, [r_ps[pi]], [r_ob[oc]])
                    resid_update([(a, b, w)], lambda kc, a_, b_: obuf[:, kc, 0:b_ - a_], lambda kc, a_, b_: [r_ob[kc]], G1)
                if l == 0:
                    dump("d_x1", lambda kc: xs[:, kc, :], lambda kc: trs(r_xs, kc, 0, NT))

                P.barrier()
                maybe_stop(10 * l + 5)
                hT = scr[:, 0:24576].rearrange("p (f t) -> p f t", t=768)
                r_hT = [R() for _ in range(32)]
                ob2 = scr[:, 24576:30720].rearrange("p (o t) -> p o t", t=768)
                r_ob2 = [R() for _ in range(KC)]
                w1b = [scr[:, 30720 + i * 2048:30720 + (i + 1) * 2048].rearrange("p (g k c) -> p g k c", g=2, k=KC) for i in range(2)]
                w2b = [scr[:, 34816 + i * 4096:34816 + (i + 1) * 4096].rearrange("p (f c) -> p f c", c=128) for i in range(2)]
                r_w1b = [R(), R()]
                r_w2b = [R(), R()]
                if need_ctx:
                    supers = [[(0, 256, 1), (256, 768, 0)], [(768, 1280, 0), (1280, 1536, 0)], [(1536, 2048, 0), (2048, 2304, 0)]]
                else:
                    supers = [[(256, 768, 0), (768, 1024, 0)], [(1024, 1536, 0), (1536, 1792, 0)], [(1792, 2304, 0)]]
                n1 = 0
                n2 = 0
                for sup in supers:
                    a0 = sup[0][0]
                    prenorm(sup, A2, 24, l)
                    for g in range(16):
                        i = n1 % 2
                        n1 += 1
                        DMA("pool", w1b[i].rearrange("p g k c -> p (g k c)"), w1c[l, g].rearrange("p g k c -> p (g k c)"), [],
                            [r_w1b[i]])
                        for f2 in range(2):
                            f = g * 2 + f2
                            for (a, b, w) in sup:
                                n = b - a
                                pi = next_ps()
                                for kc in range(KC):
                                    MM(psb[pi][:, 0:n], w1b[i][:, f2, kc, :], hx[:, kc, a:b], kc == 0, kc == KC - 1,
                                       [r_w1b[i]] + trs(r_hx, kc, a, b), [r_ps[pi]])
                                ti = next_tf()
                                ACT(tmpf[ti][:, 0:n], psb[pi][:, 0:n], AF.Relu, [r_ps[pi]], [r_tmpf[ti]])
                                TT("dve", hT[:, f, a - a0:b - a0], tmpf[ti][:, 0:n], tmpf[ti][:, 0:n], ALU.mult, [r_tmpf[ti]], [r_hT[f]])
                    ssb = []
                    for _ in sup:
                        pss = next_ps()
                        ps_reserved.add(pss)
                        ssb.append(pss)
                    for oc in range(KC):
                        i = n2 % 2
                        n2 += 1
                        DMA("pool", w2b[i].rearrange("p f c -> p (f c)"), w2c[l, oc].rearrange("p f c -> p (f c)"), [],
                            [r_w2b[i]])
                        for si_, (a, b, w) in enumerate(sup):
                            n = b - a
                            pi = next_ps()
                            for f in range(32):
                                MM(psb[pi][:, 0:n], w2b[i][:, f, :], hT[:, f, a - a0:b - a0], f == 0, f == 31,
                                   [r_w2b[i], r_hT[f]], [r_ps[pi]])
                            ACT(ob2[:, oc, a - a0:b - a0], psb[pi][:, 0:n], AF.Copy, [r_ps[pi]], [r_ob2[oc]])
                            sq_i = next_sq()
                            ACT(sqb[sq_i][:, 0:n], psb[pi][:, 0:n], AF.Square, [r_ps[pi]], [r_sqb[sq_i]])
                            MM(psb[ssb[si_]][:, 0:n], ones_b[:], sqb[sq_i][:, 0:n], oc == 0, oc == KC - 1,
                               [r_sqb[sq_i], r_const], [r_ps[ssb[si_]]])
                    for si_, (a, b, w) in enumerate(sup):
                        n = b - a
                        ACT(rs[:, 0:n], psb[ssb[si_]][:, 0:n], AF.Sqrt, [r_ps[ssb[si_]], r_const], [r_rs], bias=cst[:, 0:1], scale=1.0)
                        RECIP(rs[:, 0:n], rs[:, 0:n], [r_rs], [r_rs])
                        for kc in range(KC):
                            ti = next_tf()
                            TT("dve", tmpf[ti][:, 0:n], ob2[:, kc, a - a0:b - a0], rs[:, 0:n], ALU.mult,
                               [r_ob2[kc], r_rs], [r_tmpf[ti]])
                            STT("dve", xs[:, kc, a:b], tmpf[ti][:, 0:n], G2[:, kc, w:w + 1], xs[:, kc, a:b], ALU.mult, ALU.add,
                                [r_tmpf[ti], r_lay] + trs(r_xs, kc, a, b), trs(r_xs, kc, a, b))
                    for pss in ssb:
                        ps_reserved.discard(pss)
                if l == 0:
                    dump("d_x2", lambda kc: xs[:, kc, :], lambda kc: trs(r_xs, kc, 0, NT))
                P.barrier()

        except _Stop:
            pass
        for kc in range(KC):
            DMA("sp", outT[kc], xs[:, kc, CTX:NT], trs(r_xs, kc, CTX, NT), [])
        P.emit()
    return nc


_CACHE = {}


def _consts():
    if "c" in _CACHE:
        return _CACHE["c"]
    c = {}
    c["ident"] = np.eye(128, dtype=np.float32)
    s = np.arange(128)
    c["umask"] = (s[:, None] <= s[None, :]).astype(np.float32)
    c["lmask"] = (s[:, None] >= s[None, :]).astype(np.float32)
    k = np.arange(64)
    ang = 2.0 * np.pi * np.outer(k, k) / 64.0
    cs = np.zeros((128, 256), np.float64)
    for hh in range(2):
        cs[hh * 64:(hh + 1) * 64, hh * 64:(hh + 1) * 64] = np.cos(ang) / 8.0
        cs[hh * 64:(hh + 1) * 64, 128 + hh * 64:128 + (hh + 1) * 64] = np.sin(ang) / 8.0
    c["cs64"] = cs.astype(np.float32)
    t = np.arange(SEQ, dtype=np.int64)
    ph = (np.outer(t, t) % SEQ).astype(np.float64) * (2.0 * np.pi / SEQ)
    dc = (np.cos(ph) / math.sqrt(SEQ)).astype(np.float32)
    ds = (-np.sin(ph) / math.sqrt(SEQ)).astype(np.float32)
    both = np.stack([dc, ds], 0)
    both = both.reshape(2, 4, 4, 128, 4, 512)
    c["dftx"] = np.ascontiguousarray(both.transpose(4, 1, 3, 0, 2, 5)).astype(ml_dtypes.bfloat16)
    t = np.arange(CTX, dtype=np.int64)
    ph = (np.outer(t, t) % CTX).astype(np.float64) * (2.0 * np.pi / CTX)
    both = np.stack([np.cos(ph), -np.sin(ph)], 0) / math.sqrt(CTX)
    both = both.reshape(2, 2, 128, CTX)
    c["dftc"] = np.ascontiguousarray(both.transpose(2, 0, 1, 3)).astype(ml_dtypes.bfloat16)
    rows = SEQ // 64
    quarter = D // 4
    freq = np.exp(-math.log(10000.0) * np.arange(quarter, dtype=np.float32) / quarter).astype(np.float32)
    r = np.broadcast_to(np.arange(rows, dtype=np.float32)[:, None], (rows, 64)).reshape(-1)
    col = np.broadcast_to(np.arange(64, dtype=np.float32)[None, :], (rows, 64)).reshape(-1)
    ar = r[:, None] * freq
    ac = col[:, None] * freq
    pos = np.concatenate([np.sin(ar), np.cos(ar), np.sin(ac), np.cos(ac)], axis=-1).astype(np.float32)
    c["posT"] = np.ascontiguousarray(pos.T).reshape(KC, 128, SEQ)
    _CACHE["c"] = c
    return c


def _chunk_w(w, cols):
    return np.ascontiguousarray(w[:, cols].reshape(KC, 128, -1).transpose(1, 0, 2))


def _prep_shared(inp):
    f = np.float32
    L = DEPTH
    w_in = np.asarray(inp["w_in"], f)
    sh = {}
    wada = np.asarray(inp["w_ada"], f)
    sh["wada"] = np.ascontiguousarray(wada.reshape(L, KC, 128, 12, 512).transpose(0, 3, 2, 1, 4))
    sh["bada"] = np.ascontiguousarray(np.asarray(inp["b_ada"], f).reshape(L, 48, 128).transpose(0, 2, 1))
    gs = np.stack([np.asarray(inp[k], f) for k in ("g_pre_mix", "g_post_mix", "g_pre_mlp", "g_post_mlp")], 1)
    sh["gvec"] = np.ascontiguousarray(gs.reshape(L, 4, KC, 128).transpose(0, 3, 1, 2))
    offs = [Q_OFF + 128 * i for i in range(4)] + [K_OFF + 128 * i for i in range(4)] + \
           [F_OFF, F_OFF + 128, CA_OFF, CA_OFF + 128, CG_OFF, CG_OFF + 128]
    sh["winc"] = np.stack([np.stack([_chunk_w(w_in[l], np.arange(o, o + 128)) for o in offs]) for l in range(L)])
    sh["wvo"] = np.stack([np.stack([_chunk_w(w_in[l], np.concatenate([np.arange(V_OFF + 128 * h, V_OFF + 128 * h + 128),
                                                                      np.arange(O_OFF + 128 * h, O_OFF + 128 * h + 128)]))
                                    for h in range(NH)]) for l in range(L)])
    sh["wgate"] = np.stack([_chunk_w(w_in[l], np.arange(G_OFF, G_OFF + 16)) for l in range(L)])
    sh["bgate"] = np.ascontiguousarray(np.asarray(inp["b_gate"], f).reshape(L, 16))
    wqk = np.asarray(inp["w_qk_conv"], f)
    sh["wqkc"] = np.ascontiguousarray(wqk.reshape(L, 3, 8, 128).transpose(0, 2, 1, 3))
    sh["gmn"] = np.ascontiguousarray(np.asarray(inp["g_mlstm_norm"], f))
    wdw = np.asarray(inp["w_dw"], f)
    sh["wdw"] = np.ascontiguousarray(wdw.reshape(L, CONV_K, 2, 128).transpose(0, 3, 2, 1))
    cv = np.stack([np.asarray(inp[k], f) for k in ("b_dw", "g_conv_ln", "b_conv_ln")], 1)
    sh["cvp"] = np.ascontiguousarray(cv.reshape(L, 3, 2, 128).transpose(0, 3, 1, 2))
    wout = np.asarray(inp["w_out"], f)
    sh["woutc"] = np.ascontiguousarray(wout.reshape(L, KC, 128, KC, 128).transpose(0, 2, 3, 1, 4))
    w1 = np.asarray(inp["w_mlp1"], f)
    sh["w1c"] = np.ascontiguousarray(w1.reshape(L, KC, 128, 16, 2, 128).transpose(0, 3, 2, 4, 1, 5))
    w2 = np.asarray(inp["w_mlp2"], f)
    sh["w2c"] = np.ascontiguousarray(w2.reshape(L, 32, 128, KC, 128).transpose(0, 3, 2, 1, 4))
    c = _consts()
    for k in ("ident", "umask", "lmask", "cs64", "dftx", "dftc", "posT"):
        sh[k] = c[k]
    return sh


def make_in_maps(inp, cores):
    sh = _prep_shared(inp)
    x = np.asarray(inp["x"], np.float32)
    ctx = np.asarray(inp["ctx"], np.float32)
    c = np.asarray(inp["c"], np.float32)
    c_ctx = np.asarray(inp["c_ctx"], np.float32)
    maps = []
    for b in cores:
        m = dict(sh)
        m["xT"] = np.ascontiguousarray(x[b].T).reshape(KC, 128, SEQ)
        m["ctxT"] = np.ascontiguousarray(ctx[b].T).reshape(KC, 128, CTX)
        cv = np.stack([c[b], c_ctx], -1)
        m["cvec"] = np.ascontiguousarray(cv.reshape(KC, 128, 2).transpose(1, 0, 2))
        maps.append(m)
    return maps


def kernel(**inputs):
    if "nc" not in _CACHE:
        _CACHE["nc"] = build_program(dbg=False)
    nc = _CACHE["nc"]
    in_maps = make_in_maps(inputs, list(range(NB)))
    res = run_bass_kernel_spmd(nc, in_maps, core_ids=list(range(NB)))
    out = np.empty((NB, SEQ, D), np.float32)
    for b in range(NB):
        oT = np.asarray(res.results[b]["outT"], np.float32).reshape(D, SEQ)
        out[b] = oT.T
    return out
```

```python
import math
from contextlib import ExitStack

import numpy as np
import ml_dtypes

import concourse.bass as bass
import concourse.mybir as mybir
from concourse.bass_utils import run_bass_kernel_spmd

F32 = mybir.dt.float32
BF16 = mybir.dt.bfloat16
AF = mybir.ActivationFunctionType
ALU = mybir.AluOpType
AX = mybir.AxisListType

ENGS = ("pe", "act", "dve", "pool", "sp")
SEM_LIM = 2000
N_DMA_SEMS = 16


class Res:
    __slots__ = ("name", "w", "r", "excl")

    def __init__(self, name, excl=False):
        self.name = name
        self.w = None
        self.r = []
        self.excl = excl


class Op:
    __slots__ = ("eng", "idx", "fn", "deps", "dma", "signal", "semref", "dma_slot", "dma_val")

    def __init__(self, eng, idx, fn, dma):
        self.eng = eng
        self.idx = idx
        self.fn = fn
        self.deps = []
        self.dma = dma
        self.signal = False
        self.semref = None
        self.dma_slot = None
        self.dma_val = None


class Prog:
    def __init__(self, nc, same_engine_sync=True):
        self.nc = nc
        self.ops = {e: [] for e in ENGS}
        self.n_dma_q = {}
        self.same_engine_sync = same_engine_sync

    def res(self, name="", excl=False):
        return Res(name, excl)

    def op(self, eng, fn, reads=(), writes=(), dma=False):
        o = Op(eng, len(self.ops[eng]), fn, dma)
        if dma:
            half = N_DMA_SEMS // 2
            k = self.n_dma_q.get(eng, 0)
            self.n_dma_q[eng] = k + 1
            o.dma_slot = (k % half) + (half if eng == "pool" else 0)
            o.dma_val = 16 * (k // half + 1)
        deps = []
        for r in reads:
            if r.w is not None:
                deps.append(r.w)
            if r.excl:
                deps.extend(x for x in r.r if x.eng != eng)
        for r in writes:
            if r.w is not None:
                deps.append(r.w)
            deps.extend(r.r)
        for r in reads:
            r.r.append(o)
        for r in writes:
            r.w = o
            r.r = []
        seen = set()
        for d in deps:
            if d is o or id(d) in seen:
                continue
            seen.add(id(d))
            if (not d.dma) and (not dma) and d.eng == eng:
                if eng == "pe" or not self.same_engine_sync:
                    continue
            o.deps.append(d)
        self.ops[eng].append(o)
        return o

    def barrier(self):
        targets = []
        for e in ENGS:
            cs = [o for o in self.ops[e] if not o.dma and o.fn is not None]
            if cs:
                targets.append(cs[-1])
        by_slot = {}
        for e in ENGS:
            for o in self.ops[e]:
                if o.dma and (o.dma_slot not in by_slot or o.dma_val > by_slot[o.dma_slot].dma_val):
                    by_slot[o.dma_slot] = o
        targets += list(by_slot.values())
        for e in ENGS:
            o = Op(e, len(self.ops[e]), None, False)
            o.deps = list(targets) if e != "pe" else [t for t in targets if t.dma or t.eng != e]
            self.ops[e].append(o)

    def emit(self):
        nc = self.nc
        for e in ENGS:
            for o in self.ops[e]:
                for d in o.deps:
                    d.signal = True
        with ExitStack() as st:
            dma_sems = [st.enter_context(nc.semaphore(f"dq{i}")) for i in range(N_DMA_SEMS)]
            for e in ENGS:
                n = sum(1 for o in self.ops[e] if o.signal and not o.dma)
                k = max((n + SEM_LIM - 1) // SEM_LIM, 1)
                sems = [st.enter_context(nc.semaphore(f"s_{e}{i}")) for i in range(k)]
                c = 0
                for o in self.ops[e]:
                    if o.signal and not o.dma:
                        o.semref = (sems[c // SEM_LIM], c % SEM_LIM + 1, c)
                        c += 1
            block = st.enter_context(nc.Block())

            def run(e, eng):
                waited_c = {p: -1 for p in ENGS}
                waited_d = [0] * N_DMA_SEMS
                for o in self.ops[e]:
                    for d in o.deps:
                        if d.dma:
                            if waited_d[d.dma_slot] >= d.dma_val:
                                continue
                            waited_d[d.dma_slot] = d.dma_val
                            eng.wait_ge(dma_sems[d.dma_slot], d.dma_val)
                        else:
                            sem, val, gc = d.semref
                            if waited_c[d.eng] >= gc:
                                continue
                            waited_c[d.eng] = gc
                            eng.wait_ge(sem, val)
                    if o.fn is None:
                        continue
                    ins = o.fn(eng)
                    if o.dma:
                        ins.then_inc(dma_sems[o.dma_slot], 16)
                    elif o.signal:
                        ins.then_inc(o.semref[0], 1)

            fin_dma = {}
            for e in ENGS:
                for o in self.ops[e]:
                    if o.dma:
                        fin_dma[o.dma_slot] = max(fin_dma.get(o.dma_slot, 0), o.dma_val)

            @block.tensor
            def _(eng):
                run("pe", eng)

            @block.scalar
            def _(eng):
                run("act", eng)

            @block.vector
            def _(eng):
                run("dve", eng)

            @block.gpsimd
            def _(eng):
                run("pool", eng)

            @block.sync
            def _(eng):
                run("sp", eng)
                for slot, val in sorted(fin_dma.items()):
                    eng.wait_ge(dma_sems[slot], val)


D = 1024
NB = 8
SEQ = 2048
CTX = 256
NT = CTX + SEQ
DEPTH = 2
KC = 8
NJ = NT // 128
HD = 128
NH = 4
DFF = 4096
EPS = 1e-6
CONV_K = 31
PADC = 15
Q_OFF, K_OFF, V_OFF, O_OFF, G_OFF = 0, 512, 1024, 1536, 2048
F_OFF = 2064
CA_OFF = 2320
CG_OFF = 2576
UPW = (CTX + 2 * PADC) + (SEQ + 2 * PADC)


def tiles_for(need_ctx):
    t = [(256, 768, 0), (768, 1280, 0), (1280, 1792, 0), (1792, 2304, 0)]
    if need_ctx:
        t = [(0, 256, 1)] + t
    return t


class _Stop(Exception):
    pass


def build_program(dbg=False, stop=None):
    nc = bass.Bass("TRN2", target_bir_lowering=False)
    P = Prog(nc)

    def din(name, shape, dt=F32):
        return nc.dram_tensor(name, list(shape), dt, kind="ExternalInput").ap()

    def dout(name, shape, dt=F32):
        return nc.dram_tensor(name, list(shape), dt, kind="ExternalOutput").ap()

    xT = din("xT", [KC, 128, SEQ])
    ctxT = din("ctxT", [KC, 128, CTX])
    posT = din("posT", [KC, 128, SEQ])
    cvec = din("cvec", [128, KC, 2])
    wada = din("wada", [DEPTH, 12, 128, KC, 512])
    bada = din("bada", [DEPTH, 128, 48])
    gvec = din("gvec", [DEPTH, 128, 4, KC])
    winc = din("winc", [DEPTH, 14, 128, KC, 128])
    wvo = din("wvo", [DEPTH, NH, 128, KC, 256])
    wgate = din("wgate", [DEPTH, 128, KC, 16])
    bgate = din("bgate", [DEPTH, 16])
    wqkc = din("wqkc", [DEPTH, 8, 3, 128])
    gmn = din("gmn", [DEPTH, 512])
    wdw = din("wdw", [DEPTH, 128, 2, CONV_K])
    cvp = din("cvp", [DEPTH, 128, 3, 2])
    woutc = din("woutc", [DEPTH, 128, KC, KC, 128])
    w1c = din("w1c", [DEPTH, 16, 128, 2, KC, 128])
    w2c = din("w2c", [DEPTH, KC, 128, 32, 128])
    ident_d = din("ident", [128, 128])
    umask_d = din("umask", [128, 128])
    lmask_d = din("lmask", [128, 128])
    cs64_d = din("cs64", [128, 256])
    dftx_d = din("dftx", [4, 4, 128, 2, 4, 512], BF16)
    dftc_d = din("dftc", [128, 2, 2, 256], BF16)
    outT = dout("outT", [KC, 128, SEQ])
    dbg_out = {}
    if dbg:
        for nm in ("d_hx", "d_ym", "d_x1", "d_x2"):
            dbg_out[nm] = dout(nm, [KC, 128, NT])

    with ExitStack() as st:
        def sb(name, shape, dt):
            return st.enter_context(nc.sbuf_tensor(name, list(shape), dt))

        xs = sb("xs", [128, KC, NT], F32)
        hx = sb("hx", [128, KC, NT], BF16)
        SCR_ARENA = 25088
        SCR_N = SCR_ARENA + KC * NT
        scr = sb("scr", [128, SCR_N], BF16)
        ym_flat = scr[:, SCR_ARENA:SCR_N]
        ym = ym_flat.rearrange("p (c t) -> p c t", t=NT)
        ident_f = sb("ident_f", [128, 128], F32)
        ident_b = sb("ident_b", [128, 128], BF16)
        umask_f = sb("umask_f", [128, 128], F32)
        lmask_f = sb("lmask_f", [128, 128], F32)
        negm_f = sb("negm_f", [128, 128], BF16)
        negm_b = sb("negm_b", [128, 128], BF16)
        ones_f = sb("ones_f", [128, 128], F32)
        ones_b = sb("ones_b", [128, 128], BF16)
        cs64 = sb("cs64b", [128, 256], BF16)
        cst = sb("cst", [128, 4], F32)
        cv_f = sb("cv_f", [128, KC, 2], F32)
        sc_b = sb("sc_b", [128, KC, 2], BF16)
        mod = sb("mod", [128, DEPTH, 48, 2], F32)
        bada_s = sb("bada_s", [128, DEPTH, 48], F32)
        gv = sb("gv", [128, DEPTH, 4, KC], F32)
        A1 = sb("A1", [128, KC, 2], F32)
        A2 = sb("A2", [128, KC, 2], F32)
        G1 = sb("G1", [128, KC, 2], F32)
        G2 = sb("G2", [128, KC, 2], F32)
        rs = sb("rs", [128, 512], F32)
        sqb = [sb(f"sqb{i}", [128, 512], BF16) for i in range(2)]
        tmpf = [sb(f"tmpf{i}", [128, 512], F32) for i in range(2)]
        psb = [st.enter_context(nc.psum_tensor(f"ps{i}", [128, 512], F32)) for i in range(8)]

        R = P.res
        r_xs = [[R() for _ in range(NJ)] for _ in range(KC)]
        r_hx = [[R() for _ in range(NJ)] for _ in range(KC)]
        r_ym = [[R() for _ in range(NJ)] for _ in range(KC)]
        r_ps = [P.res("ps", excl=True) for _ in range(8)]
        r_const = R()
        r_mod = R()
        r_lay = R()
        r_rs = R()
        r_sqb = [R(), R()]
        r_tmpf = [R(), R()]
        r_arena = R()

        def trs(rl, kc, a, b):
            return [rl[kc][j] for j in range(a // 128, (b + 127) // 128)]

        def trs_all(rl, a, b):
            out = []
            for kc in range(KC):
                out += trs(rl, kc, a, b)
            return out

        cnt = {"ps": 0, "sq": 0, "tf": 0}

        ps_reserved = set()

        def next_ps():
            while True:
                i = cnt["ps"] % 8
                cnt["ps"] += 1
                if i not in ps_reserved:
                    return i

        def next_sq():
            i = cnt["sq"] % 2
            cnt["sq"] += 1
            return i

        def next_tf():
            i = cnt["tf"] % 2
            cnt["tf"] += 1
            return i

        def MM(out, lhsT, rhs, start, stop, reads, writes):
            P.op("pe", lambda e: e.matmul(out, lhsT=lhsT, rhs=rhs, start=start, stop=stop), reads, writes)

        def TR(out, in_, ident, reads, writes):
            P.op("pe", lambda e: e.transpose(out, in_, ident), reads, writes)

        def ACT(out, in_, func, reads, writes, bias=None, scale=None, accum=None):
            kw = {}
            if bias is not None:
                kw["bias"] = bias
            if scale is not None:
                kw["scale"] = scale
            if accum is not None:
                kw["accum_out"] = accum
            P.op("act", lambda e: e.activation(out=out, in_=in_, func=func, **kw), reads, writes)

        def TT(eng, out, in0, in1, op, reads, writes):
            P.op(eng, lambda e: e.tensor_tensor(out=out, in0=in0, in1=in1, op=op), reads, writes)

        def TS(eng, out, in0, s1, s2, op0, op1, reads, writes):
            if s2 is None:
                P.op(eng, lambda e: e.tensor_scalar(out=out, in0=in0, scalar1=s1, scalar2=None, op0=op0), reads, writes)
            else:
                P.op(eng, lambda e: e.tensor_scalar(out=out, in0=in0, scalar1=s1, scalar2=s2, op0=op0, op1=op1), reads, writes)

        def STT(eng, out, in0, scalar, in1, op0, op1, reads, writes):
            P.op(eng, lambda e: e.scalar_tensor_tensor(out=out, in0=in0, scalar=scalar, in1=in1, op0=op0, op1=op1), reads, writes)

        def CP(eng, out, in_, reads, writes):
            P.op(eng, lambda e: e.tensor_copy(out=out, in_=in_), reads, writes)

        def MS(eng, ap, val, writes):
            P.op(eng, lambda e: e.memset(ap, val), (), writes)

        def RECIP(out, in_, reads, writes):
            P.op("dve", lambda e: e.reciprocal(out=out, in_=in_), reads, writes)

        def DMA(q, out, in_, reads, writes):
            P.op(q, lambda e: e.dma_start(out=out, in_=in_), reads, writes, dma=True)

        def arena(off, n, dt=BF16):
            if dt == BF16:
                return scr[:, off:off + n]
            assert off % 2 == 0
            return scr[:, off:off + 2 * n].bitcast(F32)

        DMA("sp", ident_f[:], ident_d, [], [r_const])
        DMA("sp", umask_f[:], umask_d, [], [r_const])
        DMA("sp", lmask_f[:], lmask_d, [], [r_const])
        DMA("pool", cs64[:], cs64_d, [], [r_const])
        DMA("sp", cv_f[:], cvec, [], [r_const])
        DMA("sp", bada_s[:], bada.rearrange("l p n -> p l n"), [], [r_const])
        DMA("sp", gv[:], gvec.rearrange("l p a k -> p l a k"), [], [r_const])
        CP("dve", ident_b[:], ident_f[:], [r_const], [r_const])
        TS("dve", negm_f[:], umask_f[:], -1.0, 30000.0, ALU.add, ALU.mult, [r_const], [r_const])
        TS("dve", negm_b[:], lmask_f[:], -1.0, 30000.0, ALU.add, ALU.mult, [r_const], [r_const])
        MS("dve", ones_f[:], 1.0, [r_const])
        MS("dve", ones_b[:], 1.0, [r_const])
        MS("dve", cst[:, 0:1], 1024.0 * EPS, [r_const])
        MS("dve", cst[:, 1:2], EPS, [r_const])
        MS("dve", cst[:, 2:3], 1.0, [r_const])
        MS("dve", cst[:, 3:4], 0.0, [r_const])

        for kc in range(KC):
            DMA("sp", xs[:, kc, CTX:NT], xT[kc], [], trs(r_xs, kc, CTX, NT))
            DMA("sp", xs[:, kc, 0:CTX], ctxT[kc], [], trs(r_xs, kc, 0, CTX))
        pos_buf = [arena(0, SEQ, F32), arena(2 * SEQ, SEQ, F32)]
        r_pos = [R(), R()]
        for kc in range(KC):
            i = kc % 2
            DMA("sp", pos_buf[i], posT[kc], [], [r_pos[i]])
            TT("dve", xs[:, kc, CTX:NT], xs[:, kc, CTX:NT], pos_buf[i], ALU.add,
               [r_pos[i]] + trs(r_xs, kc, CTX, NT), trs(r_xs, kc, CTX, NT))

        ACT(sc_b[:], cv_f[:], AF.Silu, [r_const], [r_const])
        wa_off = 4 * SEQ
        wa_buf = [arena(wa_off + i * KC * 512, KC * 512).rearrange("p (k c) -> p k c", c=512) for i in range(2)]
        r_wa = [R(), R()]
        for l in range(DEPTH):
            pi = next_ps()
            pv = psb[pi][:, 0:96].rearrange("p (n w) -> p n w", w=2)
            for blk in range(12):
                i = (l * 12 + blk) % 2
                DMA("pool", wa_buf[i], wada[l, blk], [], [r_wa[i]])
                for n4 in range(4):
                    n = blk * 4 + n4
                    for kc in range(KC):
                        MM(pv[:, n, :], wa_buf[i][:, kc, n4 * 128:(n4 + 1) * 128], sc_b[:, kc, :], kc == 0, kc == KC - 1,
                           [r_wa[i], r_const], [r_ps[pi]])
            TT("dve", mod[:, l], pv, bada_s[:, l].unsqueeze(2).to_broadcast([128, 48, 2]), ALU.add,
               [r_ps[pi], r_const], [r_mod])

        def rstd_of(src_fn, n, reads):
            pi = next_ps()
            for kc in range(KC):
                si = next_sq()
                ACT(sqb[si][:, 0:n], src_fn(kc), AF.Square, reads(kc), [r_sqb[si]])
                MM(psb[pi][:, 0:n], ones_b[:], sqb[si][:, 0:n], kc == 0, kc == KC - 1, [r_sqb[si], r_const], [r_ps[pi]])
            ACT(rs[:, 0:n], psb[pi][:, 0:n], AF.Sqrt, [r_ps[pi], r_const], [r_rs], bias=cst[:, 0:1], scale=1.0)
            RECIP(rs[:, 0:n], rs[:, 0:n], [r_rs], [r_rs])

        def prenorm(tl, Amod, shift_base, l):
            for (a, b, w) in tl:
                n = b - a
                rstd_of(lambda kc: xs[:, kc, a:b], n, lambda kc: trs(r_xs, kc, a, b))
                for kc in range(KC):
                    ti = next_tf()
                    TT("dve", tmpf[ti][:, 0:n], xs[:, kc, a:b], rs[:, 0:n], ALU.mult,
                       trs(r_xs, kc, a, b) + [r_rs], [r_tmpf[ti]])
                    ACT(hx[:, kc, a:b], tmpf[ti][:, 0:n], AF.Identity, [r_tmpf[ti], r_lay, r_mod], trs(r_hx, kc, a, b),
                        bias=mod[:, l, shift_base + kc, w:w + 1], scale=Amod[:, kc, w:w + 1])

        def resid_update(tl, src_fn, src_reads, Gm):
            for (a, b, w) in tl:
                n = b - a
                rstd_of(lambda kc: src_fn(kc, a, b), n, lambda kc: src_reads(kc, a, b))
                for kc in range(KC):
                    ti = next_tf()
                    TT("dve", tmpf[ti][:, 0:n], src_fn(kc, a, b), rs[:, 0:n], ALU.mult,
                       src_reads(kc, a, b) + [r_rs], [r_tmpf[ti]])
                    STT("dve", xs[:, kc, a:b], tmpf[ti][:, 0:n], Gm[:, kc, w:w + 1], xs[:, kc, a:b], ALU.mult, ALU.add,
                        [r_tmpf[ti], r_lay] + trs(r_xs, kc, a, b), trs(r_xs, kc, a, b))

        def dump(name, src_fn, reads_fn):
            if not dbg:
                return
            for kc in range(KC):
                DMA("pool", dbg_out[name][kc], src_fn(kc), reads_fn(kc), [])

        def maybe_stop(k):
            if stop is not None and stop == k:
                raise _Stop()

        try:
            P.barrier()
            for l in range(DEPTH):
                need_ctx = l < DEPTH - 1
                TL_all = tiles_for(True)
                TL = tiles_for(need_ctx)
                j0 = 0 if need_ctx else 2

                def mk(dst, gidx, mbase, plus1):
                    for w in range(2):
                        if plus1:
                            STT("dve", dst[:, :, w], mod[:, l, mbase:mbase + KC, w], 1.0, gv[:, l, gidx, :], ALU.add, ALU.mult,
                                [r_mod, r_const, r_lay], [r_lay])
                        else:
                            TT("dve", dst[:, :, w], mod[:, l, mbase:mbase + KC, w], gv[:, l, gidx, :], ALU.mult,
                               [r_mod, r_const, r_lay], [r_lay])
                    TS("dve", dst[:], dst[:], 32.0, None, ALU.mult, None, [r_lay], [r_lay])

                mk(A1, 0, 8, True)
                mk(G1, 1, 16, False)
                mk(A2, 2, 32, True)
                mk(G2, 3, 40, False)

                prenorm(TL_all, A1, 0, l)
                maybe_stop(10 * l + 1)
                if l == 0:
                    dump("d_hx", lambda kc: hx[:, kc, :], lambda kc: trs(r_hx, kc, 0, NT))

                o_w = 0
                wbuf = [arena(o_w + i * 1024, 1024).rearrange("p (k c) -> p k c", c=128) for i in range(4)]
                r_wbuf = [R() for _ in range(4)]
                wcnt = {"n": 0}

                def load_w(src):
                    i = wcnt["n"] % 4
                    wcnt["n"] += 1
                    DMA("pool", wbuf[i], src, [], [r_wbuf[i]])
                    return i

                o_up = 4096
                upad = arena(o_up, 2 * UPW).rearrange("p (c t) -> p c t", t=UPW)
                r_up = R()
                o_vb = o_up + 2 * UPW
                vbuf = arena(o_vb, 2 * NT, F32).rearrange("p (c t) -> p c t", t=NT)
                r_vb = [R(), R()]
                o_cs = o_vb + 4 * NT
                wdw_s = arena(o_cs, 2 * CONV_K, F32).rearrange("p (c k) -> p c k", k=CONV_K)
                cvp_s = arena(o_cs + 4 * CONV_K, 6, F32).rearrange("p (a c) -> p a c", c=2)
                r_cs = R()
                lnt = [ym_flat[:, i * 1024:(i + 1) * 1024].bitcast(F32) for i in range(5)]
                r_lnt = [R() for _ in range(5)]
                dgm = ym_flat[:, 5120:5120 + 2 * CONV_K * 128].rearrange("p (c k m) -> p c k m", k=CONV_K, m=128)
                r_dg = R()

                DMA("sp", wdw_s, wdw[l], [], [r_cs])
                DMA("sp", cvp_s, cvp[l], [], [r_cs])
                MS("dve", upad, 0.0, [r_up])
                for cc in range(2):
                    for k in range(CONV_K):
                        TS("dve", dgm[:, cc, k, :], ident_f[:], wdw_s[:, cc, k:k + 1], None, ALU.mult, None,
                           [r_const, r_cs], [r_dg])

                def upos(a):
                    return a + PADC if a < CTX else (CTX + 2 * PADC) + (a - CTX) + PADC

                for cc in range(2):
                    ia = load_w(winc[l, 10 + cc])
                    ig = load_w(winc[l, 12 + cc])
                    for (a, b, w) in TL:
                        n = b - a
                        pa = next_ps()
                        for kc in range(KC):
                            MM(psb[pa][:, 0:n], wbuf[ia][:, kc, :], hx[:, kc, a:b], kc == 0, kc == KC - 1,
                               [r_wbuf[ia]] + trs(r_hx, kc, a, b), [r_ps[pa]])
                        pg = next_ps()
                        for kc in range(KC):
                            MM(psb[pg][:, 0:n], wbuf[ig][:, kc, :], hx[:, kc, a:b], kc == 0, kc == KC - 1,
                               [r_wbuf[ig]] + trs(r_hx, kc, a, b), [r_ps[pg]])
                        ti = next_tf()
                        ACT(tmpf[ti][:, 0:n], psb[pg][:, 0:n], AF.Sigmoid, [r_ps[pg]], [r_tmpf[ti]])
                        TT("dve", upad[:, cc, upos(a):upos(a) + n], psb[pa][:, 0:n], tmpf[ti][:, 0:n], ALU.mult,
                           [r_ps[pa], r_tmpf[ti]], [r_up])
                for cc in range(2):
                    for (a, b, w) in TL:
                        n = b - a
                        pi = next_ps()
                        p0 = upos(a) - PADC
                        for k in range(CONV_K):
                            MM(psb[pi][:, 0:n], dgm[:, cc, k, :], upad[:, cc, p0 + k:p0 + k + n], k == 0, k == CONV_K - 1,
                               [r_dg, r_up], [r_ps[pi]])
                        ACT(vbuf[:, cc, a:b], psb[pi][:, 0:n], AF.Identity, [r_ps[pi], r_cs], [r_vb[cc]],
                            bias=cvp_s[:, 0, cc:cc + 1], scale=1.0)
                for (a, b, w) in TL:
                    n = b - a
                    p1 = next_ps()
                    p2 = next_ps()
                    for cc in range(2):
                        MM(psb[p1][:, 0:n], ones_f[:], vbuf[:, cc, a:b], cc == 0, cc == 1, [r_const, r_vb[cc]], [r_ps[p1]])
                    for cc in range(2):
                        TT("dve", lnt[0][:, 0:n], vbuf[:, cc, a:b], vbuf[:, cc, a:b], ALU.mult, [r_vb[cc]], [r_lnt[0]])
                        MM(psb[p2][:, 0:n], ones_f[:], lnt[0][:, 0:n], cc == 0, cc == 1, [r_const, r_lnt[0]], [r_ps[p2]])
                    mean = lnt[1]
                    TS("dve", mean[:, 0:n], psb[p1][:, 0:n], 1.0 / 256.0, None, ALU.mult, None, [r_ps[p1]], [r_lnt[1]])
                    TT("dve", lnt[2][:, 0:n], mean[:, 0:n], mean[:, 0:n], ALU.mult, [r_lnt[1]], [r_lnt[2]])
                    STT("dve", lnt[2][:, 0:n], psb[p2][:, 0:n], 1.0 / 256.0, lnt[2][:, 0:n], ALU.mult, ALU.subtract,
                        [r_ps[p2], r_lnt[2]], [r_lnt[2]])
                    ACT(lnt[2][:, 0:n], lnt[2][:, 0:n], AF.Sqrt, [r_lnt[2], r_const], [r_lnt[2]], bias=cst[:, 1:2], scale=1.0)
                    RECIP(lnt[2][:, 0:n], lnt[2][:, 0:n], [r_lnt[2]], [r_lnt[2]])
                    for cc in range(2):
                        TT("dve", lnt[3 + cc][:, 0:n], vbuf[:, cc, a:b], mean[:, 0:n], ALU.subtract,
                           [r_vb[cc], r_lnt[1]], [r_lnt[3 + cc]])
                        TT("dve", lnt[3 + cc][:, 0:n], lnt[3 + cc][:, 0:n], lnt[2][:, 0:n], ALU.mult,
                           [r_lnt[3 + cc], r_lnt[2]], [r_lnt[3 + cc]])
                        ACT(ym[:, 6 + cc, a:b], lnt[3 + cc][:, 0:n], AF.Silu, [r_lnt[3 + cc], r_cs], trs(r_ym, 6 + cc, a, b),
                            bias=cvp_s[:, 2, cc:cc + 1], scale=cvp_s[:, 1, cc:cc + 1])
                P.barrier()

                maybe_stop(10 * l + 2)
                o_uf = 4096
                uF = arena(o_uf, 2 * NT).rearrange("p (c t) -> p c t", t=NT)
                r_uf = [R(), R()]
                o_dp = o_uf + 2 * NT
                dpc = [arena(o_dp + i * 4096, 4096).rearrange("p (s k t) -> p s k t", s=2, k=4) for i in range(3)]
                r_dpc = [R() for _ in range(3)]
                o_dc = o_dp + 3 * 4096
                dcc = arena(o_dc, 1024).rearrange("p (s k t) -> p s k t", s=2, k=2)
                r_dcc = R()
                AB = ym_flat[:, 0:NJ * 512].rearrange("p (j c m) -> p j c m", c=2, m=256)
                r_ab = [R() for _ in range(NJ)]

                for cc in range(2):
                    iw = load_w(winc[l, 8 + cc])
                    for (a, b, w) in TL:
                        n = b - a
                        pi = next_ps()
                        for kc in range(KC):
                            MM(psb[pi][:, 0:n], wbuf[iw][:, kc, :], hx[:, kc, a:b], kc == 0, kc == KC - 1,
                               [r_wbuf[iw]] + trs(r_hx, kc, a, b), [r_ps[pi]])
                        ACT(uF[:, cc, a:b], psb[pi][:, 0:n], AF.Copy, [r_ps[pi]], [r_uf[cc]])
                for j in range(j0, NJ):
                    pi = next_ps()
                    for cc in range(2):
                        MM(psb[pi][:, cc * 256:(cc + 1) * 256], uF[:, cc, j * 128:(j + 1) * 128], cs64[:], True, True,
                           [r_uf[cc], r_const], [r_ps[pi]])
                    CP("dve", AB[:, j].rearrange("p c m -> p (c m)"), psb[pi][:, :], [r_ps[pi]], [r_ab[j]])
                npc = 0
                for tq in range(4):
                    pp = [next_ps(), next_ps()]
                    for kg in range(4):
                        i = npc % 3
                        npc += 1
                        DMA("sp", dpc[i].rearrange("p s k t -> p (s k t)"),
                            dftx_d[tq, kg].rearrange("p s k t -> p (s k t)"), [], [r_dpc[i]])
                        for ki in range(4):
                            j = 2 + kg * 4 + ki
                            for s in range(2):
                                first = (kg == 0 and ki == 0 and s == 0)
                                last = (kg == 3 and ki == 3 and s == 1)
                                for cc in range(2):
                                    MM(psb[pp[cc]][:, :], AB[:, j, cc, s * 128:(s + 1) * 128], dpc[i][:, s, ki, :], first, last,
                                       [r_ab[j], r_dpc[i]], [r_ps[pp[cc]]])
                    for cc in range(2):
                        a = CTX + tq * 512
                        ACT(ym[:, 4 + cc, a:a + 512], psb[pp[cc]][:, :], AF.Copy, [r_ps[pp[cc]]], trs(r_ym, 4 + cc, a, a + 512))
                if need_ctx:
                    DMA("sp", dcc.rearrange("p s k t -> p (s k t)"), dftc_d.rearrange("p s k t -> p (s k t)"), [], [r_dcc])
                    pp = [next_ps(), next_ps()]
                    for ki in range(2):
                        for s in range(2):
                            for cc in range(2):
                                MM(psb[pp[cc]][:, 0:256], AB[:, ki, cc, s * 128:(s + 1) * 128], dcc[:, s, ki, :],
                                   ki == 0 and s == 0, ki == 1 and s == 1, [r_ab[ki], r_dcc], [r_ps[pp[cc]]])
                    for cc in range(2):
                        ACT(ym[:, 4 + cc, 0:CTX], psb[pp[cc]][:, 0:256], AF.Copy, [r_ps[pp[cc]]], trs(r_ym, 4 + cc, 0, CTX))

                P.barrier()
                maybe_stop(10 * l + 3)
                o_s = 0
                dgq = [arena(o_s + i * 256, 128, F32) for i in range(2)]
                qs = [arena(o_s + 512 + i * 128, 128) for i in range(2)]
                sTt = [arena(o_s + 768 + i * 128, 128) for i in range(2)]
                kts = [arena(o_s + 1024 + i * 128, 128) for i in range(2)]
                Et = [arena(o_s + 1280 + i * 128, 128) for i in range(2)]
                Cst = [arena(o_s + 1536 + i * 260, 130, F32) for i in range(2)]
                Cbf = [arena(o_s + 2056 + i * 130, 130) for i in range(2)]
                rden = [arena(o_s + 2316 + i * 4, 2, F32) for i in range(2)]
                st1 = arena(o_s + 2324, 2 * NJ, F32)
                st2 = arena(o_s + 2396, 2 * NJ, F32)
                r_dgq, r_qs, r_sT, r_kts = [R(), R()], [R(), R()], [R(), R()], [R(), R()]
                r_dgb, r_Et = [R(), R()], [R(), R()]
                r_C, r_Cb, r_rden = [R(), R()], [R(), R()], [R(), R()]
                r_st = R()
                wqk_raw = arena(2468, 1024).rearrange("p (k c) -> p k c", c=128)
                r_wqk = R()
                w3 = arena(3492, 3072).rearrange("p (t k c) -> p t k c", t=3, k=KC)
                wtap = arena(6564, 384, F32).rearrange("p (t c) -> p t c", c=128)
                r_w3, r_wtap = R(), R()
                o_g = 7332
                Gtok = arena(o_g, NJ * 16, F32).rearrange("p (j g) -> p j g", g=16)
                dgb = [arena(o_g + i * 256, 128, F32) for i in range(2)]
                LF = arena(o_g + 576, NJ * 8, F32).rearrange("p (j g) -> p j g", g=8)
                CM = arena(o_g + 864, NJ * 8, F32).rearrange("p (j g) -> p j g", g=8)
                Bc = arena(o_g + 1152, NJ * 8, F32).rearrange("p (j g) -> p j g", g=8)
                EB = arena(o_g + 1440, NJ * 8, F32).rearrange("p (j g) -> p j g", g=8)
                EBL = arena(o_g + 1728, NJ * 8, F32).rearrange("p (j g) -> p j g", g=8)
                ECL = arena(o_g + 2016, NJ * 8, F32).rearrange("p (j g) -> p j g", g=8)
                bg_s = arena(o_g + 2304, 16, F32)
                wg_s = arena(o_g + 2336, KC * 16).rearrange("p (k g) -> p k g", g=16)
                gmn_s = arena(o_g + 2464, 512, F32)
                r_gate = R()
                o_h = o_g + 3488
                qT = arena(o_h, NT)
                kT = arena(o_h + NT, NT)
                vaug = arena(o_h + 2 * NT, NJ * 130).rearrange("p (j e) -> p j e", e=130)
                sgo = arena(o_h + 2 * NT + 2340, NT).rearrange("p (j e) -> p j e", e=128)
                hsum = arena(o_h + 3 * NT + 2340, NT, F32).rearrange("p (j e) -> p j e", e=128)
                assert o_h + 5 * NT + 2340 <= SCR_ARENA, (o_h + 5 * NT + 2340)
                r_q, r_k, r_v, r_sg, r_hs = R(), R(), R(), R(), R()
                wvo_s = ym_flat[:, 3 * NT:3 * NT + KC * 256].rearrange("p (k c) -> p k c", c=256)
                r_wvo = R()

                DMA("pool", wg_s, wgate[l], [], [r_gate])
                DMA("sp", bg_s, bgate[l].partition_broadcast(128), [], [r_gate])
                DMA("sp", gmn_s, gmn[l].partition_broadcast(128), [], [r_gate])
                for jg in range(0, NJ, 6):
                    pi = next_ps()
                    for jj in range(6):
                        j = jg + jj
                        for kc in range(KC):
                            MM(psb[pi][:, jj * 16:(jj + 1) * 16], hx[:, kc, j * 128:(j + 1) * 128], wg_s[:, kc, :], kc == 0, kc == KC - 1,
                               [r_gate] + trs(r_hx, kc, j * 128, (j + 1) * 128), [r_ps[pi]])
                    TT("dve", Gtok[:, jg:jg + 6, :], psb[pi][:, 0:96].rearrange("p (j g) -> p j g", g=16),
                       bg_s.unsqueeze(1).to_broadcast([128, 6, 16]), ALU.add, [r_ps[pi], r_gate], [r_gate])
                CP("dve", CM[:, :, 0:4], Gtok[:, :, 0:4], [r_gate], [r_gate])
                CP("dve", CM[:, :, 4:8], Gtok[:, :, 8:12], [r_gate], [r_gate])
                ACT(LF[:, :, 0:4], Gtok[:, :, 4:8], AF.Abs, [r_gate], [r_gate])
                ACT(LF[:, :, 4:8], Gtok[:, :, 12:16], AF.Abs, [r_gate], [r_gate])
                ACT(LF[:], LF[:], AF.Exp, [r_gate], [r_gate], scale=-1.0)
                ACT(LF[:], LF[:], AF.Ln, [r_gate, r_const], [r_gate], bias=cst[:, 2:3], scale=1.0)
                TS("dve", EB[:, :, 0:4], Gtok[:, :, 4:8], 0.0, None, ALU.min, None, [r_gate], [r_gate])
                TS("dve", EB[:, :, 4:8], Gtok[:, :, 12:16], 0.0, None, ALU.min, None, [r_gate], [r_gate])
                TT("dve", LF[:], EB[:], LF[:], ALU.subtract, [r_gate], [r_gate])
                pi = next_ps()
                pbv = psb[pi][:, 0:NJ * 8].rearrange("p (j g) -> p j g", g=8)
                for j in range(NJ):
                    MM(pbv[:, j, 0:4], umask_f[:], LF[:, j, 0:4], True, True, [r_const, r_gate], [r_ps[pi]])
                    MM(pbv[:, j, 4:8], lmask_f[:], LF[:, j, 4:8], True, True, [r_const, r_gate], [r_ps[pi]])
                CP("dve", Bc[:], pbv, [r_ps[pi]], [r_gate])
                pi = next_ps()
                ptv = psb[pi][:, 0:NJ * 8].rearrange("p (j g) -> p j g", g=8)
                MM(psb[pi][:, 0:NJ * 8], ones_f[:], LF[:].rearrange("p j g -> p (j g)"), True, True, [r_const, r_gate], [r_ps[pi]])
                ACT(EBL[:], ptv, AF.Exp, [r_ps[pi]], [r_gate])
                ACT(EB[:], Bc[:], AF.Exp, [r_gate], [r_gate])
                TT("dve", CM[:], CM[:], Bc[:], ALU.subtract, [r_gate], [r_gate])
                TT("dve", ECL[:], CM[:], ptv, ALU.add, [r_gate, r_ps[pi]], [r_gate])
                ACT(ECL[:], ECL[:], AF.Exp, [r_gate], [r_gate])
                P.barrier()

                MS("dve", vaug[:, :, 128:129], 1.0, [r_v])

                for h in range(NH):
                    for qk in range(2):
                        dst, r_dst = (qT, r_q) if qk == 0 else (kT, r_k)
                        ci = qk * 4 + h
                        DMA("pool", wqk_raw, winc[l, ci], [], [r_wqk])
                        DMA("sp", wtap, wqkc[l, ci].partition_broadcast(128), [], [r_wtap])
                        if qk == 1:
                            TS("dve", wtap, wtap, HD ** -0.5, None, ALU.mult, None, [r_wtap], [r_wtap])
                        for t in range(3):
                            TT("dve", w3[:, t], wqk_raw, wtap[:, t, :].unsqueeze(1).to_broadcast([128, KC, 128]), ALU.mult,
                               [r_wqk, r_wtap], [r_w3])
                        for (a, b, w) in TL_all:
                            n = b - a
                            s0, s1 = (0, CTX) if a < CTX else (CTX, NT)
                            pi = next_ps()
                            for kc in range(KC):
                                MM(psb[pi][:, 0:n], w3[:, 1, kc, :], hx[:, kc, a:b], kc == 0, False,
                                   [r_w3] + trs(r_hx, kc, a, b), [r_ps[pi]])
                            lo = max(a, s0 + 1)
                            for kc in range(KC):
                                MM(psb[pi][:, lo - a:n], w3[:, 0, kc, :], hx[:, kc, lo - 1:b - 1], False, False,
                                   [r_w3] + trs(r_hx, kc, lo - 1, b - 1), [r_ps[pi]])
                            hi = min(b, s1 - 1)
                            for kc in range(KC):
                                MM(psb[pi][:, 0:hi - a], w3[:, 2, kc, :], hx[:, kc, a + 1:hi + 1], False, kc == KC - 1,
                                   [r_w3] + trs(r_hx, kc, a + 1, hi + 1), [r_ps[pi]])
                            ACT(dst[:, a:b], psb[pi][:, 0:n], AF.Copy, [r_ps[pi]], [r_dst])
                    DMA("pool", wvo_s, wvo[l, h], [], [r_wvo])
                    for j in range(NJ):
                        pi = next_ps()
                        for kc in range(KC):
                            MM(psb[pi][:, 0:256], hx[:, kc, j * 128:(j + 1) * 128], wvo_s[:, kc, :], kc == 0, kc == KC - 1,
                               [r_wvo] + trs(r_hx, kc, j * 128, (j + 1) * 128), [r_ps[pi]])
                        CP("dve", vaug[:, j, 0:128], psb[pi][:, 0:128], [r_ps[pi]], [r_v])
                        ACT(sgo[:, j, :], psb[pi][:, 128:256], AF.Sigmoid, [r_ps[pi]], [r_sg])
                        TT("dve", sgo[:, j, :], sgo[:, j, :], gmn_s[:, h * 128:(h + 1) * 128], ALU.mult, [r_sg, r_gate], [r_sg])
                    MS("dve", hsum, 0.0, [r_hs])
                    for d_ in range(2):
                        MS("dve", Cst[d_], 0.0, [r_C[d_]])
                        MS("dve", Cbf[d_], 0.0, [r_Cb[d_]])
                    order = [list(range(NJ)), [1, 0] + list(range(NJ - 1, 1, -1))]
                    t0f = tmpf[0]
                    t1b = tmpf[1][:, :].bitcast(BF16)
                    dgb2 = [dgb, [dgq[0], dgq[1]]]
                    sT2 = [sTt, [t1b[:, 256:384], t1b[:, 384:512]]]
                    kts2 = [kts, [t1b[:, 512:640], t1b[:, 640:768]]]
                    Et2 = [Et, [t1b[:, 768:896], t1b[:, 896:1024]]]
                    ndi = [t0f[:, 0:130], t0f[:, 130:260]]
                    ndi_den = t0f[:, 0:260].rearrange("p (d e) -> p d e", e=130)[:, :, 128:129]
                    rden_j = t0f[:, 392:394].unsqueeze(2)
                    r_rdj = R()
                    r_ndi = [R(), R()]
                    t0b = t0f[:, 260:390].bitcast(BF16)
                    Cbf2 = [Cbf, [t0b[:, 0:130], t0b[:, 130:260]]]
                    r_Cb2 = [r_Cb, [R(), R()]]
                    rr = lambda: [[R(), R()], [R(), R()]]
                    r_dgb2, r_sT2, r_kts2, r_Et2 = rr(), rr(), rr(), rr()

                    def info(step, d_):
                        j = order[d_][step]
                        return j, step % 2, d_ * 4 + h, (need_ctx or j >= 2), step == NJ - 1

                    ps_hold = {}

                    def a1(step, d_):
                        j, bs, col, need_out, is_last = info(step, d_)
                        if need_out:
                            TS("dve", dgb2[bs][d_], ident_f[:], Bc[:, j, col:col + 1], None, ALU.mult, None,
                               [r_const, r_gate], [r_dgb2[bs][d_]])
                        yield

                    def a2a(step, d_):
                        j, bs, col, need_out, is_last = info(step, d_)
                        c0, c1 = j * 128, (j + 1) * 128
                        if need_out:
                            pd_ = d_
                            MM(psb[pd_][:, 0:128], ones_f[:], dgb2[bs][d_], True, False, [r_const, r_dgb2[bs][d_]], [r_ps[pd_]])
                            MM(psb[pd_][:, 0:128], ident_b[:], (negm_f if d_ == 0 else negm_b)[:], False, True,
                               [r_const], [r_ps[pd_]])
                            yield
                            ACT(Et2[bs][d_], psb[pd_][:, 0:128], AF.Exp, [r_ps[pd_], r_gate], [r_Et2[bs][d_]],
                                bias=CM[:, j, col:col + 1], scale=1.0)
                            yield
                            p_s = 2 + d_
                            MM(psb[p_s][:, 0:128], kT[:, c0:c1], qT[:, c0:c1], True, True, [r_k, r_q], [r_ps[p_s]])
                            yield
                        if not is_last:
                            p_t = next_ps()
                            ptb = psb[p_t][:, 0:64].bitcast(BF16)
                            TR(ptb, kT[:, c0:c1], ident_b[:], [r_k, r_const], [r_ps[p_t]])
                            yield
                            ACT(kts2[bs][d_], ptb, AF.Copy, [r_ps[p_t], r_gate], [r_kts2[bs][d_]], scale=ECL[:, j, col:col + 1])
                            yield

                    def a2b(step, d_):
                        j, bs, col, need_out, is_last = info(step, d_)
                        if need_out:
                            p_s = 2 + d_
                            TT("dve", sT2[bs][d_], psb[p_s][:, 0:128], Et2[bs][d_], ALU.mult,
                               [r_ps[p_s], r_Et2[bs][d_]], [r_sT2[bs][d_]])
                        yield

                    def stage_b(step, d_):
                        j, bs, col, need_out, is_last = info(step, d_)
                        cur, nxt = Cbf2[step % 2][d_], Cbf2[(step + 1) % 2][d_]
                        r_cur, r_nxt = r_Cb2[step % 2][d_], r_Cb2[(step + 1) % 2][d_]
                        if not is_last:
                            p_u = next_ps()
                            MM(psb[p_u][:, 0:129], kts2[bs][d_], vaug[:, j, 0:129], True, True, [r_kts2[bs][d_], r_v], [r_ps[p_u]])
                            yield
                            STT("dve", Cst[d_][:, 0:129], Cst[d_][:, 0:129], EBL[:, j, col:col + 1], psb[p_u][:, 0:129],
                                ALU.mult, ALU.add, [r_C[d_], r_gate, r_ps[p_u]], [r_C[d_]])
                            yield
                            ACT(nxt[:, 0:129], Cst[d_][:, 0:129], AF.Copy, [r_C[d_]], [r_nxt])
                            yield
                        if need_out:
                            c0, c1 = j * 128, (j + 1) * 128
                            p_i = next_ps()
                            MM(psb[p_i][:, 0:129], qT[:, c0:c1], cur[:, 0:129], True, True, [r_q, r_cur], [r_ps[p_i]])
                            p_n = next_ps()
                            MM(psb[p_n][:, 0:129], sT2[bs][d_], vaug[:, j, 0:129], True, True, [r_sT2[bs][d_], r_v], [r_ps[p_n]])
                            yield
                            ACT(ndi[d_][:, 0:129], psb[p_i][:, 0:129], AF.Copy, [r_ps[p_i], r_gate], [r_ndi[d_]], scale=EB[:, j, col:col + 1])
                            yield
                            TT("dve", ndi[d_][:, 0:129], ndi[d_][:, 0:129], psb[p_n][:, 0:129], ALU.add, [r_ndi[d_], r_ps[p_n]], [r_ndi[d_]])
                            yield

                    def den_ops(step):
                        j, bs, col, need_out, is_last = info(step, 0)
                        if need_out:
                            STT("dve", rden_j, ndi_den, -1.0, ndi_den, ALU.mult, ALU.max, [r_ndi[0], r_ndi[1]], [r_rdj])
                            TS("dve", rden_j, rden_j, 1.0, None, ALU.max, None, [r_rdj], [r_rdj])
                            RECIP(rden_j, rden_j, [r_rdj], [r_rdj])

                    def stage_b2(step, d_):
                        j, bs, col, need_out, is_last = info(step, d_)
                        if need_out:
                            STT("dve", hsum[:, j, :], ndi[d_][:, 0:128], t0f[:, 392 + d_:393 + d_], hsum[:, j, :], ALU.mult, ALU.add,
                                [r_ndi[d_], r_rdj, r_hs], [r_hs])
                        yield

                    def interleave(*gens):
                        gens = list(gens)
                        while gens:
                            for g in list(gens):
                                try:
                                    next(g)
                                except StopIteration:
                                    gens.remove(g)

                    for bnk in range(4):
                        ps_reserved.add(bnk)
                    interleave(a1(0, 0), a1(0, 1))
                    interleave(a2a(0, 0), a2a(0, 1))
                    interleave(a2b(0, 0), a2b(0, 1))
                    interleave(a1(1, 0), a1(1, 1))
                    for step in range(NJ):
                        if step + 2 < NJ:
                            interleave(a1(step + 2, 0), a1(step + 2, 1))
                        if step + 1 < NJ:
                            interleave(a2a(step + 1, 0), a2a(step + 1, 1))
                        interleave(stage_b(step, 0), stage_b(step, 1))
                        den_ops(step)
                        interleave(stage_b2(step, 0), stage_b2(step, 1))
                        if step + 1 < NJ:
                            interleave(a2b(step + 1, 0), a2b(step + 1, 1))
                    for bnk in range(4):
                        ps_reserved.discard(bnk)
                    nj = NJ - j0
                    hv = hsum[:, j0:NJ, :]
                    sqv = ym[:, h, j0 * 128:NT].rearrange("p (j e) -> p j e", e=128)
                    ymh_res = trs(r_ym, h, j0 * 128, NT) + ([r_wvo] if h == 3 else [])
                    P.op("dve", (lambda hv=hv, nj=nj: lambda e: e.tensor_reduce(out=st1[:, 0:nj], in_=hv, axis=AX.X, op=ALU.add))(),
                         [r_hs], [r_st])
                    TT("dve", sqv, hv, hv, ALU.mult, [r_hs], ymh_res)
                    P.op("dve", (lambda sqv=sqv, nj=nj: lambda e: e.tensor_reduce(out=st1[:, NJ:NJ + nj], in_=sqv, axis=AX.X, op=ALU.add))(),
                         ymh_res, [r_st])
                    mean_ = st2[:, 0:nj]
                    var_ = st2[:, NJ:NJ + nj]
                    TS("dve", mean_, st1[:, 0:nj], 1.0 / HD, None, ALU.mult, None, [r_st], [r_st])
                    TT("dve", var_, mean_, mean_, ALU.mult, [r_st], [r_st])
                    STT("dve", var_, st1[:, NJ:NJ + nj], 1.0 / HD, var_, ALU.mult, ALU.subtract, [r_st], [r_st])
                    ACT(var_, var_, AF.Sqrt, [r_st, r_const], [r_st], bias=cst[:, 1:2], scale=1.0)
                    RECIP(var_, var_, [r_st], [r_st])
                    TT("dve", hv, hv, mean_.unsqueeze(2).to_broadcast([128, nj, 128]), ALU.subtract, [r_hs, r_st], [r_hs])
                    TT("dve", hv, hv, var_.unsqueeze(2).to_broadcast([128, nj, 128]), ALU.mult, [r_hs, r_st], [r_hs])
                    TT("dve", sgo[:, j0:NJ, :], hv, sgo[:, j0:NJ, :], ALU.mult, [r_hs, r_sg], [r_sg])
                    for jg in range(j0, NJ, 4):
                        p_t = next_ps()
                        ptb = psb[p_t][:, 0:256].bitcast(BF16)
                        jn = min(4, NJ - jg)
                        for jj in range(jn):
                            TR(ptb[:, jj * 128:(jj + 1) * 128], sgo[:, jg + jj, :], ident_b[:], [r_sg, r_const], [r_ps[p_t]])
                        ACT(ym[:, h, jg * 128:(jg + jn) * 128], ptb[:, 0:jn * 128], AF.Copy, [r_ps[p_t]],
                            trs(r_ym, h, jg * 128, (jg + jn) * 128) + ([r_wvo] if h == 3 else []))

                if l == 0:
                    dump("d_ym", lambda kc: ym[:, kc, :], lambda kc: trs(r_ym, kc, 0, NT))

                P.barrier()
                maybe_stop(10 * l + 4)
                o_wo = 4096
                wo_s = arena(o_wo, KC * KC * 128).rearrange("p (o k c) -> p o k c", o=KC, k=KC)
                r_wo = R()
                obuf = arena(o_wo + 8192, KC * 512, F32).rearrange("p (o t) -> p o t", t=512)
                r_ob = [R() for _ in range(KC)]
                DMA("pool", wo_s.rearrange("p o k c -> p (o k c)"), woutc[l].rearrange("p o k c -> p (o k c)"), [],
                    [r_wo])
                for (a, b, w) in TL:
                    n = b - a
                    for oc in range(KC):
                        pi = next_ps()
                        for kc in range(KC):
                            MM(psb[pi][:, 0:n], wo_s[:, oc, kc, :], ym[:, kc, a:b], kc == 0, kc == KC - 1,
                               [r_wo] + trs(r_ym, kc, a, b), [r_ps[pi]])
                        ACT(obuf[:, oc, 0:n], psb[pi][:, 0:n], AF.Copy, [r_ps[pi]], [r_ob[oc]])
                    resid_update([(a, b, w)], lambda kc, a_, b_: obuf[:, kc, 0:b_ - a_], lambda kc, a_, b_: [r_ob[kc]], G1)
                if l == 0:
                    dump("d_x1", lambda kc: xs[:, kc, :], lambda kc: trs(r_xs, kc, 0, NT))

                P.barrier()
                maybe_stop(10 * l + 5)
                hT = scr[:, 0:24576].rearrange("p (f t) -> p f t", t=768)
                r_hT = [R() for _ in range(32)]
                ob2 = scr[:, 24576:30720].rearrange("p (o t) -> p o t", t=768)
                r_ob2 = [R() for _ in range(KC)]
                w1b = [scr[:, 30720 + i * 2048:30720 + (i + 1) * 2048].rearrange("p (g k c) -> p g k c", g=2, k=KC) for i in range(2)]
                w2b = [scr[:, 34816 + i * 4096:34816 + (i + 1) * 4096].rearrange("p (f c) -> p f c", c=128) for i in range(2)]
                r_w1b = [R(), R()]
                r_w2b = [R(), R()]
                if need_ctx:
                    supers = [[(0, 256, 1), (256, 768, 0)], [(768, 1280, 0), (1280, 1536, 0)], [(1536, 2048, 0), (2048, 2304, 0)]]
                else:
                    supers = [[(256, 768, 0), (768, 1024, 0)], [(1024, 1536, 0), (1536, 1792, 0)], [(1792, 2304, 0)]]
                n1 = 0
                n2 = 0
                for sup in supers:
                    a0 = sup[0][0]
                    prenorm(sup, A2, 24, l)
                    for g in range(16):
                        i = n1 % 2
                        n1 += 1
                        DMA("pool", w1b[i].rearrange("p g k c -> p (g k c)"), w1c[l, g].rearrange("p g k c -> p (g k c)"), [],
                            [r_w1b[i]])
                        for f2 in range(2):
                            f = g * 2 + f2
                            for (a, b, w) in sup:
                                n = b - a
                                pi = next_ps()
                                for kc in range(KC):
                                    MM(psb[pi][:, 0:n], w1b[i][:, f2, kc, :], hx[:, kc, a:b], kc == 0, kc == KC - 1,
                                       [r_w1b[i]] + trs(r_hx, kc, a, b), [r_ps[pi]])
                                ti = next_tf()
                                ACT(tmpf[ti][:, 0:n], psb[pi][:, 0:n], AF.Relu, [r_ps[pi]], [r_tmpf[ti]])
                                TT("dve", hT[:, f, a - a0:b - a0], tmpf[ti][:, 0:n], tmpf[ti][:, 0:n], ALU.mult, [r_tmpf[ti]], [r_hT[f]])
                    ssb = []
                    for _ in sup:
                        pss = next_ps()
                        ps_reserved.add(pss)
                        ssb.append(pss)
                    for oc in range(KC):
                        i = n2 % 2
                        n2 += 1
                        DMA("pool", w2b[i].rearrange("p f c -> p (f c)"), w2c[l, oc].rearrange("p f c -> p (f c)"), [],
                            [r_w2b[i]])
                        for si_, (a, b, w) in enumerate(sup):
                            n = b - a
                            pi = next_ps()
                            for f in range(32):
                                MM(psb[pi][:, 0:n], w2b[i][:, f, :], hT[:, f, a - a0:b - a0], f == 0, f == 31,
                                   [r_w2b[i], r_hT[f]], [r_ps[pi]])
                            ACT(ob2[:, oc, a - a0:b - a0], psb[pi][:, 0:n], AF.Copy, [r_ps[pi]], [r_ob2[oc]])
                            sq_i = next_sq()
                            ACT(sqb[sq_i][:, 0:n], psb[pi][:, 0:n], AF.Square, [r_ps[pi]], [r_sqb[sq_i]])
                            MM(psb[ssb[si_]][:, 0:n], ones_b[:], sqb[sq_i][:, 0:n], oc == 0, oc == KC - 1,
                               [r_sqb[sq_i], r_const], [r_ps[ssb[si_]]])
                    for si_, (a, b, w) in enumerate(sup):
                        n = b - a
                        ACT(rs[:, 0:n], psb[ssb[si_]][:, 0:n], AF.Sqrt, [r_ps[ssb[si_]], r_const], [r_rs], bias=cst[:, 0:1], scale=1.0)
                        RECIP(rs[:, 0:n], rs[:, 0:n], [r_rs], [r_rs])
                        for kc in range(KC):
                            ti = next_tf()
                            TT("dve", tmpf[ti][:, 0:n], ob2[:, kc, a - a0:b - a0], rs[:, 0:n], ALU.mult,
                               [r_ob2[kc], r_rs], [r_tmpf[ti]])
                            STT("dve", xs[:, kc, a:b], tmpf[ti][:, 0:n], G2[:, kc, w:w + 1], xs[:, kc, a:b], ALU.mult, ALU.add,
                                [r_tmpf[ti], r_lay] + trs(r_xs, kc, a, b), trs(r_xs, kc, a, b))
                    for pss in ssb:
                        ps_reserved.discard(pss)
                if l == 0:
                    dump("d_x2", lambda kc: xs[:, kc, :], lambda kc: trs(r_xs, kc, 0, NT))
                P.barrier()

        except _Stop:
            pass
        for kc in range(KC):
            DMA("sp", outT[kc], xs[:, kc, CTX:NT], trs(r_xs, kc, CTX, NT), [])
        P.emit()
    return nc


_CACHE = {}


def _consts():
    if "c" in _CACHE:
        return _CACHE["c"]
    c = {}
    c["ident"] = np.eye(128, dtype=np.float32)
    s = np.arange(128)
    c["umask"] = (s[:, None] <= s[None, :]).astype(np.float32)
    c["lmask"] = (s[:, None] >= s[None, :]).astype(np.float32)
    k = np.arange(64)
    ang = 2.0 * np.pi * np.outer(k, k) / 64.0
    cs = np.zeros((128, 256), np.float64)
    for hh in range(2):
        cs[hh * 64:(hh + 1) * 64, hh * 64:(hh + 1) * 64] = np.cos(ang) / 8.0
        cs[hh * 64:(hh + 1) * 64, 128 + hh * 64:128 + (hh + 1) * 64] = np.sin(ang) / 8.0
    c["cs64"] = cs.astype(np.float32)
    t = np.arange(SEQ, dtype=np.int64)
    ph = (np.outer(t, t) % SEQ).astype(np.float64) * (2.0 * np.pi / SEQ)
    dc = (np.cos(ph) / math.sqrt(SEQ)).astype(np.float32)
    ds = (-np.sin(ph) / math.sqrt(SEQ)).astype(np.float32)
    both = np.stack([dc, ds], 0)
    both = both.reshape(2, 4, 4, 128, 4, 512)
    c["dftx"] = np.ascontiguousarray(both.transpose(4, 1, 3, 0, 2, 5)).astype(ml_dtypes.bfloat16)
    t = np.arange(CTX, dtype=np.int64)
    ph = (np.outer(t, t) % CTX).astype(np.float64) * (2.0 * np.pi / CTX)
    both = np.stack([np.cos(ph), -np.sin(ph)], 0) / math.sqrt(CTX)
    both = both.reshape(2, 2, 128, CTX)
    c["dftc"] = np.ascontiguousarray(both.transpose(2, 0, 1, 3)).astype(ml_dtypes.bfloat16)
    rows = SEQ // 64
    quarter = D // 4
    freq = np.exp(-math.log(10000.0) * np.arange(quarter, dtype=np.float32) / quarter).astype(np.float32)
    r = np.broadcast_to(np.arange(rows, dtype=np.float32)[:, None], (rows, 64)).reshape(-1)
    col = np.broadcast_to(np.arange(64, dtype=np.float32)[None, :], (rows, 64)).reshape(-1)
    ar = r[:, None] * freq
    ac = col[:, None] * freq
    pos = np.concatenate([np.sin(ar), np.cos(ar), np.sin(ac), np.cos(ac)], axis=-1).astype(np.float32)
    c["posT"] = np.ascontiguousarray(pos.T).reshape(KC, 128, SEQ)
    _CACHE["c"] = c
    return c


def _chunk_w(w, cols):
    return np.ascontiguousarray(w[:, cols].reshape(KC, 128, -1).transpose(1, 0, 2))


def _prep_shared(inp):
    f = np.float32
    L = DEPTH
    w_in = np.asarray(inp["w_in"], f)
    sh = {}
    wada = np.asarray(inp["w_ada"], f)
    sh["wada"] = np.ascontiguousarray(wada.reshape(L, KC, 128, 12, 512).transpose(0, 3, 2, 1, 4))
    sh["bada"] = np.ascontiguousarray(np.asarray(inp["b_ada"], f).reshape(L, 48, 128).transpose(0, 2, 1))
    gs = np.stack([np.asarray(inp[k], f) for k in ("g_pre_mix", "g_post_mix", "g_pre_mlp", "g_post_mlp")], 1)
    sh["gvec"] = np.ascontiguousarray(gs.reshape(L, 4, KC, 128).transpose(0, 3, 1, 2))
    offs = [Q_OFF + 128 * i for i in range(4)] + [K_OFF + 128 * i for i in range(4)] + \
           [F_OFF, F_OFF + 128, CA_OFF, CA_OFF + 128, CG_OFF, CG_OFF + 128]
    sh["winc"] = np.stack([np.stack([_chunk_w(w_in[l], np.arange(o, o + 128)) for o in offs]) for l in range(L)])
    sh["wvo"] = np.stack([np.stack([_chunk_w(w_in[l], np.concatenate([np.arange(V_OFF + 128 * h, V_OFF + 128 * h + 128),
                                                                      np.arange(O_OFF + 128 * h, O_OFF + 128 * h + 128)]))
                                    for h in range(NH)]) for l in range(L)])
    sh["wgate"] = np.stack([_chunk_w(w_in[l], np.arange(G_OFF, G_OFF + 16)) for l in range(L)])
    sh["bgate"] = np.ascontiguousarray(np.asarray(inp["b_gate"], f).reshape(L, 16))
    wqk = np.asarray(inp["w_qk_conv"], f)
    sh["wqkc"] = np.ascontiguousarray(wqk.reshape(L, 3, 8, 128).transpose(0, 2, 1, 3))
    sh["gmn"] = np.ascontiguousarray(np.asarray(inp["g_mlstm_norm"], f))
    wdw = np.asarray(inp["w_dw"], f)
    sh["wdw"] = np.ascontiguousarray(wdw.reshape(L, CONV_K, 2, 128).transpose(0, 3, 2, 1))
    cv = np.stack([np.asarray(inp[k], f) for k in ("b_dw", "g_conv_ln", "b_conv_ln")], 1)
    sh["cvp"] = np.ascontiguousarray(cv.reshape(L, 3, 2, 128).transpose(0, 3, 1, 2))
    wout = np.asarray(inp["w_out"], f)
    sh["woutc"] = np.ascontiguousarray(wout.reshape(L, KC, 128, KC, 128).transpose(0, 2, 3, 1, 4))
    w1 = np.asarray(inp["w_mlp1"], f)
    sh["w1c"] = np.ascontiguousarray(w1.reshape(L, KC, 128, 16, 2, 128).transpose(0, 3, 2, 4, 1, 5))
    w2 = np.asarray(inp["w_mlp2"], f)
    sh["w2c"] = np.ascontiguousarray(w2.reshape(L, 32, 128, KC, 128).transpose(0, 3, 2, 1, 4))
    c = _consts()
    for k in ("ident", "umask", "lmask", "cs64", "dftx", "dftc", "posT"):
        sh[k] = c[k]
    return sh


def make_in_maps(inp, cores):
    sh = _prep_shared(inp)
    x = np.asarray(inp["x"], np.float32)
    ctx = np.asarray(inp["ctx"], np.float32)
    c = np.asarray(inp["c"], np.float32)
    c_ctx = np.asarray(inp["c_ctx"], np.float32)
    maps = []
    for b in cores:
        m = dict(sh)
        m["xT"] = np.ascontiguousarray(x[b].T).reshape(KC, 128, SEQ)
        m["ctxT"] = np.ascontiguousarray(ctx[b].T).reshape(KC, 128, CTX)
        cv = np.stack([c[b], c_ctx], -1)
        m["cvec"] = np.ascontiguousarray(cv.reshape(KC, 128, 2).transpose(1, 0, 2))
        maps.append(m)
    return maps


def kernel(**inputs):
    if "nc" not in _CACHE:
        _CACHE["nc"] = build_program(dbg=False)
    nc = _CACHE["nc"]
    in_maps = make_in_maps(inputs, list(range(NB)))
    res = run_bass_kernel_spmd(nc, in_maps, core_ids=list(range(NB)))
    out = np.empty((NB, SEQ, D), np.float32)
    for b in range(NB):
        oT = np.asarray(res.results[b]["outT"], np.float32).reshape(D, SEQ)
        out[b] = oT.T
    return out
```

```python
import math
from contextlib import ExitStack

import numpy as np
import ml_dtypes

import concourse.bass as bass
import concourse.mybir as mybir
from concourse.bass_utils import run_bass_kernel_spmd

F32 = mybir.dt.float32
BF16 = mybir.dt.bfloat16
AF = mybir.ActivationFunctionType
ALU = mybir.AluOpType
AX = mybir.AxisListType

ENGS = ("pe", "act", "dve", "pool", "sp")
SEM_LIM = 2000
N_DMA_SEMS = 16


class Res:
    __slots__ = ("name", "w", "r", "excl")

    def __init__(self, name, excl=False):
        self.name = name
        self.w = None
        self.r = []
        self.excl = excl


class Op:
    __slots__ = ("eng", "idx", "fn", "deps", "dma", "signal", "semref", "dma_slot", "dma_val")

    def __init__(self, eng, idx, fn, dma):
        self.eng = eng
        self.idx = idx
        self.fn = fn
        self.deps = []
        self.dma = dma
        self.signal = False
        self.semref = None
        self.dma_slot = None
        self.dma_val = None


class Prog:
    def __init__(self, nc, same_engine_sync=True):
        self.nc = nc
        self.ops = {e: [] for e in ENGS}
        self.n_dma_q = {}
        self.same_engine_sync = same_engine_sync

    def res(self, name="", excl=False):
        return Res(name, excl)

    def op(self, eng, fn, reads=(), writes=(), dma=False):
        o = Op(eng, len(self.ops[eng]), fn, dma)
        if dma:
            half = N_DMA_SEMS // 2
            k = self.n_dma_q.get(eng, 0)
            self.n_dma_q[eng] = k + 1
            o.dma_slot = (k % half) + (half if eng == "pool" else 0)
            o.dma_val = 16 * (k // half + 1)
        deps = []
        for r in reads:
            if r.w is not None:
                deps.append(r.w)
            if r.excl:
                deps.extend(x for x in r.r if x.eng != eng)
        for r in writes:
            if r.w is not None:
                deps.append(r.w)
            deps.extend(r.r)
        for r in reads:
            r.r.append(o)
        for r in writes:
            r.w = o
            r.r = []
        seen = set()
        for d in deps:
            if d is o or id(d) in seen:
                continue
            seen.add(id(d))
            if (not d.dma) and (not dma) and d.eng == eng:
                if eng == "pe" or not self.same_engine_sync:
                    continue
            o.deps.append(d)
        self.ops[eng].append(o)
        return o

    def barrier(self):
        targets = []
        for e in ENGS:
            cs = [o for o in self.ops[e] if not o.dma and o.fn is not None]
            if cs:
                targets.append(cs[-1])
        by_slot = {}
        for e in ENGS:
            for o in self.ops[e]:
                if o.dma and (o.dma_slot not in by_slot or o.dma_val > by_slot[o.dma_slot].dma_val):
                    by_slot[o.dma_slot] = o
        targets += list(by_slot.values())
        for e in ENGS:
            o = Op(e, len(self.ops[e]), None, False)
            o.deps = list(targets) if e != "pe" else [t for t in targets if t.dma or t.eng != e]
            self.ops[e].append(o)

    def emit(self):
        nc = self.nc
        for e in ENGS:
            for o in self.ops[e]:
                for d in o.deps:
                    d.signal = True
        with ExitStack() as st:
            dma_sems = [st.enter_context(nc.semaphore(f"dq{i}")) for i in range(N_DMA_SEMS)]
            for e in ENGS:
                n = sum(1 for o in self.ops[e] if o.signal and not o.dma)
                k = max((n + SEM_LIM - 1) // SEM_LIM, 1)
                sems = [st.enter_context(nc.semaphore(f"s_{e}{i}")) for i in range(k)]
                c = 0
                for o in self.ops[e]:
                    if o.signal and not o.dma:
                        o.semref = (sems[c // SEM_LIM], c % SEM_LIM + 1, c)
                        c += 1
            block = st.enter_context(nc.Block())

            def run(e, eng):
                waited_c = {p: -1 for p in ENGS}
                waited_d = [0] * N_DMA_SEMS
                for o in self.ops[e]:
                    for d in o.deps:
                        if d.dma:
                            if waited_d[d.dma_slot] >= d.dma_val:
                                continue
                            waited_d[d.dma_slot] = d.dma_val
                            eng.wait_ge(dma_sems[d.dma_slot], d.dma_val)
                        else:
                            sem, val, gc = d.semref
                            if waited_c[d.eng] >= gc:
                                continue
                            waited_c[d.eng] = gc
                            eng.wait_ge(sem, val)
                    if o.fn is None:
                        continue
                    ins = o.fn(eng)
                    if o.dma:
                        ins.then_inc(dma_sems[o.dma_slot], 16)
                    elif o.signal:
                        ins.then_inc(o.semref[0], 1)

            fin_dma = {}
            for e in ENGS:
                for o in self.ops[e]:
                    if o.dma:
                        fin_dma[o.dma_slot] = max(fin_dma.get(o.dma_slot, 0), o.dma_val)

            @block.tensor
            def _(eng):
                run("pe", eng)

            @block.scalar
            def _(eng):
                run("act", eng)

            @block.vector
            def _(eng):
                run("dve", eng)

            @block.gpsimd
            def _(eng):
                run("pool", eng)

            @block.sync
            def _(eng):
                run("sp", eng)
                for slot, val in sorted(fin_dma.items()):
                    eng.wait_ge(dma_sems[slot], val)


D = 1024
NB = 8
SEQ = 2048
CTX = 256
NT = CTX + SEQ
DEPTH = 2
KC = 8
NJ = NT // 128
HD = 128
NH = 4
DFF = 4096
EPS = 1e-6
CONV_K = 31
PADC = 15
Q_OFF, K_OFF, V_OFF, O_OFF, G_OFF = 0, 512, 1024, 1536, 2048
F_OFF = 2064
CA_OFF = 2320
CG_OFF = 2576
UPW = (CTX + 2 * PADC) + (SEQ + 2 * PADC)


def tiles_for(need_ctx):
    t = [(256, 768, 0), (768, 1280, 0), (1280, 1792, 0), (1792, 2304, 0)]
    if need_ctx:
        t = [(0, 256, 1)] + t
    return t


class _Stop(Exception):
    pass


def build_program(dbg=False, stop=None):
    nc = bass.Bass("TRN2", target_bir_lowering=False)
    P = Prog(nc)

    def din(name, shape, dt=F32):
        return nc.dram_tensor(name, list(shape), dt, kind="ExternalInput").ap()

    def dout(name, shape, dt=F32):
        return nc.dram_tensor(name, list(shape), dt, kind="ExternalOutput").ap()

    xT = din("xT", [KC, 128, SEQ])
    ctxT = din("ctxT", [KC, 128, CTX])
    posT = din("posT", [KC, 128, SEQ])
    cvec = din("cvec", [128, KC, 2])
    wada = din("wada", [DEPTH, 12, 128, KC, 512])
    bada = din("bada", [DEPTH, 128, 48])
    gvec = din("gvec", [DEPTH, 128, 4, KC])
    winc = din("winc", [DEPTH, 14, 128, KC, 128])
    wvo = din("wvo", [DEPTH, NH, 128, KC, 256])
    wgate = din("wgate", [DEPTH, 128, KC, 16])
    bgate = din("bgate", [DEPTH, 16])
    wqkp = din("wqkp", [DEPTH, 8, 128, 3])
    gmn = din("gmn", [DEPTH, 512])
    wdw = din("wdw", [DEPTH, 128, 2, CONV_K])
    cvp = din("cvp", [DEPTH, 128, 3, 2])
    woutc = din("woutc", [DEPTH, 128, KC, KC, 128])
    w1c = din("w1c", [DEPTH, 16, 128, 2, KC, 128])
    w2c = din("w2c", [DEPTH, KC, 128, 32, 128])
    ident_d = din("ident", [128, 128])
    umask_d = din("umask", [128, 128])
    lmask_d = din("lmask", [128, 128])
    cs64_d = din("cs64", [128, 256])
    dftx_d = din("dftx", [4, 4, 128, 2, 4, 512], BF16)
    dftc_d = din("dftc", [128, 2, 2, 256], BF16)
    outT = dout("outT", [KC, 128, SEQ])
    dbg_out = {}
    if dbg:
        for nm in ("d_hx", "d_ym", "d_x1", "d_x2"):
            dbg_out[nm] = dout(nm, [KC, 128, NT])

    with ExitStack() as st:
        def sb(name, shape, dt):
            return st.enter_context(nc.sbuf_tensor(name, list(shape), dt))

        xs = sb("xs", [128, KC, NT], F32)
        hx = sb("hx", [128, KC, NT], BF16)
        SCR_ARENA = 25088
        SCR_N = SCR_ARENA + KC * NT
        scr = sb("scr", [128, SCR_N], BF16)
        ym_flat = scr[:, SCR_ARENA:SCR_N]
        ym = ym_flat.rearrange("p (c t) -> p c t", t=NT)
        ident_f = sb("ident_f", [128, 128], F32)
        ident_b = sb("ident_b", [128, 128], BF16)
        umask_f = sb("umask_f", [128, 128], F32)
        lmask_f = sb("lmask_f", [128, 128], F32)
        negm_f = sb("negm_f", [128, 128], BF16)
        negm_b = sb("negm_b", [128, 128], BF16)
        ones_f = sb("ones_f", [128, 128], F32)
        ones_b = sb("ones_b", [128, 128], BF16)
        cs64 = sb("cs64b", [128, 256], BF16)
        cst = sb("cst", [128, 4], F32)
        cv_f = sb("cv_f", [128, KC, 2], F32)
        sc_b = sb("sc_b", [128, KC, 2], BF16)
        mod = sb("mod", [128, DEPTH, 48, 2], F32)
        bada_s = sb("bada_s", [128, DEPTH, 48], F32)
        gv = sb("gv", [128, DEPTH, 4, KC], F32)
        A1 = sb("A1", [128, KC, 2], F32)
        A2 = sb("A2", [128, KC, 2], F32)
        G1 = sb("G1", [128, KC, 2], F32)
        G2 = sb("G2", [128, KC, 2], F32)
        rs = sb("rs", [128, 512], F32)
        sqb = [sb(f"sqb{i}", [128, 512], BF16) for i in range(2)]
        tmpf = [sb(f"tmpf{i}", [128, 512], F32) for i in range(2)]
        psb = [st.enter_context(nc.psum_tensor(f"ps{i}", [128, 512], F32)) for i in range(8)]

        R = P.res
        r_xs = [[R() for _ in range(NJ)] for _ in range(KC)]
        r_hx = [[R() for _ in range(NJ)] for _ in range(KC)]
        r_ym = [[R() for _ in range(NJ)] for _ in range(KC)]
        r_ps = [P.res("ps", excl=True) for _ in range(8)]
        r_const = R()
        r_mod = R()
        r_lay = R()
        r_rs = R()
        r_sqb = [R(), R()]
        r_tmpf = [R(), R()]
        r_arena = R()

        def trs(rl, kc, a, b):
            return [rl[kc][j] for j in range(a // 128, (b + 127) // 128)]

        def trs_all(rl, a, b):
            out = []
            for kc in range(KC):
                out += trs(rl, kc, a, b)
            return out

        cnt = {"ps": 0, "sq": 0, "tf": 0}

        ps_reserved = set()

        def next_ps():
            while True:
                i = cnt["ps"] % 8
                cnt["ps"] += 1
                if i not in ps_reserved:
                    return i

        def next_sq():
            i = cnt["sq"] % 2
            cnt["sq"] += 1
            return i

        def next_tf():
            i = cnt["tf"] % 2
            cnt["tf"] += 1
            return i

        def MM(out, lhsT, rhs, start, stop, reads, writes):
            P.op("pe", lambda e: e.matmul(out, lhsT=lhsT, rhs=rhs, start=start, stop=stop), reads, writes)

        def TR(out, in_, ident, reads, writes):
            P.op("pe", lambda e: e.transpose(out, in_, ident), reads, writes)

        def ACT(out, in_, func, reads, writes, bias=None, scale=None, accum=None):
            kw = {}
            if bias is not None:
                kw["bias"] = bias
            if scale is not None:
                kw["scale"] = scale
            if accum is not None:
                kw["accum_out"] = accum
            P.op("act", lambda e: e.activation(out=out, in_=in_, func=func, **kw), reads, writes)

        def TT(eng, out, in0, in1, op, reads, writes):
            P.op(eng, lambda e: e.tensor_tensor(out=out, in0=in0, in1=in1, op=op), reads, writes)

        def TS(eng, out, in0, s1, s2, op0, op1, reads, writes):
            if s2 is None:
                P.op(eng, lambda e: e.tensor_scalar(out=out, in0=in0, scalar1=s1, scalar2=None, op0=op0), reads, writes)
            else:
                P.op(eng, lambda e: e.tensor_scalar(out=out, in0=in0, scalar1=s1, scalar2=s2, op0=op0, op1=op1), reads, writes)

        def STT(eng, out, in0, scalar, in1, op0, op1, reads, writes):
            P.op(eng, lambda e: e.scalar_tensor_tensor(out=out, in0=in0, scalar=scalar, in1=in1, op0=op0, op1=op1), reads, writes)

        def CP(eng, out, in_, reads, writes):
            P.op(eng, lambda e: e.tensor_copy(out=out, in_=in_), reads, writes)

        def MS(eng, ap, val, writes):
            P.op(eng, lambda e: e.memset(ap, val), (), writes)

        def RECIP(out, in_, reads, writes):
            P.op("dve", lambda e: e.reciprocal(out=out, in_=in_), reads, writes)

        def DMA(q, out, in_, reads, writes):
            P.op(q, lambda e: e.dma_start(out=out, in_=in_), reads, writes, dma=True)

        def arena(off, n, dt=BF16):
            if dt == BF16:
                return scr[:, off:off + n]
            assert off % 2 == 0
            return scr[:, off:off + 2 * n].bitcast(F32)

        DMA("sp", ident_f[:], ident_d, [], [r_const])
        DMA("sp", umask_f[:], umask_d, [], [r_const])
        DMA("sp", lmask_f[:], lmask_d, [], [r_const])
        DMA("pool", cs64[:], cs64_d, [], [r_const])
        DMA("sp", cv_f[:], cvec, [], [r_const])
        DMA("sp", bada_s[:], bada.rearrange("l p n -> p l n"), [], [r_const])
        DMA("sp", gv[:], gvec.rearrange("l p a k -> p l a k"), [], [r_const])
        CP("dve", ident_b[:], ident_f[:], [r_const], [r_const])
        TS("dve", negm_f[:], umask_f[:], -1.0, 30000.0, ALU.add, ALU.mult, [r_const], [r_const])
        TS("dve", negm_b[:], lmask_f[:], -1.0, 30000.0, ALU.add, ALU.mult, [r_const], [r_const])
        MS("dve", ones_f[:], 1.0, [r_const])
        MS("dve", ones_b[:], 1.0, [r_const])
        MS("dve", cst[:, 0:1], 1024.0 * EPS, [r_const])
        MS("dve", cst[:, 1:2], EPS, [r_const])
        MS("dve", cst[:, 2:3], 1.0, [r_const])
        MS("dve", cst[:, 3:4], 0.0, [r_const])

        for kc in range(KC):
            DMA("sp", xs[:, kc, CTX:NT], xT[kc], [], trs(r_xs, kc, CTX, NT))
            DMA("sp", xs[:, kc, 0:CTX], ctxT[kc], [], trs(r_xs, kc, 0, CTX))
        pos_buf = [arena(0, SEQ, F32), arena(2 * SEQ, SEQ, F32)]
        r_pos = [R(), R()]
        for kc in range(KC):
            i = kc % 2
            DMA("sp", pos_buf[i], posT[kc], [], [r_pos[i]])
            TT("dve", xs[:, kc, CTX:NT], xs[:, kc, CTX:NT], pos_buf[i], ALU.add,
               [r_pos[i]] + trs(r_xs, kc, CTX, NT), trs(r_xs, kc, CTX, NT))

        ACT(sc_b[:], cv_f[:], AF.Silu, [r_const], [r_const])
        wa_off = 4 * SEQ
        wa_buf = [arena(wa_off + i * KC * 512, KC * 512).rearrange("p (k c) -> p k c", c=512) for i in range(2)]
        r_wa = [R(), R()]
        for l in range(DEPTH):
            pi = next_ps()
            pv = psb[pi][:, 0:96].rearrange("p (n w) -> p n w", w=2)
            for blk in range(12):
                i = (l * 12 + blk) % 2
                DMA("pool", wa_buf[i], wada[l, blk], [], [r_wa[i]])
                for n4 in range(4):
                    n = blk * 4 + n4
                    for kc in range(KC):
                        MM(pv[:, n, :], wa_buf[i][:, kc, n4 * 128:(n4 + 1) * 128], sc_b[:, kc, :], kc == 0, kc == KC - 1,
                           [r_wa[i], r_const], [r_ps[pi]])
            TT("dve", mod[:, l], pv, bada_s[:, l].unsqueeze(2).to_broadcast([128, 48, 2]), ALU.add,
               [r_ps[pi], r_const], [r_mod])

        def rstd_of(src_fn, n, reads):
            pi = next_ps()
            for kc in range(KC):
                si = next_sq()
                ACT(sqb[si][:, 0:n], src_fn(kc), AF.Square, reads(kc), [r_sqb[si]])
                MM(psb[pi][:, 0:n], ones_b[:], sqb[si][:, 0:n], kc == 0, kc == KC - 1, [r_sqb[si], r_const], [r_ps[pi]])
            ACT(rs[:, 0:n], psb[pi][:, 0:n], AF.Sqrt, [r_ps[pi], r_const], [r_rs], bias=cst[:, 0:1], scale=1.0)
            RECIP(rs[:, 0:n], rs[:, 0:n], [r_rs], [r_rs])

        def prenorm(tl, Amod, shift_base, l):
            for (a, b, w) in tl:
                n = b - a
                rstd_of(lambda kc: xs[:, kc, a:b], n, lambda kc: trs(r_xs, kc, a, b))
                for kc in range(KC):
                    ti = next_tf()
                    TT("dve", tmpf[ti][:, 0:n], xs[:, kc, a:b], rs[:, 0:n], ALU.mult,
                       trs(r_xs, kc, a, b) + [r_rs], [r_tmpf[ti]])
                    ACT(hx[:, kc, a:b], tmpf[ti][:, 0:n], AF.Identity, [r_tmpf[ti], r_lay, r_mod], trs(r_hx, kc, a, b),
                        bias=mod[:, l, shift_base + kc, w:w + 1], scale=Amod[:, kc, w:w + 1])

        def resid_update(tl, src_fn, src_reads, Gm):
            for (a, b, w) in tl:
                n = b - a
                rstd_of(lambda kc: src_fn(kc, a, b), n, lambda kc: src_reads(kc, a, b))
                for kc in range(KC):
                    ti = next_tf()
                    TT("dve", tmpf[ti][:, 0:n], src_fn(kc, a, b), rs[:, 0:n], ALU.mult,
                       src_reads(kc, a, b) + [r_rs], [r_tmpf[ti]])
                    STT("dve", xs[:, kc, a:b], tmpf[ti][:, 0:n], Gm[:, kc, w:w + 1], xs[:, kc, a:b], ALU.mult, ALU.add,
                        [r_tmpf[ti], r_lay] + trs(r_xs, kc, a, b), trs(r_xs, kc, a, b))

        def dump(name, src_fn, reads_fn):
            if not dbg:
                return
            for kc in range(KC):
                DMA("pool", dbg_out[name][kc], src_fn(kc), reads_fn(kc), [])

        def maybe_stop(k):
            if stop is not None and stop == k:
                raise _Stop()

        try:
            P.barrier()
            for l in range(DEPTH):
                need_ctx = l < DEPTH - 1
                TL_all = tiles_for(True)
                TL = tiles_for(need_ctx)
                j0 = 0 if need_ctx else 2

                def mk(dst, gidx, mbase, plus1):
                    for w in range(2):
                        if plus1:
                            STT("dve", dst[:, :, w], mod[:, l, mbase:mbase + KC, w], 1.0, gv[:, l, gidx, :], ALU.add, ALU.mult,
                                [r_mod, r_const, r_lay], [r_lay])
                        else:
                            TT("dve", dst[:, :, w], mod[:, l, mbase:mbase + KC, w], gv[:, l, gidx, :], ALU.mult,
                               [r_mod, r_const, r_lay], [r_lay])
                    TS("dve", dst[:], dst[:], 32.0, None, ALU.mult, None, [r_lay], [r_lay])

                mk(A1, 0, 8, True)
                mk(G1, 1, 16, False)
                mk(A2, 2, 32, True)
                mk(G2, 3, 40, False)

                prenorm(TL_all, A1, 0, l)
                maybe_stop(10 * l + 1)
                if l == 0:
                    dump("d_hx", lambda kc: hx[:, kc, :], lambda kc: trs(r_hx, kc, 0, NT))

                o_w = 0
                wbuf = [arena(o_w + i * 1024, 1024).rearrange("p (k c) -> p k c", c=128) for i in range(4)]
                r_wbuf = [R() for _ in range(4)]
                wcnt = {"n": 0}

                def load_w(src):
                    i = wcnt["n"] % 4
                    wcnt["n"] += 1
                    DMA("pool", wbuf[i], src, [], [r_wbuf[i]])
                    return i

                o_up = 4096
                upad = arena(o_up, 2 * UPW).rearrange("p (c t) -> p c t", t=UPW)
                r_up = R()
                o_vb = o_up + 2 * UPW
                vbuf = arena(o_vb, 2 * NT, F32).rearrange("p (c t) -> p c t", t=NT)
                r_vb = [R(), R()]
                o_cs = o_vb + 4 * NT
                wdw_s = arena(o_cs, 2 * CONV_K, F32).rearrange("p (c k) -> p c k", k=CONV_K)
                cvp_s = arena(o_cs + 4 * CONV_K, 6, F32).rearrange("p (a c) -> p a c", c=2)
                r_cs = R()
                lnt = [ym_flat[:, i * 1024:(i + 1) * 1024].bitcast(F32) for i in range(5)]
                r_lnt = [R() for _ in range(5)]
                dgm = ym_flat[:, 5120:5120 + 2 * CONV_K * 128].rearrange("p (c k m) -> p c k m", k=CONV_K, m=128)
                r_dg = R()

                DMA("sp", wdw_s, wdw[l], [], [r_cs])
                DMA("sp", cvp_s, cvp[l], [], [r_cs])
                MS("dve", upad, 0.0, [r_up])
                for cc in range(2):
                    for k in range(CONV_K):
                        TS("dve", dgm[:, cc, k, :], ident_f[:], wdw_s[:, cc, k:k + 1], None, ALU.mult, None,
                           [r_const, r_cs], [r_dg])

                def upos(a):
                    return a + PADC if a < CTX else (CTX + 2 * PADC) + (a - CTX) + PADC

                for cc in range(2):
                    ia = load_w(winc[l, 10 + cc])
                    ig = load_w(winc[l, 12 + cc])
                    for (a, b, w) in TL:
                        n = b - a
                        pa = next_ps()
                        for kc in range(KC):
                            MM(psb[pa][:, 0:n], wbuf[ia][:, kc, :], hx[:, kc, a:b], kc == 0, kc == KC - 1,
                               [r_wbuf[ia]] + trs(r_hx, kc, a, b), [r_ps[pa]])
                        pg = next_ps()
                        for kc in range(KC):
                            MM(psb[pg][:, 0:n], wbuf[ig][:, kc, :], hx[:, kc, a:b], kc == 0, kc == KC - 1,
                               [r_wbuf[ig]] + trs(r_hx, kc, a, b), [r_ps[pg]])
                        ti = next_tf()
                        ACT(tmpf[ti][:, 0:n], psb[pg][:, 0:n], AF.Sigmoid, [r_ps[pg]], [r_tmpf[ti]])
                        TT("dve", upad[:, cc, upos(a):upos(a) + n], psb[pa][:, 0:n], tmpf[ti][:, 0:n], ALU.mult,
                           [r_ps[pa], r_tmpf[ti]], [r_up])
                for cc in range(2):
                    for (a, b, w) in TL:
                        n = b - a
                        pi = next_ps()
                        p0 = upos(a) - PADC
                        for k in range(CONV_K):
                            MM(psb[pi][:, 0:n], dgm[:, cc, k, :], upad[:, cc, p0 + k:p0 + k + n], k == 0, k == CONV_K - 1,
                               [r_dg, r_up], [r_ps[pi]])
                        ACT(vbuf[:, cc, a:b], psb[pi][:, 0:n], AF.Identity, [r_ps[pi], r_cs], [r_vb[cc]],
                            bias=cvp_s[:, 0, cc:cc + 1], scale=1.0)
                for (a, b, w) in TL:
                    n = b - a
                    p1 = next_ps()
                    p2 = next_ps()
                    for cc in range(2):
                        MM(psb[p1][:, 0:n], ones_f[:], vbuf[:, cc, a:b], cc == 0, cc == 1, [r_const, r_vb[cc]], [r_ps[p1]])
                    for cc in range(2):
                        TT("dve", lnt[0][:, 0:n], vbuf[:, cc, a:b], vbuf[:, cc, a:b], ALU.mult, [r_vb[cc]], [r_lnt[0]])
                        MM(psb[p2][:, 0:n], ones_f[:], lnt[0][:, 0:n], cc == 0, cc == 1, [r_const, r_lnt[0]], [r_ps[p2]])
                    mean = lnt[1]
                    TS("dve", mean[:, 0:n], psb[p1][:, 0:n], 1.0 / 256.0, None, ALU.mult, None, [r_ps[p1]], [r_lnt[1]])
                    TT("dve", lnt[2][:, 0:n], mean[:, 0:n], mean[:, 0:n], ALU.mult, [r_lnt[1]], [r_lnt[2]])
                    STT("dve", lnt[2][:, 0:n], psb[p2][:, 0:n], 1.0 / 256.0, lnt[2][:, 0:n], ALU.mult, ALU.subtract,
                        [r_ps[p2], r_lnt[2]], [r_lnt[2]])
                    ACT(lnt[2][:, 0:n], lnt[2][:, 0:n], AF.Sqrt, [r_lnt[2], r_const], [r_lnt[2]], bias=cst[:, 1:2], scale=1.0)
                    RECIP(lnt[2][:, 0:n], lnt[2][:, 0:n], [r_lnt[2]], [r_lnt[2]])
                    for cc in range(2):
                        TT("dve", lnt[3 + cc][:, 0:n], vbuf[:, cc, a:b], mean[:, 0:n], ALU.subtract,
                           [r_vb[cc], r_lnt[1]], [r_lnt[3 + cc]])
                        TT("dve", lnt[3 + cc][:, 0:n], lnt[3 + cc][:, 0:n], lnt[2][:, 0:n], ALU.mult,
                           [r_lnt[3 + cc], r_lnt[2]], [r_lnt[3 + cc]])
                        ACT(ym[:, 6 + cc, a:b], lnt[3 + cc][:, 0:n], AF.Silu, [r_lnt[3 + cc], r_cs], trs(r_ym, 6 + cc, a, b),
                            bias=cvp_s[:, 2, cc:cc + 1], scale=cvp_s[:, 1, cc:cc + 1])
                P.barrier()

                maybe_stop(10 * l + 2)
                o_uf = 4096
                uF = arena(o_uf, 2 * NT).rearrange("p (c t) -> p c t", t=NT)
                r_uf = [R(), R()]
                o_dp = o_uf + 2 * NT
                dpc = [arena(o_dp + i * 4096, 4096).rearrange("p (s k t) -> p s k t", s=2, k=4) for i in range(3)]
                r_dpc = [R() for _ in range(3)]
                o_dc = o_dp + 3 * 4096
                dcc = arena(o_dc, 1024).rearrange("p (s k t) -> p s k t", s=2, k=2)
                r_dcc = R()
                AB = ym_flat[:, 0:NJ * 512].rearrange("p (j c m) -> p j c m", c=2, m=256)
                r_ab = [R() for _ in range(NJ)]

                for cc in range(2):
                    iw = load_w(winc[l, 8 + cc])
                    for (a, b, w) in TL:
                        n = b - a
                        pi = next_ps()
                        for kc in range(KC):
                            MM(psb[pi][:, 0:n], wbuf[iw][:, kc, :], hx[:, kc, a:b], kc == 0, kc == KC - 1,
                               [r_wbuf[iw]] + trs(r_hx, kc, a, b), [r_ps[pi]])
                        ACT(uF[:, cc, a:b], psb[pi][:, 0:n], AF.Copy, [r_ps[pi]], [r_uf[cc]])
                for j in range(j0, NJ):
                    pi = next_ps()
                    for cc in range(2):
                        MM(psb[pi][:, cc * 256:(cc + 1) * 256], uF[:, cc, j * 128:(j + 1) * 128], cs64[:], True, True,
                           [r_uf[cc], r_const], [r_ps[pi]])
                    CP("dve", AB[:, j].rearrange("p c m -> p (c m)"), psb[pi][:, :], [r_ps[pi]], [r_ab[j]])
                npc = 0
                for tq in range(4):
                    pp = [next_ps(), next_ps()]
                    for kg in range(4):
                        i = npc % 3
                        npc += 1
                        DMA("sp", dpc[i].rearrange("p s k t -> p (s k t)"),
                            dftx_d[tq, kg].rearrange("p s k t -> p (s k t)"), [], [r_dpc[i]])
                        for ki in range(4):
                            j = 2 + kg * 4 + ki
                            for s in range(2):
                                first = (kg == 0 and ki == 0 and s == 0)
                                last = (kg == 3 and ki == 3 and s == 1)
                                for cc in range(2):
                                    MM(psb[pp[cc]][:, :], AB[:, j, cc, s * 128:(s + 1) * 128], dpc[i][:, s, ki, :], first, last,
                                       [r_ab[j], r_dpc[i]], [r_ps[pp[cc]]])
                    for cc in range(2):
                        a = CTX + tq * 512
                        ACT(ym[:, 4 + cc, a:a + 512], psb[pp[cc]][:, :], AF.Copy, [r_ps[pp[cc]]], trs(r_ym, 4 + cc, a, a + 512))
                if need_ctx:
                    DMA("sp", dcc.rearrange("p s k t -> p (s k t)"), dftc_d.rearrange("p s k t -> p (s k t)"), [], [r_dcc])
                    pp = [next_ps(), next_ps()]
                    for ki in range(2):
                        for s in range(2):
                            for cc in range(2):
                                MM(psb[pp[cc]][:, 0:256], AB[:, ki, cc, s * 128:(s + 1) * 128], dcc[:, s, ki, :],
                                   ki == 0 and s == 0, ki == 1 and s == 1, [r_ab[ki], r_dcc], [r_ps[pp[cc]]])
                    for cc in range(2):
                        ACT(ym[:, 4 + cc, 0:CTX], psb[pp[cc]][:, 0:256], AF.Copy, [r_ps[pp[cc]]], trs(r_ym, 4 + cc, 0, CTX))

                P.barrier()
                maybe_stop(10 * l + 3)
                o_s = 0
                dgq = [arena(o_s + i * 256, 128, F32) for i in range(2)]
                qs = [arena(o_s + 512 + i * 128, 128) for i in range(2)]
                sTt = [arena(o_s + 768 + i * 128, 128) for i in range(2)]
                kts = [arena(o_s + 1024 + i * 128, 128) for i in range(2)]
                Et = [arena(o_s + 1280 + i * 128, 128) for i in range(2)]
                Cst = [arena(o_s + 1536 + i * 260, 130, F32) for i in range(2)]
                Cbf = [arena(o_s + 2056 + i * 130, 130) for i in range(2)]
                rden = [arena(o_s + 2316 + i * 4, 2, F32) for i in range(2)]
                st1 = arena(o_s + 2324, 2 * NJ, F32)
                st2 = arena(o_s + 2396, 2 * NJ, F32)
                r_dgq, r_qs, r_sT, r_kts = [R(), R()], [R(), R()], [R(), R()], [R(), R()]
                r_dgb, r_Et = [R(), R()], [R(), R()]
                r_C, r_Cb, r_rden = [R(), R()], [R(), R()], [R(), R()]
                r_st = R()
                wqk_raw = arena(2468, 1024).rearrange("p (k c) -> p k c", c=128)
                r_wqk = R()
                dg3 = arena(3492, 384).rearrange("p (t c) -> p t c", c=128)
                wtapc = arena(3876, 4, F32)[:, 0:3]
                zraw = arena(3884, 2308)
                r_w3, r_wtap, r_zraw = R(), R(), R()
                o_g = 7332
                Gtok = arena(o_g, NJ * 16, F32).rearrange("p (j g) -> p j g", g=16)
                dgb = [arena(o_g + i * 256, 128, F32) for i in range(2)]
                LF = arena(o_g + 576, NJ * 8, F32).rearrange("p (j g) -> p j g", g=8)
                CM = arena(o_g + 864, NJ * 8, F32).rearrange("p (j g) -> p j g", g=8)
                Bc = arena(o_g + 1152, NJ * 8, F32).rearrange("p (j g) -> p j g", g=8)
                EB = arena(o_g + 1440, NJ * 8, F32).rearrange("p (j g) -> p j g", g=8)
                EBL = arena(o_g + 1728, NJ * 8, F32).rearrange("p (j g) -> p j g", g=8)
                ECL = arena(o_g + 2016, NJ * 8, F32).rearrange("p (j g) -> p j g", g=8)
                bg_s = arena(o_g + 2304, 16, F32)
                wg_s = arena(o_g + 2336, KC * 16).rearrange("p (k g) -> p k g", g=16)
                gmn_s = arena(o_g + 2464, 512, F32)
                r_gate = R()
                o_h = o_g + 3488
                qT = arena(o_h, NT)
                kT = arena(o_h + NT, NT)
                vaug = arena(o_h + 2 * NT, NJ * 130).rearrange("p (j e) -> p j e", e=130)
                sgo = arena(o_h + 2 * NT + 2340, NT).rearrange("p (j e) -> p j e", e=128)
                hsum = arena(o_h + 3 * NT + 2340, NT, F32).rearrange("p (j e) -> p j e", e=128)
                assert o_h + 5 * NT + 2340 <= SCR_ARENA, (o_h + 5 * NT + 2340)
                r_q, r_k, r_v, r_sg, r_hs = R(), R(), R(), R(), R()
                wvo_s = ym_flat[:, 3 * NT:3 * NT + KC * 256].rearrange("p (k c) -> p k c", c=256)
                r_wvo = R()

                DMA("pool", wg_s, wgate[l], [], [r_gate])
                DMA("sp", bg_s, bgate[l].partition_broadcast(128), [], [r_gate])
                DMA("sp", gmn_s, gmn[l].partition_broadcast(128), [], [r_gate])
                for jg in range(0, NJ, 6):
                    pi = next_ps()
                    for jj in range(6):
                        j = jg + jj
                        for kc in range(KC):
                            MM(psb[pi][:, jj * 16:(jj + 1) * 16], hx[:, kc, j * 128:(j + 1) * 128], wg_s[:, kc, :], kc == 0, kc == KC - 1,
                               [r_gate] + trs(r_hx, kc, j * 128, (j + 1) * 128), [r_ps[pi]])
                    TT("dve", Gtok[:, jg:jg + 6, :], psb[pi][:, 0:96].rearrange("p (j g) -> p j g", g=16),
                       bg_s.unsqueeze(1).to_broadcast([128, 6, 16]), ALU.add, [r_ps[pi], r_gate], [r_gate])
                CP("dve", CM[:, :, 0:4], Gtok[:, :, 0:4], [r_gate], [r_gate])
                CP("dve", CM[:, :, 4:8], Gtok[:, :, 8:12], [r_gate], [r_gate])
                ACT(LF[:, :, 0:4], Gtok[:, :, 4:8], AF.Abs, [r_gate], [r_gate])
                ACT(LF[:, :, 4:8], Gtok[:, :, 12:16], AF.Abs, [r_gate], [r_gate])
                ACT(LF[:], LF[:], AF.Exp, [r_gate], [r_gate], scale=-1.0)
                ACT(LF[:], LF[:], AF.Ln, [r_gate, r_const], [r_gate], bias=cst[:, 2:3], scale=1.0)
                TS("dve", EB[:, :, 0:4], Gtok[:, :, 4:8], 0.0, None, ALU.min, None, [r_gate], [r_gate])
                TS("dve", EB[:, :, 4:8], Gtok[:, :, 12:16], 0.0, None, ALU.min, None, [r_gate], [r_gate])
                TT("dve", LF[:], EB[:], LF[:], ALU.subtract, [r_gate], [r_gate])
                pi = next_ps()
                pbv = psb[pi][:, 0:NJ * 8].rearrange("p (j g) -> p j g", g=8)
                for j in range(NJ):
                    MM(pbv[:, j, 0:4], umask_f[:], LF[:, j, 0:4], True, True, [r_const, r_gate], [r_ps[pi]])
                    MM(pbv[:, j, 4:8], lmask_f[:], LF[:, j, 4:8], True, True, [r_const, r_gate], [r_ps[pi]])
                CP("dve", Bc[:], pbv, [r_ps[pi]], [r_gate])
                pi = next_ps()
                ptv = psb[pi][:, 0:NJ * 8].rearrange("p (j g) -> p j g", g=8)
                MM(psb[pi][:, 0:NJ * 8], ones_f[:], LF[:].rearrange("p j g -> p (j g)"), True, True, [r_const, r_gate], [r_ps[pi]])
                ACT(EBL[:], ptv, AF.Exp, [r_ps[pi]], [r_gate])
                ACT(EB[:], Bc[:], AF.Exp, [r_gate], [r_gate])
                TT("dve", CM[:], CM[:], Bc[:], ALU.subtract, [r_gate], [r_gate])
                TT("dve", ECL[:], CM[:], ptv, ALU.add, [r_gate, r_ps[pi]], [r_gate])
                ACT(ECL[:], ECL[:], AF.Exp, [r_gate], [r_gate])
                P.barrier()

                MS("dve", vaug[:, :, 128:129], 1.0, [r_v])
                MS("dve", zraw, 0.0, [r_zraw])

                for h in range(NH):
                    for qk in range(2):
                        dst, r_dst = (qT, r_q) if qk == 0 else (kT, r_k)
                        ci = qk * 4 + h
                        DMA("pool", wqk_raw, winc[l, ci], [], [r_wqk])
                        DMA("sp", wtapc, wqkp[l, ci], [], [r_wtap])
                        if qk == 1:
                            TS("dve", wtapc, wtapc, HD ** -0.5, None, ALU.mult, None, [r_wtap], [r_wtap])
                        for t in range(3):
                            TS("dve", dg3[:, t, :], ident_f[:], wtapc[:, t:t + 1], None, ALU.mult, None,
                               [r_const, r_wtap], [r_w3])

                        def zpos(a_):
                            return 1 + a_ if a_ < CTX else 3 + a_
                        for (a, b, w) in TL_all:
                            n = b - a
                            pi = next_ps()
                            for kc in range(KC):
                                MM(psb[pi][:, 0:n], wqk_raw[:, kc, :], hx[:, kc, a:b], kc == 0, kc == KC - 1,
                                   [r_wqk] + trs(r_hx, kc, a, b), [r_ps[pi]])
                            ACT(zraw[:, zpos(a):zpos(a) + n], psb[pi][:, 0:n], AF.Copy, [r_ps[pi]], [r_zraw])
                        for (a, b, w) in TL_all:
                            n = b - a
                            pi = next_ps()
                            for t in range(3):
                                MM(psb[pi][:, 0:n], dg3[:, t, :], zraw[:, zpos(a) + t - 1:zpos(a) + t - 1 + n], t == 0, t == 2,
                                   [r_w3, r_zraw], [r_ps[pi]])
                            ACT(dst[:, a:b], psb[pi][:, 0:n], AF.Copy, [r_ps[pi]], [r_dst])
                    DMA("pool", wvo_s, wvo[l, h], [], [r_wvo])
                    for j in range(NJ):
                        pi = next_ps()
                        for kc in range(KC):
                            MM(psb[pi][:, 0:256], hx[:, kc, j * 128:(j + 1) * 128], wvo_s[:, kc, :], kc == 0, kc == KC - 1,
                               [r_wvo] + trs(r_hx, kc, j * 128, (j + 1) * 128), [r_ps[pi]])
                        CP("dve", vaug[:, j, 0:128], psb[pi][:, 0:128], [r_ps[pi]], [r_v])
                        ACT(sgo[:, j, :], psb[pi][:, 128:256], AF.Sigmoid, [r_ps[pi]], [r_sg])
                        TT("dve", sgo[:, j, :], sgo[:, j, :], gmn_s[:, h * 128:(h + 1) * 128], ALU.mult, [r_sg, r_gate], [r_sg])
                    MS("dve", hsum, 0.0, [r_hs])
                    for d_ in range(2):
                        MS("dve", Cst[d_], 0.0, [r_C[d_]])
                        MS("dve", Cbf[d_], 0.0, [r_Cb[d_]])
                    order = [list(range(NJ)), [1, 0] + list(range(NJ - 1, 1, -1))]
                    t0f = tmpf[0]
                    t1b = tmpf[1][:, :].bitcast(BF16)
                    dgb2 = [dgb, [dgq[0], dgq[1]]]
                    sT2 = [sTt, [t1b[:, 256:384], t1b[:, 384:512]]]
                    kts2 = [kts, [t1b[:, 512:640], t1b[:, 640:768]]]
                    Et2 = [Et, [t1b[:, 768:896], t1b[:, 896:1024]]]
                    ndi = [t0f[:, 0:130], t0f[:, 130:260]]
                    ndi_den = t0f[:, 0:260].rearrange("p (d e) -> p d e", e=130)[:, :, 128:129]
                    rden_j = t0f[:, 392:394].unsqueeze(2)
                    r_rdj = R()
                    r_ndi = [R(), R()]
                    t0b = t0f[:, 260:390].bitcast(BF16)
                    Cbf2 = [Cbf, [t0b[:, 0:130], t0b[:, 130:260]]]
                    r_Cb2 = [r_Cb, [R(), R()]]
                    rr = lambda: [[R(), R()], [R(), R()]]
                    r_dgb2, r_sT2, r_kts2, r_Et2 = rr(), rr(), rr(), rr()

                    def info(step, d_):
                        j = order[d_][step]
                        return j, step % 2, d_ * 4 + h, (need_ctx or j >= 2), step == NJ - 1

                    ps_hold = {}

                    def a1(step, d_):
                        j, bs, col, need_out, is_last = info(step, d_)
                        if need_out:
                            TS("dve", dgb2[bs][d_], ident_f[:], Bc[:, j, col:col + 1], None, ALU.mult, None,
                               [r_const, r_gate], [r_dgb2[bs][d_]])
                        yield

                    def a2a(step, d_):
                        j, bs, col, need_out, is_last = info(step, d_)
                        c0, c1 = j * 128, (j + 1) * 128
                        if need_out:
                            pd_ = d_
                            MM(psb[pd_][:, 0:128], ones_f[:], dgb2[bs][d_], True, False, [r_const, r_dgb2[bs][d_]], [r_ps[pd_]])
                            MM(psb[pd_][:, 0:128], ident_b[:], (negm_f if d_ == 0 else negm_b)[:], False, True,
                               [r_const], [r_ps[pd_]])
                            yield
                            ACT(Et2[bs][d_], psb[pd_][:, 0:128], AF.Exp, [r_ps[pd_], r_gate], [r_Et2[bs][d_]],
                                bias=CM[:, j, col:col + 1], scale=1.0)
                            yield
                            p_s = 2 + d_
                            MM(psb[p_s][:, 0:128], kT[:, c0:c1], qT[:, c0:c1], True, True, [r_k, r_q], [r_ps[p_s]])
                            yield
                        if not is_last:
                            p_t = next_ps()
                            ptb = psb[p_t][:, 0:64].bitcast(BF16)
                            TR(ptb, kT[:, c0:c1], ident_b[:], [r_k, r_const], [r_ps[p_t]])
                            yield
                            ACT(kts2[bs][d_], ptb, AF.Copy, [r_ps[p_t], r_gate], [r_kts2[bs][d_]], scale=ECL[:, j, col:col + 1])
                            yield

                    def a2b(step, d_):
                        j, bs, col, need_out, is_last = info(step, d_)
                        if need_out:
                            p_s = 2 + d_
                            TT("dve", sT2[bs][d_], psb[p_s][:, 0:128], Et2[bs][d_], ALU.mult,
                               [r_ps[p_s], r_Et2[bs][d_]], [r_sT2[bs][d_]])
                        yield

                    def stage_b(step, d_):
                        j, bs, col, need_out, is_last = info(step, d_)
                        cur, nxt = Cbf2[step % 2][d_], Cbf2[(step + 1) % 2][d_]
                        r_cur, r_nxt = r_Cb2[step % 2][d_], r_Cb2[(step + 1) % 2][d_]
                        if not is_last:
                            p_u = next_ps()
                            MM(psb[p_u][:, 0:129], kts2[bs][d_], vaug[:, j, 0:129], True, True, [r_kts2[bs][d_], r_v], [r_ps[p_u]])
                            yield
                            STT("dve", Cst[d_][:, 0:129], Cst[d_][:, 0:129], EBL[:, j, col:col + 1], psb[p_u][:, 0:129],
                                ALU.mult, ALU.add, [r_C[d_], r_gate, r_ps[p_u]], [r_C[d_]])
                            yield
                            ACT(nxt[:, 0:129], Cst[d_][:, 0:129], AF.Copy, [r_C[d_]], [r_nxt])
                            yield
                        if need_out:
                            c0, c1 = j * 128, (j + 1) * 128
                            p_i = next_ps()
                            MM(psb[p_i][:, 0:129], qT[:, c0:c1], cur[:, 0:129], True, True, [r_q, r_cur], [r_ps[p_i]])
                            p_n = next_ps()
                            MM(psb[p_n][:, 0:129], sT2[bs][d_], vaug[:, j, 0:129], True, True, [r_sT2[bs][d_], r_v], [r_ps[p_n]])
                            yield
                            ACT(ndi[d_][:, 0:129], psb[p_i][:, 0:129], AF.Copy, [r_ps[p_i], r_gate], [r_ndi[d_]], scale=EB[:, j, col:col + 1])
                            yield
                            TT("dve", ndi[d_][:, 0:129], ndi[d_][:, 0:129], psb[p_n][:, 0:129], ALU.add, [r_ndi[d_], r_ps[p_n]], [r_ndi[d_]])
                            yield

                    def den_ops(step):
                        j, bs, col, need_out, is_last = info(step, 0)
                        if need_out:
                            STT("dve", rden_j, ndi_den, -1.0, ndi_den, ALU.mult, ALU.max, [r_ndi[0], r_ndi[1]], [r_rdj])
                            TS("dve", rden_j, rden_j, 1.0, None, ALU.max, None, [r_rdj], [r_rdj])
                            RECIP(rden_j, rden_j, [r_rdj], [r_rdj])

                    def stage_b2(step, d_):
                        j, bs, col, need_out, is_last = info(step, d_)
                        if need_out:
                            STT("dve", hsum[:, j, :], ndi[d_][:, 0:128], t0f[:, 392 + d_:393 + d_], hsum[:, j, :], ALU.mult, ALU.add,
                                [r_ndi[d_], r_rdj, r_hs], [r_hs])
                        yield

                    def interleave(*gens):
                        gens = list(gens)
                        while gens:
                            for g in list(gens):
                                try:
                                    next(g)
                                except StopIteration:
                                    gens.remove(g)

                    for bnk in range(4):
                        ps_reserved.add(bnk)
                    interleave(a1(0, 0), a1(0, 1))
                    interleave(a2a(0, 0), a2a(0, 1))
                    interleave(a2b(0, 0), a2b(0, 1))
                    interleave(a1(1, 0), a1(1, 1))
                    for step in range(NJ):
                        if step + 2 < NJ:
                            interleave(a1(step + 2, 0), a1(step + 2, 1))
                        if step + 1 < NJ:
                            interleave(a2a(step + 1, 0), a2a(step + 1, 1))
                        interleave(stage_b(step, 0), stage_b(step, 1))
                        den_ops(step)
                        interleave(stage_b2(step, 0), stage_b2(step, 1))
                        if step + 1 < NJ:
                            interleave(a2b(step + 1, 0), a2b(step + 1, 1))
                    for bnk in range(4):
                        ps_reserved.discard(bnk)
                    nj = NJ - j0
                    hv = hsum[:, j0:NJ, :]
                    sqv = ym[:, h, j0 * 128:NT].rearrange("p (j e) -> p j e", e=128)
                    ymh_res = trs(r_ym, h, j0 * 128, NT) + ([r_wvo] if h == 3 else [])
                    P.op("dve", (lambda hv=hv, nj=nj: lambda e: e.tensor_reduce(out=st1[:, 0:nj], in_=hv, axis=AX.X, op=ALU.add))(),
                         [r_hs], [r_st])
                    TT("dve", sqv, hv, hv, ALU.mult, [r_hs], ymh_res)
                    P.op("dve", (lambda sqv=sqv, nj=nj: lambda e: e.tensor_reduce(out=st1[:, NJ:NJ + nj], in_=sqv, axis=AX.X, op=ALU.add))(),
                         ymh_res, [r_st])
                    mean_ = st2[:, 0:nj]
                    var_ = st2[:, NJ:NJ + nj]
                    TS("dve", mean_, st1[:, 0:nj], 1.0 / HD, None, ALU.mult, None, [r_st], [r_st])
                    TT("dve", var_, mean_, mean_, ALU.mult, [r_st], [r_st])
                    STT("dve", var_, st1[:, NJ:NJ + nj], 1.0 / HD, var_, ALU.mult, ALU.subtract, [r_st], [r_st])
                    ACT(var_, var_, AF.Sqrt, [r_st, r_const], [r_st], bias=cst[:, 1:2], scale=1.0)
                    RECIP(var_, var_, [r_st], [r_st])
                    TT("dve", hv, hv, mean_.unsqueeze(2).to_broadcast([128, nj, 128]), ALU.subtract, [r_hs, r_st], [r_hs])
                    TT("dve", hv, hv, var_.unsqueeze(2).to_broadcast([128, nj, 128]), ALU.mult, [r_hs, r_st], [r_hs])
                    TT("dve", sgo[:, j0:NJ, :], hv, sgo[:, j0:NJ, :], ALU.mult, [r_hs, r_sg], [r_sg])
                    for jg in range(j0, NJ, 4):
                        p_t = next_ps()
                        ptb = psb[p_t][:, 0:256].bitcast(BF16)
                        jn = min(4, NJ - jg)
                        for jj in range(jn):
                            TR(ptb[:, jj * 128:(jj + 1) * 128], sgo[:, jg + jj, :], ident_b[:], [r_sg, r_const], [r_ps[p_t]])
                        ACT(ym[:, h, jg * 128:(jg + jn) * 128], ptb[:, 0:jn * 128], AF.Copy, [r_ps[p_t]],
                            trs(r_ym, h, jg * 128, (jg + jn) * 128) + ([r_wvo] if h == 3 else []))

                if l == 0:
                    dump("d_ym", lambda kc: ym[:, kc, :], lambda kc: trs(r_ym, kc, 0, NT))

                P.barrier()
                maybe_stop(10 * l + 4)
                o_wo = 4096
                wo_s = arena(o_wo, KC * KC * 128).rearrange("p (o k c) -> p o k c", o=KC, k=KC)
                r_wo = R()
                obuf = arena(o_wo + 8192, KC * 512, F32).rearrange("p (o t) -> p o t", t=512)
                r_ob = [R() for _ in range(KC)]
                DMA("pool", wo_s.rearrange("p o k c -> p (o k c)"), woutc[l].rearrange("p o k c -> p (o k c)"), [],
                    [r_wo])
                for (a, b, w) in TL:
                    n = b - a
                    for oc in range(KC):
                        pi = next_ps()
                        for kc in range(KC):
                            MM(psb[pi][:, 0:n], wo_s[:, oc, kc, :], ym[:, kc, a:b], kc == 0, kc == KC - 1,
                               [r_wo] + trs(r_ym, kc, a, b), [r_ps[pi]])
                        ACT(obuf[:, oc, 0:n], psb[pi][:, 0:n], AF.Copy, [r_ps[pi]], [r_ob[oc]])
                    resid_update([(a, b, w)], lambda kc, a_, b_: obuf[:, kc, 0:b_ - a_], lambda kc, a_, b_: [r_ob[kc]], G1)
                if l == 0:
                    dump("d_x1", lambda kc: xs[:, kc, :], lambda kc: trs(r_xs, kc, 0, NT))

                P.barrier()
                maybe_stop(10 * l + 5)
                hT = scr[:, 0:24576].rearrange("p (f t) -> p f t", t=768)
                r_hT = [R() for _ in range(32)]
                ob2 = scr[:, 24576:30720].rearrange("p (o t) -> p o t", t=768)
                r_ob2 = [R() for _ in range(KC)]
                w1b = [scr[:, 30720 + i * 2048:30720 + (i + 1) * 2048].rearrange("p (g k c) -> p g k c", g=2, k=KC) for i in range(2)]
                w2b = [scr[:, 34816 + i * 4096:34816 + (i + 1) * 4096].rearrange("p (f c) -> p f c", c=128) for i in range(2)]
                r_w1b = [R(), R()]
                r_w2b = [R(), R()]
                if need_ctx:
                    supers = [[(0, 256, 1), (256, 768, 0)], [(768, 1280, 0), (1280, 1536, 0)], [(1536, 2048, 0), (2048, 2304, 0)]]
                else:
                    supers = [[(256, 768, 0), (768, 1024, 0)], [(1024, 1536, 0), (1536, 1792, 0)], [(1792, 2304, 0)]]
                n1 = 0
                n2 = 0
                prenorm(supers[0], A2, 24, l)
                for isup, sup in enumerate(supers):
                    a0 = sup[0][0]
                    for g in range(16):
                        i = n1 % 2
                        n1 += 1
                        DMA("pool", w1b[i].rearrange("p g k c -> p (g k c)"), w1c[l, g].rearrange("p g k c -> p (g k c)"), [],
                            [r_w1b[i]])
                        for f2 in range(2):
                            f = g * 2 + f2
                            for (a, b, w) in sup:
                                n = b - a
                                pi = next_ps()
                                for kc in range(KC):
                                    MM(psb[pi][:, 0:n], w1b[i][:, f2, kc, :], hx[:, kc, a:b], kc == 0, kc == KC - 1,
                                       [r_w1b[i]] + trs(r_hx, kc, a, b), [r_ps[pi]])
                                ti = next_tf()
                                ACT(tmpf[ti][:, 0:n], psb[pi][:, 0:n], AF.Relu, [r_ps[pi]], [r_tmpf[ti]])
                                TT("dve", hT[:, f, a - a0:b - a0], tmpf[ti][:, 0:n], tmpf[ti][:, 0:n], ALU.mult, [r_tmpf[ti]], [r_hT[f]])
                    if isup + 1 < len(supers):
                        prenorm(supers[isup + 1], A2, 24, l)
                    ssb = []
                    for _ in sup:
                        pss = next_ps()
                        ps_reserved.add(pss)
                        ssb.append(pss)
                    for oc in range(KC):
                        i = n2 % 2
                        n2 += 1
                        DMA("pool", w2b[i].rearrange("p f c -> p (f c)"), w2c[l, oc].rearrange("p f c -> p (f c)"), [],
                            [r_w2b[i]])
                        for si_, (a, b, w) in enumerate(sup):
                            n = b - a
                            pi = next_ps()
                            for f in range(32):
                                MM(psb[pi][:, 0:n], w2b[i][:, f, :], hT[:, f, a - a0:b - a0], f == 0, f == 31,
                                   [r_w2b[i], r_hT[f]], [r_ps[pi]])
                            ACT(ob2[:, oc, a - a0:b - a0], psb[pi][:, 0:n], AF.Copy, [r_ps[pi]], [r_ob2[oc]])
                            sq_i = next_sq()
                            ACT(sqb[sq_i][:, 0:n], psb[pi][:, 0:n], AF.Square, [r_ps[pi]], [r_sqb[sq_i]])
                            MM(psb[ssb[si_]][:, 0:n], ones_b[:], sqb[sq_i][:, 0:n], oc == 0, oc == KC - 1,
                               [r_sqb[sq_i], r_const], [r_ps[ssb[si_]]])
                    for si_, (a, b, w) in enumerate(sup):
                        n = b - a
                        ACT(rs[:, 0:n], psb[ssb[si_]][:, 0:n], AF.Sqrt, [r_ps[ssb[si_]], r_const], [r_rs], bias=cst[:, 0:1], scale=1.0)
                        RECIP(rs[:, 0:n], rs[:, 0:n], [r_rs], [r_rs])
                        for kc in range(KC):
                            ti = next_tf()
                            TT("dve", tmpf[ti][:, 0:n], ob2[:, kc, a - a0:b - a0], rs[:, 0:n], ALU.mult,
                               [r_ob2[kc], r_rs], [r_tmpf[ti]])
                            STT("dve", xs[:, kc, a:b], tmpf[ti][:, 0:n], G2[:, kc, w:w + 1], xs[:, kc, a:b], ALU.mult, ALU.add,
                                [r_tmpf[ti], r_lay] + trs(r_xs, kc, a, b), trs(r_xs, kc, a, b))
                    for pss in ssb:
                        ps_reserved.discard(pss)
                if l == 0:
                    dump("d_x2", lambda kc: xs[:, kc, :], lambda kc: trs(r_xs, kc, 0, NT))
                P.barrier()

        except _Stop:
            pass
        for kc in range(KC):
            DMA("sp", outT[kc], xs[:, kc, CTX:NT], trs(r_xs, kc, CTX, NT), [])
        P.emit()
    return nc


_CACHE = {}


def _consts():
    if "c" in _CACHE:
        return _CACHE["c"]
    c = {}
    c["ident"] = np.eye(128, dtype=np.float32)
    s = np.arange(128)
    c["umask"] = (s[:, None] <= s[None, :]).astype(np.float32)
    c["lmask"] = (s[:, None] >= s[None, :]).astype(np.float32)
    k = np.arange(64)
    ang = 2.0 * np.pi * np.outer(k, k) / 64.0
    cs = np.zeros((128, 256), np.float64)
    for hh in range(2):
        cs[hh * 64:(hh + 1) * 64, hh * 64:(hh + 1) * 64] = np.cos(ang) / 8.0
        cs[hh * 64:(hh + 1) * 64, 128 + hh * 64:128 + (hh + 1) * 64] = np.sin(ang) / 8.0
    c["cs64"] = cs.astype(np.float32)
    t = np.arange(SEQ, dtype=np.int64)
    ph = (np.outer(t, t) % SEQ).astype(np.float64) * (2.0 * np.pi / SEQ)
    dc = (np.cos(ph) / math.sqrt(SEQ)).astype(np.float32)
    ds = (-np.sin(ph) / math.sqrt(SEQ)).astype(np.float32)
    both = np.stack([dc, ds], 0)
    both = both.reshape(2, 4, 4, 128, 4, 512)
    c["dftx"] = np.ascontiguousarray(both.transpose(4, 1, 3, 0, 2, 5)).astype(ml_dtypes.bfloat16)
    t = np.arange(CTX, dtype=np.int64)
    ph = (np.outer(t, t) % CTX).astype(np.float64) * (2.0 * np.pi / CTX)
    both = np.stack([np.cos(ph), -np.sin(ph)], 0) / math.sqrt(CTX)
    both = both.reshape(2, 2, 128, CTX)
    c["dftc"] = np.ascontiguousarray(both.transpose(2, 0, 1, 3)).astype(ml_dtypes.bfloat16)
    rows = SEQ // 64
    quarter = D // 4
    freq = np.exp(-math.log(10000.0) * np.arange(quarter, dtype=np.float32) / quarter).astype(np.float32)
    r = np.broadcast_to(np.arange(rows, dtype=np.float32)[:, None], (rows, 64)).reshape(-1)
    col = np.broadcast_to(np.arange(64, dtype=np.float32)[None, :], (rows, 64)).reshape(-1)
    ar = r[:, None] * freq
    ac = col[:, None] * freq
    pos = np.concatenate([np.sin(ar), np.cos(ar), np.sin(ac), np.cos(ac)], axis=-1).astype(np.float32)
    c["posT"] = np.ascontiguousarray(pos.T).reshape(KC, 128, SEQ)
    _CACHE["c"] = c
    return c


def _chunk_w(w, cols):
    return np.ascontiguousarray(w[:, cols].reshape(KC, 128, -1).transpose(1, 0, 2))


def _prep_shared(inp):
    f = np.float32
    L = DEPTH
    w_in = np.asarray(inp["w_in"], f)
    sh = {}
    wada = np.asarray(inp["w_ada"], f)
    sh["wada"] = np.ascontiguousarray(wada.reshape(L, KC, 128, 12, 512).transpose(0, 3, 2, 1, 4))
    sh["bada"] = np.ascontiguousarray(np.asarray(inp["b_ada"], f).reshape(L, 48, 128).transpose(0, 2, 1))
    gs = np.stack([np.asarray(inp[k], f) for k in ("g_pre_mix", "g_post_mix", "g_pre_mlp", "g_post_mlp")], 1)
    sh["gvec"] = np.ascontiguousarray(gs.reshape(L, 4, KC, 128).transpose(0, 3, 1, 2))
    offs = [Q_OFF + 128 * i for i in range(4)] + [K_OFF + 128 * i for i in range(4)] + \
           [F_OFF, F_OFF + 128, CA_OFF, CA_OFF + 128, CG_OFF, CG_OFF + 128]
    sh["winc"] = np.stack([np.stack([_chunk_w(w_in[l], np.arange(o, o + 128)) for o in offs]) for l in range(L)])
    sh["wvo"] = np.stack([np.stack([_chunk_w(w_in[l], np.concatenate([np.arange(V_OFF + 128 * h, V_OFF + 128 * h + 128),
                                                                      np.arange(O_OFF + 128 * h, O_OFF + 128 * h + 128)]))
                                    for h in range(NH)]) for l in range(L)])
    sh["wgate"] = np.stack([_chunk_w(w_in[l], np.arange(G_OFF, G_OFF + 16)) for l in range(L)])
    sh["bgate"] = np.ascontiguousarray(np.asarray(inp["b_gate"], f).reshape(L, 16))
    wqk = np.asarray(inp["w_qk_conv"], f)
    sh["wqkp"] = np.ascontiguousarray(wqk.reshape(L, 3, 8, 128).transpose(0, 2, 3, 1))
    sh["gmn"] = np.ascontiguousarray(np.asarray(inp["g_mlstm_norm"], f))
    wdw = np.asarray(inp["w_dw"], f)
    sh["wdw"] = np.ascontiguousarray(wdw.reshape(L, CONV_K, 2, 128).transpose(0, 3, 2, 1))
    cv = np.stack([np.asarray(inp[k], f) for k in ("b_dw", "g_conv_ln", "b_conv_ln")], 1)
    sh["cvp"] = np.ascontiguousarray(cv.reshape(L, 3, 2, 128).transpose(0, 3, 1, 2))
    wout = np.asarray(inp["w_out"], f)
    sh["woutc"] = np.ascontiguousarray(wout.reshape(L, KC, 128, KC, 128).transpose(0, 2, 3, 1, 4))
    w1 = np.asarray(inp["w_mlp1"], f)
    sh["w1c"] = np.ascontiguousarray(w1.reshape(L, KC, 128, 16, 2, 128).transpose(0, 3, 2, 4, 1, 5))
    w2 = np.asarray(inp["w_mlp2"], f)
    sh["w2c"] = np.ascontiguousarray(w2.reshape(L, 32, 128, KC, 128).transpose(0, 3, 2, 1, 4))
    c = _consts()
    for k in ("ident", "umask", "lmask", "cs64", "dftx", "dftc", "posT"):
        sh[k] = c[k]
    return sh


def make_in_maps(inp, cores):
    sh = _prep_shared(inp)
    x = np.asarray(inp["x"], np.float32)
    ctx = np.asarray(inp["ctx"], np.float32)
    c = np.asarray(inp["c"], np.float32)
    c_ctx = np.asarray(inp["c_ctx"], np.float32)
    maps = []
    for b in cores:
        m = dict(sh)
        m["xT"] = np.ascontiguousarray(x[b].T).reshape(KC, 128, SEQ)
        m["ctxT"] = np.ascontiguousarray(ctx[b].T).reshape(KC, 128, CTX)
        cv = np.stack([c[b], c_ctx], -1)
        m["cvec"] = np.ascontiguousarray(cv.reshape(KC, 128, 2).transpose(1, 0, 2))
        maps.append(m)
    return maps


def kernel(**inputs):
    if "nc" not in _CACHE:
        _CACHE["nc"] = build_program(dbg=False)
    nc = _CACHE["nc"]
    in_maps = make_in_maps(inputs, list(range(NB)))
    res = run_bass_kernel_spmd(nc, in_maps, core_ids=list(range(NB)))
    out = np.empty((NB, SEQ, D), np.float32)
    for b in range(NB):
        oT = np.asarray(res.results[b]["outT"], np.float32).reshape(D, SEQ)
        out[b] = oT.T
    return out
```

```python
import math
from contextlib import ExitStack

import numpy as np
import ml_dtypes

import concourse.bass as bass
import concourse.mybir as mybir
from concourse.bass_utils import run_bass_kernel_spmd

F32 = mybir.dt.float32
BF16 = mybir.dt.bfloat16
AF = mybir.ActivationFunctionType
ALU = mybir.AluOpType
AX = mybir.AxisListType

ENGS = ("pe", "act", "dve", "pool", "sp")
SEM_LIM = 2000
N_DMA_SEMS = 16


class Res:
    __slots__ = ("name", "w", "r", "excl")

    def __init__(self, name, excl=False):
        self.name = name
        self.w = None
        self.r = []
        self.excl = excl


class Op:
    __slots__ = ("eng", "idx", "fn", "deps", "dma", "signal", "semref", "dma_slot", "dma_val")

    def __init__(self, eng, idx, fn, dma):
        self.eng = eng
        self.idx = idx
        self.fn = fn
        self.deps = []
        self.dma = dma
        self.signal = False
        self.semref = None
        self.dma_slot = None
        self.dma_val = None


class Prog:
    def __init__(self, nc, same_engine_sync=True):
        self.nc = nc
        self.ops = {e: [] for e in ENGS}
        self.n_dma_q = {}
        self.same_engine_sync = same_engine_sync

    def res(self, name="", excl=False):
        return Res(name, excl)

    def op(self, eng, fn, reads=(), writes=(), dma=False):
        o = Op(eng, len(self.ops[eng]), fn, dma)
        if dma:
            half = N_DMA_SEMS // 2
            k = self.n_dma_q.get(eng, 0)
            self.n_dma_q[eng] = k + 1
            o.dma_slot = (k % half) + (half if eng == "pool" else 0)
            o.dma_val = 16 * (k // half + 1)
        deps = []
        for r in reads:
            if r.w is not None:
                deps.append(r.w)
            if r.excl:
                deps.extend(x for x in r.r if x.eng != eng)
        for r in writes:
            if r.w is not None:
                deps.append(r.w)
            deps.extend(r.r)
        for r in reads:
            r.r.append(o)
        for r in writes:
            r.w = o
            r.r = []
        seen = set()
        for d in deps:
            if d is o or id(d) in seen:
                continue
            seen.add(id(d))
            if (not d.dma) and (not dma) and d.eng == eng:
                if eng == "pe" or not self.same_engine_sync:
                    continue
            o.deps.append(d)
        self.ops[eng].append(o)
        return o

    def barrier(self):
        targets = []
        for e in ENGS:
            cs = [o for o in self.ops[e] if not o.dma and o.fn is not None]
            if cs:
                targets.append(cs[-1])
        by_slot = {}
        for e in ENGS:
            for o in self.ops[e]:
                if o.dma and (o.dma_slot not in by_slot or o.dma_val > by_slot[o.dma_slot].dma_val):
                    by_slot[o.dma_slot] = o
        targets += list(by_slot.values())
        for e in ENGS:
            o = Op(e, len(self.ops[e]), None, False)
            o.deps = list(targets) if e != "pe" else [t for t in targets if t.dma or t.eng != e]
            self.ops[e].append(o)

    def emit(self):
        nc = self.nc
        for e in ENGS:
            for o in self.ops[e]:
                for d in o.deps:
                    d.signal = True
        with ExitStack() as st:
            dma_sems = [st.enter_context(nc.semaphore(f"dq{i}")) for i in range(N_DMA_SEMS)]
            for e in ENGS:
                n = sum(1 for o in self.ops[e] if o.signal and not o.dma)
                k = max((n + SEM_LIM - 1) // SEM_LIM, 1)
                sems = [st.enter_context(nc.semaphore(f"s_{e}{i}")) for i in range(k)]
                c = 0
                for o in self.ops[e]:
                    if o.signal and not o.dma:
                        o.semref = (sems[c // SEM_LIM], c % SEM_LIM + 1, c)
                        c += 1
            block = st.enter_context(nc.Block())

            def run(e, eng):
                waited_c = {p: -1 for p in ENGS}
                waited_d = [0] * N_DMA_SEMS
                for o in self.ops[e]:
                    for d in o.deps:
                        if d.dma:
                            if waited_d[d.dma_slot] >= d.dma_val:
                                continue
                            waited_d[d.dma_slot] = d.dma_val
                            eng.wait_ge(dma_sems[d.dma_slot], d.dma_val)
                        else:
                            sem, val, gc = d.semref
                            if waited_c[d.eng] >= gc:
                                continue
                            waited_c[d.eng] = gc
                            eng.wait_ge(sem, val)
                    if o.fn is None:
                        continue
                    ins = o.fn(eng)
                    if o.dma:
                        ins.then_inc(dma_sems[o.dma_slot], 16)
                    elif o.signal:
                        ins.then_inc(o.semref[0], 1)

            fin_dma = {}
            for e in ENGS:
                for o in self.ops[e]:
                    if o.dma:
                        fin_dma[o.dma_slot] = max(fin_dma.get(o.dma_slot, 0), o.dma_val)

            @block.tensor
            def _(eng):
                run("pe", eng)

            @block.scalar
            def _(eng):
                run("act", eng)

            @block.vector
            def _(eng):
                run("dve", eng)

            @block.gpsimd
            def _(eng):
                run("pool", eng)

            @block.sync
            def _(eng):
                run("sp", eng)
                for slot, val in sorted(fin_dma.items()):
                    eng.wait_ge(dma_sems[slot], val)


D = 1024
NB = 8
SEQ = 2048
CTX = 256
NT = CTX + SEQ
DEPTH = 2
KC = 8
NJ = NT // 128
HD = 128
NH = 4
DFF = 4096
EPS = 1e-6
CONV_K = 31
PADC = 15
Q_OFF, K_OFF, V_OFF, O_OFF, G_OFF = 0, 512, 1024, 1536, 2048
F_OFF = 2064
CA_OFF = 2320
CG_OFF = 2576
UPW = (CTX + 2 * PADC) + (SEQ + 2 * PADC)


def tiles_for(need_ctx):
    t = [(256, 768, 0), (768, 1280, 0), (1280, 1792, 0), (1792, 2304, 0)]
    if need_ctx:
        t = [(0, 256, 1)] + t
    return t


class _Stop(Exception):
    pass


def build_program(dbg=False, stop=None):
    nc = bass.Bass("TRN2", target_bir_lowering=False)
    P = Prog(nc)

    def din(name, shape, dt=F32):
        return nc.dram_tensor(name, list(shape), dt, kind="ExternalInput").ap()

    def dout(name, shape, dt=F32):
        return nc.dram_tensor(name, list(shape), dt, kind="ExternalOutput").ap()

    xT = din("xT", [KC, 128, SEQ])
    ctxT = din("ctxT", [KC, 128, CTX])
    posT = din("posT", [KC, 128, SEQ])
    cvec = din("cvec", [128, KC, 2])
    wada = din("wada", [DEPTH, 12, 128, KC, 512])
    bada = din("bada", [DEPTH, 128, 48])
    gvec = din("gvec", [DEPTH, 128, 4, KC])
    winc = din("winc", [DEPTH, 14, 128, KC, 128])
    wvo = din("wvo", [DEPTH, NH, 128, KC, 256])
    wgate = din("wgate", [DEPTH, 128, KC, 16])
    bgate = din("bgate", [DEPTH, 16])
    wqkp = din("wqkp", [DEPTH, 8, 128, 3])
    gmn = din("gmn", [DEPTH, 512])
    wdw = din("wdw", [DEPTH, 128, 2, CONV_K])
    cvp = din("cvp", [DEPTH, 128, 3, 2])
    woutc = din("woutc", [DEPTH, 128, KC, KC, 128])
    w1c = din("w1c", [DEPTH, 16, 128, 2, KC, 128])
    w2c = din("w2c", [DEPTH, KC, 128, 32, 128])
    ident_d = din("ident", [128, 128])
    umask_d = din("umask", [128, 128])
    lmask_d = din("lmask", [128, 128])
    cs64_d = din("cs64", [128, 256])
    dftx_d = din("dftx", [4, 4, 128, 2, 4, 512], BF16)
    dftc_d = din("dftc", [128, 2, 2, 256], BF16)
    outT = dout("outT", [KC, 128, SEQ])
    dbg_out = {}
    if dbg:
        for nm in ("d_hx", "d_ym", "d_x1", "d_x2"):
            dbg_out[nm] = dout(nm, [KC, 128, NT])

    with ExitStack() as st:
        def sb(name, shape, dt):
            return st.enter_context(nc.sbuf_tensor(name, list(shape), dt))

        xs = sb("xs", [128, KC, NT], F32)
        hx = sb("hx", [128, KC, NT], BF16)
        SCR_ARENA = 25088
        SCR_N = SCR_ARENA + KC * NT
        scr = sb("scr", [128, SCR_N], BF16)
        ym_flat = scr[:, SCR_ARENA:SCR_N]
        ym = ym_flat.rearrange("p (c t) -> p c t", t=NT)
        ident_f = sb("ident_f", [128, 128], F32)
        ident_b = sb("ident_b", [128, 128], BF16)
        umask_f = sb("umask_f", [128, 128], F32)
        lmask_f = sb("lmask_f", [128, 128], F32)
        negm_f = sb("negm_f", [128, 128], BF16)
        negm_b = sb("negm_b", [128, 128], BF16)
        ones_f = sb("ones_f", [128, 128], F32)
        ones_b = sb("ones_b", [128, 128], BF16)
        cs64 = sb("cs64b", [128, 256], BF16)
        cst = sb("cst", [128, 4], F32)
        cv_f = sb("cv_f", [128, KC, 2], F32)
        sc_b = sb("sc_b", [128, KC, 2], BF16)
        mod = sb("mod", [128, DEPTH, 48, 2], F32)
        bada_s = sb("bada_s", [128, DEPTH, 48], F32)
        gv = sb("gv", [128, DEPTH, 4, KC], F32)
        A1 = sb("A1", [128, KC, 2], F32)
        A2 = sb("A2", [128, KC, 2], F32)
        G1 = sb("G1", [128, KC, 2], F32)
        G2 = sb("G2", [128, KC, 2], F32)
        rs = sb("rs", [128, 512], F32)
        sqb = [sb(f"sqb{i}", [128, 512], BF16) for i in range(2)]
        tmpf = [sb(f"tmpf{i}", [128, 512], F32) for i in range(2)]
        psb = [st.enter_context(nc.psum_tensor(f"ps{i}", [128, 512], F32)) for i in range(8)]

        R = P.res
        r_xs = [[R() for _ in range(NJ)] for _ in range(KC)]
        r_hx = [[R() for _ in range(NJ)] for _ in range(KC)]
        r_ym = [[R() for _ in range(NJ)] for _ in range(KC)]
        r_ps = [P.res("ps", excl=True) for _ in range(8)]
        r_const = R()
        r_mod = R()
        r_lay = R()
        r_rs = R()
        r_sqb = [R(), R()]
        r_tmpf = [R(), R()]
        r_arena = R()

        def trs(rl, kc, a, b):
            return [rl[kc][j] for j in range(a // 128, (b + 127) // 128)]

        def trs_all(rl, a, b):
            out = []
            for kc in range(KC):
                out += trs(rl, kc, a, b)
            return out

        cnt = {"ps": 0, "sq": 0, "tf": 0}

        ps_reserved = set()

        def next_ps():
            while True:
                i = cnt["ps"] % 8
                cnt["ps"] += 1
                if i not in ps_reserved:
                    return i

        def next_sq():
            i = cnt["sq"] % 2
            cnt["sq"] += 1
            return i

        def next_tf():
            i = cnt["tf"] % 2
            cnt["tf"] += 1
            return i

        def MM(out, lhsT, rhs, start, stop, reads, writes):
            P.op("pe", lambda e: e.matmul(out, lhsT=lhsT, rhs=rhs, start=start, stop=stop), reads, writes)

        def TR(out, in_, ident, reads, writes):
            P.op("pe", lambda e: e.transpose(out, in_, ident), reads, writes)

        def ACT(out, in_, func, reads, writes, bias=None, scale=None, accum=None):
            kw = {}
            if bias is not None:
                kw["bias"] = bias
            if scale is not None:
                kw["scale"] = scale
            if accum is not None:
                kw["accum_out"] = accum
            P.op("act", lambda e: e.activation(out=out, in_=in_, func=func, **kw), reads, writes)

        def TT(eng, out, in0, in1, op, reads, writes):
            P.op(eng, lambda e: e.tensor_tensor(out=out, in0=in0, in1=in1, op=op), reads, writes)

        def TS(eng, out, in0, s1, s2, op0, op1, reads, writes):
            if s2 is None:
                P.op(eng, lambda e: e.tensor_scalar(out=out, in0=in0, scalar1=s1, scalar2=None, op0=op0), reads, writes)
            else:
                P.op(eng, lambda e: e.tensor_scalar(out=out, in0=in0, scalar1=s1, scalar2=s2, op0=op0, op1=op1), reads, writes)

        def STT(eng, out, in0, scalar, in1, op0, op1, reads, writes):
            P.op(eng, lambda e: e.scalar_tensor_tensor(out=out, in0=in0, scalar=scalar, in1=in1, op0=op0, op1=op1), reads, writes)

        def CP(eng, out, in_, reads, writes):
            P.op(eng, lambda e: e.tensor_copy(out=out, in_=in_), reads, writes)

        def MS(eng, ap, val, writes):
            P.op(eng, lambda e: e.memset(ap, val), (), writes)

        def RECIP(out, in_, reads, writes):
            P.op("dve", lambda e: e.reciprocal(out=out, in_=in_), reads, writes)

        def DMA(q, out, in_, reads, writes):
            P.op(q, lambda e: e.dma_start(out=out, in_=in_), reads, writes, dma=True)

        def arena(off, n, dt=BF16):
            if dt == BF16:
                return scr[:, off:off + n]
            assert off % 2 == 0
            return scr[:, off:off + 2 * n].bitcast(F32)

        DMA("sp", ident_f[:], ident_d, [], [r_const])
        DMA("sp", umask_f[:], umask_d, [], [r_const])
        DMA("sp", lmask_f[:], lmask_d, [], [r_const])
        DMA("pool", cs64[:], cs64_d, [], [r_const])
        DMA("sp", cv_f[:], cvec, [], [r_const])
        DMA("sp", bada_s[:], bada.rearrange("l p n -> p l n"), [], [r_const])
        DMA("sp", gv[:], gvec.rearrange("l p a k -> p l a k"), [], [r_const])
        CP("dve", ident_b[:], ident_f[:], [r_const], [r_const])
        TS("dve", negm_f[:], umask_f[:], -1.0, 30000.0, ALU.add, ALU.mult, [r_const], [r_const])
        TS("dve", negm_b[:], lmask_f[:], -1.0, 30000.0, ALU.add, ALU.mult, [r_const], [r_const])
        MS("dve", ones_f[:], 1.0, [r_const])
        MS("dve", ones_b[:], 1.0, [r_const])
        MS("dve", cst[:, 0:1], 1024.0 * EPS, [r_const])
        MS("dve", cst[:, 1:2], EPS, [r_const])
        MS("dve", cst[:, 2:3], 1.0, [r_const])
        MS("dve", cst[:, 3:4], 0.0, [r_const])

        for kc in range(KC):
            DMA("sp", xs[:, kc, CTX:NT], xT[kc], [], trs(r_xs, kc, CTX, NT))
            DMA("sp", xs[:, kc, 0:CTX], ctxT[kc], [], trs(r_xs, kc, 0, CTX))
        pos_buf = [arena(0, SEQ, F32), arena(2 * SEQ, SEQ, F32)]
        r_pos = [R(), R()]
        for kc in range(KC):
            i = kc % 2
            DMA("sp", pos_buf[i], posT[kc], [], [r_pos[i]])
            TT("dve", xs[:, kc, CTX:NT], xs[:, kc, CTX:NT], pos_buf[i], ALU.add,
               [r_pos[i]] + trs(r_xs, kc, CTX, NT), trs(r_xs, kc, CTX, NT))

        ACT(sc_b[:], cv_f[:], AF.Silu, [r_const], [r_const])
        wa_off = 4 * SEQ
        wa_buf = [arena(wa_off + i * KC * 512, KC * 512).rearrange("p (k c) -> p k c", c=512) for i in range(2)]
        r_wa = [R(), R()]
        for l in range(DEPTH):
            pi = next_ps()
            pv = psb[pi][:, 0:96].rearrange("p (n w) -> p n w", w=2)
            for blk in range(12):
                i = (l * 12 + blk) % 2
                DMA("pool", wa_buf[i], wada[l, blk], [], [r_wa[i]])
                for n4 in range(4):
                    n = blk * 4 + n4
                    for kc in range(KC):
                        MM(pv[:, n, :], wa_buf[i][:, kc, n4 * 128:(n4 + 1) * 128], sc_b[:, kc, :], kc == 0, kc == KC - 1,
                           [r_wa[i], r_const], [r_ps[pi]])
            TT("dve", mod[:, l], pv, bada_s[:, l].unsqueeze(2).to_broadcast([128, 48, 2]), ALU.add,
               [r_ps[pi], r_const], [r_mod])

        def rstd_of(src_fn, n, reads):
            pi = next_ps()
            for kc in range(KC):
                si = next_sq()
                ACT(sqb[si][:, 0:n], src_fn(kc), AF.Square, reads(kc), [r_sqb[si]])
                MM(psb[pi][:, 0:n], ones_b[:], sqb[si][:, 0:n], kc == 0, kc == KC - 1, [r_sqb[si], r_const], [r_ps[pi]])
            ACT(rs[:, 0:n], psb[pi][:, 0:n], AF.Sqrt, [r_ps[pi], r_const], [r_rs], bias=cst[:, 0:1], scale=1.0)
            RECIP(rs[:, 0:n], rs[:, 0:n], [r_rs], [r_rs])

        def prenorm(tl, Amod, shift_base, l):
            for (a, b, w) in tl:
                n = b - a
                rstd_of(lambda kc: xs[:, kc, a:b], n, lambda kc: trs(r_xs, kc, a, b))
                for kc in range(KC):
                    ti = next_tf()
                    TT("dve", tmpf[ti][:, 0:n], xs[:, kc, a:b], rs[:, 0:n], ALU.mult,
                       trs(r_xs, kc, a, b) + [r_rs], [r_tmpf[ti]])
                    ACT(hx[:, kc, a:b], tmpf[ti][:, 0:n], AF.Identity, [r_tmpf[ti], r_lay, r_mod], trs(r_hx, kc, a, b),
                        bias=mod[:, l, shift_base + kc, w:w + 1], scale=Amod[:, kc, w:w + 1])

        def resid_update(tl, src_fn, src_reads, Gm):
            for (a, b, w) in tl:
                n = b - a
                rstd_of(lambda kc: src_fn(kc, a, b), n, lambda kc: src_reads(kc, a, b))
                for kc in range(KC):
                    ti = next_tf()
                    TT("dve", tmpf[ti][:, 0:n], src_fn(kc, a, b), rs[:, 0:n], ALU.mult,
                       src_reads(kc, a, b) + [r_rs], [r_tmpf[ti]])
                    STT("dve", xs[:, kc, a:b], tmpf[ti][:, 0:n], Gm[:, kc, w:w + 1], xs[:, kc, a:b], ALU.mult, ALU.add,
                        [r_tmpf[ti], r_lay] + trs(r_xs, kc, a, b), trs(r_xs, kc, a, b))

        def dump(name, src_fn, reads_fn):
            if not dbg:
                return
            for kc in range(KC):
                DMA("pool", dbg_out[name][kc], src_fn(kc), reads_fn(kc), [])

        def maybe_stop(k):
            if stop is not None and stop == k:
                raise _Stop()

        try:
            P.barrier()
            for l in range(DEPTH):
                need_ctx = l < DEPTH - 1
                TL_all = tiles_for(True)
                TL = tiles_for(need_ctx)
                j0 = 0 if need_ctx else 2

                def mk(dst, gidx, mbase, plus1):
                    for w in range(2):
                        if plus1:
                            STT("dve", dst[:, :, w], mod[:, l, mbase:mbase + KC, w], 1.0, gv[:, l, gidx, :], ALU.add, ALU.mult,
                                [r_mod, r_const, r_lay], [r_lay])
                        else:
                            TT("dve", dst[:, :, w], mod[:, l, mbase:mbase + KC, w], gv[:, l, gidx, :], ALU.mult,
                               [r_mod, r_const, r_lay], [r_lay])
                    TS("dve", dst[:], dst[:], 32.0, None, ALU.mult, None, [r_lay], [r_lay])

                mk(A1, 0, 8, True)
                mk(G1, 1, 16, False)
                mk(A2, 2, 32, True)
                mk(G2, 3, 40, False)

                maybe_stop(10 * l + 1)

                o_w = 0
                wbuf = [arena(o_w + i * 1024, 1024).rearrange("p (k c) -> p k c", c=128) for i in range(4)]
                r_wbuf = [R() for _ in range(4)]
                wcnt = {"n": 0}

                def load_w(src):
                    i = wcnt["n"] % 4
                    wcnt["n"] += 1
                    DMA("pool", wbuf[i], src, [], [r_wbuf[i]])
                    return i

                o_up = 4096
                upad = arena(o_up, 2 * UPW).rearrange("p (c t) -> p c t", t=UPW)
                r_up = R()
                o_vb = o_up + 2 * UPW
                vbuf = arena(o_vb, 2 * NT, F32).rearrange("p (c t) -> p c t", t=NT)
                r_vb = [R(), R()]
                o_cs = o_vb + 4 * NT
                wdw_s = arena(o_cs, 2 * CONV_K, F32).rearrange("p (c k) -> p c k", k=CONV_K)
                cvp_s = arena(o_cs + 4 * CONV_K, 6, F32).rearrange("p (a c) -> p a c", c=2)
                r_cs = R()
                lnt = [ym_flat[:, i * 1024:(i + 1) * 1024].bitcast(F32) for i in range(5)]
                r_lnt = [R() for _ in range(5)]
                dgm = ym_flat[:, 5120:5120 + 2 * CONV_K * 128].rearrange("p (c k m) -> p c k m", k=CONV_K, m=128)
                r_dg = R()

                DMA("sp", wdw_s, wdw[l], [], [r_cs])
                DMA("sp", cvp_s, cvp[l], [], [r_cs])
                MS("dve", upad, 0.0, [r_up])
                for cc in range(2):
                    for k in range(CONV_K):
                        TS("dve", dgm[:, cc, k, :], ident_f[:], wdw_s[:, cc, k:k + 1], None, ALU.mult, None,
                           [r_const, r_cs], [r_dg])

                def upos(a):
                    return a + PADC if a < CTX else (CTX + 2 * PADC) + (a - CTX) + PADC

                iag = [(load_w(winc[l, 10 + cc]), load_w(winc[l, 12 + cc])) for cc in range(2)]
                for (a, b, w) in TL:
                    prenorm([(a, b, w)], A1, 0, l)
                    for cc in range(2):
                        ia, ig = iag[cc]
                        n = b - a
                        pa = next_ps()
                        for kc in range(KC):
                            MM(psb[pa][:, 0:n], wbuf[ia][:, kc, :], hx[:, kc, a:b], kc == 0, kc == KC - 1,
                               [r_wbuf[ia]] + trs(r_hx, kc, a, b), [r_ps[pa]])
                        pg = next_ps()
                        for kc in range(KC):
                            MM(psb[pg][:, 0:n], wbuf[ig][:, kc, :], hx[:, kc, a:b], kc == 0, kc == KC - 1,
                               [r_wbuf[ig]] + trs(r_hx, kc, a, b), [r_ps[pg]])
                        ti = next_tf()
                        ACT(tmpf[ti][:, 0:n], psb[pg][:, 0:n], AF.Sigmoid, [r_ps[pg]], [r_tmpf[ti]])
                        TT("dve", upad[:, cc, upos(a):upos(a) + n], psb[pa][:, 0:n], tmpf[ti][:, 0:n], ALU.mult,
                           [r_ps[pa], r_tmpf[ti]], [r_up])
                if not need_ctx:
                    prenorm([(0, 256, 1)], A1, 0, l)
                if l == 0:
                    dump("d_hx", lambda kc: hx[:, kc, :], lambda kc: trs(r_hx, kc, 0, NT))
                for cc in range(2):
                    for (a, b, w) in TL:
                        n = b - a
                        pi = next_ps()
                        p0 = upos(a) - PADC
                        for k in range(CONV_K):
                            MM(psb[pi][:, 0:n], dgm[:, cc, k, :], upad[:, cc, p0 + k:p0 + k + n], k == 0, k == CONV_K - 1,
                               [r_dg, r_up], [r_ps[pi]])
                        ACT(vbuf[:, cc, a:b], psb[pi][:, 0:n], AF.Identity, [r_ps[pi], r_cs], [r_vb[cc]],
                            bias=cvp_s[:, 0, cc:cc + 1], scale=1.0)
                for (a, b, w) in TL:
                    n = b - a
                    p1 = next_ps()
                    p2 = next_ps()
                    for cc in range(2):
                        MM(psb[p1][:, 0:n], ones_f[:], vbuf[:, cc, a:b], cc == 0, cc == 1, [r_const, r_vb[cc]], [r_ps[p1]])
                    for cc in range(2):
                        TT("dve", lnt[0][:, 0:n], vbuf[:, cc, a:b], vbuf[:, cc, a:b], ALU.mult, [r_vb[cc]], [r_lnt[0]])
                        MM(psb[p2][:, 0:n], ones_f[:], lnt[0][:, 0:n], cc == 0, cc == 1, [r_const, r_lnt[0]], [r_ps[p2]])
                    mean = lnt[1]
                    TS("dve", mean[:, 0:n], psb[p1][:, 0:n], 1.0 / 256.0, None, ALU.mult, None, [r_ps[p1]], [r_lnt[1]])
                    TT("dve", lnt[2][:, 0:n], mean[:, 0:n], mean[:, 0:n], ALU.mult, [r_lnt[1]], [r_lnt[2]])
                    STT("dve", lnt[2][:, 0:n], psb[p2][:, 0:n], 1.0 / 256.0, lnt[2][:, 0:n], ALU.mult, ALU.subtract,
                        [r_ps[p2], r_lnt[2]], [r_lnt[2]])
                    ACT(lnt[2][:, 0:n], lnt[2][:, 0:n], AF.Sqrt, [r_lnt[2], r_const], [r_lnt[2]], bias=cst[:, 1:2], scale=1.0)
                    RECIP(lnt[2][:, 0:n], lnt[2][:, 0:n], [r_lnt[2]], [r_lnt[2]])
                    for cc in range(2):
                        TT("dve", lnt[3 + cc][:, 0:n], vbuf[:, cc, a:b], mean[:, 0:n], ALU.subtract,
                           [r_vb[cc], r_lnt[1]], [r_lnt[3 + cc]])
                        TT("dve", lnt[3 + cc][:, 0:n], lnt[3 + cc][:, 0:n], lnt[2][:, 0:n], ALU.mult,
                           [r_lnt[3 + cc], r_lnt[2]], [r_lnt[3 + cc]])
                        ACT(ym[:, 6 + cc, a:b], lnt[3 + cc][:, 0:n], AF.Silu, [r_lnt[3 + cc], r_cs], trs(r_ym, 6 + cc, a, b),
                            bias=cvp_s[:, 2, cc:cc + 1], scale=cvp_s[:, 1, cc:cc + 1])
                P.barrier()

                maybe_stop(10 * l + 2)
                o_uf = 4096
                uF = arena(o_uf, 2 * NT).rearrange("p (c t) -> p c t", t=NT)
                r_uf = [R(), R()]
                o_dp = o_uf + 2 * NT
                dpc = [arena(o_dp + i * 4096, 4096).rearrange("p (s k t) -> p s k t", s=2, k=4) for i in range(3)]
                r_dpc = [R() for _ in range(3)]
                o_dc = o_dp + 3 * 4096
                dcc = arena(o_dc, 1024).rearrange("p (s k t) -> p s k t", s=2, k=2)
                r_dcc = R()
                AB = ym_flat[:, 0:NJ * 512].rearrange("p (j c m) -> p j c m", c=2, m=256)
                r_ab = [R() for _ in range(NJ)]

                for cc in range(2):
                    iw = load_w(winc[l, 8 + cc])
                    for (a, b, w) in TL:
                        n = b - a
                        pi = next_ps()
                        for kc in range(KC):
                            MM(psb[pi][:, 0:n], wbuf[iw][:, kc, :], hx[:, kc, a:b], kc == 0, kc == KC - 1,
                               [r_wbuf[iw]] + trs(r_hx, kc, a, b), [r_ps[pi]])
                        ACT(uF[:, cc, a:b], psb[pi][:, 0:n], AF.Copy, [r_ps[pi]], [r_uf[cc]])
                for j in range(j0, NJ):
                    pi = next_ps()
                    for cc in range(2):
                        MM(psb[pi][:, cc * 256:(cc + 1) * 256], uF[:, cc, j * 128:(j + 1) * 128], cs64[:], True, True,
                           [r_uf[cc], r_const], [r_ps[pi]])
                    CP("dve", AB[:, j].rearrange("p c m -> p (c m)"), psb[pi][:, :], [r_ps[pi]], [r_ab[j]])
                npc = 0
                for tq in range(4):
                    pp = [next_ps(), next_ps()]
                    for kg in range(4):
                        i = npc % 3
                        npc += 1
                        DMA("sp", dpc[i].rearrange("p s k t -> p (s k t)"),
                            dftx_d[tq, kg].rearrange("p s k t -> p (s k t)"), [], [r_dpc[i]])
                        for ki in range(4):
                            j = 2 + kg * 4 + ki
                            for s in range(2):
                                first = (kg == 0 and ki == 0 and s == 0)
                                last = (kg == 3 and ki == 3 and s == 1)
                                for cc in range(2):
                                    MM(psb[pp[cc]][:, :], AB[:, j, cc, s * 128:(s + 1) * 128], dpc[i][:, s, ki, :], first, last,
                                       [r_ab[j], r_dpc[i]], [r_ps[pp[cc]]])
                    for cc in range(2):
                        a = CTX + tq * 512
                        ACT(ym[:, 4 + cc, a:a + 512], psb[pp[cc]][:, :], AF.Copy, [r_ps[pp[cc]]], trs(r_ym, 4 + cc, a, a + 512))
                if need_ctx:
                    DMA("sp", dcc.rearrange("p s k t -> p (s k t)"), dftc_d.rearrange("p s k t -> p (s k t)"), [], [r_dcc])
                    pp = [next_ps(), next_ps()]
                    for ki in range(2):
                        for s in range(2):
                            for cc in range(2):
                                MM(psb[pp[cc]][:, 0:256], AB[:, ki, cc, s * 128:(s + 1) * 128], dcc[:, s, ki, :],
                                   ki == 0 and s == 0, ki == 1 and s == 1, [r_ab[ki], r_dcc], [r_ps[pp[cc]]])
                    for cc in range(2):
                        ACT(ym[:, 4 + cc, 0:CTX], psb[pp[cc]][:, 0:256], AF.Copy, [r_ps[pp[cc]]], trs(r_ym, 4 + cc, 0, CTX))

                P.barrier()
                maybe_stop(10 * l + 3)
                o_s = 0
                dgq = [arena(o_s + i * 256, 128, F32) for i in range(2)]
                qs = [arena(o_s + 512 + i * 128, 128) for i in range(2)]
                sTt = [arena(o_s + 768 + i * 128, 128) for i in range(2)]
                kts = [arena(o_s + 1024 + i * 128, 128) for i in range(2)]
                Et = [arena(o_s + 1280 + i * 128, 128) for i in range(2)]
                Cst = [arena(o_s + 1536 + i * 260, 130, F32) for i in range(2)]
                Cbf = [arena(o_s + 2056 + i * 130, 130) for i in range(2)]
                rden = [arena(o_s + 2316 + i * 4, 2, F32) for i in range(2)]
                st1 = arena(o_s + 2324, 2 * NJ, F32)
                st2 = arena(o_s + 2396, 2 * NJ, F32)
                r_dgq, r_qs, r_sT, r_kts = [R(), R()], [R(), R()], [R(), R()], [R(), R()]
                r_dgb, r_Et = [R(), R()], [R(), R()]
                r_C, r_Cb, r_rden = [R(), R()], [R(), R()], [R(), R()]
                r_st = R()
                wqk_raw = arena(2468, 1024).rearrange("p (k c) -> p k c", c=128)
                r_wqk = R()
                dg3 = arena(3492, 384).rearrange("p (t c) -> p t c", c=128)
                wtapc = arena(3876, 4, F32)[:, 0:3]
                zraw = arena(3884, 2308)
                r_w3, r_wtap, r_zraw = R(), R(), R()
                o_g = 7332
                Gtok = arena(o_g, NJ * 16, F32).rearrange("p (j g) -> p j g", g=16)
                dgb = [arena(o_g + i * 256, 128, F32) for i in range(2)]
                LF = arena(o_g + 576, NJ * 8, F32).rearrange("p (j g) -> p j g", g=8)
                CM = arena(o_g + 864, NJ * 8, F32).rearrange("p (j g) -> p j g", g=8)
                Bc = arena(o_g + 1152, NJ * 8, F32).rearrange("p (j g) -> p j g", g=8)
                EB = arena(o_g + 1440, NJ * 8, F32).rearrange("p (j g) -> p j g", g=8)
                EBL = arena(o_g + 1728, NJ * 8, F32).rearrange("p (j g) -> p j g", g=8)
                ECL = arena(o_g + 2016, NJ * 8, F32).rearrange("p (j g) -> p j g", g=8)
                bg_s = arena(o_g + 2304, 16, F32)
                wg_s = arena(o_g + 2336, KC * 16).rearrange("p (k g) -> p k g", g=16)
                gmn_s = arena(o_g + 2464, 512, F32)
                r_gate = R()
                o_h = o_g + 3488
                qT = arena(o_h, NT)
                kT = arena(o_h + NT, NT)
                vaug = arena(o_h + 2 * NT, NJ * 130).rearrange("p (j e) -> p j e", e=130)
                sgo = arena(o_h + 2 * NT + 2340, NT).rearrange("p (j e) -> p j e", e=128)
                hsum = arena(o_h + 3 * NT + 2340, NT, F32).rearrange("p (j e) -> p j e", e=128)
                assert o_h + 5 * NT + 2340 <= SCR_ARENA, (o_h + 5 * NT + 2340)
                r_q, r_k, r_v, r_sg, r_hs = R(), R(), R(), R(), R()
                wvo_s = ym_flat[:, 3 * NT:3 * NT + KC * 256].rearrange("p (k c) -> p k c", c=256)
                r_wvo = R()

                DMA("pool", wg_s, wgate[l], [], [r_gate])
                DMA("sp", bg_s, bgate[l].partition_broadcast(128), [], [r_gate])
                DMA("sp", gmn_s, gmn[l].partition_broadcast(128), [], [r_gate])
                for jg in range(0, NJ, 6):
                    pi = next_ps()
                    for jj in range(6):
                        j = jg + jj
                        for kc in range(KC):
                            MM(psb[pi][:, jj * 16:(jj + 1) * 16], hx[:, kc, j * 128:(j + 1) * 128], wg_s[:, kc, :], kc == 0, kc == KC - 1,
                               [r_gate] + trs(r_hx, kc, j * 128, (j + 1) * 128), [r_ps[pi]])
                    TT("dve", Gtok[:, jg:jg + 6, :], psb[pi][:, 0:96].rearrange("p (j g) -> p j g", g=16),
                       bg_s.unsqueeze(1).to_broadcast([128, 6, 16]), ALU.add, [r_ps[pi], r_gate], [r_gate])
                CP("dve", CM[:, :, 0:4], Gtok[:, :, 0:4], [r_gate], [r_gate])
                CP("dve", CM[:, :, 4:8], Gtok[:, :, 8:12], [r_gate], [r_gate])
                ACT(LF[:, :, 0:4], Gtok[:, :, 4:8], AF.Abs, [r_gate], [r_gate])
                ACT(LF[:, :, 4:8], Gtok[:, :, 12:16], AF.Abs, [r_gate], [r_gate])
                ACT(LF[:], LF[:], AF.Exp, [r_gate], [r_gate], scale=-1.0)
                ACT(LF[:], LF[:], AF.Ln, [r_gate, r_const], [r_gate], bias=cst[:, 2:3], scale=1.0)
                TS("dve", EB[:, :, 0:4], Gtok[:, :, 4:8], 0.0, None, ALU.min, None, [r_gate], [r_gate])
                TS("dve", EB[:, :, 4:8], Gtok[:, :, 12:16], 0.0, None, ALU.min, None, [r_gate], [r_gate])
                TT("dve", LF[:], EB[:], LF[:], ALU.subtract, [r_gate], [r_gate])
                pi = next_ps()
                pbv = psb[pi][:, 0:NJ * 8].rearrange("p (j g) -> p j g", g=8)
                for j in range(NJ):
                    MM(pbv[:, j, 0:4], umask_f[:], LF[:, j, 0:4], True, True, [r_const, r_gate], [r_ps[pi]])
                    MM(pbv[:, j, 4:8], lmask_f[:], LF[:, j, 4:8], True, True, [r_const, r_gate], [r_ps[pi]])
                CP("dve", Bc[:], pbv, [r_ps[pi]], [r_gate])
                pi = next_ps()
                ptv = psb[pi][:, 0:NJ * 8].rearrange("p (j g) -> p j g", g=8)
                MM(psb[pi][:, 0:NJ * 8], ones_f[:], LF[:].rearrange("p j g -> p (j g)"), True, True, [r_const, r_gate], [r_ps[pi]])
                ACT(EBL[:], ptv, AF.Exp, [r_ps[pi]], [r_gate])
                ACT(EB[:], Bc[:], AF.Exp, [r_gate], [r_gate])
                TT("dve", CM[:], CM[:], Bc[:], ALU.subtract, [r_gate], [r_gate])
                TT("dve", ECL[:], CM[:], ptv, ALU.add, [r_gate, r_ps[pi]], [r_gate])
                ACT(ECL[:], ECL[:], AF.Exp, [r_gate], [r_gate])
                P.barrier()

                MS("dve", vaug[:, :, 128:129], 1.0, [r_v])
                MS("dve", zraw, 0.0, [r_zraw])

                for h in range(NH):
                    for qk in range(2):
                        dst, r_dst = (qT, r_q) if qk == 0 else (kT, r_k)
                        ci = qk * 4 + h
                        DMA("pool", wqk_raw, winc[l, ci], [], [r_wqk])
                        DMA("sp", wtapc, wqkp[l, ci], [], [r_wtap])
                        if qk == 1:
                            TS("dve", wtapc, wtapc, HD ** -0.5, None, ALU.mult, None, [r_wtap], [r_wtap])
                        for t in range(3):
                            TS("dve", dg3[:, t, :], ident_f[:], wtapc[:, t:t + 1], None, ALU.mult, None,
                               [r_const, r_wtap], [r_w3])

                        def zpos(a_):
                            return 1 + a_ if a_ < CTX else 3 + a_
                        for (a, b, w) in TL_all:
                            n = b - a
                            pi = next_ps()
                            for kc in range(KC):
                                MM(psb[pi][:, 0:n], wqk_raw[:, kc, :], hx[:, kc, a:b], kc == 0, kc == KC - 1,
                                   [r_wqk] + trs(r_hx, kc, a, b), [r_ps[pi]])
                            ACT(zraw[:, zpos(a):zpos(a) + n], psb[pi][:, 0:n], AF.Copy, [r_ps[pi]], [r_zraw])
                        for (a, b, w) in TL_all:
                            n = b - a
                            pi = next_ps()
                            for t in range(3):
                                MM(psb[pi][:, 0:n], dg3[:, t, :], zraw[:, zpos(a) + t - 1:zpos(a) + t - 1 + n], t == 0, t == 2,
                                   [r_w3, r_zraw], [r_ps[pi]])
                            ACT(dst[:, a:b], psb[pi][:, 0:n], AF.Copy, [r_ps[pi]], [r_dst])
                    DMA("pool", wvo_s, wvo[l, h], [], [r_wvo])
                    for j in range(NJ):
                        pi = next_ps()
                        for kc in range(KC):
                            MM(psb[pi][:, 0:256], hx[:, kc, j * 128:(j + 1) * 128], wvo_s[:, kc, :], kc == 0, kc == KC - 1,
                               [r_wvo] + trs(r_hx, kc, j * 128, (j + 1) * 128), [r_ps[pi]])
                        CP("dve", vaug[:, j, 0:128], psb[pi][:, 0:128], [r_ps[pi]], [r_v])
                        ACT(sgo[:, j, :], psb[pi][:, 128:256], AF.Sigmoid, [r_ps[pi]], [r_sg])
                        TT("dve", sgo[:, j, :], sgo[:, j, :], gmn_s[:, h * 128:(h + 1) * 128], ALU.mult, [r_sg, r_gate], [r_sg])
                    MS("dve", hsum, 0.0, [r_hs])
                    for d_ in range(2):
                        MS("dve", Cst[d_], 0.0, [r_C[d_]])
                        MS("dve", Cbf[d_], 0.0, [r_Cb[d_]])
                    order = [list(range(NJ)), [1, 0] + list(range(NJ - 1, 1, -1))]
                    t0f = tmpf[0]
                    t1b = tmpf[1][:, :].bitcast(BF16)
                    dgb2 = [dgb, [dgq[0], dgq[1]]]
                    sT2 = [sTt, [t1b[:, 256:384], t1b[:, 384:512]]]
                    kts2 = [kts, [t1b[:, 512:640], t1b[:, 640:768]]]
                    Et2 = [Et, [t1b[:, 768:896], t1b[:, 896:1024]]]
                    ndi = [t0f[:, 0:130], t0f[:, 130:260]]
                    ndi_den = t0f[:, 0:260].rearrange("p (d e) -> p d e", e=130)[:, :, 128:129]
                    rden_j = t0f[:, 392:394].unsqueeze(2)
                    r_rdj = R()
                    r_ndi = [R(), R()]
                    t0b = t0f[:, 260:390].bitcast(BF16)
                    Cbf2 = [Cbf, [t0b[:, 0:130], t0b[:, 130:260]]]
                    r_Cb2 = [r_Cb, [R(), R()]]
                    rr = lambda: [[R(), R()], [R(), R()]]
                    r_dgb2, r_sT2, r_kts2, r_Et2 = rr(), rr(), rr(), rr()

                    def info(step, d_):
                        j = order[d_][step]
                        return j, step % 2, d_ * 4 + h, (need_ctx or j >= 2), step == NJ - 1

                    ps_hold = {}

                    def a1(step, d_):
                        j, bs, col, need_out, is_last = info(step, d_)
                        if need_out:
                            TS("dve", dgb2[bs][d_], ident_f[:], Bc[:, j, col:col + 1], None, ALU.mult, None,
                               [r_const, r_gate], [r_dgb2[bs][d_]])
                        yield

                    def a2a(step, d_):
                        j, bs, col, need_out, is_last = info(step, d_)
                        c0, c1 = j * 128, (j + 1) * 128
                        if need_out:
                            pd_ = d_
                            MM(psb[pd_][:, 0:128], ones_f[:], dgb2[bs][d_], True, False, [r_const, r_dgb2[bs][d_]], [r_ps[pd_]])
                            MM(psb[pd_][:, 0:128], ident_b[:], (negm_f if d_ == 0 else negm_b)[:], False, True,
                               [r_const], [r_ps[pd_]])
                            yield
                            ACT(Et2[bs][d_], psb[pd_][:, 0:128], AF.Exp, [r_ps[pd_], r_gate], [r_Et2[bs][d_]],
                                bias=CM[:, j, col:col + 1], scale=1.0)
                            yield
                            p_s = 2 + d_
                            MM(psb[p_s][:, 0:128], kT[:, c0:c1], qT[:, c0:c1], True, True, [r_k, r_q], [r_ps[p_s]])
                            yield
                        if not is_last:
                            p_t = next_ps()
                            ptb = psb[p_t][:, 0:64].bitcast(BF16)
                            TR(ptb, kT[:, c0:c1], ident_b[:], [r_k, r_const], [r_ps[p_t]])
                            yield
                            ACT(kts2[bs][d_], ptb, AF.Copy, [r_ps[p_t], r_gate], [r_kts2[bs][d_]], scale=ECL[:, j, col:col + 1])
                            yield

                    def a2b(step, d_):
                        j, bs, col, need_out, is_last = info(step, d_)
                        if need_out:
                            p_s = 2 + d_
                            TT("dve", sT2[bs][d_], psb[p_s][:, 0:128], Et2[bs][d_], ALU.mult,
                               [r_ps[p_s], r_Et2[bs][d_]], [r_sT2[bs][d_]])
                        yield

                    def stage_b(step, d_):
                        j, bs, col, need_out, is_last = info(step, d_)
                        cur, nxt = Cbf2[step % 2][d_], Cbf2[(step + 1) % 2][d_]
                        r_cur, r_nxt = r_Cb2[step % 2][d_], r_Cb2[(step + 1) % 2][d_]
                        if not is_last:
                            p_u = next_ps()
                            MM(psb[p_u][:, 0:129], kts2[bs][d_], vaug[:, j, 0:129], True, True, [r_kts2[bs][d_], r_v], [r_ps[p_u]])
                            yield
                            STT("dve", Cst[d_][:, 0:129], Cst[d_][:, 0:129], EBL[:, j, col:col + 1], psb[p_u][:, 0:129],
                                ALU.mult, ALU.add, [r_C[d_], r_gate, r_ps[p_u]], [r_C[d_]])
                            yield
                            ACT(nxt[:, 0:129], Cst[d_][:, 0:129], AF.Copy, [r_C[d_]], [r_nxt])
                            yield

                    def stage_bo(step, d_):
                        j, bs, col, need_out, is_last = info(step, d_)
                        cur, r_cur = Cbf2[step % 2][d_], r_Cb2[step % 2][d_]
                        if need_out:
                            c0, c1 = j * 128, (j + 1) * 128
                            p_i = next_ps()
                            MM(psb[p_i][:, 0:129], qT[:, c0:c1], cur[:, 0:129], True, True, [r_q, r_cur], [r_ps[p_i]])
                            p_n = next_ps()
                            MM(psb[p_n][:, 0:129], sT2[bs][d_], vaug[:, j, 0:129], True, True, [r_sT2[bs][d_], r_v], [r_ps[p_n]])
                            yield
                            ACT(ndi[d_][:, 0:129], psb[p_i][:, 0:129], AF.Copy, [r_ps[p_i], r_gate], [r_ndi[d_]], scale=EB[:, j, col:col + 1])
                            yield
                            TT("dve", ndi[d_][:, 0:129], ndi[d_][:, 0:129], psb[p_n][:, 0:129], ALU.add, [r_ndi[d_], r_ps[p_n]], [r_ndi[d_]])
                            yield

                    def den_ops(step):
                        j, bs, col, need_out, is_last = info(step, 0)
                        if need_out:
                            STT("dve", rden_j, ndi_den, -1.0, ndi_den, ALU.mult, ALU.max, [r_ndi[0], r_ndi[1]], [r_rdj])
                            TS("dve", rden_j, rden_j, 1.0, None, ALU.max, None, [r_rdj], [r_rdj])
                            RECIP(rden_j, rden_j, [r_rdj], [r_rdj])

                    def stage_b2(step, d_):
                        j, bs, col, need_out, is_last = info(step, d_)
                        if need_out:
                            STT("dve", hsum[:, j, :], ndi[d_][:, 0:128], t0f[:, 392 + d_:393 + d_], hsum[:, j, :], ALU.mult, ALU.add,
                                [r_ndi[d_], r_rdj, r_hs], [r_hs])
                        yield

                    def interleave(*gens):
                        gens = list(gens)
                        while gens:
                            for g in list(gens):
                                try:
                                    next(g)
                                except StopIteration:
                                    gens.remove(g)

                    for bnk in range(4):
                        ps_reserved.add(bnk)
                    interleave(a1(0, 0), a1(0, 1))
                    interleave(a2a(0, 0), a2a(0, 1))
                    interleave(a2b(0, 0), a2b(0, 1))
                    interleave(a1(1, 0), a1(1, 1))
                    for step in range(NJ):
                        if step + 2 < NJ:
                            interleave(a1(step + 2, 0), a1(step + 2, 1))
                        if step + 1 < NJ:
                            interleave(a2a(step + 1, 0), a2a(step + 1, 1))
                        interleave(stage_b(step, 0), stage_b(step, 1))
                        if step + 1 < NJ:
                            interleave(a2b(step + 1, 0), a2b(step + 1, 1))
                        interleave(stage_bo(step, 0), stage_bo(step, 1))
                        den_ops(step)
                        interleave(stage_b2(step, 0), stage_b2(step, 1))
                    for bnk in range(4):
                        ps_reserved.discard(bnk)
                    nj = NJ - j0
                    hv = hsum[:, j0:NJ, :]
                    sqv = ym[:, h, j0 * 128:NT].rearrange("p (j e) -> p j e", e=128)
                    ymh_res = trs(r_ym, h, j0 * 128, NT) + ([r_wvo] if h == 3 else [])
                    P.op("dve", (lambda hv=hv, nj=nj: lambda e: e.tensor_reduce(out=st1[:, 0:nj], in_=hv, axis=AX.X, op=ALU.add))(),
                         [r_hs], [r_st])
                    TT("dve", sqv, hv, hv, ALU.mult, [r_hs], ymh_res)
                    P.op("dve", (lambda sqv=sqv, nj=nj: lambda e: e.tensor_reduce(out=st1[:, NJ:NJ + nj], in_=sqv, axis=AX.X, op=ALU.add))(),
                         ymh_res, [r_st])
                    mean_ = st2[:, 0:nj]
                    var_ = st2[:, NJ:NJ + nj]
                    TS("dve", mean_, st1[:, 0:nj], 1.0 / HD, None, ALU.mult, None, [r_st], [r_st])
                    TT("dve", var_, mean_, mean_, ALU.mult, [r_st], [r_st])
                    STT("dve", var_, st1[:, NJ:NJ + nj], 1.0 / HD, var_, ALU.mult, ALU.subtract, [r_st], [r_st])
                    ACT(var_, var_, AF.Sqrt, [r_st, r_const], [r_st], bias=cst[:, 1:2], scale=1.0)
                    RECIP(var_, var_, [r_st], [r_st])
                    TT("dve", hv, hv, mean_.unsqueeze(2).to_broadcast([128, nj, 128]), ALU.subtract, [r_hs, r_st], [r_hs])
                    TT("dve", hv, hv, var_.unsqueeze(2).to_broadcast([128, nj, 128]), ALU.mult, [r_hs, r_st], [r_hs])
                    TT("dve", sgo[:, j0:NJ, :], hv, sgo[:, j0:NJ, :], ALU.mult, [r_hs, r_sg], [r_sg])
                    for jg in range(j0, NJ, 4):
                        p_t = next_ps()
                        ptb = psb[p_t][:, 0:256].bitcast(BF16)
                        jn = min(4, NJ - jg)
                        for jj in range(jn):
                            TR(ptb[:, jj * 128:(jj + 1) * 128], sgo[:, jg + jj, :], ident_b[:], [r_sg, r_const], [r_ps[p_t]])
                        ACT(ym[:, h, jg * 128:(jg + jn) * 128], ptb[:, 0:jn * 128], AF.Copy, [r_ps[p_t]],
                            trs(r_ym, h, jg * 128, (jg + jn) * 128) + ([r_wvo] if h == 3 else []))

                if l == 0:
                    dump("d_ym", lambda kc: ym[:, kc, :], lambda kc: trs(r_ym, kc, 0, NT))

                P.barrier()
                maybe_stop(10 * l + 4)
                o_wo = 4096
                wo_s = arena(o_wo, KC * KC * 128).rearrange("p (o k c) -> p o k c", o=KC, k=KC)
                r_wo = R()
                obuf = arena(o_wo + 8192, KC * 512, F32).rearrange("p (o t) -> p o t", t=512)
                r_ob = [R() for _ in range(KC)]
                DMA("pool", wo_s.rearrange("p o k c -> p (o k c)"), woutc[l].rearrange("p o k c -> p (o k c)"), [],
                    [r_wo])
                for (a, b, w) in TL:
                    n = b - a
                    for oc in range(KC):
                        pi = next_ps()
                        for kc in range(KC):
                            MM(psb[pi][:, 0:n], wo_s[:, oc, kc, :], ym[:, kc, a:b], kc == 0, kc == KC - 1,
                               [r_wo] + trs(r_ym, kc, a, b), [r_ps[pi]])
                        ACT(obuf[:, oc, 0:n], psb[pi][:, 0:n], AF.Copy, [r_ps[pi]], [r_ob[oc]])
                    resid_update([(a, b, w)], lambda kc, a_, b_: obuf[:, kc, 0:b_ - a_], lambda kc, a_, b_: [r_ob[kc]], G1)
                if l == 0:
                    dump("d_x1", lambda kc: xs[:, kc, :], lambda kc: trs(r_xs, kc, 0, NT))

                P.barrier()
                maybe_stop(10 * l + 5)
                hT = scr[:, 0:24576].rearrange("p (f t) -> p f t", t=768)
                r_hT = [R() for _ in range(32)]
                ob2 = scr[:, 24576:30720].rearrange("p (o t) -> p o t", t=768)
                r_ob2 = [R() for _ in range(KC)]
                w1b = [scr[:, 30720 + i * 2048:30720 + (i + 1) * 2048].rearrange("p (g k c) -> p g k c", g=2, k=KC) for i in range(2)]
                w2b = [scr[:, 34816 + i * 4096:34816 + (i + 1) * 4096].rearrange("p (f c) -> p f c", c=128) for i in range(2)]
                r_w1b = [R(), R()]
                r_w2b = [R(), R()]
                if need_ctx:
                    supers = [[(0, 256, 1), (256, 768, 0)], [(768, 1280, 0), (1280, 1536, 0)], [(1536, 2048, 0), (2048, 2304, 0)]]
                else:
                    supers = [[(256, 768, 0), (768, 1024, 0)], [(1024, 1536, 0), (1536, 1792, 0)], [(1792, 2304, 0)]]
                n1 = 0
                n2 = 0
                prenorm(supers[0], A2, 24, l)
                for isup, sup in enumerate(supers):
                    a0 = sup[0][0]
                    for g in range(16):
                        i = n1 % 2
                        n1 += 1
                        DMA("pool", w1b[i].rearrange("p g k c -> p (g k c)"), w1c[l, g].rearrange("p g k c -> p (g k c)"), [],
                            [r_w1b[i]])
                        for f2 in range(2):
                            f = g * 2 + f2
                            for (a, b, w) in sup:
                                n = b - a
                                pi = next_ps()
                                for kc in range(KC):
                                    MM(psb[pi][:, 0:n], w1b[i][:, f2, kc, :], hx[:, kc, a:b], kc == 0, kc == KC - 1,
                                       [r_w1b[i]] + trs(r_hx, kc, a, b), [r_ps[pi]])
                                ti = next_tf()
                                ACT(tmpf[ti][:, 0:n], psb[pi][:, 0:n], AF.Relu, [r_ps[pi]], [r_tmpf[ti]])
                                TT("dve", hT[:, f, a - a0:b - a0], tmpf[ti][:, 0:n], tmpf[ti][:, 0:n], ALU.mult, [r_tmpf[ti]], [r_hT[f]])
                    if isup + 1 < len(supers):
                        prenorm(supers[isup + 1], A2, 24, l)
                    ssb = []
                    for _ in sup:
                        pss = next_ps()
                        ps_reserved.add(pss)
                        ssb.append(pss)
                    for oc in range(KC):
                        i = n2 % 2
                        n2 += 1
                        DMA("pool", w2b[i].rearrange("p f c -> p (f c)"), w2c[l, oc].rearrange("p f c -> p (f c)"), [],
                            [r_w2b[i]])
                        for si_, (a, b, w) in enumerate(sup):
                            n = b - a
                            pi = next_ps()
                            for f in range(32):
                                MM(psb[pi][:, 0:n], w2b[i][:, f, :], hT[:, f, a - a0:b - a0], f == 0, f == 31,
                                   [r_w2b[i], r_hT[f]], [r_ps[pi]])
                            ACT(ob2[:, oc, a - a0:b - a0], psb[pi][:, 0:n], AF.Copy, [r_ps[pi]], [r_ob2[oc]])
                            sq_i = next_sq()
                            ACT(sqb[sq_i][:, 0:n], psb[pi][:, 0:n], AF.Square, [r_ps[pi]], [r_sqb[sq_i]])
                            MM(psb[ssb[si_]][:, 0:n], ones_b[:], sqb[sq_i][:, 0:n], oc == 0, oc == KC - 1,
                               [r_sqb[sq_i], r_const], [r_ps[ssb[si_]]])
                    for si_, (a, b, w) in enumerate(sup):
                        n = b - a
                        ACT(rs[:, 0:n], psb[ssb[si_]][:, 0:n], AF.Sqrt, [r_ps[ssb[si_]], r_const], [r_rs], bias=cst[:, 0:1], scale=1.0)
                        RECIP(rs[:, 0:n], rs[:, 0:n], [r_rs], [r_rs])
                        for kc in range(KC):
                            ti = next_tf()
                            TT("dve", tmpf[ti][:, 0:n], ob2[:, kc, a - a0:b - a0], rs[:, 0:n], ALU.mult,
                               [r_ob2[kc], r_rs], [r_tmpf[ti]])
                            STT("dve", xs[:, kc, a:b], tmpf[ti][:, 0:n], G2[:, kc, w:w + 1], xs[:, kc, a:b], ALU.mult, ALU.add,
                                [r_tmpf[ti], r_lay] + trs(r_xs, kc, a, b), trs(r_xs, kc, a, b))
                    for pss in ssb:
                        ps_reserved.discard(pss)
                if l == 0:
                    dump("d_x2", lambda kc: xs[:, kc, :], lambda kc: trs(r_xs, kc, 0, NT))
                P.barrier()

        except _Stop:
            pass
        for kc in range(KC):
            DMA("sp", outT[kc], xs[:, kc, CTX:NT], trs(r_xs, kc, CTX, NT), [])
        P.emit()
    return nc


_CACHE = {}


def _consts():
    if "c" in _CACHE:
        return _CACHE["c"]
    c = {}
    c["ident"] = np.eye(128, dtype=np.float32)
    s = np.arange(128)
    c["umask"] = (s[:, None] <= s[None, :]).astype(np.float32)
    c["lmask"] = (s[:, None] >= s[None, :]).astype(np.float32)
    k = np.arange(64)
    ang = 2.0 * np.pi * np.outer(k, k) / 64.0
    cs = np.zeros((128, 256), np.float64)
    for hh in range(2):
        cs[hh * 64:(hh + 1) * 64, hh * 64:(hh + 1) * 64] = np.cos(ang) / 8.0
        cs[hh * 64:(hh + 1) * 64, 128 + hh * 64:128 + (hh + 1) * 64] = np.sin(ang) / 8.0
    c["cs64"] = cs.astype(np.float32)
    t = np.arange(SEQ, dtype=np.int64)
    ph = (np.outer(t, t) % SEQ).astype(np.float64) * (2.0 * np.pi / SEQ)
    dc = (np.cos(ph) / math.sqrt(SEQ)).astype(np.float32)
    ds = (-np.sin(ph) / math.sqrt(SEQ)).astype(np.float32)
    both = np.stack([dc, ds], 0)
    both = both.reshape(2, 4, 4, 128, 4, 512)
    c["dftx"] = np.ascontiguousarray(both.transpose(4, 1, 3, 0, 2, 5)).astype(ml_dtypes.bfloat16)
    t = np.arange(CTX, dtype=np.int64)
    ph = (np.outer(t, t) % CTX).astype(np.float64) * (2.0 * np.pi / CTX)
    both = np.stack([np.cos(ph), -np.sin(ph)], 0) / math.sqrt(CTX)
    both = both.reshape(2, 2, 128, CTX)
    c["dftc"] = np.ascontiguousarray(both.transpose(2, 0, 1, 3)).astype(ml_dtypes.bfloat16)
    rows = SEQ // 64
    quarter = D // 4
    freq = np.exp(-math.log(10000.0) * np.arange(quarter, dtype=np.float32) / quarter).astype(np.float32)
    r = np.broadcast_to(np.arange(rows, dtype=np.float32)[:, None], (rows, 64)).reshape(-1)
    col = np.broadcast_to(np.arange(64, dtype=np.float32)[None, :], (rows, 64)).reshape(-1)
    ar = r[:, None] * freq
    ac = col[:, None] * freq
    pos = np.concatenate([np.sin(ar), np.cos(ar), np.sin(ac), np.cos(ac)], axis=-1).astype(np.float32)
    c["posT"] = np.ascontiguousarray(pos.T).reshape(KC, 128, SEQ)
    _CACHE["c"] = c
    return c


def _chunk_w(w, cols):
    return np.ascontiguousarray(w[:, cols].reshape(KC, 128, -1).transpose(1, 0, 2))


def _prep_shared(inp):
    f = np.float32
    L = DEPTH
    w_in = np.asarray(inp["w_in"], f)
    sh = {}
    wada = np.asarray(inp["w_ada"], f)
    sh["wada"] = np.ascontiguousarray(wada.reshape(L, KC, 128, 12, 512).transpose(0, 3, 2, 1, 4))
    sh["bada"] = np.ascontiguousarray(np.asarray(inp["b_ada"], f).reshape(L, 48, 128).transpose(0, 2, 1))
    gs = np.stack([np.asarray(inp[k], f) for k in ("g_pre_mix", "g_post_mix", "g_pre_mlp", "g_post_mlp")], 1)
    sh["gvec"] = np.ascontiguousarray(gs.reshape(L, 4, KC, 128).transpose(0, 3, 1, 2))
    offs = [Q_OFF + 128 * i for i in range(4)] + [K_OFF + 128 * i for i in range(4)] + \
           [F_OFF, F_OFF + 128, CA_OFF, CA_OFF + 128, CG_OFF, CG_OFF + 128]
    sh["winc"] = np.stack([np.stack([_chunk_w(w_in[l], np.arange(o, o + 128)) for o in offs]) for l in range(L)])
    sh["wvo"] = np.stack([np.stack([_chunk_w(w_in[l], np.concatenate([np.arange(V_OFF + 128 * h, V_OFF + 128 * h + 128),
                                                                      np.arange(O_OFF + 128 * h, O_OFF + 128 * h + 128)]))
                                    for h in range(NH)]) for l in range(L)])
    sh["wgate"] = np.stack([_chunk_w(w_in[l], np.arange(G_OFF, G_OFF + 16)) for l in range(L)])
    sh["bgate"] = np.ascontiguousarray(np.asarray(inp["b_gate"], f).reshape(L, 16))
    wqk = np.asarray(inp["w_qk_conv"], f)
    sh["wqkp"] = np.ascontiguousarray(wqk.reshape(L, 3, 8, 128).transpose(0, 2, 3, 1))
    sh["gmn"] = np.ascontiguousarray(np.asarray(inp["g_mlstm_norm"], f))
    wdw = np.asarray(inp["w_dw"], f)
    sh["wdw"] = np.ascontiguousarray(wdw.reshape(L, CONV_K, 2, 128).transpose(0, 3, 2, 1))
    cv = np.stack([np.asarray(inp[k], f) for k in ("b_dw", "g_conv_ln", "b_conv_ln")], 1)
    sh["cvp"] = np.ascontiguousarray(cv.reshape(L, 3, 2, 128).transpose(0, 3, 1, 2))
    wout = np.asarray(inp["w_out"], f)
    sh["woutc"] = np.ascontiguousarray(wout.reshape(L, KC, 128, KC, 128).transpose(0, 2, 3, 1, 4))
    w1 = np.asarray(inp["w_mlp1"], f)
    sh["w1c"] = np.ascontiguousarray(w1.reshape(L, KC, 128, 16, 2, 128).transpose(0, 3, 2, 4, 1, 5))
    w2 = np.asarray(inp["w_mlp2"], f)
    sh["w2c"] = np.ascontiguousarray(w2.reshape(L, 32, 128, KC, 128).transpose(0, 3, 2, 1, 4))
    c = _consts()
    for k in ("ident", "umask", "lmask", "cs64", "dftx", "dftc", "posT"):
        sh[k] = c[k]
    return sh


def make_in_maps(inp, cores):
    sh = _prep_shared(inp)
    x = np.asarray(inp["x"], np.float32)
    ctx = np.asarray(inp["ctx"], np.float32)
    c = np.asarray(inp["c"], np.float32)
    c_ctx = np.asarray(inp["c_ctx"], np.float32)
    maps = []
    for b in cores:
        m = dict(sh)
        m["xT"] = np.ascontiguousarray(x[b].T).reshape(KC, 128, SEQ)
        m["ctxT"] = np.ascontiguousarray(ctx[b].T).reshape(KC, 128, CTX)
        cv = np.stack([c[b], c_ctx], -1)
        m["cvec"] = np.ascontiguousarray(cv.reshape(KC, 128, 2).transpose(1, 0, 2))
        maps.append(m)
    return maps


def kernel(**inputs):
    if "nc" not in _CACHE:
        _CACHE["nc"] = build_program(dbg=False)
    nc = _CACHE["nc"]
    in_maps = make_in_maps(inputs, list(range(NB)))
    res = run_bass_kernel_spmd(nc, in_maps, core_ids=list(range(NB)))
    out = np.empty((NB, SEQ, D), np.float32)
    for b in range(NB):
        oT = np.asarray(res.results[b]["outT"], np.float32).reshape(D, SEQ)
        out[b] = oT.T
    return out
```

```python
import math
from contextlib import ExitStack

import numpy as np
import ml_dtypes

import concourse.bass as bass
import concourse.mybir as mybir
from concourse.bass_utils import run_bass_kernel_spmd

F32 = mybir.dt.float32
BF16 = mybir.dt.bfloat16
AF = mybir.ActivationFunctionType
ALU = mybir.AluOpType
AX = mybir.AxisListType

ENGS = ("pe", "act", "dve", "pool", "sp")
SEM_LIM = 2000
N_DMA_SEMS = 16


class Res:
    __slots__ = ("name", "w", "r", "excl")

    def __init__(self, name, excl=False):
        self.name = name
        self.w = None
        self.r = []
        self.excl = excl


class Op:
    __slots__ = ("eng", "idx", "fn", "deps", "dma", "signal", "semref", "dma_slot", "dma_val")

    def __init__(self, eng, idx, fn, dma):
        self.eng = eng
        self.idx = idx
        self.fn = fn
        self.deps = []
        self.dma = dma
        self.signal = False
        self.semref = None
        self.dma_slot = None
        self.dma_val = None


class Prog:
    def __init__(self, nc, same_engine_sync=True):
        self.nc = nc
        self.ops = {e: [] for e in ENGS}
        self.n_dma_q = {}
        self.same_engine_sync = same_engine_sync

    def res(self, name="", excl=False):
        return Res(name, excl)

    def op(self, eng, fn, reads=(), writes=(), dma=False):
        o = Op(eng, len(self.ops[eng]), fn, dma)
        if dma:
            half = N_DMA_SEMS // 2
            k = self.n_dma_q.get(eng, 0)
            self.n_dma_q[eng] = k + 1
            o.dma_slot = (k % half) + (half if eng == "pool" else 0)
            o.dma_val = 16 * (k // half + 1)
        deps = []
        for r in reads:
            if r.w is not None:
                deps.append(r.w)
            if r.excl:
                deps.extend(x for x in r.r if x.eng != eng)
        for r in writes:
            if r.w is not None:
                deps.append(r.w)
            deps.extend(r.r)
        for r in reads:
            r.r.append(o)
        for r in writes:
            r.w = o
            r.r = []
        seen = set()
        for d in deps:
            if d is o or id(d) in seen:
                continue
            seen.add(id(d))
            if (not d.dma) and (not dma) and d.eng == eng:
                if eng == "pe" or not self.same_engine_sync:
                    continue
            o.deps.append(d)
        self.ops[eng].append(o)
        return o

    def barrier(self):
        targets = []
        for e in ENGS:
            cs = [o for o in self.ops[e] if not o.dma and o.fn is not None]
            if cs:
                targets.append(cs[-1])
        by_slot = {}
        for e in ENGS:
            for o in self.ops[e]:
                if o.dma and (o.dma_slot not in by_slot or o.dma_val > by_slot[o.dma_slot].dma_val):
                    by_slot[o.dma_slot] = o
        targets += list(by_slot.values())
        for e in ENGS:
            o = Op(e, len(self.ops[e]), None, False)
            o.deps = list(targets) if e != "pe" else [t for t in targets if t.dma or t.eng != e]
            self.ops[e].append(o)

    def emit(self):
        nc = self.nc
        for e in ENGS:
            for o in self.ops[e]:
                for d in o.deps:
                    d.signal = True
        with ExitStack() as st:
            dma_sems = [st.enter_context(nc.semaphore(f"dq{i}")) for i in range(N_DMA_SEMS)]
            for e in ENGS:
                n = sum(1 for o in self.ops[e] if o.signal and not o.dma)
                k = max((n + SEM_LIM - 1) // SEM_LIM, 1)
                sems = [st.enter_context(nc.semaphore(f"s_{e}{i}")) for i in range(k)]
                c = 0
                for o in self.ops[e]:
                    if o.signal and not o.dma:
                        o.semref = (sems[c // SEM_LIM], c % SEM_LIM + 1, c)
                        c += 1
            block = st.enter_context(nc.Block())

            def run(e, eng):
                waited_c = {p: -1 for p in ENGS}
                waited_d = [0] * N_DMA_SEMS
                for o in self.ops[e]:
                    for d in o.deps:
                        if d.dma:
                            if waited_d[d.dma_slot] >= d.dma_val:
                                continue
                            waited_d[d.dma_slot] = d.dma_val
                            eng.wait_ge(dma_sems[d.dma_slot], d.dma_val)
                        else:
                            sem, val, gc = d.semref
                            if waited_c[d.eng] >= gc:
                                continue
                            waited_c[d.eng] = gc
                            eng.wait_ge(sem, val)
                    if o.fn is None:
                        continue
                    if o.dma and o.dma_val > 16 and waited_d[o.dma_slot] < o.dma_val - 16:
                        waited_d[o.dma_slot] = o.dma_val - 16
                        eng.wait_ge(dma_sems[o.dma_slot], o.dma_val - 16)
                    ins = o.fn(eng)
                    if o.dma:
                        ins.then_inc(dma_sems[o.dma_slot], 16)
                    elif o.signal:
                        ins.then_inc(o.semref[0], 1)

            fin_dma = {}
            for e in ENGS:
                for o in self.ops[e]:
                    if o.dma:
                        fin_dma[o.dma_slot] = max(fin_dma.get(o.dma_slot, 0), o.dma_val)

            @block.tensor
            def _(eng):
                run("pe", eng)

            @block.scalar
            def _(eng):
                run("act", eng)

            @block.vector
            def _(eng):
                run("dve", eng)

            @block.gpsimd
            def _(eng):
                run("pool", eng)

            @block.sync
            def _(eng):
                run("sp", eng)
                for slot, val in sorted(fin_dma.items()):
                    eng.wait_ge(dma_sems[slot], val)


D = 1024
NB = 8
SEQ = 2048
CTX = 256
NT = CTX + SEQ
DEPTH = 2
KC = 8
NJ = NT // 128
HD = 128
NH = 4
DFF = 4096
EPS = 1e-6
CONV_K = 31
PADC = 15
Q_OFF, K_OFF, V_OFF, O_OFF, G_OFF = 0, 512, 1024, 1536, 2048
F_OFF = 2064
CA_OFF = 2320
CG_OFF = 2576
UPW = (CTX + 2 * PADC) + (SEQ + 2 * PADC)


def tiles_for(need_ctx):
    t = [(256, 768, 0), (768, 1280, 0), (1280, 1792, 0), (1792, 2304, 0)]
    if need_ctx:
        t = [(0, 256, 1)] + t
    return t


class _Stop(Exception):
    pass


def build_program(dbg=False, stop=None):
    nc = bass.Bass("TRN2", target_bir_lowering=False)
    P = Prog(nc)

    def din(name, shape, dt=F32):
        return nc.dram_tensor(name, list(shape), dt, kind="ExternalInput").ap()

    def dout(name, shape, dt=F32):
        return nc.dram_tensor(name, list(shape), dt, kind="ExternalOutput").ap()

    xT = din("xT", [KC, 128, SEQ])
    ctxT = din("ctxT", [KC, 128, CTX])
    posT = din("posT", [KC, 128, SEQ])
    cvec = din("cvec", [128, KC, 2])
    wada = din("wada", [DEPTH, 12, 128, KC, 512])
    bada = din("bada", [DEPTH, 128, 48])
    gvec = din("gvec", [DEPTH, 128, 4, KC])
    winc = din("winc", [DEPTH, 14, 128, KC, 128])
    wvo = din("wvo", [DEPTH, NH, 128, KC, 256])
    wgate = din("wgate", [DEPTH, 128, KC, 16])
    bgate = din("bgate", [DEPTH, 16])
    wqkp = din("wqkp", [DEPTH, 8, 128, 3])
    gmn = din("gmn", [DEPTH, 512])
    wdw = din("wdw", [DEPTH, 128, 2, CONV_K])
    cvp = din("cvp", [DEPTH, 128, 3, 2])
    woutc = din("woutc", [DEPTH, 128, KC, KC, 128])
    w1c = din("w1c", [DEPTH, 16, 128, 2, KC, 128])
    w2c = din("w2c", [DEPTH, KC, 128, 32, 128])
    ident_d = din("ident", [128, 128])
    umask_d = din("umask", [128, 128])
    lmask_d = din("lmask", [128, 128])
    cs64_d = din("cs64", [128, 256])
    dftx_d = din("dftx", [4, 4, 128, 2, 4, 512], BF16)
    dftc_d = din("dftc", [128, 2, 2, 256], BF16)
    outT = dout("outT", [KC, 128, SEQ])
    dbg_out = {}
    if dbg:
        for nm in ("d_hx", "d_ym", "d_x1", "d_x2"):
            dbg_out[nm] = dout(nm, [KC, 128, NT])

    with ExitStack() as st:
        def sb(name, shape, dt):
            return st.enter_context(nc.sbuf_tensor(name, list(shape), dt))

        xs = sb("xs", [128, KC, NT], F32)
        hx = sb("hx", [128, KC, NT], BF16)
        SCR_ARENA = 25088
        SCR_N = SCR_ARENA + KC * NT
        scr = sb("scr", [128, SCR_N], BF16)
        ym_flat = scr[:, SCR_ARENA:SCR_N]
        ym = ym_flat.rearrange("p (c t) -> p c t", t=NT)
        ident_f = sb("ident_f", [128, 128], F32)
        ident_b = sb("ident_b", [128, 128], BF16)
        umask_f = sb("umask_f", [128, 128], F32)
        lmask_f = sb("lmask_f", [128, 128], F32)
        negm_f = sb("negm_f", [128, 128], BF16)
        negm_b = sb("negm_b", [128, 128], BF16)
        ones_f = sb("ones_f", [128, 128], F32)
        ones_b = sb("ones_b", [128, 128], BF16)
        cs64 = sb("cs64b", [128, 256], BF16)
        cst = sb("cst", [128, 4], F32)
        cv_f = sb("cv_f", [128, KC, 2], F32)
        sc_b = sb("sc_b", [128, KC, 2], BF16)
        mod = sb("mod", [128, DEPTH, 48, 2], F32)
        bada_s = sb("bada_s", [128, DEPTH, 48], F32)
        gv = sb("gv", [128, DEPTH, 4, KC], F32)
        A1 = sb("A1", [128, KC, 2], F32)
        A2 = sb("A2", [128, KC, 2], F32)
        G1 = sb("G1", [128, KC, 2], F32)
        G2 = sb("G2", [128, KC, 2], F32)
        rs = sb("rs", [128, 512], F32)
        sqb = [sb(f"sqb{i}", [128, 512], BF16) for i in range(2)]
        tmpf = [sb(f"tmpf{i}", [128, 512], F32) for i in range(2)]
        psb = [st.enter_context(nc.psum_tensor(f"ps{i}", [128, 512], F32)) for i in range(8)]

        R = P.res
        r_xs = [[R() for _ in range(NJ)] for _ in range(KC)]
        r_hx = [[R() for _ in range(NJ)] for _ in range(KC)]
        r_ym = [[R() for _ in range(NJ)] for _ in range(KC)]
        r_ps = [P.res("ps", excl=True) for _ in range(8)]
        r_const = R()
        r_mod = R()
        r_lay = R()
        r_rs = R()
        r_sqb = [R(), R()]
        r_tmpf = [R(), R()]
        r_arena = R()

        def trs(rl, kc, a, b):
            return [rl[kc][j] for j in range(a // 128, (b + 127) // 128)]

        def trs_all(rl, a, b):
            out = []
            for kc in range(KC):
                out += trs(rl, kc, a, b)
            return out

        cnt = {"ps": 0, "sq": 0, "tf": 0}

        ps_reserved = set()

        def next_ps():
            while True:
                i = cnt["ps"] % 8
                cnt["ps"] += 1
                if i not in ps_reserved:
                    return i

        def next_sq():
            i = cnt["sq"] % 2
            cnt["sq"] += 1
            return i

        def next_tf():
            i = cnt["tf"] % 2
            cnt["tf"] += 1
            return i

        def MM(out, lhsT, rhs, start, stop, reads, writes):
            P.op("pe", lambda e: e.matmul(out, lhsT=lhsT, rhs=rhs, start=start, stop=stop), reads, writes)

        def TR(out, in_, ident, reads, writes):
            P.op("pe", lambda e: e.transpose(out, in_, ident), reads, writes)

        def ACT(out, in_, func, reads, writes, bias=None, scale=None, accum=None):
            kw = {}
            if bias is not None:
                kw["bias"] = bias
            if scale is not None:
                kw["scale"] = scale
            if accum is not None:
                kw["accum_out"] = accum
            P.op("act", lambda e: e.activation(out=out, in_=in_, func=func, **kw), reads, writes)

        def TT(eng, out, in0, in1, op, reads, writes):
            P.op(eng, lambda e: e.tensor_tensor(out=out, in0=in0, in1=in1, op=op), reads, writes)

        def TS(eng, out, in0, s1, s2, op0, op1, reads, writes):
            if s2 is None:
                P.op(eng, lambda e: e.tensor_scalar(out=out, in0=in0, scalar1=s1, scalar2=None, op0=op0), reads, writes)
            else:
                P.op(eng, lambda e: e.tensor_scalar(out=out, in0=in0, scalar1=s1, scalar2=s2, op0=op0, op1=op1), reads, writes)

        def STT(eng, out, in0, scalar, in1, op0, op1, reads, writes):
            P.op(eng, lambda e: e.scalar_tensor_tensor(out=out, in0=in0, scalar=scalar, in1=in1, op0=op0, op1=op1), reads, writes)

        def CP(eng, out, in_, reads, writes):
            P.op(eng, lambda e: e.tensor_copy(out=out, in_=in_), reads, writes)

        def MS(eng, ap, val, writes):
            P.op(eng, lambda e: e.memset(ap, val), (), writes)

        def RECIP(out, in_, reads, writes):
            P.op("dve", lambda e: e.reciprocal(out=out, in_=in_), reads, writes)

        def DMA(q, out, in_, reads, writes):
            P.op(q, lambda e: e.dma_start(out=out, in_=in_), reads, writes, dma=True)

        def arena(off, n, dt=BF16):
            if dt == BF16:
                return scr[:, off:off + n]
            assert off % 2 == 0
            return scr[:, off:off + 2 * n].bitcast(F32)

        DMA("sp", ident_f[:], ident_d, [], [r_const])
        DMA("sp", umask_f[:], umask_d, [], [r_const])
        DMA("sp", lmask_f[:], lmask_d, [], [r_const])
        DMA("pool", cs64[:], cs64_d, [], [r_const])
        DMA("sp", cv_f[:], cvec, [], [r_const])
        DMA("sp", bada_s[:], bada.rearrange("l p n -> p l n"), [], [r_const])
        DMA("sp", gv[:], gvec.rearrange("l p a k -> p l a k"), [], [r_const])
        CP("dve", ident_b[:], ident_f[:], [r_const], [r_const])
        TS("dve", negm_f[:], umask_f[:], -1.0, 30000.0, ALU.add, ALU.mult, [r_const], [r_const])
        TS("dve", negm_b[:], lmask_f[:], -1.0, 30000.0, ALU.add, ALU.mult, [r_const], [r_const])
        MS("dve", ones_f[:], 1.0, [r_const])
        MS("dve", ones_b[:], 1.0, [r_const])
        MS("dve", cst[:, 0:1], 1024.0 * EPS, [r_const])
        MS("dve", cst[:, 1:2], EPS, [r_const])
        MS("dve", cst[:, 2:3], 1.0, [r_const])
        MS("dve", cst[:, 3:4], 0.0, [r_const])

        for kc in range(KC):
            DMA("sp", xs[:, kc, CTX:NT], xT[kc], [], trs(r_xs, kc, CTX, NT))
            DMA("sp", xs[:, kc, 0:CTX], ctxT[kc], [], trs(r_xs, kc, 0, CTX))
        pos_buf = [arena(0, SEQ, F32), arena(2 * SEQ, SEQ, F32)]
        r_pos = [R(), R()]
        for kc in range(KC):
            i = kc % 2
            DMA("sp", pos_buf[i], posT[kc], [], [r_pos[i]])
            TT("dve", xs[:, kc, CTX:NT], xs[:, kc, CTX:NT], pos_buf[i], ALU.add,
               [r_pos[i]] + trs(r_xs, kc, CTX, NT), trs(r_xs, kc, CTX, NT))

        ACT(sc_b[:], cv_f[:], AF.Silu, [r_const], [r_const])
        wa_off = 4 * SEQ
        wa_buf = [arena(wa_off + i * KC * 512, KC * 512).rearrange("p (k c) -> p k c", c=512) for i in range(2)]
        r_wa = [R(), R()]
        for l in range(DEPTH):
            pi = next_ps()
            pv = psb[pi][:, 0:96].rearrange("p (n w) -> p n w", w=2)
            for blk in range(12):
                i = (l * 12 + blk) % 2
                DMA("pool", wa_buf[i], wada[l, blk], [], [r_wa[i]])
                for n4 in range(4):
                    n = blk * 4 + n4
                    for kc in range(KC):
                        MM(pv[:, n, :], wa_buf[i][:, kc, n4 * 128:(n4 + 1) * 128], sc_b[:, kc, :], kc == 0, kc == KC - 1,
                           [r_wa[i], r_const], [r_ps[pi]])
            TT("dve", mod[:, l], pv, bada_s[:, l].unsqueeze(2).to_broadcast([128, 48, 2]), ALU.add,
               [r_ps[pi], r_const], [r_mod])

        def rstd_of(src_fn, n, reads):
            pi = next_ps()
            for kc in range(KC):
                si = next_sq()
                ACT(sqb[si][:, 0:n], src_fn(kc), AF.Square, reads(kc), [r_sqb[si]])
                MM(psb[pi][:, 0:n], ones_b[:], sqb[si][:, 0:n], kc == 0, kc == KC - 1, [r_sqb[si], r_const], [r_ps[pi]])
            ACT(rs[:, 0:n], psb[pi][:, 0:n], AF.Sqrt, [r_ps[pi], r_const], [r_rs], bias=cst[:, 0:1], scale=1.0)
            RECIP(rs[:, 0:n], rs[:, 0:n], [r_rs], [r_rs])

        def prenorm(tl, Amod, shift_base, l):
            for (a, b, w) in tl:
                n = b - a
                rstd_of(lambda kc: xs[:, kc, a:b], n, lambda kc: trs(r_xs, kc, a, b))
                for kc in range(KC):
                    ti = next_tf()
                    TT("dve", tmpf[ti][:, 0:n], xs[:, kc, a:b], rs[:, 0:n], ALU.mult,
                       trs(r_xs, kc, a, b) + [r_rs], [r_tmpf[ti]])
                    ACT(hx[:, kc, a:b], tmpf[ti][:, 0:n], AF.Identity, [r_tmpf[ti], r_lay, r_mod], trs(r_hx, kc, a, b),
                        bias=mod[:, l, shift_base + kc, w:w + 1], scale=Amod[:, kc, w:w + 1])

        def resid_update(tl, src_fn, src_reads, Gm):
            for (a, b, w) in tl:
                n = b - a
                rstd_of(lambda kc: src_fn(kc, a, b), n, lambda kc: src_reads(kc, a, b))
                for kc in range(KC):
                    ti = next_tf()
                    TT("dve", tmpf[ti][:, 0:n], src_fn(kc, a, b), rs[:, 0:n], ALU.mult,
                       src_reads(kc, a, b) + [r_rs], [r_tmpf[ti]])
                    STT("dve", xs[:, kc, a:b], tmpf[ti][:, 0:n], Gm[:, kc, w:w + 1], xs[:, kc, a:b], ALU.mult, ALU.add,
                        [r_tmpf[ti], r_lay] + trs(r_xs, kc, a, b), trs(r_xs, kc, a, b))

        def dump(name, src_fn, reads_fn):
            if not dbg:
                return
            for kc in range(KC):
                DMA("pool", dbg_out[name][kc], src_fn(kc), reads_fn(kc), [])

        def maybe_stop(k):
            if stop is not None and stop == k:
                raise _Stop()

        try:
            P.barrier()
            for l in range(DEPTH):
                need_ctx = l < DEPTH - 1
                TL_all = tiles_for(True)
                TL = tiles_for(need_ctx)
                j0 = 0 if need_ctx else 2

                def mk(dst, gidx, mbase, plus1):
                    for w in range(2):
                        if plus1:
                            STT("dve", dst[:, :, w], mod[:, l, mbase:mbase + KC, w], 1.0, gv[:, l, gidx, :], ALU.add, ALU.mult,
                                [r_mod, r_const, r_lay], [r_lay])
                        else:
                            TT("dve", dst[:, :, w], mod[:, l, mbase:mbase + KC, w], gv[:, l, gidx, :], ALU.mult,
                               [r_mod, r_const, r_lay], [r_lay])
                    TS("dve", dst[:], dst[:], 32.0, None, ALU.mult, None, [r_lay], [r_lay])

                mk(A1, 0, 8, True)
                mk(G1, 1, 16, False)
                mk(A2, 2, 32, True)
                mk(G2, 3, 40, False)

                maybe_stop(10 * l + 1)

                o_w = 0
                wbuf = [arena(o_w + i * 1024, 1024).rearrange("p (k c) -> p k c", c=128) for i in range(4)]
                r_wbuf = [R() for _ in range(4)]
                wcnt = {"n": 0}

                def load_w(src):
                    i = wcnt["n"] % 4
                    wcnt["n"] += 1
                    DMA("pool", wbuf[i], src, [], [r_wbuf[i]])
                    return i

                o_up = 4096
                upad = arena(o_up, 2 * UPW).rearrange("p (c t) -> p c t", t=UPW)
                r_up = R()
                o_vb = o_up + 2 * UPW
                vbuf = arena(o_vb, 2 * NT, F32).rearrange("p (c t) -> p c t", t=NT)
                r_vb = [R(), R()]
                o_cs = o_vb + 4 * NT
                wdw_s = arena(o_cs, 2 * CONV_K, F32).rearrange("p (c k) -> p c k", k=CONV_K)
                cvp_s = arena(o_cs + 4 * CONV_K, 6, F32).rearrange("p (a c) -> p a c", c=2)
                r_cs = R()
                lnt = [ym_flat[:, i * 1024:(i + 1) * 1024].bitcast(F32) for i in range(5)]
                r_lnt = [R() for _ in range(5)]
                dgm = ym_flat[:, 5120:5120 + 2 * CONV_K * 128].rearrange("p (c k m) -> p c k m", k=CONV_K, m=128)
                r_dg = R()

                DMA("sp", wdw_s, wdw[l], [], [r_cs])
                DMA("sp", cvp_s, cvp[l], [], [r_cs])
                MS("dve", upad, 0.0, [r_up])
                for cc in range(2):
                    for k in range(CONV_K):
                        TS("dve", dgm[:, cc, k, :], ident_f[:], wdw_s[:, cc, k:k + 1], None, ALU.mult, None,
                           [r_const, r_cs], [r_dg])

                def upos(a):
                    return a + PADC if a < CTX else (CTX + 2 * PADC) + (a - CTX) + PADC

                iag = [(load_w(winc[l, 10 + cc]), load_w(winc[l, 12 + cc])) for cc in range(2)]
                for (a, b, w) in TL:
                    prenorm([(a, b, w)], A1, 0, l)
                    for cc in range(2):
                        ia, ig = iag[cc]
                        n = b - a
                        pa = next_ps()
                        for kc in range(KC):
                            MM(psb[pa][:, 0:n], wbuf[ia][:, kc, :], hx[:, kc, a:b], kc == 0, kc == KC - 1,
                               [r_wbuf[ia]] + trs(r_hx, kc, a, b), [r_ps[pa]])
                        pg = next_ps()
                        for kc in range(KC):
                            MM(psb[pg][:, 0:n], wbuf[ig][:, kc, :], hx[:, kc, a:b], kc == 0, kc == KC - 1,
                               [r_wbuf[ig]] + trs(r_hx, kc, a, b), [r_ps[pg]])
                        ti = next_tf()
                        ACT(tmpf[ti][:, 0:n], psb[pg][:, 0:n], AF.Sigmoid, [r_ps[pg]], [r_tmpf[ti]])
                        TT("dve", upad[:, cc, upos(a):upos(a) + n], psb[pa][:, 0:n], tmpf[ti][:, 0:n], ALU.mult,
                           [r_ps[pa], r_tmpf[ti]], [r_up])
                if not need_ctx:
                    prenorm([(0, 256, 1)], A1, 0, l)
                if l == 0:
                    dump("d_hx", lambda kc: hx[:, kc, :], lambda kc: trs(r_hx, kc, 0, NT))
                for cc in range(2):
                    for (a, b, w) in TL:
                        n = b - a
                        pi = next_ps()
                        p0 = upos(a) - PADC
                        for k in range(CONV_K):
                            MM(psb[pi][:, 0:n], dgm[:, cc, k, :], upad[:, cc, p0 + k:p0 + k + n], k == 0, k == CONV_K - 1,
                               [r_dg, r_up], [r_ps[pi]])
                        ACT(vbuf[:, cc, a:b], psb[pi][:, 0:n], AF.Identity, [r_ps[pi], r_cs], [r_vb[cc]],
                            bias=cvp_s[:, 0, cc:cc + 1], scale=1.0)
                for (a, b, w) in TL:
                    n = b - a
                    p1 = next_ps()
                    p2 = next_ps()
                    for cc in range(2):
                        MM(psb[p1][:, 0:n], ones_f[:], vbuf[:, cc, a:b], cc == 0, cc == 1, [r_const, r_vb[cc]], [r_ps[p1]])
                    for cc in range(2):
                        TT("dve", lnt[0][:, 0:n], vbuf[:, cc, a:b], vbuf[:, cc, a:b], ALU.mult, [r_vb[cc]], [r_lnt[0]])
                        MM(psb[p2][:, 0:n], ones_f[:], lnt[0][:, 0:n], cc == 0, cc == 1, [r_const, r_lnt[0]], [r_ps[p2]])
                    mean = lnt[1]
                    TS("dve", mean[:, 0:n], psb[p1][:, 0:n], 1.0 / 256.0, None, ALU.mult, None, [r_ps[p1]], [r_lnt[1]])
                    TT("dve", lnt[2][:, 0:n], mean[:, 0:n], mean[:, 0:n], ALU.mult, [r_lnt[1]], [r_lnt[2]])
                    STT("dve", lnt[2][:, 0:n], psb[p2][:, 0:n], 1.0 / 256.0, lnt[2][:, 0:n], ALU.mult, ALU.subtract,
                        [r_ps[p2], r_lnt[2]], [r_lnt[2]])
                    ACT(lnt[2][:, 0:n], lnt[2][:, 0:n], AF.Sqrt, [r_lnt[2], r_const], [r_lnt[2]], bias=cst[:, 1:2], scale=1.0)
                    RECIP(lnt[2][:, 0:n], lnt[2][:, 0:n], [r_lnt[2]], [r_lnt[2]])
                    for cc in range(2):
                        TT("dve", lnt[3 + cc][:, 0:n], vbuf[:, cc, a:b], mean[:, 0:n], ALU.subtract,
                           [r_vb[cc], r_lnt[1]], [r_lnt[3 + cc]])
                        TT("dve", lnt[3 + cc][:, 0:n], lnt[3 + cc][:, 0:n], lnt[2][:, 0:n], ALU.mult,
                           [r_lnt[3 + cc], r_lnt[2]], [r_lnt[3 + cc]])
                        ACT(ym[:, 6 + cc, a:b], lnt[3 + cc][:, 0:n], AF.Silu, [r_lnt[3 + cc], r_cs], trs(r_ym, 6 + cc, a, b),
                            bias=cvp_s[:, 2, cc:cc + 1], scale=cvp_s[:, 1, cc:cc + 1])
                P.barrier()

                maybe_stop(10 * l + 2)
                o_uf = 4096
                uF = arena(o_uf, 2 * NT).rearrange("p (c t) -> p c t", t=NT)
                r_uf = [R(), R()]
                o_dp = o_uf + 2 * NT
                dpc = [arena(o_dp + i * 4096, 4096).rearrange("p (s k t) -> p s k t", s=2, k=4) for i in range(3)]
                r_dpc = [R() for _ in range(3)]
                o_dc = o_dp + 3 * 4096
                dcc = arena(o_dc, 1024).rearrange("p (s k t) -> p s k t", s=2, k=2)
                r_dcc = R()
                AB = ym_flat[:, 0:NJ * 512].rearrange("p (j c m) -> p j c m", c=2, m=256)
                r_ab = [R() for _ in range(NJ)]

                for cc in range(2):
                    iw = load_w(winc[l, 8 + cc])
                    for (a, b, w) in TL:
                        n = b - a
                        pi = next_ps()
                        for kc in range(KC):
                            MM(psb[pi][:, 0:n], wbuf[iw][:, kc, :], hx[:, kc, a:b], kc == 0, kc == KC - 1,
                               [r_wbuf[iw]] + trs(r_hx, kc, a, b), [r_ps[pi]])
                        ACT(uF[:, cc, a:b], psb[pi][:, 0:n], AF.Copy, [r_ps[pi]], [r_uf[cc]])
                for j in range(j0, NJ):
                    pi = next_ps()
                    for cc in range(2):
                        MM(psb[pi][:, cc * 256:(cc + 1) * 256], uF[:, cc, j * 128:(j + 1) * 128], cs64[:], True, True,
                           [r_uf[cc], r_const], [r_ps[pi]])
                    CP("dve", AB[:, j].rearrange("p c m -> p (c m)"), psb[pi][:, :], [r_ps[pi]], [r_ab[j]])
                npc = 0
                for tq in range(4):
                    pp = [next_ps(), next_ps()]
                    for kg in range(4):
                        i = npc % 3
                        npc += 1
                        DMA("sp", dpc[i].rearrange("p s k t -> p (s k t)"),
                            dftx_d[tq, kg].rearrange("p s k t -> p (s k t)"), [], [r_dpc[i]])
                        for ki in range(4):
                            j = 2 + kg * 4 + ki
                            for s in range(2):
                                first = (kg == 0 and ki == 0 and s == 0)
                                last = (kg == 3 and ki == 3 and s == 1)
                                for cc in range(2):
                                    MM(psb[pp[cc]][:, :], AB[:, j, cc, s * 128:(s + 1) * 128], dpc[i][:, s, ki, :], first, last,
                                       [r_ab[j], r_dpc[i]], [r_ps[pp[cc]]])
                    for cc in range(2):
                        a = CTX + tq * 512
                        ACT(ym[:, 4 + cc, a:a + 512], psb[pp[cc]][:, :], AF.Copy, [r_ps[pp[cc]]], trs(r_ym, 4 + cc, a, a + 512))
                if need_ctx:
                    DMA("sp", dcc.rearrange("p s k t -> p (s k t)"), dftc_d.rearrange("p s k t -> p (s k t)"), [], [r_dcc])
                    pp = [next_ps(), next_ps()]
                    for ki in range(2):
                        for s in range(2):
                            for cc in range(2):
                                MM(psb[pp[cc]][:, 0:256], AB[:, ki, cc, s * 128:(s + 1) * 128], dcc[:, s, ki, :],
                                   ki == 0 and s == 0, ki == 1 and s == 1, [r_ab[ki], r_dcc], [r_ps[pp[cc]]])
                    for cc in range(2):
                        ACT(ym[:, 4 + cc, 0:CTX], psb[pp[cc]][:, 0:256], AF.Copy, [r_ps[pp[cc]]], trs(r_ym, 4 + cc, 0, CTX))

                P.barrier()
                maybe_stop(10 * l + 3)
                o_s = 0
                dgq = [arena(o_s + i * 256, 128, F32) for i in range(2)]
                qs = [arena(o_s + 512 + i * 128, 128) for i in range(2)]
                sTt = [arena(o_s + 768 + i * 128, 128) for i in range(2)]
                kts = [arena(o_s + 1024 + i * 128, 128) for i in range(2)]
                Et = [arena(o_s + 1280 + i * 128, 128) for i in range(2)]
                Cst = [arena(o_s + 1536 + i * 260, 130, F32) for i in range(2)]
                Cbf = [arena(o_s + 2056 + i * 130, 130) for i in range(2)]
                rden = [arena(o_s + 2316 + i * 4, 2, F32) for i in range(2)]
                st1 = arena(o_s + 2324, 2 * NJ, F32)
                st2 = arena(o_s + 2396, 2 * NJ, F32)
                r_dgq, r_qs, r_sT, r_kts = [R(), R()], [R(), R()], [R(), R()], [R(), R()]
                r_dgb, r_Et = [R(), R()], [R(), R()]
                r_C, r_Cb, r_rden = [R(), R()], [R(), R()], [R(), R()]
                r_st = R()
                wqk_raw = arena(2468, 1024).rearrange("p (k c) -> p k c", c=128)
                r_wqk = R()
                dg3 = arena(3492, 384).rearrange("p (t c) -> p t c", c=128)
                wtapc = arena(3876, 4, F32)[:, 0:3]
                zraw = arena(3884, 2308)
                r_w3, r_wtap, r_zraw = R(), R(), R()
                o_g = 7332
                Gtok = arena(o_g, NJ * 16, F32).rearrange("p (j g) -> p j g", g=16)
                dgb = [arena(o_g + i * 256, 128, F32) for i in range(2)]
                LF = arena(o_g + 576, NJ * 8, F32).rearrange("p (j g) -> p j g", g=8)
                CM = arena(o_g + 864, NJ * 8, F32).rearrange("p (j g) -> p j g", g=8)
                Bc = arena(o_g + 1152, NJ * 8, F32).rearrange("p (j g) -> p j g", g=8)
                EB = arena(o_g + 1440, NJ * 8, F32).rearrange("p (j g) -> p j g", g=8)
                EBL = arena(o_g + 1728, NJ * 8, F32).rearrange("p (j g) -> p j g", g=8)
                ECL = arena(o_g + 2016, NJ * 8, F32).rearrange("p (j g) -> p j g", g=8)
                bg_s = arena(o_g + 2304, 16, F32)
                wg_s = arena(o_g + 2336, KC * 16).rearrange("p (k g) -> p k g", g=16)
                gmn_s = arena(o_g + 2464, 512, F32)
                r_gate = R()
                o_h = o_g + 3488
                qT = arena(o_h, NT)
                kT = arena(o_h + NT, NT)
                vaug = arena(o_h + 2 * NT, NJ * 130).rearrange("p (j e) -> p j e", e=130)
                sgo = arena(o_h + 2 * NT + 2340, NT).rearrange("p (j e) -> p j e", e=128)
                hsum = arena(o_h + 3 * NT + 2340, NT, F32).rearrange("p (j e) -> p j e", e=128)
                assert o_h + 5 * NT + 2340 <= SCR_ARENA, (o_h + 5 * NT + 2340)
                r_q, r_k, r_v, r_sg, r_hs = R(), R(), R(), R(), R()
                wvo_s = ym_flat[:, 3 * NT:3 * NT + KC * 256].rearrange("p (k c) -> p k c", c=256)
                r_wvo = R()

                DMA("pool", wg_s, wgate[l], [], [r_gate])
                DMA("sp", bg_s, bgate[l].partition_broadcast(128), [], [r_gate])
                DMA("sp", gmn_s, gmn[l].partition_broadcast(128), [], [r_gate])
                for jg in range(0, NJ, 6):
                    pi = next_ps()
                    for jj in range(6):
                        j = jg + jj
                        for kc in range(KC):
                            MM(psb[pi][:, jj * 16:(jj + 1) * 16], hx[:, kc, j * 128:(j + 1) * 128], wg_s[:, kc, :], kc == 0, kc == KC - 1,
                               [r_gate] + trs(r_hx, kc, j * 128, (j + 1) * 128), [r_ps[pi]])
                    TT("dve", Gtok[:, jg:jg + 6, :], psb[pi][:, 0:96].rearrange("p (j g) -> p j g", g=16),
                       bg_s.unsqueeze(1).to_broadcast([128, 6, 16]), ALU.add, [r_ps[pi], r_gate], [r_gate])
                CP("dve", CM[:, :, 0:4], Gtok[:, :, 0:4], [r_gate], [r_gate])
                CP("dve", CM[:, :, 4:8], Gtok[:, :, 8:12], [r_gate], [r_gate])
                ACT(LF[:, :, 0:4], Gtok[:, :, 4:8], AF.Abs, [r_gate], [r_gate])
                ACT(LF[:, :, 4:8], Gtok[:, :, 12:16], AF.Abs, [r_gate], [r_gate])
                ACT(LF[:], LF[:], AF.Exp, [r_gate], [r_gate], scale=-1.0)
                ACT(LF[:], LF[:], AF.Ln, [r_gate, r_const], [r_gate], bias=cst[:, 2:3], scale=1.0)
                TS("dve", EB[:, :, 0:4], Gtok[:, :, 4:8], 0.0, None, ALU.min, None, [r_gate], [r_gate])
                TS("dve", EB[:, :, 4:8], Gtok[:, :, 12:16], 0.0, None, ALU.min, None, [r_gate], [r_gate])
                TT("dve", LF[:], EB[:], LF[:], ALU.subtract, [r_gate], [r_gate])
                pi = next_ps()
                pbv = psb[pi][:, 0:NJ * 8].rearrange("p (j g) -> p j g", g=8)
                for j in range(NJ):
                    MM(pbv[:, j, 0:4], umask_f[:], LF[:, j, 0:4], True, True, [r_const, r_gate], [r_ps[pi]])
                    MM(pbv[:, j, 4:8], lmask_f[:], LF[:, j, 4:8], True, True, [r_const, r_gate], [r_ps[pi]])
                CP("dve", Bc[:], pbv, [r_ps[pi]], [r_gate])
                pi = next_ps()
                ptv = psb[pi][:, 0:NJ * 8].rearrange("p (j g) -> p j g", g=8)
                MM(psb[pi][:, 0:NJ * 8], ones_f[:], LF[:].rearrange("p j g -> p (j g)"), True, True, [r_const, r_gate], [r_ps[pi]])
                ACT(EBL[:], ptv, AF.Exp, [r_ps[pi]], [r_gate])
                ACT(EB[:], Bc[:], AF.Exp, [r_gate], [r_gate])
                TT("dve", CM[:], CM[:], Bc[:], ALU.subtract, [r_gate], [r_gate])
                TT("dve", ECL[:], CM[:], ptv, ALU.add, [r_gate, r_ps[pi]], [r_gate])
                ACT(ECL[:], ECL[:], AF.Exp, [r_gate], [r_gate])
                P.barrier()

                MS("dve", vaug[:, :, 128:129], 1.0, [r_v])
                MS("dve", zraw, 0.0, [r_zraw])

                def head_qk(h):
                    for qk in range(2):
                        dst, r_dst = (qT, r_q) if qk == 0 else (kT, r_k)
                        ci = qk * 4 + h
                        DMA("pool", wqk_raw, winc[l, ci], [], [r_wqk])
                        DMA("sp", wtapc, wqkp[l, ci], [], [r_wtap])
                        if qk == 1:
                            TS("dve", wtapc, wtapc, HD ** -0.5, None, ALU.mult, None, [r_wtap], [r_wtap])
                        for t in range(3):
                            TS("dve", dg3[:, t, :], ident_f[:], wtapc[:, t:t + 1], None, ALU.mult, None,
                               [r_const, r_wtap], [r_w3])

                        def zpos(a_):
                            return 1 + a_ if a_ < CTX else 3 + a_
                        for (a, b, w) in TL_all:
                            n = b - a
                            pi = next_ps()
                            for kc in range(KC):
                                MM(psb[pi][:, 0:n], wqk_raw[:, kc, :], hx[:, kc, a:b], kc == 0, kc == KC - 1,
                                   [r_wqk] + trs(r_hx, kc, a, b), [r_ps[pi]])
                            ACT(zraw[:, zpos(a):zpos(a) + n], psb[pi][:, 0:n], AF.Copy, [r_ps[pi]], [r_zraw])
                        for (a, b, w) in TL_all:
                            n = b - a
                            pi = next_ps()
                            for t in range(3):
                                MM(psb[pi][:, 0:n], dg3[:, t, :], zraw[:, zpos(a) + t - 1:zpos(a) + t - 1 + n], t == 0, t == 2,
                                   [r_w3, r_zraw], [r_ps[pi]])
                            ACT(dst[:, a:b], psb[pi][:, 0:n], AF.Copy, [r_ps[pi]], [r_dst])
                def head_vo(h):
                    DMA("pool", wvo_s, wvo[l, h], [], [r_wvo])
                    for j in range(NJ):
                        pi = next_ps()
                        for kc in range(KC):
                            MM(psb[pi][:, 0:256], hx[:, kc, j * 128:(j + 1) * 128], wvo_s[:, kc, :], kc == 0, kc == KC - 1,
                               [r_wvo] + trs(r_hx, kc, j * 128, (j + 1) * 128), [r_ps[pi]])
                        CP("dve", vaug[:, j, 0:128], psb[pi][:, 0:128], [r_ps[pi]], [r_v])
                        ACT(sgo[:, j, :], psb[pi][:, 128:256], AF.Sigmoid, [r_ps[pi]], [r_sg])
                        TT("dve", sgo[:, j, :], sgo[:, j, :], gmn_s[:, h * 128:(h + 1) * 128], ALU.mult, [r_sg, r_gate], [r_sg])
                def head_loop(h):
                    MS("dve", hsum, 0.0, [r_hs])
                    for d_ in range(2):
                        MS("dve", Cst[d_], 0.0, [r_C[d_]])
                        MS("dve", Cbf[d_], 0.0, [r_Cb[d_]])
                    order = [list(range(NJ)), [1, 0] + list(range(NJ - 1, 1, -1))]
                    t0f = tmpf[0]
                    t1b = tmpf[1][:, :].bitcast(BF16)
                    dgb2 = [dgb, [dgq[0], dgq[1]]]
                    sT2 = [sTt, [t1b[:, 256:384], t1b[:, 384:512]]]
                    kts2 = [kts, [t1b[:, 512:640], t1b[:, 640:768]]]
                    Et2 = [Et, [t1b[:, 768:896], t1b[:, 896:1024]]]
                    ndi = [t0f[:, 0:130], t0f[:, 130:260]]
                    ndi_den = t0f[:, 0:260].rearrange("p (d e) -> p d e", e=130)[:, :, 128:129]
                    rden_j = t0f[:, 392:394].unsqueeze(2)
                    r_rdj = R()
                    r_ndi = [R(), R()]
                    t0b = t0f[:, 260:390].bitcast(BF16)
                    Cbf2 = [Cbf, [t0b[:, 0:130], t0b[:, 130:260]]]
                    r_Cb2 = [r_Cb, [R(), R()]]
                    rr = lambda: [[R(), R()], [R(), R()]]
                    r_dgb2, r_sT2, r_kts2, r_Et2 = rr(), rr(), rr(), rr()

                    def info(step, d_):
                        j = order[d_][step]
                        return j, step % 2, d_ * 4 + h, (need_ctx or j >= 2), step == NJ - 1

                    ps_hold = {}

                    def a1(step, d_):
                        j, bs, col, need_out, is_last = info(step, d_)
                        if need_out:
                            TS("dve", dgb2[bs][d_], ident_f[:], Bc[:, j, col:col + 1], None, ALU.mult, None,
                               [r_const, r_gate], [r_dgb2[bs][d_]])
                        yield

                    def a2a(step, d_):
                        j, bs, col, need_out, is_last = info(step, d_)
                        c0, c1 = j * 128, (j + 1) * 128
                        if need_out:
                            pd_ = d_
                            MM(psb[pd_][:, 0:128], ones_f[:], dgb2[bs][d_], True, False, [r_const, r_dgb2[bs][d_]], [r_ps[pd_]])
                            MM(psb[pd_][:, 0:128], ident_b[:], (negm_f if d_ == 0 else negm_b)[:], False, True,
                               [r_const], [r_ps[pd_]])
                            yield
                            ACT(Et2[bs][d_], psb[pd_][:, 0:128], AF.Exp, [r_ps[pd_], r_gate], [r_Et2[bs][d_]],
                                bias=CM[:, j, col:col + 1], scale=1.0)
                            yield
                            p_s = 2 + d_
                            MM(psb[p_s][:, 0:128], kT[:, c0:c1], qT[:, c0:c1], True, True, [r_k, r_q], [r_ps[p_s]])
                            yield
                        if not is_last:
                            p_t = next_ps()
                            ptb = psb[p_t][:, 0:64].bitcast(BF16)
                            TR(ptb, kT[:, c0:c1], ident_b[:], [r_k, r_const], [r_ps[p_t]])
                            yield
                            ACT(kts2[bs][d_], ptb, AF.Copy, [r_ps[p_t], r_gate], [r_kts2[bs][d_]], scale=ECL[:, j, col:col + 1])
                            yield

                    def a2b(step, d_):
                        j, bs, col, need_out, is_last = info(step, d_)
                        if need_out:
                            p_s = 2 + d_
                            TT("dve", sT2[bs][d_], psb[p_s][:, 0:128], Et2[bs][d_], ALU.mult,
                               [r_ps[p_s], r_Et2[bs][d_]], [r_sT2[bs][d_]])
                        yield

                    def stage_b(step, d_):
                        j, bs, col, need_out, is_last = info(step, d_)
                        cur, nxt = Cbf2[step % 2][d_], Cbf2[(step + 1) % 2][d_]
                        r_cur, r_nxt = r_Cb2[step % 2][d_], r_Cb2[(step + 1) % 2][d_]
                        if not is_last:
                            p_u = next_ps()
                            MM(psb[p_u][:, 0:129], kts2[bs][d_], vaug[:, j, 0:129], True, True, [r_kts2[bs][d_], r_v], [r_ps[p_u]])
                            yield
                            STT("dve", Cst[d_][:, 0:129], Cst[d_][:, 0:129], EBL[:, j, col:col + 1], psb[p_u][:, 0:129],
                                ALU.mult, ALU.add, [r_C[d_], r_gate, r_ps[p_u]], [r_C[d_]])
                            yield
                            ACT(nxt[:, 0:129], Cst[d_][:, 0:129], AF.Copy, [r_C[d_]], [r_nxt])
                            yield

                    def stage_bo(step, d_):
                        j, bs, col, need_out, is_last = info(step, d_)
                        cur, r_cur = Cbf2[step % 2][d_], r_Cb2[step % 2][d_]
                        if need_out:
                            c0, c1 = j * 128, (j + 1) * 128
                            p_i = next_ps()
                            MM(psb[p_i][:, 0:129], qT[:, c0:c1], cur[:, 0:129], True, True, [r_q, r_cur], [r_ps[p_i]])
                            p_n = next_ps()
                            MM(psb[p_n][:, 0:129], sT2[bs][d_], vaug[:, j, 0:129], True, True, [r_sT2[bs][d_], r_v], [r_ps[p_n]])
                            yield
                            ACT(ndi[d_][:, 0:129], psb[p_i][:, 0:129], AF.Copy, [r_ps[p_i], r_gate], [r_ndi[d_]], scale=EB[:, j, col:col + 1])
                            yield
                            TT("dve", ndi[d_][:, 0:129], ndi[d_][:, 0:129], psb[p_n][:, 0:129], ALU.add, [r_ndi[d_], r_ps[p_n]], [r_ndi[d_]])
                            yield

                    def den_ops(step):
                        j, bs, col, need_out, is_last = info(step, 0)
                        if need_out:
                            STT("dve", rden_j, ndi_den, -1.0, ndi_den, ALU.mult, ALU.max, [r_ndi[0], r_ndi[1]], [r_rdj])
                            TS("dve", rden_j, rden_j, 1.0, None, ALU.max, None, [r_rdj], [r_rdj])
                            RECIP(rden_j, rden_j, [r_rdj], [r_rdj])

                    def stage_b2(step, d_):
                        j, bs, col, need_out, is_last = info(step, d_)
                        if need_out:
                            STT("dve", hsum[:, j, :], ndi[d_][:, 0:128], t0f[:, 392 + d_:393 + d_], hsum[:, j, :], ALU.mult, ALU.add,
                                [r_ndi[d_], r_rdj, r_hs], [r_hs])
                        yield

                    def interleave(*gens):
                        gens = list(gens)
                        while gens:
                            for g in list(gens):
                                try:
                                    next(g)
                                except StopIteration:
                                    gens.remove(g)

                    for bnk in range(4):
                        ps_reserved.add(bnk)
                    interleave(a1(0, 0), a1(0, 1))
                    interleave(a2a(0, 0), a2a(0, 1))
                    interleave(a2b(0, 0), a2b(0, 1))
                    interleave(a1(1, 0), a1(1, 1))
                    for step in range(NJ):
                        if step + 2 < NJ:
                            interleave(a1(step + 2, 0), a1(step + 2, 1))
                        if step + 1 < NJ:
                            interleave(a2a(step + 1, 0), a2a(step + 1, 1))
                        interleave(stage_b(step, 0), stage_b(step, 1))
                        if step + 1 < NJ:
                            interleave(a2b(step + 1, 0), a2b(step + 1, 1))
                        interleave(stage_bo(step, 0), stage_bo(step, 1))
                        den_ops(step)
                        interleave(stage_b2(step, 0), stage_b2(step, 1))
                    for bnk in range(4):
                        ps_reserved.discard(bnk)
                def head_out(h):
                    nj = NJ - j0
                    hv = hsum[:, j0:NJ, :]
                    sqv = ym[:, h, j0 * 128:NT].rearrange("p (j e) -> p j e", e=128)
                    ymh_res = trs(r_ym, h, j0 * 128, NT) + ([r_wvo] if h == 3 else [])
                    P.op("dve", (lambda hv=hv, nj=nj: lambda e: e.tensor_reduce(out=st1[:, 0:nj], in_=hv, axis=AX.X, op=ALU.add))(),
                         [r_hs], [r_st])
                    TT("dve", sqv, hv, hv, ALU.mult, [r_hs], ymh_res)
                    P.op("dve", (lambda sqv=sqv, nj=nj: lambda e: e.tensor_reduce(out=st1[:, NJ:NJ + nj], in_=sqv, axis=AX.X, op=ALU.add))(),
                         ymh_res, [r_st])
                    mean_ = st2[:, 0:nj]
                    var_ = st2[:, NJ:NJ + nj]
                    TS("dve", mean_, st1[:, 0:nj], 1.0 / HD, None, ALU.mult, None, [r_st], [r_st])
                    TT("dve", var_, mean_, mean_, ALU.mult, [r_st], [r_st])
                    STT("dve", var_, st1[:, NJ:NJ + nj], 1.0 / HD, var_, ALU.mult, ALU.subtract, [r_st], [r_st])
                    ACT(var_, var_, AF.Sqrt, [r_st, r_const], [r_st], bias=cst[:, 1:2], scale=1.0)
                    RECIP(var_, var_, [r_st], [r_st])
                    TT("dve", hv, hv, mean_.unsqueeze(2).to_broadcast([128, nj, 128]), ALU.subtract, [r_hs, r_st], [r_hs])
                    TT("dve", hv, hv, var_.unsqueeze(2).to_broadcast([128, nj, 128]), ALU.mult, [r_hs, r_st], [r_hs])
                    TT("dve", sgo[:, j0:NJ, :], hv, sgo[:, j0:NJ, :], ALU.mult, [r_hs, r_sg], [r_sg])
                    for jg in range(j0, NJ, 4):
                        p_t = next_ps()
                        ptb = psb[p_t][:, 0:256].bitcast(BF16)
                        jn = min(4, NJ - jg)
                        for jj in range(jn):
                            TR(ptb[:, jj * 128:(jj + 1) * 128], sgo[:, jg + jj, :], ident_b[:], [r_sg, r_const], [r_ps[p_t]])
                        ACT(ym[:, h, jg * 128:(jg + jn) * 128], ptb[:, 0:jn * 128], AF.Copy, [r_ps[p_t]],
                            trs(r_ym, h, jg * 128, (jg + jn) * 128) + ([r_wvo] if h == 3 else []))

                head_qk(0)
                head_vo(0)
                for h in range(NH):
                    head_loop(h)
                    if h + 1 < NH:
                        head_qk(h + 1)
                    head_out(h)
                    if h + 1 < NH:
                        head_vo(h + 1)
                if l == 0:
                    dump("d_ym", lambda kc: ym[:, kc, :], lambda kc: trs(r_ym, kc, 0, NT))

                P.barrier()
                maybe_stop(10 * l + 4)
                o_wo = 4096
                wo_s = arena(o_wo, KC * KC * 128).rearrange("p (o k c) -> p o k c", o=KC, k=KC)
                r_wo = R()
                obuf = arena(o_wo + 8192, KC * 512, F32).rearrange("p (o t) -> p o t", t=512)
                r_ob = [R() for _ in range(KC)]
                DMA("pool", wo_s.rearrange("p o k c -> p (o k c)"), woutc[l].rearrange("p o k c -> p (o k c)"), [],
                    [r_wo])
                for (a, b, w) in TL:
                    n = b - a
                    for oc in range(KC):
                        pi = next_ps()
                        for kc in range(KC):
                            MM(psb[pi][:, 0:n], wo_s[:, oc, kc, :], ym[:, kc, a:b], kc == 0, kc == KC - 1,
                               [r_wo] + trs(r_ym, kc, a, b), [r_ps[pi]])
                        ACT(obuf[:, oc, 0:n], psb[pi][:, 0:n], AF.Copy, [r_ps[pi]], [r_ob[oc]])
                    resid_update([(a, b, w)], lambda kc, a_, b_: obuf[:, kc, 0:b_ - a_], lambda kc, a_, b_: [r_ob[kc]], G1)
                if l == 0:
                    dump("d_x1", lambda kc: xs[:, kc, :], lambda kc: trs(r_xs, kc, 0, NT))

                P.barrier()
                maybe_stop(10 * l + 5)
                hT = scr[:, 0:24576].rearrange("p (f t) -> p f t", t=768)
                r_hT = [R() for _ in range(32)]
                ob2 = scr[:, 24576:30720].rearrange("p (o t) -> p o t", t=768)
                r_ob2 = [R() for _ in range(KC)]
                w1b = [scr[:, 30720 + i * 2048:30720 + (i + 1) * 2048].rearrange("p (g k c) -> p g k c", g=2, k=KC) for i in range(2)]
                w2b = [scr[:, 34816 + i * 4096:34816 + (i + 1) * 4096].rearrange("p (f c) -> p f c", c=128) for i in range(2)]
                r_w1b = [R(), R()]
                r_w2b = [R(), R()]
                if need_ctx:
                    supers = [[(0, 256, 1), (256, 768, 0)], [(768, 1280, 0), (1280, 1536, 0)], [(1536, 2048, 0), (2048, 2304, 0)]]
                else:
                    supers = [[(256, 768, 0), (768, 1024, 0)], [(1024, 1536, 0), (1536, 1792, 0)], [(1792, 2304, 0)]]
                n1 = 0
                n2 = 0
                prenorm(supers[0], A2, 24, l)
                for isup, sup in enumerate(supers):
                    a0 = sup[0][0]
                    for g in range(16):
                        i = n1 % 2
                        n1 += 1
                        DMA("pool", w1b[i].rearrange("p g k c -> p (g k c)"), w1c[l, g].rearrange("p g k c -> p (g k c)"), [],
                            [r_w1b[i]])
                        for f2 in range(2):
                            f = g * 2 + f2
                            for (a, b, w) in sup:
                                n = b - a
                                pi = next_ps()
                                for kc in range(KC):
                                    MM(psb[pi][:, 0:n], w1b[i][:, f2, kc, :], hx[:, kc, a:b], kc == 0, kc == KC - 1,
                                       [r_w1b[i]] + trs(r_hx, kc, a, b), [r_ps[pi]])
                                ti = next_tf()
                                ACT(tmpf[ti][:, 0:n], psb[pi][:, 0:n], AF.Relu, [r_ps[pi]], [r_tmpf[ti]])
                                TT("dve", hT[:, f, a - a0:b - a0], tmpf[ti][:, 0:n], tmpf[ti][:, 0:n], ALU.mult, [r_tmpf[ti]], [r_hT[f]])
                    if isup + 1 < len(supers):
                        prenorm(supers[isup + 1], A2, 24, l)
                    ssb = []
                    for _ in sup:
                        pss = next_ps()
                        ps_reserved.add(pss)
                        ssb.append(pss)
                    for oc in range(KC):
                        i = n2 % 2
                        n2 += 1
                        DMA("pool", w2b[i].rearrange("p f c -> p (f c)"), w2c[l, oc].rearrange("p f c -> p (f c)"), [],
                            [r_w2b[i]])
                        for si_, (a, b, w) in enumerate(sup):
                            n = b - a
                            pi = next_ps()
                            for f in range(32):
                                MM(psb[pi][:, 0:n], w2b[i][:, f, :], hT[:, f, a - a0:b - a0], f == 0, f == 31,
                                   [r_w2b[i], r_hT[f]], [r_ps[pi]])
                            ACT(ob2[:, oc, a - a0:b - a0], psb[pi][:, 0:n], AF.Copy, [r_ps[pi]], [r_ob2[oc]])
                            sq_i = next_sq()
                            ACT(sqb[sq_i][:, 0:n], psb[pi][:, 0:n], AF.Square, [r_ps[pi]], [r_sqb[sq_i]])
                            MM(psb[ssb[si_]][:, 0:n], ones_b[:], sqb[sq_i][:, 0:n], oc == 0, oc == KC - 1,
                               [r_sqb[sq_i], r_const], [r_ps[ssb[si_]]])
                    for si_, (a, b, w) in enumerate(sup):
                        n = b - a
                        ACT(rs[:, 0:n], psb[ssb[si_]][:, 0:n], AF.Sqrt, [r_ps[ssb[si_]], r_const], [r_rs], bias=cst[:, 0:1], scale=1.0)
                        RECIP(rs[:, 0:n], rs[:, 0:n], [r_rs], [r_rs])
                        for kc in range(KC):
                            ti = next_tf()
                            TT("dve", tmpf[ti][:, 0:n], ob2[:, kc, a - a0:b - a0], rs[:, 0:n], ALU.mult,
                               [r_ob2[kc], r_rs], [r_tmpf[ti]])
                            STT("dve", xs[:, kc, a:b], tmpf[ti][:, 0:n], G2[:, kc, w:w + 1], xs[:, kc, a:b], ALU.mult, ALU.add,
                                [r_tmpf[ti], r_lay] + trs(r_xs, kc, a, b), trs(r_xs, kc, a, b))
                    for pss in ssb:
                        ps_reserved.discard(pss)
                if l == 0:
                    dump("d_x2", lambda kc: xs[:, kc, :], lambda kc: trs(r_xs, kc, 0, NT))
                P.barrier()

        except _Stop:
            pass
        for kc in range(KC):
            DMA("sp", outT[kc], xs[:, kc, CTX:NT], trs(r_xs, kc, CTX, NT), [])
        P.emit()
    return nc


_CACHE = {}


def _consts():
    if "c" in _CACHE:
        return _CACHE["c"]
    c = {}
    c["ident"] = np.eye(128, dtype=np.float32)
    s = np.arange(128)
    c["umask"] = (s[:, None] <= s[None, :]).astype(np.float32)
    c["lmask"] = (s[:, None] >= s[None, :]).astype(np.float32)
    k = np.arange(64)
    ang = 2.0 * np.pi * np.outer(k, k) / 64.0
    cs = np.zeros((128, 256), np.float64)
    for hh in range(2):
        cs[hh * 64:(hh + 1) * 64, hh * 64:(hh + 1) * 64] = np.cos(ang) / 8.0
        cs[hh * 64:(hh + 1) * 64, 128 + hh * 64:128 + (hh + 1) * 64] = np.sin(ang) / 8.0
    c["cs64"] = cs.astype(np.float32)
    t = np.arange(SEQ, dtype=np.int64)
    ph = (np.outer(t, t) % SEQ).astype(np.float64) * (2.0 * np.pi / SEQ)
    dc = (np.cos(ph) / math.sqrt(SEQ)).astype(np.float32)
    ds = (-np.sin(ph) / math.sqrt(SEQ)).astype(np.float32)
    both = np.stack([dc, ds], 0)
    both = both.reshape(2, 4, 4, 128, 4, 512)
    c["dftx"] = np.ascontiguousarray(both.transpose(4, 1, 3, 0, 2, 5)).astype(ml_dtypes.bfloat16)
    t = np.arange(CTX, dtype=np.int64)
    ph = (np.outer(t, t) % CTX).astype(np.float64) * (2.0 * np.pi / CTX)
    both = np.stack([np.cos(ph), -np.sin(ph)], 0) / math.sqrt(CTX)
    both = both.reshape(2, 2, 128, CTX)
    c["dftc"] = np.ascontiguousarray(both.transpose(2, 0, 1, 3)).astype(ml_dtypes.bfloat16)
    rows = SEQ // 64
    quarter = D // 4
    freq = np.exp(-math.log(10000.0) * np.arange(quarter, dtype=np.float32) / quarter).astype(np.float32)
    r = np.broadcast_to(np.arange(rows, dtype=np.float32)[:, None], (rows, 64)).reshape(-1)
    col = np.broadcast_to(np.arange(64, dtype=np.float32)[None, :], (rows, 64)).reshape(-1)
    ar = r[:, None] * freq
    ac = col[:, None] * freq
    pos = np.concatenate([np.sin(ar), np.cos(ar), np.sin(ac), np.cos(ac)], axis=-1).astype(np.float32)
    c["posT"] = np.ascontiguousarray(pos.T).reshape(KC, 128, SEQ)
    _CACHE["c"] = c
    return c


def _chunk_w(w, cols):
    return np.ascontiguousarray(w[:, cols].reshape(KC, 128, -1).transpose(1, 0, 2))


def _prep_shared(inp):
    f = np.float32
    L = DEPTH
    w_in = np.asarray(inp["w_in"], f)
    sh = {}
    wada = np.asarray(inp["w_ada"], f)
    sh["wada"] = np.ascontiguousarray(wada.reshape(L, KC, 128, 12, 512).transpose(0, 3, 2, 1, 4))
    sh["bada"] = np.ascontiguousarray(np.asarray(inp["b_ada"], f).reshape(L, 48, 128).transpose(0, 2, 1))
    gs = np.stack([np.asarray(inp[k], f) for k in ("g_pre_mix", "g_post_mix", "g_pre_mlp", "g_post_mlp")], 1)
    sh["gvec"] = np.ascontiguousarray(gs.reshape(L, 4, KC, 128).transpose(0, 3, 1, 2))
    offs = [Q_OFF + 128 * i for i in range(4)] + [K_OFF + 128 * i for i in range(4)] + \
           [F_OFF, F_OFF + 128, CA_OFF, CA_OFF + 128, CG_OFF, CG_OFF + 128]
    sh["winc"] = np.stack([np.stack([_chunk_w(w_in[l], np.arange(o, o + 128)) for o in offs]) for l in range(L)])
    sh["wvo"] = np.stack([np.stack([_chunk_w(w_in[l], np.concatenate([np.arange(V_OFF + 128 * h, V_OFF + 128 * h + 128),
                                                                      np.arange(O_OFF + 128 * h, O_OFF + 128 * h + 128)]))
                                    for h in range(NH)]) for l in range(L)])
    sh["wgate"] = np.stack([_chunk_w(w_in[l], np.arange(G_OFF, G_OFF + 16)) for l in range(L)])
    sh["bgate"] = np.ascontiguousarray(np.asarray(inp["b_gate"], f).reshape(L, 16))
    wqk = np.asarray(inp["w_qk_conv"], f)
    sh["wqkp"] = np.ascontiguousarray(wqk.reshape(L, 3, 8, 128).transpose(0, 2, 3, 1))
    sh["gmn"] = np.ascontiguousarray(np.asarray(inp["g_mlstm_norm"], f))
    wdw = np.asarray(inp["w_dw"], f)
    sh["wdw"] = np.ascontiguousarray(wdw.reshape(L, CONV_K, 2, 128).transpose(0, 3, 2, 1))
    cv = np.stack([np.asarray(inp[k], f) for k in ("b_dw", "g_conv_ln", "b_conv_ln")], 1)
    sh["cvp"] = np.ascontiguousarray(cv.reshape(L, 3, 2, 128).transpose(0, 3, 1, 2))
    wout = np.asarray(inp["w_out"], f)
    sh["woutc"] = np.ascontiguousarray(wout.reshape(L, KC, 128, KC, 128).transpose(0, 2, 3, 1, 4))
    w1 = np.asarray(inp["w_mlp1"], f)
    sh["w1c"] = np.ascontiguousarray(w1.reshape(L, KC, 128, 16, 2, 128).transpose(0, 3, 2, 4, 1, 5))
    w2 = np.asarray(inp["w_mlp2"], f)
    sh["w2c"] = np.ascontiguousarray(w2.reshape(L, 32, 128, KC, 128).transpose(0, 3, 2, 1, 4))
    c = _consts()
    for k in ("ident", "umask", "lmask", "cs64", "dftx", "dftc", "posT"):
        sh[k] = c[k]
    return sh


def make_in_maps(inp, cores):
    sh = _prep_shared(inp)
    x = np.asarray(inp["x"], np.float32)
    ctx = np.asarray(inp["ctx"], np.float32)
    c = np.asarray(inp["c"], np.float32)
    c_ctx = np.asarray(inp["c_ctx"], np.float32)
    maps = []
    for b in cores:
        m = dict(sh)
        m["xT"] = np.ascontiguousarray(x[b].T).reshape(KC, 128, SEQ)
        m["ctxT"] = np.ascontiguousarray(ctx[b].T).reshape(KC, 128, CTX)
        cv = np.stack([c[b], c_ctx], -1)
        m["cvec"] = np.ascontiguousarray(cv.reshape(KC, 128, 2).transpose(1, 0, 2))
        maps.append(m)
    return maps


def kernel(**inputs):
    if "nc" not in _CACHE:
        _CACHE["nc"] = build_program(dbg=False)
    nc = _CACHE["nc"]
    in_maps = make_in_maps(inputs, list(range(NB)))
    res = run_bass_kernel_spmd(nc, in_maps, core_ids=list(range(NB)))
    out = np.empty((NB, SEQ, D), np.float32)
    for b in range(NB):
        oT = np.asarray(res.results[b]["outT"], np.float32).reshape(D, SEQ)
        out[b] = oT.T
    return out
```

```python
import math
from contextlib import ExitStack

import numpy as np
import ml_dtypes

import concourse.bass as bass
import concourse.mybir as mybir
from concourse.bass_utils import run_bass_kernel_spmd

F32 = mybir.dt.float32
BF16 = mybir.dt.bfloat16
AF = mybir.ActivationFunctionType
ALU = mybir.AluOpType
AX = mybir.AxisListType

ENGS = ("pe", "act", "dve", "pool", "sp")
SEM_LIM = 2000
N_DMA_SEMS = 16


class Res:
    __slots__ = ("name", "w", "r", "excl")

    def __init__(self, name, excl=False):
        self.name = name
        self.w = None
        self.r = []
        self.excl = excl


class Op:
    __slots__ = ("eng", "idx", "fn", "deps", "dma", "signal", "semref", "dma_slot", "dma_val")

    def __init__(self, eng, idx, fn, dma):
        self.eng = eng
        self.idx = idx
        self.fn = fn
        self.deps = []
        self.dma = dma
        self.signal = False
        self.semref = None
        self.dma_slot = None
        self.dma_val = None


class Prog:
    def __init__(self, nc, same_engine_sync=True):
        self.nc = nc
        self.ops = {e: [] for e in ENGS}
        self.n_dma_q = {}
        self.same_engine_sync = same_engine_sync

    def res(self, name="", excl=False):
        return Res(name, excl)

    def op(self, eng, fn, reads=(), writes=(), dma=False):
        o = Op(eng, len(self.ops[eng]), fn, dma)
        if dma:
            half = N_DMA_SEMS // 2
            k = self.n_dma_q.get(eng, 0)
            self.n_dma_q[eng] = k + 1
            o.dma_slot = (k % half) + (half if eng == "pool" else 0)
            o.dma_val = 16 * (k // half + 1)
        deps = []
        for r in reads:
            if r.w is not None:
                deps.append(r.w)
            if r.excl:
                deps.extend(x for x in r.r if x.eng != eng)
        for r in writes:
            if r.w is not None:
                deps.append(r.w)
            deps.extend(r.r)
        for r in reads:
            r.r.append(o)
        for r in writes:
            r.w = o
            r.r = []
        seen = set()
        for d in deps:
            if d is o or id(d) in seen:
                continue
            seen.add(id(d))
            if (not d.dma) and (not dma) and d.eng == eng:
                if eng == "pe" or not self.same_engine_sync:
                    continue
            o.deps.append(d)
        self.ops[eng].append(o)
        return o

    def barrier(self):
        targets = []
        for e in ENGS:
            cs = [o for o in self.ops[e] if not o.dma and o.fn is not None]
            if cs:
                targets.append(cs[-1])
        by_slot = {}
        for e in ENGS:
            for o in self.ops[e]:
                if o.dma and (o.dma_slot not in by_slot or o.dma_val > by_slot[o.dma_slot].dma_val):
                    by_slot[o.dma_slot] = o
        targets += list(by_slot.values())
        for e in ENGS:
            o = Op(e, len(self.ops[e]), None, False)
            o.deps = list(targets) if e != "pe" else [t for t in targets if t.dma or t.eng != e]
            self.ops[e].append(o)

    def emit(self):
        nc = self.nc
        for e in ENGS:
            for o in self.ops[e]:
                for d in o.deps:
                    d.signal = True
        with ExitStack() as st:
            dma_sems = [st.enter_context(nc.semaphore(f"dq{i}")) for i in range(N_DMA_SEMS)]
            for e in ENGS:
                n = sum(1 for o in self.ops[e] if o.signal and not o.dma)
                k = max((n + SEM_LIM - 1) // SEM_LIM, 1)
                sems = [st.enter_context(nc.semaphore(f"s_{e}{i}")) for i in range(k)]
                c = 0
                for o in self.ops[e]:
                    if o.signal and not o.dma:
                        o.semref = (sems[c // SEM_LIM], c % SEM_LIM + 1, c)
                        c += 1
            block = st.enter_context(nc.Block())

            def run(e, eng):
                waited_c = {p: -1 for p in ENGS}
                waited_d = [0] * N_DMA_SEMS
                for o in self.ops[e]:
                    for d in o.deps:
                        if d.dma:
                            if waited_d[d.dma_slot] >= d.dma_val:
                                continue
                            waited_d[d.dma_slot] = d.dma_val
                            eng.wait_ge(dma_sems[d.dma_slot], d.dma_val)
                        else:
                            sem, val, gc = d.semref
                            if waited_c[d.eng] >= gc:
                                continue
                            waited_c[d.eng] = gc
                            eng.wait_ge(sem, val)
                    if o.fn is None:
                        continue
                    if o.dma and o.dma_val > 16 and waited_d[o.dma_slot] < o.dma_val - 16:
                        waited_d[o.dma_slot] = o.dma_val - 16
                        eng.wait_ge(dma_sems[o.dma_slot], o.dma_val - 16)
                    ins = o.fn(eng)
                    if o.dma:
                        ins.then_inc(dma_sems[o.dma_slot], 16)
                    elif o.signal:
                        ins.then_inc(o.semref[0], 1)

            fin_dma = {}
            for e in ENGS:
                for o in self.ops[e]:
                    if o.dma:
                        fin_dma[o.dma_slot] = max(fin_dma.get(o.dma_slot, 0), o.dma_val)

            @block.tensor
            def _(eng):
                run("pe", eng)

            @block.scalar
            def _(eng):
                run("act", eng)

            @block.vector
            def _(eng):
                run("dve", eng)

            @block.gpsimd
            def _(eng):
                run("pool", eng)

            @block.sync
            def _(eng):
                run("sp", eng)
                for slot, val in sorted(fin_dma.items()):
                    eng.wait_ge(dma_sems[slot], val)


D = 1024
NB = 8
SEQ = 2048
CTX = 256
NT = CTX + SEQ
DEPTH = 2
KC = 8
NJ = NT // 128
HD = 128
NH = 4
DFF = 4096
EPS = 1e-6
CONV_K = 31
PADC = 15
Q_OFF, K_OFF, V_OFF, O_OFF, G_OFF = 0, 512, 1024, 1536, 2048
F_OFF = 2064
CA_OFF = 2320
CG_OFF = 2576
UPW = (CTX + 2 * PADC) + (SEQ + 2 * PADC)


def tiles_for(need_ctx):
    t = [(256, 768, 0), (768, 1280, 0), (1280, 1792, 0), (1792, 2304, 0)]
    if need_ctx:
        t = [(0, 256, 1)] + t
    return t


class _Stop(Exception):
    pass


def build_program(dbg=False, stop=None):
    nc = bass.Bass("TRN2", target_bir_lowering=False)
    P = Prog(nc)

    def din(name, shape, dt=F32):
        return nc.dram_tensor(name, list(shape), dt, kind="ExternalInput").ap()

    def dout(name, shape, dt=F32):
        return nc.dram_tensor(name, list(shape), dt, kind="ExternalOutput").ap()

    xT = din("xT", [KC, 128, SEQ])
    ctxT = din("ctxT", [KC, 128, CTX])
    posT = din("posT", [KC, 128, SEQ])
    cvec = din("cvec", [128, KC, 2])
    wada = din("wada", [DEPTH, 12, 128, KC, 512])
    bada = din("bada", [DEPTH, 128, 48])
    gvec = din("gvec", [DEPTH, 128, 4, KC])
    winc = din("winc", [DEPTH, 14, 128, KC, 128])
    wvo = din("wvo", [DEPTH, NH, 128, KC, 256])
    wgate = din("wgate", [DEPTH, 128, KC, 16])
    bgate = din("bgate", [DEPTH, 16])
    wqkp = din("wqkp", [DEPTH, 8, 128, 3])
    gmn = din("gmn", [DEPTH, 512])
    wdw = din("wdw", [DEPTH, 128, 2, CONV_K])
    cvp = din("cvp", [DEPTH, 128, 3, 2])
    woutc = din("woutc", [DEPTH, 128, KC, KC, 128])
    w1c = din("w1c", [DEPTH, 16, 128, 2, KC, 128])
    w2c = din("w2c", [DEPTH, KC, 128, 32, 128])
    ident_d = din("ident", [128, 128])
    umask_d = din("umask", [128, 128])
    lmask_d = din("lmask", [128, 128])
    cs64_d = din("cs64", [128, 256])
    dftx_d = din("dftx", [4, 4, 128, 2, 4, 512], BF16)
    dftc_d = din("dftc", [128, 2, 2, 256], BF16)
    outT = dout("outT", [KC, 128, SEQ])
    dbg_out = {}
    if dbg:
        for nm in ("d_hx", "d_ym", "d_x1", "d_x2"):
            dbg_out[nm] = dout(nm, [KC, 128, NT])

    with ExitStack() as st:
        def sb(name, shape, dt):
            return st.enter_context(nc.sbuf_tensor(name, list(shape), dt))

        xs = sb("xs", [128, KC, NT], F32)
        hx = sb("hx", [128, KC, NT], BF16)
        SCR_ARENA = 25088
        SCR_N = SCR_ARENA + KC * NT
        scr = sb("scr", [128, SCR_N], BF16)
        ym_flat = scr[:, SCR_ARENA:SCR_N]
        ym = ym_flat.rearrange("p (c t) -> p c t", t=NT)
        ident_f = sb("ident_f", [128, 128], F32)
        ident_b = sb("ident_b", [128, 128], BF16)
        umask_f = sb("umask_f", [128, 128], F32)
        lmask_f = sb("lmask_f", [128, 128], F32)
        negm_f = sb("negm_f", [128, 128], BF16)
        negm_b = sb("negm_b", [128, 128], BF16)
        ones_f = sb("ones_f", [128, 128], F32)
        ones_b = sb("ones_b", [128, 128], BF16)
        cs64 = sb("cs64b", [128, 256], BF16)
        cst = sb("cst", [128, 4], F32)
        cv_f = sb("cv_f", [128, KC, 2], F32)
        sc_b = sb("sc_b", [128, KC, 2], BF16)
        mod = sb("mod", [128, DEPTH, 48, 2], F32)
        bada_s = sb("bada_s", [128, DEPTH, 48], F32)
        gv = sb("gv", [128, DEPTH, 4, KC], F32)
        A1 = sb("A1", [128, KC, 2], F32)
        A2 = sb("A2", [128, KC, 2], F32)
        G1 = sb("G1", [128, KC, 2], F32)
        G2 = sb("G2", [128, KC, 2], F32)
        rs = sb("rs", [128, 512], F32)
        sqb = [sb(f"sqb{i}", [128, 512], BF16) for i in range(2)]
        tmpf = [sb(f"tmpf{i}", [128, 512], F32) for i in range(2)]
        psb = [st.enter_context(nc.psum_tensor(f"ps{i}", [128, 512], F32)) for i in range(8)]

        R = P.res
        r_xs = [[R() for _ in range(NJ)] for _ in range(KC)]
        r_hx = [[R() for _ in range(NJ)] for _ in range(KC)]
        r_ym = [[R() for _ in range(NJ)] for _ in range(KC)]
        r_ps = [P.res("ps", excl=True) for _ in range(8)]
        r_const = R()
        r_mod = R()
        r_lay = R()
        r_rs = R()
        r_sqb = [R(), R()]
        r_tmpf = [R(), R()]
        r_arena = R()

        def trs(rl, kc, a, b):
            return [rl[kc][j] for j in range(a // 128, (b + 127) // 128)]

        def trs_all(rl, a, b):
            out = []
            for kc in range(KC):
                out += trs(rl, kc, a, b)
            return out

        cnt = {"ps": 0, "sq": 0, "tf": 0}

        ps_reserved = set()

        def next_ps():
            while True:
                i = cnt["ps"] % 8
                cnt["ps"] += 1
                if i not in ps_reserved:
                    return i

        def next_sq():
            i = cnt["sq"] % 2
            cnt["sq"] += 1
            return i

        def next_tf():
            i = cnt["tf"] % 2
            cnt["tf"] += 1
            return i

        def MM(out, lhsT, rhs, start, stop, reads, writes):
            P.op("pe", lambda e: e.matmul(out, lhsT=lhsT, rhs=rhs, start=start, stop=stop), reads, writes)

        def TR(out, in_, ident, reads, writes):
            P.op("pe", lambda e: e.transpose(out, in_, ident), reads, writes)

        def ACT(out, in_, func, reads, writes, bias=None, scale=None, accum=None):
            kw = {}
            if bias is not None:
                kw["bias"] = bias
            if scale is not None:
                kw["scale"] = scale
            if accum is not None:
                kw["accum_out"] = accum
            P.op("act", lambda e: e.activation(out=out, in_=in_, func=func, **kw), reads, writes)

        def TT(eng, out, in0, in1, op, reads, writes):
            P.op(eng, lambda e: e.tensor_tensor(out=out, in0=in0, in1=in1, op=op), reads, writes)

        def TS(eng, out, in0, s1, s2, op0, op1, reads, writes):
            if s2 is None:
                P.op(eng, lambda e: e.tensor_scalar(out=out, in0=in0, scalar1=s1, scalar2=None, op0=op0), reads, writes)
            else:
                P.op(eng, lambda e: e.tensor_scalar(out=out, in0=in0, scalar1=s1, scalar2=s2, op0=op0, op1=op1), reads, writes)

        def STT(eng, out, in0, scalar, in1, op0, op1, reads, writes):
            P.op(eng, lambda e: e.scalar_tensor_tensor(out=out, in0=in0, scalar=scalar, in1=in1, op0=op0, op1=op1), reads, writes)

        def CP(eng, out, in_, reads, writes):
            P.op(eng, lambda e: e.tensor_copy(out=out, in_=in_), reads, writes)

        def MS(eng, ap, val, writes):
            P.op(eng, lambda e: e.memset(ap, val), (), writes)

        def RECIP(out, in_, reads, writes):
            P.op("dve", lambda e: e.reciprocal(out=out, in_=in_), reads, writes)

        def DMA(q, out, in_, reads, writes):
            P.op(q, lambda e: e.dma_start(out=out, in_=in_), reads, writes, dma=True)

        def arena(off, n, dt=BF16):
            if dt == BF16:
                return scr[:, off:off + n]
            assert off % 2 == 0
            return scr[:, off:off + 2 * n].bitcast(F32)

        DMA("sp", ident_f[:], ident_d, [], [r_const])
        DMA("sp", umask_f[:], umask_d, [], [r_const])
        DMA("sp", lmask_f[:], lmask_d, [], [r_const])
        DMA("pool", cs64[:], cs64_d, [], [r_const])
        DMA("sp", cv_f[:], cvec, [], [r_const])
        DMA("sp", bada_s[:], bada.rearrange("l p n -> p l n"), [], [r_const])
        DMA("sp", gv[:], gvec.rearrange("l p a k -> p l a k"), [], [r_const])
        CP("dve", ident_b[:], ident_f[:], [r_const], [r_const])
        TS("dve", negm_f[:], umask_f[:], -1.0, 30000.0, ALU.add, ALU.mult, [r_const], [r_const])
        TS("dve", negm_b[:], lmask_f[:], -1.0, 30000.0, ALU.add, ALU.mult, [r_const], [r_const])
        MS("dve", ones_f[:], 1.0, [r_const])
        MS("dve", ones_b[:], 1.0, [r_const])
        MS("dve", cst[:, 0:1], 1024.0 * EPS, [r_const])
        MS("dve", cst[:, 1:2], EPS, [r_const])
        MS("dve", cst[:, 2:3], 1.0, [r_const])
        MS("dve", cst[:, 3:4], 0.0, [r_const])

        for kc in range(KC):
            DMA("sp", xs[:, kc, CTX:NT], xT[kc], [], trs(r_xs, kc, CTX, NT))
            DMA("sp", xs[:, kc, 0:CTX], ctxT[kc], [], trs(r_xs, kc, 0, CTX))
        pos_buf = [arena(0, SEQ, F32), arena(2 * SEQ, SEQ, F32)]
        r_pos = [R(), R()]
        for kc in range(KC):
            i = kc % 2
            DMA("sp", pos_buf[i], posT[kc], [], [r_pos[i]])
            TT("dve", xs[:, kc, CTX:NT], xs[:, kc, CTX:NT], pos_buf[i], ALU.add,
               [r_pos[i]] + trs(r_xs, kc, CTX, NT), trs(r_xs, kc, CTX, NT))

        ACT(sc_b[:], cv_f[:], AF.Silu, [r_const], [r_const])
        wa_off = 4 * SEQ
        wa_buf = [arena(wa_off + i * KC * 512, KC * 512).rearrange("p (k c) -> p k c", c=512) for i in range(2)]
        r_wa = [R(), R()]
        def ada_block(l_, blk, buf, r_buf, pv_, r_pv):
            DMA("pool", buf, wada[l_, blk], [], [r_buf])
            for n4 in range(4):
                n = blk * 4 + n4
                for kc in range(KC):
                    MM(pv_[:, n, :], buf[:, kc, n4 * 128:(n4 + 1) * 128], sc_b[:, kc, :], kc == 0, kc == KC - 1,
                       [r_buf, r_const], [r_pv])

        pi = next_ps()
        pv = psb[pi][:, 0:96].rearrange("p (n w) -> p n w", w=2)
        for blk in range(4):
            ada_block(0, blk, wa_buf[blk % 2], r_wa[blk % 2], pv, r_ps[pi])
        TT("dve", mod[:, 0, 0:16], pv[:, 0:16], bada_s[:, 0, 0:16].unsqueeze(2).to_broadcast([128, 16, 2]), ALU.add,
           [r_ps[pi], r_const], [r_mod])
        ada_todo = [(0, blk) for blk in range(4, 12)] + [(1, blk) for blk in range(12)]

        def rstd_of(src_fn, n, reads):
            pi = next_ps()
            for kc in range(KC):
                si = next_sq()
                ACT(sqb[si][:, 0:n], src_fn(kc), AF.Square, reads(kc), [r_sqb[si]])
                MM(psb[pi][:, 0:n], ones_b[:], sqb[si][:, 0:n], kc == 0, kc == KC - 1, [r_sqb[si], r_const], [r_ps[pi]])
            ACT(rs[:, 0:n], psb[pi][:, 0:n], AF.Sqrt, [r_ps[pi], r_const], [r_rs], bias=cst[:, 0:1], scale=1.0)
            RECIP(rs[:, 0:n], rs[:, 0:n], [r_rs], [r_rs])

        def prenorm(tl, Amod, shift_base, l):
            for (a, b, w) in tl:
                n = b - a
                rstd_of(lambda kc: xs[:, kc, a:b], n, lambda kc: trs(r_xs, kc, a, b))
                for kc in range(KC):
                    ti = next_tf()
                    TT("dve", tmpf[ti][:, 0:n], xs[:, kc, a:b], rs[:, 0:n], ALU.mult,
                       trs(r_xs, kc, a, b) + [r_rs], [r_tmpf[ti]])
                    ACT(hx[:, kc, a:b], tmpf[ti][:, 0:n], AF.Identity, [r_tmpf[ti], r_lay, r_mod], trs(r_hx, kc, a, b),
                        bias=mod[:, l, shift_base + kc, w:w + 1], scale=Amod[:, kc, w:w + 1])

        def resid_update(tl, src_fn, src_reads, Gm):
            for (a, b, w) in tl:
                n = b - a
                rstd_of(lambda kc: src_fn(kc, a, b), n, lambda kc: src_reads(kc, a, b))
                for kc in range(KC):
                    ti = next_tf()
                    TT("dve", tmpf[ti][:, 0:n], src_fn(kc, a, b), rs[:, 0:n], ALU.mult,
                       src_reads(kc, a, b) + [r_rs], [r_tmpf[ti]])
                    STT("dve", xs[:, kc, a:b], tmpf[ti][:, 0:n], Gm[:, kc, w:w + 1], xs[:, kc, a:b], ALU.mult, ALU.add,
                        [r_tmpf[ti], r_lay] + trs(r_xs, kc, a, b), trs(r_xs, kc, a, b))

        def dump(name, src_fn, reads_fn):
            if not dbg:
                return
            for kc in range(KC):
                DMA("pool", dbg_out[name][kc], src_fn(kc), reads_fn(kc), [])

        def maybe_stop(k):
            if stop is not None and stop == k:
                raise _Stop()

        try:
            P.barrier()
            for l in range(DEPTH):
                need_ctx = l < DEPTH - 1
                TL_all = tiles_for(True)
                TL = tiles_for(need_ctx)
                j0 = 0 if need_ctx else 2

                def mk(dst, gidx, mbase, plus1):
                    for w in range(2):
                        if plus1:
                            STT("dve", dst[:, :, w], mod[:, l, mbase:mbase + KC, w], 1.0, gv[:, l, gidx, :], ALU.add, ALU.mult,
                                [r_mod, r_const, r_lay], [r_lay])
                        else:
                            TT("dve", dst[:, :, w], mod[:, l, mbase:mbase + KC, w], gv[:, l, gidx, :], ALU.mult,
                               [r_mod, r_const, r_lay], [r_lay])
                    TS("dve", dst[:], dst[:], 32.0, None, ALU.mult, None, [r_lay], [r_lay])

                mk(A1, 0, 8, True)
                if l > 0:
                    mk(G1, 1, 16, False)
                    mk(A2, 2, 32, True)
                    mk(G2, 3, 40, False)

                maybe_stop(10 * l + 1)

                o_w = 0
                wbuf = [arena(o_w + i * 1024, 1024).rearrange("p (k c) -> p k c", c=128) for i in range(4)]
                r_wbuf = [R() for _ in range(4)]
                wcnt = {"n": 0}

                def load_w(src):
                    i = wcnt["n"] % 4
                    wcnt["n"] += 1
                    DMA("pool", wbuf[i], src, [], [r_wbuf[i]])
                    return i

                o_up = 4096
                upad = arena(o_up, 2 * UPW).rearrange("p (c t) -> p c t", t=UPW)
                r_up = R()
                o_vb = o_up + 2 * UPW
                vbuf = arena(o_vb, 2 * NT, F32).rearrange("p (c t) -> p c t", t=NT)
                r_vb = [R(), R()]
                o_cs = o_vb + 4 * NT
                wdw_s = arena(o_cs, 2 * CONV_K, F32).rearrange("p (c k) -> p c k", k=CONV_K)
                cvp_s = arena(o_cs + 4 * CONV_K, 6, F32).rearrange("p (a c) -> p a c", c=2)
                r_cs = R()
                lnt = [ym_flat[:, i * 1024:(i + 1) * 1024].bitcast(F32) for i in range(5)]
                r_lnt = [R() for _ in range(5)]
                dgm = ym_flat[:, 5120:5120 + 2 * CONV_K * 128].rearrange("p (c k m) -> p c k m", k=CONV_K, m=128)
                r_dg = R()

                DMA("sp", wdw_s, wdw[l], [], [r_cs])
                DMA("sp", cvp_s, cvp[l], [], [r_cs])
                MS("dve", upad, 0.0, [r_up])
                for cc in range(2):
                    for k in range(CONV_K):
                        TS("dve", dgm[:, cc, k, :], ident_f[:], wdw_s[:, cc, k:k + 1], None, ALU.mult, None,
                           [r_const, r_cs], [r_dg])

                def upos(a):
                    return a + PADC if a < CTX else (CTX + 2 * PADC) + (a - CTX) + PADC

                iag = [(load_w(winc[l, 10 + cc]), load_w(winc[l, 12 + cc])) for cc in range(2)]
                if l == 0:
                    wa_d = arena(18200, KC * 512).rearrange("p (k c) -> p k c", c=512)
                    r_wad = R()
                    pb0, pb1 = next_ps(), next_ps()
                    ps_reserved.add(pb0)
                    ps_reserved.add(pb1)
                    pvd = [psb[pb0][:, 0:96].rearrange("p (n w) -> p n w", w=2), psb[pb1][:, 0:96].rearrange("p (n w) -> p n w", w=2)]
                    r_pvd = [r_ps[pb0], r_ps[pb1]]

                def ada_some(k):
                    if l != 0:
                        return
                    for _ in range(k):
                        if ada_todo:
                            l_, blk = ada_todo.pop(0)
                            ada_block(l_, blk, wa_d, r_wad, pvd[l_], r_pvd[l_])

                for (a, b, w) in TL:
                    ada_some(2)
                    prenorm([(a, b, w)], A1, 0, l)
                    for cc in range(2):
                        ia, ig = iag[cc]
                        n = b - a
                        pa = next_ps()
                        for kc in range(KC):
                            MM(psb[pa][:, 0:n], wbuf[ia][:, kc, :], hx[:, kc, a:b], kc == 0, kc == KC - 1,
                               [r_wbuf[ia]] + trs(r_hx, kc, a, b), [r_ps[pa]])
                        pg = next_ps()
                        for kc in range(KC):
                            MM(psb[pg][:, 0:n], wbuf[ig][:, kc, :], hx[:, kc, a:b], kc == 0, kc == KC - 1,
                               [r_wbuf[ig]] + trs(r_hx, kc, a, b), [r_ps[pg]])
                        ti = next_tf()
                        ACT(tmpf[ti][:, 0:n], psb[pg][:, 0:n], AF.Sigmoid, [r_ps[pg]], [r_tmpf[ti]])
                        TT("dve", upad[:, cc, upos(a):upos(a) + n], psb[pa][:, 0:n], tmpf[ti][:, 0:n], ALU.mult,
                           [r_ps[pa], r_tmpf[ti]], [r_up])
                if not need_ctx:
                    prenorm([(0, 256, 1)], A1, 0, l)
                if l == 0:
                    dump("d_hx", lambda kc: hx[:, kc, :], lambda kc: trs(r_hx, kc, 0, NT))
                for cc in range(2):
                    for (a, b, w) in TL:
                        ada_some(1)
                        n = b - a
                        pi = next_ps()
                        p0 = upos(a) - PADC
                        for k in range(CONV_K):
                            MM(psb[pi][:, 0:n], dgm[:, cc, k, :], upad[:, cc, p0 + k:p0 + k + n], k == 0, k == CONV_K - 1,
                               [r_dg, r_up], [r_ps[pi]])
                        ACT(vbuf[:, cc, a:b], psb[pi][:, 0:n], AF.Identity, [r_ps[pi], r_cs], [r_vb[cc]],
                            bias=cvp_s[:, 0, cc:cc + 1], scale=1.0)
                if l == 0:
                    ada_some(len(ada_todo))
                    TT("dve", mod[:, 0, 16:48], pvd[0][:, 16:48], bada_s[:, 0, 16:48].unsqueeze(2).to_broadcast([128, 32, 2]), ALU.add,
                       [r_pvd[0], r_const], [r_mod])
                    TT("dve", mod[:, 1], pvd[1], bada_s[:, 1].unsqueeze(2).to_broadcast([128, 48, 2]), ALU.add,
                       [r_pvd[1], r_const], [r_mod])
                    ps_reserved.discard(pb0)
                    ps_reserved.discard(pb1)
                    mk(G1, 1, 16, False)
                    mk(A2, 2, 32, True)
                    mk(G2, 3, 40, False)
                for (a, b, w) in TL:
                    n = b - a
                    p1 = next_ps()
                    p2 = next_ps()
                    for cc in range(2):
                        MM(psb[p1][:, 0:n], ones_f[:], vbuf[:, cc, a:b], cc == 0, cc == 1, [r_const, r_vb[cc]], [r_ps[p1]])
                    for cc in range(2):
                        TT("dve", lnt[0][:, 0:n], vbuf[:, cc, a:b], vbuf[:, cc, a:b], ALU.mult, [r_vb[cc]], [r_lnt[0]])
                        MM(psb[p2][:, 0:n], ones_f[:], lnt[0][:, 0:n], cc == 0, cc == 1, [r_const, r_lnt[0]], [r_ps[p2]])
                    mean = lnt[1]
                    TS("dve", mean[:, 0:n], psb[p1][:, 0:n], 1.0 / 256.0, None, ALU.mult, None, [r_ps[p1]], [r_lnt[1]])
                    TT("dve", lnt[2][:, 0:n], mean[:, 0:n], mean[:, 0:n], ALU.mult, [r_lnt[1]], [r_lnt[2]])
                    STT("dve", lnt[2][:, 0:n], psb[p2][:, 0:n], 1.0 / 256.0, lnt[2][:, 0:n], ALU.mult, ALU.subtract,
                        [r_ps[p2], r_lnt[2]], [r_lnt[2]])
                    ACT(lnt[2][:, 0:n], lnt[2][:, 0:n], AF.Sqrt, [r_lnt[2], r_const], [r_lnt[2]], bias=cst[:, 1:2], scale=1.0)
                    RECIP(lnt[2][:, 0:n], lnt[2][:, 0:n], [r_lnt[2]], [r_lnt[2]])
                    for cc in range(2):
                        TT("dve", lnt[3 + cc][:, 0:n], vbuf[:, cc, a:b], mean[:, 0:n], ALU.subtract,
                           [r_vb[cc], r_lnt[1]], [r_lnt[3 + cc]])
                        TT("dve", lnt[3 + cc][:, 0:n], lnt[3 + cc][:, 0:n], lnt[2][:, 0:n], ALU.mult,
                           [r_lnt[3 + cc], r_lnt[2]], [r_lnt[3 + cc]])
                        ACT(ym[:, 6 + cc, a:b], lnt[3 + cc][:, 0:n], AF.Silu, [r_lnt[3 + cc], r_cs], trs(r_ym, 6 + cc, a, b),
                            bias=cvp_s[:, 2, cc:cc + 1], scale=cvp_s[:, 1, cc:cc + 1])
                P.barrier()

                maybe_stop(10 * l + 2)
                o_uf = 4096
                uF = arena(o_uf, 2 * NT).rearrange("p (c t) -> p c t", t=NT)
                r_uf = [R(), R()]
                o_dp = o_uf + 2 * NT
                dpc = [arena(o_dp + i * 4096, 4096).rearrange("p (s k t) -> p s k t", s=2, k=4) for i in range(3)]
                r_dpc = [R() for _ in range(3)]
                o_dc = o_dp + 3 * 4096
                dcc = arena(o_dc, 1024).rearrange("p (s k t) -> p s k t", s=2, k=2)
                r_dcc = R()
                AB = ym_flat[:, 0:NJ * 512].rearrange("p (j c m) -> p j c m", c=2, m=256)
                r_ab = [R() for _ in range(NJ)]

                for cc in range(2):
                    iw = load_w(winc[l, 8 + cc])
                    for (a, b, w) in TL:
                        n = b - a
                        pi = next_ps()
                        for kc in range(KC):
                            MM(psb[pi][:, 0:n], wbuf[iw][:, kc, :], hx[:, kc, a:b], kc == 0, kc == KC - 1,
                               [r_wbuf[iw]] + trs(r_hx, kc, a, b), [r_ps[pi]])
                        ACT(uF[:, cc, a:b], psb[pi][:, 0:n], AF.Copy, [r_ps[pi]], [r_uf[cc]])
                for j in range(j0, NJ):
                    pi = next_ps()
                    for cc in range(2):
                        MM(psb[pi][:, cc * 256:(cc + 1) * 256], uF[:, cc, j * 128:(j + 1) * 128], cs64[:], True, True,
                           [r_uf[cc], r_const], [r_ps[pi]])
                    CP("dve", AB[:, j].rearrange("p c m -> p (c m)"), psb[pi][:, :], [r_ps[pi]], [r_ab[j]])
                npc = 0
                for tq in range(4):
                    pp = [next_ps(), next_ps()]
                    for kg in range(4):
                        i = npc % 3
                        npc += 1
                        DMA("sp", dpc[i].rearrange("p s k t -> p (s k t)"),
                            dftx_d[tq, kg].rearrange("p s k t -> p (s k t)"), [], [r_dpc[i]])
                        for ki in range(4):
                            j = 2 + kg * 4 + ki
                            for s in range(2):
                                first = (kg == 0 and ki == 0 and s == 0)
                                last = (kg == 3 and ki == 3 and s == 1)
                                for cc in range(2):
                                    MM(psb[pp[cc]][:, :], AB[:, j, cc, s * 128:(s + 1) * 128], dpc[i][:, s, ki, :], first, last,
                                       [r_ab[j], r_dpc[i]], [r_ps[pp[cc]]])
                    for cc in range(2):
                        a = CTX + tq * 512
                        ACT(ym[:, 4 + cc, a:a + 512], psb[pp[cc]][:, :], AF.Copy, [r_ps[pp[cc]]], trs(r_ym, 4 + cc, a, a + 512))
                if need_ctx:
                    DMA("sp", dcc.rearrange("p s k t -> p (s k t)"), dftc_d.rearrange("p s k t -> p (s k t)"), [], [r_dcc])
                    pp = [next_ps(), next_ps()]
                    for ki in range(2):
                        for s in range(2):
                            for cc in range(2):
                                MM(psb[pp[cc]][:, 0:256], AB[:, ki, cc, s * 128:(s + 1) * 128], dcc[:, s, ki, :],
                                   ki == 0 and s == 0, ki == 1 and s == 1, [r_ab[ki], r_dcc], [r_ps[pp[cc]]])
                    for cc in range(2):
                        ACT(ym[:, 4 + cc, 0:CTX], psb[pp[cc]][:, 0:256], AF.Copy, [r_ps[pp[cc]]], trs(r_ym, 4 + cc, 0, CTX))

                P.barrier()
                maybe_stop(10 * l + 3)
                o_s = 0
                dgq = [arena(o_s + i * 256, 128, F32) for i in range(2)]
                qs = [arena(o_s + 512 + i * 128, 128) for i in range(2)]
                sTt = [arena(o_s + 768 + i * 128, 128) for i in range(2)]
                kts = [arena(o_s + 1024 + i * 128, 128) for i in range(2)]
                Et = [arena(o_s + 1280 + i * 128, 128) for i in range(2)]
                Cst = [arena(o_s + 1536 + i * 260, 130, F32) for i in range(2)]
                Cbf = [arena(o_s + 2056 + i * 130, 130) for i in range(2)]
                rden = [arena(o_s + 2316 + i * 4, 2, F32) for i in range(2)]
                st1 = arena(o_s + 2324, 2 * NJ, F32)
                st2 = arena(o_s + 2396, 2 * NJ, F32)
                r_dgq, r_qs, r_sT, r_kts = [R(), R()], [R(), R()], [R(), R()], [R(), R()]
                r_dgb, r_Et = [R(), R()], [R(), R()]
                r_C, r_Cb, r_rden = [R(), R()], [R(), R()], [R(), R()]
                r_st = R()
                wqk_raw = arena(2468, 1024).rearrange("p (k c) -> p k c", c=128)
                r_wqk = R()
                dg3 = arena(3492, 384).rearrange("p (t c) -> p t c", c=128)
                wtapc = arena(3876, 4, F32)[:, 0:3]
                zraw = arena(3884, 2308)
                r_w3, r_wtap, r_zraw = R(), R(), R()
                o_g = 7332
                Gtok = arena(o_g, NJ * 16, F32).rearrange("p (j g) -> p j g", g=16)
                dgb = [arena(o_g + i * 256, 128, F32) for i in range(2)]
                LF = arena(o_g + 576, NJ * 8, F32).rearrange("p (j g) -> p j g", g=8)
                CM = arena(o_g + 864, NJ * 8, F32).rearrange("p (j g) -> p j g", g=8)
                Bc = arena(o_g + 1152, NJ * 8, F32).rearrange("p (j g) -> p j g", g=8)
                EB = arena(o_g + 1440, NJ * 8, F32).rearrange("p (j g) -> p j g", g=8)
                EBL = arena(o_g + 1728, NJ * 8, F32).rearrange("p (j g) -> p j g", g=8)
                ECL = arena(o_g + 2016, NJ * 8, F32).rearrange("p (j g) -> p j g", g=8)
                bg_s = arena(o_g + 2304, 16, F32)
                wg_s = arena(o_g + 2336, KC * 16).rearrange("p (k g) -> p k g", g=16)
                gmn_s = arena(o_g + 2464, 512, F32)
                r_gate = R()
                o_h = o_g + 3488
                qT = arena(o_h, NT)
                kT = arena(o_h + NT, NT)
                vaug = arena(o_h + 2 * NT, NJ * 130).rearrange("p (j e) -> p j e", e=130)
                sgo = arena(o_h + 2 * NT + 2340, NT).rearrange("p (j e) -> p j e", e=128)
                hsum = arena(o_h + 3 * NT + 2340, NT, F32).rearrange("p (j e) -> p j e", e=128)
                assert o_h + 5 * NT + 2340 <= SCR_ARENA, (o_h + 5 * NT + 2340)
                r_q, r_k, r_v, r_sg, r_hs = R(), R(), R(), R(), R()
                wvo_s = ym_flat[:, 3 * NT:3 * NT + KC * 256].rearrange("p (k c) -> p k c", c=256)
                r_wvo = R()

                DMA("pool", wg_s, wgate[l], [], [r_gate])
                DMA("sp", bg_s, bgate[l].partition_broadcast(128), [], [r_gate])
                DMA("sp", gmn_s, gmn[l].partition_broadcast(128), [], [r_gate])
                for jg in range(0, NJ, 6):
                    pi = next_ps()
                    for jj in range(6):
                        j = jg + jj
                        for kc in range(KC):
                            MM(psb[pi][:, jj * 16:(jj + 1) * 16], hx[:, kc, j * 128:(j + 1) * 128], wg_s[:, kc, :], kc == 0, kc == KC - 1,
                               [r_gate] + trs(r_hx, kc, j * 128, (j + 1) * 128), [r_ps[pi]])
                    TT("dve", Gtok[:, jg:jg + 6, :], psb[pi][:, 0:96].rearrange("p (j g) -> p j g", g=16),
                       bg_s.unsqueeze(1).to_broadcast([128, 6, 16]), ALU.add, [r_ps[pi], r_gate], [r_gate])
                CP("dve", CM[:, :, 0:4], Gtok[:, :, 0:4], [r_gate], [r_gate])
                CP("dve", CM[:, :, 4:8], Gtok[:, :, 8:12], [r_gate], [r_gate])
                ACT(LF[:, :, 0:4], Gtok[:, :, 4:8], AF.Abs, [r_gate], [r_gate])
                ACT(LF[:, :, 4:8], Gtok[:, :, 12:16], AF.Abs, [r_gate], [r_gate])
                ACT(LF[:], LF[:], AF.Exp, [r_gate], [r_gate], scale=-1.0)
                ACT(LF[:], LF[:], AF.Ln, [r_gate, r_const], [r_gate], bias=cst[:, 2:3], scale=1.0)
                TS("dve", EB[:, :, 0:4], Gtok[:, :, 4:8], 0.0, None, ALU.min, None, [r_gate], [r_gate])
                TS("dve", EB[:, :, 4:8], Gtok[:, :, 12:16], 0.0, None, ALU.min, None, [r_gate], [r_gate])
                TT("dve", LF[:], EB[:], LF[:], ALU.subtract, [r_gate], [r_gate])
                pi = next_ps()
                pbv = psb[pi][:, 0:NJ * 8].rearrange("p (j g) -> p j g", g=8)
                for j in range(NJ):
                    MM(pbv[:, j, 0:4], umask_f[:], LF[:, j, 0:4], True, True, [r_const, r_gate], [r_ps[pi]])
                    MM(pbv[:, j, 4:8], lmask_f[:], LF[:, j, 4:8], True, True, [r_const, r_gate], [r_ps[pi]])
                CP("dve", Bc[:], pbv, [r_ps[pi]], [r_gate])
                pi = next_ps()
                ptv = psb[pi][:, 0:NJ * 8].rearrange("p (j g) -> p j g", g=8)
                MM(psb[pi][:, 0:NJ * 8], ones_f[:], LF[:].rearrange("p j g -> p (j g)"), True, True, [r_const, r_gate], [r_ps[pi]])
                ACT(EBL[:], ptv, AF.Exp, [r_ps[pi]], [r_gate])
                ACT(EB[:], Bc[:], AF.Exp, [r_gate], [r_gate])
                TT("dve", CM[:], CM[:], Bc[:], ALU.subtract, [r_gate], [r_gate])
                TT("dve", ECL[:], CM[:], ptv, ALU.add, [r_gate, r_ps[pi]], [r_gate])
                ACT(ECL[:], ECL[:], AF.Exp, [r_gate], [r_gate])
                P.barrier()

                MS("dve", vaug[:, :, 128:129], 1.0, [r_v])
                MS("dve", zraw, 0.0, [r_zraw])

                def head_qk(h):
                    for qk in range(2):
                        dst, r_dst = (qT, r_q) if qk == 0 else (kT, r_k)
                        ci = qk * 4 + h
                        DMA("pool", wqk_raw, winc[l, ci], [], [r_wqk])
                        DMA("sp", wtapc, wqkp[l, ci], [], [r_wtap])
                        if qk == 1:
                            TS("dve", wtapc, wtapc, HD ** -0.5, None, ALU.mult, None, [r_wtap], [r_wtap])
                        for t in range(3):
                            TS("dve", dg3[:, t, :], ident_f[:], wtapc[:, t:t + 1], None, ALU.mult, None,
                               [r_const, r_wtap], [r_w3])

                        def zpos(a_):
                            return 1 + a_ if a_ < CTX else 3 + a_
                        for (a, b, w) in TL_all:
                            n = b - a
                            pi = next_ps()
                            for kc in range(KC):
                                MM(psb[pi][:, 0:n], wqk_raw[:, kc, :], hx[:, kc, a:b], kc == 0, kc == KC - 1,
                                   [r_wqk] + trs(r_hx, kc, a, b), [r_ps[pi]])
                            ACT(zraw[:, zpos(a):zpos(a) + n], psb[pi][:, 0:n], AF.Copy, [r_ps[pi]], [r_zraw])
                        for (a, b, w) in TL_all:
                            n = b - a
                            pi = next_ps()
                            for t in range(3):
                                MM(psb[pi][:, 0:n], dg3[:, t, :], zraw[:, zpos(a) + t - 1:zpos(a) + t - 1 + n], t == 0, t == 2,
                                   [r_w3, r_zraw], [r_ps[pi]])
                            ACT(dst[:, a:b], psb[pi][:, 0:n], AF.Copy, [r_ps[pi]], [r_dst])
                def head_vo(h):
                    DMA("pool", wvo_s, wvo[l, h], [], [r_wvo])
                    for j in range(NJ):
                        pi = next_ps()
                        for kc in range(KC):
                            MM(psb[pi][:, 0:256], hx[:, kc, j * 128:(j + 1) * 128], wvo_s[:, kc, :], kc == 0, kc == KC - 1,
                               [r_wvo] + trs(r_hx, kc, j * 128, (j + 1) * 128), [r_ps[pi]])
                        CP("dve", vaug[:, j, 0:128], psb[pi][:, 0:128], [r_ps[pi]], [r_v])
                        ACT(sgo[:, j, :], psb[pi][:, 128:256], AF.Sigmoid, [r_ps[pi]], [r_sg])
                        TT("dve", sgo[:, j, :], sgo[:, j, :], gmn_s[:, h * 128:(h + 1) * 128], ALU.mult, [r_sg, r_gate], [r_sg])
                def head_loop(h):
                    MS("dve", hsum, 0.0, [r_hs])
                    for d_ in range(2):
                        MS("dve", Cst[d_], 0.0, [r_C[d_]])
                        MS("dve", Cbf[d_], 0.0, [r_Cb[d_]])
                    order = [list(range(NJ)), [1, 0] + list(range(NJ - 1, 1, -1))]
                    t0f = tmpf[0]
                    t1b = tmpf[1][:, :].bitcast(BF16)
                    dgb2 = [dgb, [dgq[0], dgq[1]]]
                    sT2 = [sTt, [t1b[:, 256:384], t1b[:, 384:512]]]
                    kts2 = [kts, [t1b[:, 512:640], t1b[:, 640:768]]]
                    Et2 = [Et, [t1b[:, 768:896], t1b[:, 896:1024]]]
                    ndi = [t0f[:, 0:130], t0f[:, 130:260]]
                    ndi_den = t0f[:, 0:260].rearrange("p (d e) -> p d e", e=130)[:, :, 128:129]
                    rden_j = t0f[:, 392:394].unsqueeze(2)
                    r_rdj = R()
                    r_ndi = [R(), R()]
                    t0b = t0f[:, 260:390].bitcast(BF16)
                    Cbf2 = [Cbf, [t0b[:, 0:130], t0b[:, 130:260]]]
                    r_Cb2 = [r_Cb, [R(), R()]]
                    rr = lambda: [[R(), R()], [R(), R()]]
                    r_dgb2, r_sT2, r_kts2, r_Et2 = rr(), rr(), rr(), rr()

                    def info(step, d_):
                        j = order[d_][step]
                        return j, step % 2, d_ * 4 + h, (need_ctx or j >= 2), step == NJ - 1

                    ps_hold = {}

                    def a1(step, d_):
                        j, bs, col, need_out, is_last = info(step, d_)
                        if need_out:
                            TS("dve", dgb2[bs][d_], ident_f[:], Bc[:, j, col:col + 1], None, ALU.mult, None,
                               [r_const, r_gate], [r_dgb2[bs][d_]])
                        yield

                    def a2a(step, d_):
                        j, bs, col, need_out, is_last = info(step, d_)
                        c0, c1 = j * 128, (j + 1) * 128
                        if need_out:
                            pd_ = d_
                            MM(psb[pd_][:, 0:128], ones_f[:], dgb2[bs][d_], True, False, [r_const, r_dgb2[bs][d_]], [r_ps[pd_]])
                            MM(psb[pd_][:, 0:128], ident_b[:], (negm_f if d_ == 0 else negm_b)[:], False, True,
                               [r_const], [r_ps[pd_]])
                            yield
                            ACT(Et2[bs][d_], psb[pd_][:, 0:128], AF.Exp, [r_ps[pd_], r_gate], [r_Et2[bs][d_]],
                                bias=CM[:, j, col:col + 1], scale=1.0)
                            yield
                            p_s = 2 + d_
                            MM(psb[p_s][:, 0:128], kT[:, c0:c1], qT[:, c0:c1], True, True, [r_k, r_q], [r_ps[p_s]])
                            yield
                        if not is_last:
                            p_t = next_ps()
                            ptb = psb[p_t][:, 0:64].bitcast(BF16)
                            TR(ptb, kT[:, c0:c1], ident_b[:], [r_k, r_const], [r_ps[p_t]])
                            yield
                            ACT(kts2[bs][d_], ptb, AF.Copy, [r_ps[p_t], r_gate], [r_kts2[bs][d_]], scale=ECL[:, j, col:col + 1])
                            yield

                    def a2b(step, d_):
                        j, bs, col, need_out, is_last = info(step, d_)
                        if need_out:
                            p_s = 2 + d_
                            TT("dve", sT2[bs][d_], psb[p_s][:, 0:128], Et2[bs][d_], ALU.mult,
                               [r_ps[p_s], r_Et2[bs][d_]], [r_sT2[bs][d_]])
                        yield

                    def stage_b(step, d_):
                        j, bs, col, need_out, is_last = info(step, d_)
                        cur, nxt = Cbf2[step % 2][d_], Cbf2[(step + 1) % 2][d_]
                        r_cur, r_nxt = r_Cb2[step % 2][d_], r_Cb2[(step + 1) % 2][d_]
                        if not is_last:
                            p_u = next_ps()
                            MM(psb[p_u][:, 0:129], kts2[bs][d_], vaug[:, j, 0:129], True, True, [r_kts2[bs][d_], r_v], [r_ps[p_u]])
                            yield
                            STT("dve", Cst[d_][:, 0:129], Cst[d_][:, 0:129], EBL[:, j, col:col + 1], psb[p_u][:, 0:129],
                                ALU.mult, ALU.add, [r_C[d_], r_gate, r_ps[p_u]], [r_C[d_]])
                            yield
                            ACT(nxt[:, 0:129], Cst[d_][:, 0:129], AF.Copy, [r_C[d_]], [r_nxt])
                            yield

                    def stage_bo(step, d_):
                        j, bs, col, need_out, is_last = info(step, d_)
                        cur, r_cur = Cbf2[step % 2][d_], r_Cb2[step % 2][d_]
                        if need_out:
                            c0, c1 = j * 128, (j + 1) * 128
                            p_i = next_ps()
                            MM(psb[p_i][:, 0:129], qT[:, c0:c1], cur[:, 0:129], True, True, [r_q, r_cur], [r_ps[p_i]])
                            p_n = next_ps()
                            MM(psb[p_n][:, 0:129], sT2[bs][d_], vaug[:, j, 0:129], True, True, [r_sT2[bs][d_], r_v], [r_ps[p_n]])
                            yield
                            ACT(ndi[d_][:, 0:129], psb[p_i][:, 0:129], AF.Copy, [r_ps[p_i], r_gate], [r_ndi[d_]], scale=EB[:, j, col:col + 1])
                            yield
                            TT("dve", ndi[d_][:, 0:129], ndi[d_][:, 0:129], psb[p_n][:, 0:129], ALU.add, [r_ndi[d_], r_ps[p_n]], [r_ndi[d_]])
                            yield

                    def den_ops(step):
                        j, bs, col, need_out, is_last = info(step, 0)
                        if need_out:
                            STT("dve", rden_j, ndi_den, -1.0, ndi_den, ALU.mult, ALU.max, [r_ndi[0], r_ndi[1]], [r_rdj])
                            TS("dve", rden_j, rden_j, 1.0, None, ALU.max, None, [r_rdj], [r_rdj])
                            RECIP(rden_j, rden_j, [r_rdj], [r_rdj])

                    def stage_b2(step, d_):
                        j, bs, col, need_out, is_last = info(step, d_)
                        if need_out:
                            STT("dve", hsum[:, j, :], ndi[d_][:, 0:128], t0f[:, 392 + d_:393 + d_], hsum[:, j, :], ALU.mult, ALU.add,
                                [r_ndi[d_], r_rdj, r_hs], [r_hs])
                        yield

                    def interleave(*gens):
                        gens = list(gens)
                        while gens:
                            for g in list(gens):
                                try:
                                    next(g)
                                except StopIteration:
                                    gens.remove(g)

                    for bnk in range(4):
                        ps_reserved.add(bnk)
                    interleave(a1(0, 0), a1(0, 1))
                    interleave(a2a(0, 0), a2a(0, 1))
                    interleave(a2b(0, 0), a2b(0, 1))
                    interleave(a1(1, 0), a1(1, 1))
                    for step in range(NJ):
                        if step + 2 < NJ:
                            interleave(a1(step + 2, 0), a1(step + 2, 1))
                        if step + 1 < NJ:
                            interleave(a2a(step + 1, 0), a2a(step + 1, 1))
                        interleave(stage_b(step, 0), stage_b(step, 1))
                        if step + 1 < NJ:
                            interleave(a2b(step + 1, 0), a2b(step + 1, 1))
                        interleave(stage_bo(step, 0), stage_bo(step, 1))
                        den_ops(step)
                        interleave(stage_b2(step, 0), stage_b2(step, 1))
                    for bnk in range(4):
                        ps_reserved.discard(bnk)
                def head_out(h):
                    nj = NJ - j0
                    hv = hsum[:, j0:NJ, :]
                    sqv = ym[:, h, j0 * 128:NT].rearrange("p (j e) -> p j e", e=128)
                    ymh_res = trs(r_ym, h, j0 * 128, NT) + ([r_wvo] if h == 3 else [])
                    P.op("dve", (lambda hv=hv, nj=nj: lambda e: e.tensor_reduce(out=st1[:, 0:nj], in_=hv, axis=AX.X, op=ALU.add))(),
                         [r_hs], [r_st])
                    TT("dve", sqv, hv, hv, ALU.mult, [r_hs], ymh_res)
                    P.op("dve", (lambda sqv=sqv, nj=nj: lambda e: e.tensor_reduce(out=st1[:, NJ:NJ + nj], in_=sqv, axis=AX.X, op=ALU.add))(),
                         ymh_res, [r_st])
                    mean_ = st2[:, 0:nj]
                    var_ = st2[:, NJ:NJ + nj]
                    TS("dve", mean_, st1[:, 0:nj], 1.0 / HD, None, ALU.mult, None, [r_st], [r_st])
                    TT("dve", var_, mean_, mean_, ALU.mult, [r_st], [r_st])
                    STT("dve", var_, st1[:, NJ:NJ + nj], 1.0 / HD, var_, ALU.mult, ALU.subtract, [r_st], [r_st])
                    ACT(var_, var_, AF.Sqrt, [r_st, r_const], [r_st], bias=cst[:, 1:2], scale=1.0)
                    RECIP(var_, var_, [r_st], [r_st])
                    TT("dve", hv, hv, mean_.unsqueeze(2).to_broadcast([128, nj, 128]), ALU.subtract, [r_hs, r_st], [r_hs])
                    TT("dve", hv, hv, var_.unsqueeze(2).to_broadcast([128, nj, 128]), ALU.mult, [r_hs, r_st], [r_hs])
                    TT("dve", sgo[:, j0:NJ, :], hv, sgo[:, j0:NJ, :], ALU.mult, [r_hs, r_sg], [r_sg])
                    for jg in range(j0, NJ, 4):
                        p_t = next_ps()
                        ptb = psb[p_t][:, 0:256].bitcast(BF16)
                        jn = min(4, NJ - jg)
                        for jj in range(jn):
                            TR(ptb[:, jj * 128:(jj + 1) * 128], sgo[:, jg + jj, :], ident_b[:], [r_sg, r_const], [r_ps[p_t]])
                        ACT(ym[:, h, jg * 128:(jg + jn) * 128], ptb[:, 0:jn * 128], AF.Copy, [r_ps[p_t]],
                            trs(r_ym, h, jg * 128, (jg + jn) * 128) + ([r_wvo] if h == 3 else []))

                head_qk(0)
                head_vo(0)
                for h in range(NH):
                    head_loop(h)
                    if h + 1 < NH:
                        head_qk(h + 1)
                    head_out(h)
                    if h + 1 < NH:
                        head_vo(h + 1)
                if l == 0:
                    dump("d_ym", lambda kc: ym[:, kc, :], lambda kc: trs(r_ym, kc, 0, NT))

                P.barrier()
                maybe_stop(10 * l + 4)
                o_wo = 4096
                wo_s = arena(o_wo, KC * KC * 128).rearrange("p (o k c) -> p o k c", o=KC, k=KC)
                r_wo = R()
                obuf = arena(o_wo + 8192, KC * 512, F32).rearrange("p (o t) -> p o t", t=512)
                r_ob = [R() for _ in range(KC)]
                DMA("pool", wo_s.rearrange("p o k c -> p (o k c)"), woutc[l].rearrange("p o k c -> p (o k c)"), [],
                    [r_wo])
                for (a, b, w) in TL:
                    n = b - a
                    for oc in range(KC):
                        pi = next_ps()
                        for kc in range(KC):
                            MM(psb[pi][:, 0:n], wo_s[:, oc, kc, :], ym[:, kc, a:b], kc == 0, kc == KC - 1,
                               [r_wo] + trs(r_ym, kc, a, b), [r_ps[pi]])
                        ACT(obuf[:, oc, 0:n], psb[pi][:, 0:n], AF.Copy, [r_ps[pi]], [r_ob[oc]])
                    resid_update([(a, b, w)], lambda kc, a_, b_: obuf[:, kc, 0:b_ - a_], lambda kc, a_, b_: [r_ob[kc]], G1)
                if l == 0:
                    dump("d_x1", lambda kc: xs[:, kc, :], lambda kc: trs(r_xs, kc, 0, NT))

                P.barrier()
                maybe_stop(10 * l + 5)
                hT = scr[:, 0:24576].rearrange("p (f t) -> p f t", t=768)
                r_hT = [R() for _ in range(32)]
                ob2 = scr[:, 24576:30720].rearrange("p (o t) -> p o t", t=768)
                r_ob2 = [R() for _ in range(KC)]
                w1b = [scr[:, 30720 + i * 2048:30720 + (i + 1) * 2048].rearrange("p (g k c) -> p g k c", g=2, k=KC) for i in range(2)]
                w2b = [scr[:, 34816 + i * 4096:34816 + (i + 1) * 4096].rearrange("p (f c) -> p f c", c=128) for i in range(2)]
                r_w1b = [R(), R()]
                r_w2b = [R(), R()]
                if need_ctx:
                    supers = [[(0, 256, 1), (256, 768, 0)], [(768, 1280, 0), (1280, 1536, 0)], [(1536, 2048, 0), (2048, 2304, 0)]]
                else:
                    supers = [[(256, 768, 0), (768, 1024, 0)], [(1024, 1536, 0), (1536, 1792, 0)], [(1792, 2304, 0)]]
                n1 = 0
                n2 = 0
                prenorm(supers[0], A2, 24, l)
                for isup, sup in enumerate(supers):
                    a0 = sup[0][0]
                    for g in range(16):
                        i = n1 % 2
                        n1 += 1
                        DMA("pool", w1b[i].rearrange("p g k c -> p (g k c)"), w1c[l, g].rearrange("p g k c -> p (g k c)"), [],
                            [r_w1b[i]])
                        for f2 in range(2):
                            f = g * 2 + f2
                            for (a, b, w) in sup:
                                n = b - a
                                pi = next_ps()
                                for kc in range(KC):
                                    MM(psb[pi][:, 0:n], w1b[i][:, f2, kc, :], hx[:, kc, a:b], kc == 0, kc == KC - 1,
                                       [r_w1b[i]] + trs(r_hx, kc, a, b), [r_ps[pi]])
                                ti = next_tf()
                                ACT(tmpf[ti][:, 0:n], psb[pi][:, 0:n], AF.Relu, [r_ps[pi]], [r_tmpf[ti]])
                                TT("dve", hT[:, f, a - a0:b - a0], tmpf[ti][:, 0:n], tmpf[ti][:, 0:n], ALU.mult, [r_tmpf[ti]], [r_hT[f]])
                    if isup + 1 < len(supers):
                        prenorm(supers[isup + 1], A2, 24, l)
                    ssb = []
                    for _ in sup:
                        pss = next_ps()
                        ps_reserved.add(pss)
                        ssb.append(pss)
                    for oc in range(KC):
                        i = n2 % 2
                        n2 += 1
                        DMA("pool", w2b[i].rearrange("p f c -> p (f c)"), w2c[l, oc].rearrange("p f c -> p (f c)"), [],
                            [r_w2b[i]])
                        for si_, (a, b, w) in enumerate(sup):
                            n = b - a
                            pi = next_ps()
                            for f in range(32):
                                MM(psb[pi][:, 0:n], w2b[i][:, f, :], hT[:, f, a - a0:b - a0], f == 0, f == 31,
                                   [r_w2b[i], r_hT[f]], [r_ps[pi]])
                            ACT(ob2[:, oc, a - a0:b - a0], psb[pi][:, 0:n], AF.Copy, [r_ps[pi]], [r_ob2[oc]])
                            sq_i = next_sq()
                            ACT(sqb[sq_i][:, 0:n], psb[pi][:, 0:n], AF.Square, [r_ps[pi]], [r_sqb[sq_i]])
                            MM(psb[ssb[si_]][:, 0:n], ones_b[:], sqb[sq_i][:, 0:n], oc == 0, oc == KC - 1,
                               [r_sqb[sq_i], r_const], [r_ps[ssb[si_]]])
                    for si_, (a, b, w) in enumerate(sup):
                        n = b - a
                        ACT(rs[:, 0:n], psb[ssb[si_]][:, 0:n], AF.Sqrt, [r_ps[ssb[si_]], r_const], [r_rs], bias=cst[:, 0:1], scale=1.0)
                        RECIP(rs[:, 0:n], rs[:, 0:n], [r_rs], [r_rs])
                        for kc in range(KC):
                            ti = next_tf()
                            TT("dve", tmpf[ti][:, 0:n], ob2[:, kc, a - a0:b - a0], rs[:, 0:n], ALU.mult,
                               [r_ob2[kc], r_rs], [r_tmpf[ti]])
                            STT("dve", xs[:, kc, a:b], tmpf[ti][:, 0:n], G2[:, kc, w:w + 1], xs[:, kc, a:b], ALU.mult, ALU.add,
                                [r_tmpf[ti], r_lay] + trs(r_xs, kc, a, b), trs(r_xs, kc, a, b))
                    for pss in ssb:
                        ps_reserved.discard(pss)
                if l == 0:
                    dump("d_x2", lambda kc: xs[:, kc, :], lambda kc: trs(r_xs, kc, 0, NT))
                P.barrier()

        except _Stop:
            pass
        for kc in range(KC):
            DMA("sp", outT[kc], xs[:, kc, CTX:NT], trs(r_xs, kc, CTX, NT), [])
        P.emit()
    return nc


_CACHE = {}


def _consts():
    if "c" in _CACHE:
        return _CACHE["c"]
    c = {}
    c["ident"] = np.eye(128, dtype=np.float32)
    s = np.arange(128)
    c["umask"] = (s[:, None] <= s[None, :]).astype(np.float32)
    c["lmask"] = (s[:, None] >= s[None, :]).astype(np.float32)
    k = np.arange(64)
    ang = 2.0 * np.pi * np.outer(k, k) / 64.0
    cs = np.zeros((128, 256), np.float64)
    for hh in range(2):
        cs[hh * 64:(hh + 1) * 64, hh * 64:(hh + 1) * 64] = np.cos(ang) / 8.0
        cs[hh * 64:(hh + 1) * 64, 128 + hh * 64:128 + (hh + 1) * 64] = np.sin(ang) / 8.0
    c["cs64"] = cs.astype(np.float32)
    t = np.arange(SEQ, dtype=np.int64)
    ph = (np.outer(t, t) % SEQ).astype(np.float64) * (2.0 * np.pi / SEQ)
    dc = (np.cos(ph) / math.sqrt(SEQ)).astype(np.float32)
    ds = (-np.sin(ph) / math.sqrt(SEQ)).astype(np.float32)
    both = np.stack([dc, ds], 0)
    both = both.reshape(2, 4, 4, 128, 4, 512)
    c["dftx"] = np.ascontiguousarray(both.transpose(4, 1, 3, 0, 2, 5)).astype(ml_dtypes.bfloat16)
    t = np.arange(CTX, dtype=np.int64)
    ph = (np.outer(t, t) % CTX).astype(np.float64) * (2.0 * np.pi / CTX)
    both = np.stack([np.cos(ph), -np.sin(ph)], 0) / math.sqrt(CTX)
    both = both.reshape(2, 2, 128, CTX)
    c["dftc"] = np.ascontiguousarray(both.transpose(2, 0, 1, 3)).astype(ml_dtypes.bfloat16)
    rows = SEQ // 64
    quarter = D // 4
    freq = np.exp(-math.log(10000.0) * np.arange(quarter, dtype=np.float32) / quarter).astype(np.float32)
    r = np.broadcast_to(np.arange(rows, dtype=np.float32)[:, None], (rows, 64)).reshape(-1)
    col = np.broadcast_to(np.arange(64, dtype=np.float32)[None, :], (rows, 64)).reshape(-1)
    ar = r[:, None] * freq
    ac = col[:, None] * freq
    pos = np.concatenate([np.sin(ar), np.cos(ar), np.sin(ac), np.cos(ac)], axis=-1).astype(np.float32)
    c["posT"] = np.ascontiguousarray(pos.T).reshape(KC, 128, SEQ)
    _CACHE["c"] = c
    return c


def _chunk_w(w, cols):
    return np.ascontiguousarray(w[:, cols].reshape(KC, 128, -1).transpose(1, 0, 2))


def _prep_shared(inp):
    f = np.float32
    L = DEPTH
    w_in = np.asarray(inp["w_in"], f)
    sh = {}
    wada = np.asarray(inp["w_ada"], f)
    sh["wada"] = np.ascontiguousarray(wada.reshape(L, KC, 128, 12, 512).transpose(0, 3, 2, 1, 4))
    sh["bada"] = np.ascontiguousarray(np.asarray(inp["b_ada"], f).reshape(L, 48, 128).transpose(0, 2, 1))
    gs = np.stack([np.asarray(inp[k], f) for k in ("g_pre_mix", "g_post_mix", "g_pre_mlp", "g_post_mlp")], 1)
    sh["gvec"] = np.ascontiguousarray(gs.reshape(L, 4, KC, 128).transpose(0, 3, 1, 2))
    offs = [Q_OFF + 128 * i for i in range(4)] + [K_OFF + 128 * i for i in range(4)] + \
           [F_OFF, F_OFF + 128, CA_OFF, CA_OFF + 128, CG_OFF, CG_OFF + 128]
    sh["winc"] = np.stack([np.stack([_chunk_w(w_in[l], np.arange(o, o + 128)) for o in offs]) for l in range(L)])
    sh["wvo"] = np.stack([np.stack([_chunk_w(w_in[l], np.concatenate([np.arange(V_OFF + 128 * h, V_OFF + 128 * h + 128),
                                                                      np.arange(O_OFF + 128 * h, O_OFF + 128 * h + 128)]))
                                    for h in range(NH)]) for l in range(L)])
    sh["wgate"] = np.stack([_chunk_w(w_in[l], np.arange(G_OFF, G_OFF + 16)) for l in range(L)])
    sh["bgate"] = np.ascontiguousarray(np.asarray(inp["b_gate"], f).reshape(L, 16))
    wqk = np.asarray(inp["w_qk_conv"], f)
    sh["wqkp"] = np.ascontiguousarray(wqk.reshape(L, 3, 8, 128).transpose(0, 2, 3, 1))
    sh["gmn"] = np.ascontiguousarray(np.asarray(inp["g_mlstm_norm"], f))
    wdw = np.asarray(inp["w_dw"], f)
    sh["wdw"] = np.ascontiguousarray(wdw.reshape(L, CONV_K, 2, 128).transpose(0, 3, 2, 1))
    cv = np.stack([np.asarray(inp[k], f) for k in ("b_dw", "g_conv_ln", "b_conv_ln")], 1)
    sh["cvp"] = np.ascontiguousarray(cv.reshape(L, 3, 2, 128).transpose(0, 3, 1, 2))
    wout = np.asarray(inp["w_out"], f)
    sh["woutc"] = np.ascontiguousarray(wout.reshape(L, KC, 128, KC, 128).transpose(0, 2, 3, 1, 4))
    w1 = np.asarray(inp["w_mlp1"], f)
    sh["w1c"] = np.ascontiguousarray(w1.reshape(L, KC, 128, 16, 2, 128).transpose(0, 3, 2, 4, 1, 5))
    w2 = np.asarray(inp["w_mlp2"], f)
    sh["w2c"] = np.ascontiguousarray(w2.reshape(L, 32, 128, KC, 128).transpose(0, 3, 2, 1, 4))
    c = _consts()
    for k in ("ident", "umask", "lmask", "cs64", "dftx", "dftc", "posT"):
        sh[k] = c[k]
    return sh


def make_in_maps(inp, cores):
    sh = _prep_shared(inp)
    x = np.asarray(inp["x"], np.float32)
    ctx = np.asarray(inp["ctx"], np.float32)
    c = np.asarray(inp["c"], np.float32)
    c_ctx = np.asarray(inp["c_ctx"], np.float32)
    maps = []
    for b in cores:
        m = dict(sh)
        m["xT"] = np.ascontiguousarray(x[b].T).reshape(KC, 128, SEQ)
        m["ctxT"] = np.ascontiguousarray(ctx[b].T).reshape(KC, 128, CTX)
        cv = np.stack([c[b], c_ctx], -1)
        m["cvec"] = np.ascontiguousarray(cv.reshape(KC, 128, 2).transpose(1, 0, 2))
        maps.append(m)
    return maps


def kernel(**inputs):
    if "nc" not in _CACHE:
        _CACHE["nc"] = build_program(dbg=False)
    nc = _CACHE["nc"]
    in_maps = make_in_maps(inputs, list(range(NB)))
    res = run_bass_kernel_spmd(nc, in_maps, core_ids=list(range(NB)))
    out = np.empty((NB, SEQ, D), np.float32)
    for b in range(NB):
        oT = np.asarray(res.results[b]["outT"], np.float32).reshape(D, SEQ)
        out[b] = oT.T
    return out
```

```python
import math
from contextlib import ExitStack

import numpy as np
import ml_dtypes

import concourse.bass as bass
import concourse.mybir as mybir
from concourse.bass_utils import run_bass_kernel_spmd

F32 = mybir.dt.float32
BF16 = mybir.dt.bfloat16
AF = mybir.ActivationFunctionType
ALU = mybir.AluOpType
AX = mybir.AxisListType

ENGS = ("pe", "act", "dve", "pool", "sp")
SEM_LIM = 2000
N_DMA_SEMS = 16


class Res:
    __slots__ = ("name", "w", "r", "excl")

    def __init__(self, name, excl=False):
        self.name = name
        self.w = None
        self.r = []
        self.excl = excl


class Op:
    __slots__ = ("eng", "idx", "fn", "deps", "dma", "signal", "semref", "dma_slot", "dma_val")

    def __init__(self, eng, idx, fn, dma):
        self.eng = eng
        self.idx = idx
        self.fn = fn
        self.deps = []
        self.dma = dma
        self.signal = False
        self.semref = None
        self.dma_slot = None
        self.dma_val = None


class Prog:
    def __init__(self, nc, same_engine_sync=True):
        self.nc = nc
        self.ops = {e: [] for e in ENGS}
        self.n_dma_q = {}
        self.same_engine_sync = same_engine_sync

    def res(self, name="", excl=False):
        return Res(name, excl)

    def op(self, eng, fn, reads=(), writes=(), dma=False):
        o = Op(eng, len(self.ops[eng]), fn, dma)
        if dma:
            half = N_DMA_SEMS // 2
            k = self.n_dma_q.get(eng, 0)
            self.n_dma_q[eng] = k + 1
            o.dma_slot = (k % half) + (half if eng == "pool" else 0)
            o.dma_val = 16 * (k // half + 1)
        deps = []
        for r in reads:
            if r.w is not None:
                deps.append(r.w)
            if r.excl:
                deps.extend(x for x in r.r if x.eng != eng)
        for r in writes:
            if r.w is not None:
                deps.append(r.w)
            deps.extend(r.r)
        for r in reads:
            r.r.append(o)
        for r in writes:
            r.w = o
            r.r = []
        seen = set()
        for d in deps:
            if d is o or id(d) in seen:
                continue
            seen.add(id(d))
            if (not d.dma) and (not dma) and d.eng == eng:
                if eng == "pe" or not self.same_engine_sync:
                    continue
            o.deps.append(d)
        self.ops[eng].append(o)
        return o

    def barrier(self):
        targets = []
        for e in ENGS:
            cs = [o for o in self.ops[e] if not o.dma and o.fn is not None]
            if cs:
                targets.append(cs[-1])
        by_slot = {}
        for e in ENGS:
            for o in self.ops[e]:
                if o.dma and (o.dma_slot not in by_slot or o.dma_val > by_slot[o.dma_slot].dma_val):
                    by_slot[o.dma_slot] = o
        targets += list(by_slot.values())
        for e in ENGS:
            o = Op(e, len(self.ops[e]), None, False)
            o.deps = list(targets) if e != "pe" else [t for t in targets if t.dma or t.eng != e]
            self.ops[e].append(o)

    def emit(self):
        nc = self.nc
        for e in ENGS:
            for o in self.ops[e]:
                for d in o.deps:
                    d.signal = True
        with ExitStack() as st:
            dma_sems = [st.enter_context(nc.semaphore(f"dq{i}")) for i in range(N_DMA_SEMS)]
            for e in ENGS:
                n = sum(1 for o in self.ops[e] if o.signal and not o.dma)
                k = max((n + SEM_LIM - 1) // SEM_LIM, 1)
                sems = [st.enter_context(nc.semaphore(f"s_{e}{i}")) for i in range(k)]
                c = 0
                for o in self.ops[e]:
                    if o.signal and not o.dma:
                        o.semref = (sems[c // SEM_LIM], c % SEM_LIM + 1, c)
                        c += 1
            block = st.enter_context(nc.Block())

            def run(e, eng):
                waited_c = {p: -1 for p in ENGS}
                waited_d = [0] * N_DMA_SEMS
                for o in self.ops[e]:
                    for d in o.deps:
                        if d.dma:
                            if waited_d[d.dma_slot] >= d.dma_val:
                                continue
                            waited_d[d.dma_slot] = d.dma_val
                            eng.wait_ge(dma_sems[d.dma_slot], d.dma_val)
                        else:
                            sem, val, gc = d.semref
                            if waited_c[d.eng] >= gc:
                                continue
                            waited_c[d.eng] = gc
                            eng.wait_ge(sem, val)
                    if o.fn is None:
                        continue
                    if o.dma and o.dma_val > 16 and waited_d[o.dma_slot] < o.dma_val - 16:
                        waited_d[o.dma_slot] = o.dma_val - 16
                        eng.wait_ge(dma_sems[o.dma_slot], o.dma_val - 16)
                    ins = o.fn(eng)
                    if o.dma:
                        ins.then_inc(dma_sems[o.dma_slot], 16)
                    elif o.signal:
                        ins.then_inc(o.semref[0], 1)

            fin_dma = {}
            for e in ENGS:
                for o in self.ops[e]:
                    if o.dma:
                        fin_dma[o.dma_slot] = max(fin_dma.get(o.dma_slot, 0), o.dma_val)

            @block.tensor
            def _(eng):
                run("pe", eng)

            @block.scalar
            def _(eng):
                run("act", eng)

            @block.vector
            def _(eng):
                run("dve", eng)

            @block.gpsimd
            def _(eng):
                run("pool", eng)

            @block.sync
            def _(eng):
                run("sp", eng)
                for slot, val in sorted(fin_dma.items()):
                    eng.wait_ge(dma_sems[slot], val)


D = 1024
NB = 8
SEQ = 2048
CTX = 256
NT = CTX + SEQ
DEPTH = 2
KC = 8
NJ = NT // 128
HD = 128
NH = 4
DFF = 4096
EPS = 1e-6
CONV_K = 31
PADC = 15
Q_OFF, K_OFF, V_OFF, O_OFF, G_OFF = 0, 512, 1024, 1536, 2048
F_OFF = 2064
CA_OFF = 2320
CG_OFF = 2576
UPW = (CTX + 2 * PADC) + (SEQ + 2 * PADC)


def tiles_for(need_ctx):
    t = [(256, 768, 0), (768, 1280, 0), (1280, 1792, 0), (1792, 2304, 0)]
    if need_ctx:
        t = [(0, 256, 1)] + t
    return t


class _Stop(Exception):
    pass


def build_program(dbg=False, stop=None):
    nc = bass.Bass("TRN2", target_bir_lowering=False)
    P = Prog(nc)

    def din(name, shape, dt=F32):
        return nc.dram_tensor(name, list(shape), dt, kind="ExternalInput").ap()

    def dout(name, shape, dt=F32):
        return nc.dram_tensor(name, list(shape), dt, kind="ExternalOutput").ap()

    xT = din("xT", [KC, 128, SEQ])
    ctxT = din("ctxT", [KC, 128, CTX])
    posT = din("posT", [KC, 128, SEQ])
    cvec = din("cvec", [128, KC, 2])
    wada = din("wada", [DEPTH, 12, 128, KC, 512])
    bada = din("bada", [DEPTH, 128, 48])
    gvec = din("gvec", [DEPTH, 128, 4, KC])
    winc = din("winc", [DEPTH, 14, 128, KC, 128])
    wvo = din("wvo", [DEPTH, NH, 128, KC, 256])
    wgate = din("wgate", [DEPTH, 128, KC, 16])
    bgate = din("bgate", [DEPTH, 16])
    wqkp = din("wqkp", [DEPTH, 8, 128, 3])
    gmn = din("gmn", [DEPTH, 512])
    wdw = din("wdw", [DEPTH, 128, 2, CONV_K])
    cvp = din("cvp", [DEPTH, 128, 3, 2])
    woutc = din("woutc", [DEPTH, 128, KC, KC, 128])
    w1c = din("w1c", [DEPTH, 16, 128, 2, KC, 128])
    w2c = din("w2c", [DEPTH, KC, 128, 32, 128])
    ident_d = din("ident", [128, 128])
    umask_d = din("umask", [128, 128])
    lmask_d = din("lmask", [128, 128])
    cs64_d = din("cs64", [128, 256])
    dftx_d = din("dftx", [4, 4, 128, 2, 4, 512], BF16)
    dftc_d = din("dftc", [128, 2, 2, 256], BF16)
    outT = dout("outT", [KC, 128, SEQ])
    dbg_out = {}
    if dbg:
        for nm in ("d_hx", "d_ym", "d_x1", "d_x2"):
            dbg_out[nm] = dout(nm, [KC, 128, NT])

    with ExitStack() as st:
        def sb(name, shape, dt):
            return st.enter_context(nc.sbuf_tensor(name, list(shape), dt))

        xs = sb("xs", [128, KC, NT], F32)
        hx = sb("hx", [128, KC, NT], BF16)
        SCR_ARENA = 25088
        SCR_N = SCR_ARENA + KC * NT
        scr = sb("scr", [128, SCR_N], BF16)
        ym_flat = scr[:, SCR_ARENA:SCR_N]
        ym = ym_flat.rearrange("p (c t) -> p c t", t=NT)
        ident_f = sb("ident_f", [128, 128], F32)
        ident_b = sb("ident_b", [128, 128], BF16)
        umask_f = sb("umask_f", [128, 128], F32)
        lmask_f = sb("lmask_f", [128, 128], F32)
        negm_f = sb("negm_f", [128, 128], BF16)
        negm_b = sb("negm_b", [128, 128], BF16)
        ones_f = sb("ones_f", [128, 128], F32)
        ones_b = sb("ones_b", [128, 128], BF16)
        cs64 = sb("cs64b", [128, 256], BF16)
        cst = sb("cst", [128, 4], F32)
        cv_f = sb("cv_f", [128, KC, 2], F32)
        sc_b = sb("sc_b", [128, KC, 2], BF16)
        mod = sb("mod", [128, DEPTH, 48, 2], F32)
        bada_s = sb("bada_s", [128, DEPTH, 48], F32)
        gv = sb("gv", [128, DEPTH, 4, KC], F32)
        A1 = sb("A1", [128, KC, 2], F32)
        A2 = sb("A2", [128, KC, 2], F32)
        G1 = sb("G1", [128, KC, 2], F32)
        G2 = sb("G2", [128, KC, 2], F32)
        rs = sb("rs", [128, 512], F32)
        sqb = [sb(f"sqb{i}", [128, 512], BF16) for i in range(2)]
        tmpf = [sb(f"tmpf{i}", [128, 512], F32) for i in range(2)]
        psb = [st.enter_context(nc.psum_tensor(f"ps{i}", [128, 512], F32)) for i in range(8)]

        R = P.res
        r_xs = [[R() for _ in range(NJ)] for _ in range(KC)]
        r_hx = [[R() for _ in range(NJ)] for _ in range(KC)]
        r_ym = [[R() for _ in range(NJ)] for _ in range(KC)]
        r_ps = [P.res("ps", excl=True) for _ in range(8)]
        r_const = R()
        r_mod = R()
        r_lay = R()
        r_rs = R()
        r_sqb = [R(), R()]
        r_tmpf = [R(), R()]
        r_arena = R()

        def trs(rl, kc, a, b):
            return [rl[kc][j] for j in range(a // 128, (b + 127) // 128)]

        def trs_all(rl, a, b):
            out = []
            for kc in range(KC):
                out += trs(rl, kc, a, b)
            return out

        cnt = {"ps": 0, "sq": 0, "tf": 0}

        ps_reserved = set()

        def next_ps():
            while True:
                i = cnt["ps"] % 8
                cnt["ps"] += 1
                if i not in ps_reserved:
                    return i

        def next_sq():
            i = cnt["sq"] % 2
            cnt["sq"] += 1
            return i

        def next_tf():
            i = cnt["tf"] % 2
            cnt["tf"] += 1
            return i

        def MM(out, lhsT, rhs, start, stop, reads, writes):
            P.op("pe", lambda e: e.matmul(out, lhsT=lhsT, rhs=rhs, start=start, stop=stop), reads, writes)

        def TR(out, in_, ident, reads, writes):
            P.op("pe", lambda e: e.transpose(out, in_, ident), reads, writes)

        def ACT(out, in_, func, reads, writes, bias=None, scale=None, accum=None):
            kw = {}
            if bias is not None:
                kw["bias"] = bias
            if scale is not None:
                kw["scale"] = scale
            if accum is not None:
                kw["accum_out"] = accum
            P.op("act", lambda e: e.activation(out=out, in_=in_, func=func, **kw), reads, writes)

        def TT(eng, out, in0, in1, op, reads, writes):
            P.op(eng, lambda e: e.tensor_tensor(out=out, in0=in0, in1=in1, op=op), reads, writes)

        def TS(eng, out, in0, s1, s2, op0, op1, reads, writes):
            if s2 is None:
                P.op(eng, lambda e: e.tensor_scalar(out=out, in0=in0, scalar1=s1, scalar2=None, op0=op0), reads, writes)
            else:
                P.op(eng, lambda e: e.tensor_scalar(out=out, in0=in0, scalar1=s1, scalar2=s2, op0=op0, op1=op1), reads, writes)

        def STT(eng, out, in0, scalar, in1, op0, op1, reads, writes):
            P.op(eng, lambda e: e.scalar_tensor_tensor(out=out, in0=in0, scalar=scalar, in1=in1, op0=op0, op1=op1), reads, writes)

        def CP(eng, out, in_, reads, writes):
            P.op(eng, lambda e: e.tensor_copy(out=out, in_=in_), reads, writes)

        def MS(eng, ap, val, writes):
            P.op(eng, lambda e: e.memset(ap, val), (), writes)

        def RECIP(out, in_, reads, writes):
            P.op("dve", lambda e: e.reciprocal(out=out, in_=in_), reads, writes)

        def DMA(q, out, in_, reads, writes):
            P.op(q, lambda e: e.dma_start(out=out, in_=in_), reads, writes, dma=True)

        def arena(off, n, dt=BF16):
            if dt == BF16:
                return scr[:, off:off + n]
            assert off % 2 == 0
            return scr[:, off:off + 2 * n].bitcast(F32)

        DMA("sp", ident_f[:], ident_d, [], [r_const])
        DMA("sp", umask_f[:], umask_d, [], [r_const])
        DMA("sp", lmask_f[:], lmask_d, [], [r_const])
        DMA("pool", cs64[:], cs64_d, [], [r_const])
        DMA("sp", cv_f[:], cvec, [], [r_const])
        DMA("sp", bada_s[:], bada.rearrange("l p n -> p l n"), [], [r_const])
        DMA("sp", gv[:], gvec.rearrange("l p a k -> p l a k"), [], [r_const])
        CP("dve", ident_b[:], ident_f[:], [r_const], [r_const])
        TS("dve", negm_f[:], umask_f[:], -1.0, 30000.0, ALU.add, ALU.mult, [r_const], [r_const])
        TS("dve", negm_b[:], lmask_f[:], -1.0, 30000.0, ALU.add, ALU.mult, [r_const], [r_const])
        MS("dve", ones_f[:], 1.0, [r_const])
        MS("dve", ones_b[:], 1.0, [r_const])
        MS("dve", cst[:, 0:1], 1024.0 * EPS, [r_const])
        MS("dve", cst[:, 1:2], EPS, [r_const])
        MS("dve", cst[:, 2:3], 1.0, [r_const])
        MS("dve", cst[:, 3:4], 0.0, [r_const])

        for kc in range(KC):
            DMA("sp", xs[:, kc, CTX:NT], xT[kc], [], trs(r_xs, kc, CTX, NT))
            DMA("sp", xs[:, kc, 0:CTX], ctxT[kc], [], trs(r_xs, kc, 0, CTX))
        pos_buf = [arena(0, SEQ, F32), arena(2 * SEQ, SEQ, F32)]
        r_pos = [R(), R()]
        for kc in range(KC):
            i = kc % 2
            DMA("sp", pos_buf[i], posT[kc], [], [r_pos[i]])
            TT("dve", xs[:, kc, CTX:NT], xs[:, kc, CTX:NT], pos_buf[i], ALU.add,
               [r_pos[i]] + trs(r_xs, kc, CTX, NT), trs(r_xs, kc, CTX, NT))

        ACT(sc_b[:], cv_f[:], AF.Silu, [r_const], [r_const])
        wa_off = 4 * SEQ
        wa_buf = [arena(wa_off + i * KC * 512, KC * 512).rearrange("p (k c) -> p k c", c=512) for i in range(2)]
        r_wa = [R(), R()]
        def ada_block(l_, blk, buf, r_buf, pv_, r_pv):
            DMA("pool", buf, wada[l_, blk], [], [r_buf])
            for n4 in range(4):
                n = blk * 4 + n4
                for kc in range(KC):
                    MM(pv_[:, n, :], buf[:, kc, n4 * 128:(n4 + 1) * 128], sc_b[:, kc, :], kc == 0, kc == KC - 1,
                       [r_buf, r_const], [r_pv])

        pi = next_ps()
        pv = psb[pi][:, 0:96].rearrange("p (n w) -> p n w", w=2)
        for blk in range(4):
            ada_block(0, blk, wa_buf[blk % 2], r_wa[blk % 2], pv, r_ps[pi])
        TT("dve", mod[:, 0, 0:16], pv[:, 0:16], bada_s[:, 0, 0:16].unsqueeze(2).to_broadcast([128, 16, 2]), ALU.add,
           [r_ps[pi], r_const], [r_mod])
        ada_todo = [(0, blk) for blk in range(4, 12)] + [(1, blk) for blk in range(12)]

        def rstd_of(src_fn, n, reads):
            pi = next_ps()
            for kc in range(KC):
                si = next_sq()
                ACT(sqb[si][:, 0:n], src_fn(kc), AF.Square, reads(kc), [r_sqb[si]])
                MM(psb[pi][:, 0:n], ones_b[:], sqb[si][:, 0:n], kc == 0, kc == KC - 1, [r_sqb[si], r_const], [r_ps[pi]])
            ACT(rs[:, 0:n], psb[pi][:, 0:n], AF.Sqrt, [r_ps[pi], r_const], [r_rs], bias=cst[:, 0:1], scale=1.0)
            RECIP(rs[:, 0:n], rs[:, 0:n], [r_rs], [r_rs])

        def prenorm(tl, Amod, shift_base, l):
            for (a, b, w) in tl:
                n = b - a
                rstd_of(lambda kc: xs[:, kc, a:b], n, lambda kc: trs(r_xs, kc, a, b))
                for kc in range(KC):
                    ti = next_tf()
                    TT("dve", tmpf[ti][:, 0:n], xs[:, kc, a:b], rs[:, 0:n], ALU.mult,
                       trs(r_xs, kc, a, b) + [r_rs], [r_tmpf[ti]])
                    ACT(hx[:, kc, a:b], tmpf[ti][:, 0:n], AF.Identity, [r_tmpf[ti], r_lay, r_mod], trs(r_hx, kc, a, b),
                        bias=mod[:, l, shift_base + kc, w:w + 1], scale=Amod[:, kc, w:w + 1])

        def resid_update(tl, src_fn, src_reads, Gm):
            for (a, b, w) in tl:
                n = b - a
                rstd_of(lambda kc: src_fn(kc, a, b), n, lambda kc: src_reads(kc, a, b))
                for kc in range(KC):
                    ti = next_tf()
                    TT("dve", tmpf[ti][:, 0:n], src_fn(kc, a, b), rs[:, 0:n], ALU.mult,
                       src_reads(kc, a, b) + [r_rs], [r_tmpf[ti]])
                    STT("dve", xs[:, kc, a:b], tmpf[ti][:, 0:n], Gm[:, kc, w:w + 1], xs[:, kc, a:b], ALU.mult, ALU.add,
                        [r_tmpf[ti], r_lay] + trs(r_xs, kc, a, b), trs(r_xs, kc, a, b))

        def dump(name, src_fn, reads_fn):
            if not dbg:
                return
            for kc in range(KC):
                DMA("pool", dbg_out[name][kc], src_fn(kc), reads_fn(kc), [])

        def maybe_stop(k):
            if stop is not None and stop == k:
                raise _Stop()

        try:
            P.barrier()
            for l in range(DEPTH):
                need_ctx = l < DEPTH - 1
                TL_all = tiles_for(True)
                TL = tiles_for(need_ctx)
                j0 = 0 if need_ctx else 2

                def mk(dst, gidx, mbase, plus1):
                    for w in range(2):
                        if plus1:
                            STT("dve", dst[:, :, w], mod[:, l, mbase:mbase + KC, w], 1.0, gv[:, l, gidx, :], ALU.add, ALU.mult,
                                [r_mod, r_const, r_lay], [r_lay])
                        else:
                            TT("dve", dst[:, :, w], mod[:, l, mbase:mbase + KC, w], gv[:, l, gidx, :], ALU.mult,
                               [r_mod, r_const, r_lay], [r_lay])
                    TS("dve", dst[:], dst[:], 32.0, None, ALU.mult, None, [r_lay], [r_lay])

                mk(A1, 0, 8, True)
                if l > 0:
                    mk(G1, 1, 16, False)
                    mk(A2, 2, 32, True)
                    mk(G2, 3, 40, False)

                maybe_stop(10 * l + 1)

                o_w = 0
                wbuf = [arena(o_w + i * 1024, 1024).rearrange("p (k c) -> p k c", c=128) for i in range(4)]
                r_wbuf = [R() for _ in range(4)]
                wcnt = {"n": 0}

                def load_w(src):
                    i = wcnt["n"] % 4
                    wcnt["n"] += 1
                    DMA("pool", wbuf[i], src, [], [r_wbuf[i]])
                    return i

                o_up = 4096
                upad = arena(o_up, 2 * UPW).rearrange("p (c t) -> p c t", t=UPW)
                r_up = R()
                o_vb = o_up + 2 * UPW
                vbuf = arena(o_vb, 2 * NT, F32).rearrange("p (c t) -> p c t", t=NT)
                r_vb = [R(), R()]
                o_cs = o_vb + 4 * NT
                wdw_s = arena(o_cs, 2 * CONV_K, F32).rearrange("p (c k) -> p c k", k=CONV_K)
                cvp_s = arena(o_cs + 4 * CONV_K, 6, F32).rearrange("p (a c) -> p a c", c=2)
                r_cs = R()
                lnt = [ym_flat[:, i * 1024:(i + 1) * 1024].bitcast(F32) for i in range(5)]
                r_lnt = [R() for _ in range(5)]
                dgm = ym_flat[:, 5120:5120 + 2 * CONV_K * 128].rearrange("p (c k m) -> p c k m", k=CONV_K, m=128)
                r_dg = R()

                DMA("sp", wdw_s, wdw[l], [], [r_cs])
                DMA("sp", cvp_s, cvp[l], [], [r_cs])
                MS("dve", upad, 0.0, [r_up])
                for cc in range(2):
                    for k in range(CONV_K):
                        TS("dve", dgm[:, cc, k, :], ident_f[:], wdw_s[:, cc, k:k + 1], None, ALU.mult, None,
                           [r_const, r_cs], [r_dg])

                def upos(a):
                    return a + PADC if a < CTX else (CTX + 2 * PADC) + (a - CTX) + PADC

                iag = [(load_w(winc[l, 10 + cc]), load_w(winc[l, 12 + cc])) for cc in range(2)]
                if l == 0:
                    wa_d = arena(18200, KC * 512).rearrange("p (k c) -> p k c", c=512)
                    r_wad = R()
                    pb0, pb1 = next_ps(), next_ps()
                    ps_reserved.add(pb0)
                    ps_reserved.add(pb1)
                    pvd = [psb[pb0][:, 0:96].rearrange("p (n w) -> p n w", w=2), psb[pb1][:, 0:96].rearrange("p (n w) -> p n w", w=2)]
                    r_pvd = [r_ps[pb0], r_ps[pb1]]

                def ada_some(k):
                    if l != 0:
                        return
                    for _ in range(k):
                        if ada_todo:
                            l_, blk = ada_todo.pop(0)
                            ada_block(l_, blk, wa_d, r_wad, pvd[l_], r_pvd[l_])

                for (a, b, w) in TL:
                    ada_some(2)
                    prenorm([(a, b, w)], A1, 0, l)
                    for cc in range(2):
                        ia, ig = iag[cc]
                        n = b - a
                        pa = next_ps()
                        for kc in range(KC):
                            MM(psb[pa][:, 0:n], wbuf[ia][:, kc, :], hx[:, kc, a:b], kc == 0, kc == KC - 1,
                               [r_wbuf[ia]] + trs(r_hx, kc, a, b), [r_ps[pa]])
                        pg = next_ps()
                        for kc in range(KC):
                            MM(psb[pg][:, 0:n], wbuf[ig][:, kc, :], hx[:, kc, a:b], kc == 0, kc == KC - 1,
                               [r_wbuf[ig]] + trs(r_hx, kc, a, b), [r_ps[pg]])
                        ti = next_tf()
                        ACT(tmpf[ti][:, 0:n], psb[pg][:, 0:n], AF.Sigmoid, [r_ps[pg]], [r_tmpf[ti]])
                        TT("dve", upad[:, cc, upos(a):upos(a) + n], psb[pa][:, 0:n], tmpf[ti][:, 0:n], ALU.mult,
                           [r_ps[pa], r_tmpf[ti]], [r_up])
                if not need_ctx:
                    prenorm([(0, 256, 1)], A1, 0, l)
                if l == 0:
                    dump("d_hx", lambda kc: hx[:, kc, :], lambda kc: trs(r_hx, kc, 0, NT))
                for cc in range(2):
                    for (a, b, w) in TL:
                        ada_some(1)
                        n = b - a
                        pi = next_ps()
                        p0 = upos(a) - PADC
                        for k in range(CONV_K):
                            MM(psb[pi][:, 0:n], dgm[:, cc, k, :], upad[:, cc, p0 + k:p0 + k + n], k == 0, k == CONV_K - 1,
                               [r_dg, r_up], [r_ps[pi]])
                        ACT(vbuf[:, cc, a:b], psb[pi][:, 0:n], AF.Identity, [r_ps[pi], r_cs], [r_vb[cc]],
                            bias=cvp_s[:, 0, cc:cc + 1], scale=1.0)
                if l == 0:
                    ada_some(len(ada_todo))
                    TT("dve", mod[:, 0, 16:48], pvd[0][:, 16:48], bada_s[:, 0, 16:48].unsqueeze(2).to_broadcast([128, 32, 2]), ALU.add,
                       [r_pvd[0], r_const], [r_mod])
                    TT("dve", mod[:, 1], pvd[1], bada_s[:, 1].unsqueeze(2).to_broadcast([128, 48, 2]), ALU.add,
                       [r_pvd[1], r_const], [r_mod])
                    ps_reserved.discard(pb0)
                    ps_reserved.discard(pb1)
                    mk(G1, 1, 16, False)
                    mk(A2, 2, 32, True)
                    mk(G2, 3, 40, False)
                for (a, b, w) in TL:
                    n = b - a
                    p1 = next_ps()
                    p2 = next_ps()
                    for cc in range(2):
                        MM(psb[p1][:, 0:n], ones_f[:], vbuf[:, cc, a:b], cc == 0, cc == 1, [r_const, r_vb[cc]], [r_ps[p1]])
                    for cc in range(2):
                        TT("dve", lnt[0][:, 0:n], vbuf[:, cc, a:b], vbuf[:, cc, a:b], ALU.mult, [r_vb[cc]], [r_lnt[0]])
                        MM(psb[p2][:, 0:n], ones_f[:], lnt[0][:, 0:n], cc == 0, cc == 1, [r_const, r_lnt[0]], [r_ps[p2]])
                    mean = lnt[1]
                    TS("dve", mean[:, 0:n], psb[p1][:, 0:n], 1.0 / 256.0, None, ALU.mult, None, [r_ps[p1]], [r_lnt[1]])
                    TT("dve", lnt[2][:, 0:n], mean[:, 0:n], mean[:, 0:n], ALU.mult, [r_lnt[1]], [r_lnt[2]])
                    STT("dve", lnt[2][:, 0:n], psb[p2][:, 0:n], 1.0 / 256.0, lnt[2][:, 0:n], ALU.mult, ALU.subtract,
                        [r_ps[p2], r_lnt[2]], [r_lnt[2]])
                    ACT(lnt[2][:, 0:n], lnt[2][:, 0:n], AF.Sqrt, [r_lnt[2], r_const], [r_lnt[2]], bias=cst[:, 1:2], scale=1.0)
                    RECIP(lnt[2][:, 0:n], lnt[2][:, 0:n], [r_lnt[2]], [r_lnt[2]])
                    for cc in range(2):
                        TT("dve", lnt[3 + cc][:, 0:n], vbuf[:, cc, a:b], mean[:, 0:n], ALU.subtract,
                           [r_vb[cc], r_lnt[1]], [r_lnt[3 + cc]])
                        TT("dve", lnt[3 + cc][:, 0:n], lnt[3 + cc][:, 0:n], lnt[2][:, 0:n], ALU.mult,
                           [r_lnt[3 + cc], r_lnt[2]], [r_lnt[3 + cc]])
                        ACT(ym[:, 6 + cc, a:b], lnt[3 + cc][:, 0:n], AF.Silu, [r_lnt[3 + cc], r_cs], trs(r_ym, 6 + cc, a, b),
                            bias=cvp_s[:, 2, cc:cc + 1], scale=cvp_s[:, 1, cc:cc + 1])
                P.barrier()

                maybe_stop(10 * l + 2)
                o_uf = 4096
                uF = arena(o_uf, 2 * NT).rearrange("p (c t) -> p c t", t=NT)
                r_uf = [R(), R()]
                o_dp = o_uf + 2 * NT
                dpc = [arena(o_dp + i * 4096, 4096).rearrange("p (s k t) -> p s k t", s=2, k=4) for i in range(3)]
                r_dpc = [R() for _ in range(3)]
                o_dc = o_dp + 3 * 4096
                dcc = arena(o_dc, 1024).rearrange("p (s k t) -> p s k t", s=2, k=2)
                r_dcc = R()
                AB = ym_flat[:, 0:NJ * 512].rearrange("p (j c m) -> p j c m", c=2, m=256)
                r_ab = [R() for _ in range(NJ)]

                for cc in range(2):
                    iw = load_w(winc[l, 8 + cc])
                    for (a, b, w) in TL:
                        n = b - a
                        pi = next_ps()
                        for kc in range(KC):
                            MM(psb[pi][:, 0:n], wbuf[iw][:, kc, :], hx[:, kc, a:b], kc == 0, kc == KC - 1,
                               [r_wbuf[iw]] + trs(r_hx, kc, a, b), [r_ps[pi]])
                        ACT(uF[:, cc, a:b], psb[pi][:, 0:n], AF.Copy, [r_ps[pi]], [r_uf[cc]])
                for j in range(j0, NJ):
                    pi = next_ps()
                    for cc in range(2):
                        MM(psb[pi][:, cc * 256:(cc + 1) * 256], uF[:, cc, j * 128:(j + 1) * 128], cs64[:], True, True,
                           [r_uf[cc], r_const], [r_ps[pi]])
                    CP("dve", AB[:, j].rearrange("p c m -> p (c m)"), psb[pi][:, :], [r_ps[pi]], [r_ab[j]])
                npc = 0
                for tq in range(4):
                    pp = [next_ps(), next_ps()]
                    for kg in range(4):
                        i = npc % 3
                        npc += 1
                        DMA("sp", dpc[i].rearrange("p s k t -> p (s k t)"),
                            dftx_d[tq, kg].rearrange("p s k t -> p (s k t)"), [], [r_dpc[i]])
                        for ki in range(4):
                            j = 2 + kg * 4 + ki
                            for s in range(2):
                                first = (kg == 0 and ki == 0 and s == 0)
                                last = (kg == 3 and ki == 3 and s == 1)
                                for cc in range(2):
                                    MM(psb[pp[cc]][:, :], AB[:, j, cc, s * 128:(s + 1) * 128], dpc[i][:, s, ki, :], first, last,
                                       [r_ab[j], r_dpc[i]], [r_ps[pp[cc]]])
                    for cc in range(2):
                        a = CTX + tq * 512
                        ACT(ym[:, 4 + cc, a:a + 512], psb[pp[cc]][:, :], AF.Copy, [r_ps[pp[cc]]], trs(r_ym, 4 + cc, a, a + 512))
                if need_ctx:
                    DMA("sp", dcc.rearrange("p s k t -> p (s k t)"), dftc_d.rearrange("p s k t -> p (s k t)"), [], [r_dcc])
                    pp = [next_ps(), next_ps()]
                    for ki in range(2):
                        for s in range(2):
                            for cc in range(2):
                                MM(psb[pp[cc]][:, 0:256], AB[:, ki, cc, s * 128:(s + 1) * 128], dcc[:, s, ki, :],
                                   ki == 0 and s == 0, ki == 1 and s == 1, [r_ab[ki], r_dcc], [r_ps[pp[cc]]])
                    for cc in range(2):
                        ACT(ym[:, 4 + cc, 0:CTX], psb[pp[cc]][:, 0:256], AF.Copy, [r_ps[pp[cc]]], trs(r_ym, 4 + cc, 0, CTX))

                P.barrier()
                maybe_stop(10 * l + 3)
                o_s = 0
                dgq = [arena(o_s + i * 256, 128, F32) for i in range(2)]
                qs = [arena(o_s + 512 + i * 128, 128) for i in range(2)]
                sTt = [arena(o_s + 768 + i * 128, 128) for i in range(2)]
                kts = [arena(o_s + 1024 + i * 128, 128) for i in range(2)]
                Et = [arena(o_s + 1280 + i * 128, 128) for i in range(2)]
                Cst = [arena(o_s + 1536 + i * 260, 130, F32) for i in range(2)]
                Cbf = [arena(o_s + 2056 + i * 130, 130) for i in range(2)]
                rden = [arena(o_s + 2316 + i * 4, 2, F32) for i in range(2)]
                st1 = arena(o_s + 2324, 2 * NJ, F32)
                st2 = arena(o_s + 2396, 2 * NJ, F32)
                r_dgq, r_qs, r_sT, r_kts = [R(), R()], [R(), R()], [R(), R()], [R(), R()]
                r_dgb, r_Et = [R(), R()], [R(), R()]
                r_C, r_Cb, r_rden = [R(), R()], [R(), R()], [R(), R()]
                r_st = R()
                wqk_raw = arena(2468, 1024).rearrange("p (k c) -> p k c", c=128)
                r_wqk = R()
                dg3 = arena(3492, 384).rearrange("p (t c) -> p t c", c=128)
                wtapc = arena(3876, 4, F32)[:, 0:3]
                zraw = arena(3884, 2308)
                r_w3, r_wtap, r_zraw = R(), R(), R()
                o_g = 7332
                Gtok = arena(o_g, NJ * 16, F32).rearrange("p (j g) -> p j g", g=16)
                dgb = [arena(o_g + i * 256, 128, F32) for i in range(2)]
                LF = arena(o_g + 576, NJ * 8, F32).rearrange("p (j g) -> p j g", g=8)
                CM = arena(o_g + 864, NJ * 8, F32).rearrange("p (j g) -> p j g", g=8)
                Bc = arena(o_g + 1152, NJ * 8, F32).rearrange("p (j g) -> p j g", g=8)
                EB = arena(o_g + 1440, NJ * 8, F32).rearrange("p (j g) -> p j g", g=8)
                EBL = arena(o_g + 1728, NJ * 8, F32).rearrange("p (j g) -> p j g", g=8)
                ECL = arena(o_g + 2016, NJ * 8, F32).rearrange("p (j g) -> p j g", g=8)
                bg_s = arena(o_g + 2304, 16, F32)
                wg_s = arena(o_g + 2336, KC * 16).rearrange("p (k g) -> p k g", g=16)
                gmn_s = arena(o_g + 2464, 512, F32)
                r_gate = R()
                o_h = o_g + 3488
                qT = arena(o_h, NT)
                kT = arena(o_h + NT, NT)
                vaug = arena(o_h + 2 * NT, NJ * 130).rearrange("p (j e) -> p j e", e=130)
                sgo = arena(o_h + 2 * NT + 2340, NT).rearrange("p (j e) -> p j e", e=128)
                hsum = arena(o_h + 3 * NT + 2340, NT, F32).rearrange("p (j e) -> p j e", e=128)
                assert o_h + 5 * NT + 2340 <= SCR_ARENA, (o_h + 5 * NT + 2340)
                r_q, r_k, r_v, r_sg, r_hs = R(), R(), R(), R(), R()
                wvo_s = ym_flat[:, 3 * NT:3 * NT + KC * 256].rearrange("p (k c) -> p k c", c=256)
                r_wvo = R()

                DMA("pool", wg_s, wgate[l], [], [r_gate])
                DMA("sp", bg_s, bgate[l].partition_broadcast(128), [], [r_gate])
                DMA("sp", gmn_s, gmn[l].partition_broadcast(128), [], [r_gate])
                for jg in range(0, NJ, 6):
                    pi = next_ps()
                    for jj in range(6):
                        j = jg + jj
                        for kc in range(KC):
                            MM(psb[pi][:, jj * 16:(jj + 1) * 16], hx[:, kc, j * 128:(j + 1) * 128], wg_s[:, kc, :], kc == 0, kc == KC - 1,
                               [r_gate] + trs(r_hx, kc, j * 128, (j + 1) * 128), [r_ps[pi]])
                    TT("dve", Gtok[:, jg:jg + 6, :], psb[pi][:, 0:96].rearrange("p (j g) -> p j g", g=16),
                       bg_s.unsqueeze(1).to_broadcast([128, 6, 16]), ALU.add, [r_ps[pi], r_gate], [r_gate])
                CP("dve", CM[:, :, 0:4], Gtok[:, :, 0:4], [r_gate], [r_gate])
                CP("dve", CM[:, :, 4:8], Gtok[:, :, 8:12], [r_gate], [r_gate])
                ACT(LF[:, :, 0:4], Gtok[:, :, 4:8], AF.Abs, [r_gate], [r_gate])
                ACT(LF[:, :, 4:8], Gtok[:, :, 12:16], AF.Abs, [r_gate], [r_gate])
                ACT(LF[:], LF[:], AF.Exp, [r_gate], [r_gate], scale=-1.0)
                ACT(LF[:], LF[:], AF.Ln, [r_gate, r_const], [r_gate], bias=cst[:, 2:3], scale=1.0)
                TS("dve", EB[:, :, 0:4], Gtok[:, :, 4:8], 0.0, None, ALU.min, None, [r_gate], [r_gate])
                TS("dve", EB[:, :, 4:8], Gtok[:, :, 12:16], 0.0, None, ALU.min, None, [r_gate], [r_gate])
                TT("dve", LF[:], EB[:], LF[:], ALU.subtract, [r_gate], [r_gate])
                pi = next_ps()
                pbv = psb[pi][:, 0:NJ * 8].rearrange("p (j g) -> p j g", g=8)
                for j in range(NJ):
                    MM(pbv[:, j, 0:4], umask_f[:], LF[:, j, 0:4], True, True, [r_const, r_gate], [r_ps[pi]])
                    MM(pbv[:, j, 4:8], lmask_f[:], LF[:, j, 4:8], True, True, [r_const, r_gate], [r_ps[pi]])
                CP("dve", Bc[:], pbv, [r_ps[pi]], [r_gate])
                pi = next_ps()
                ptv = psb[pi][:, 0:NJ * 8].rearrange("p (j g) -> p j g", g=8)
                MM(psb[pi][:, 0:NJ * 8], ones_f[:], LF[:].rearrange("p j g -> p (j g)"), True, True, [r_const, r_gate], [r_ps[pi]])
                ACT(EBL[:], ptv, AF.Exp, [r_ps[pi]], [r_gate])
                ACT(EB[:], Bc[:], AF.Exp, [r_gate], [r_gate])
                TT("dve", CM[:], CM[:], Bc[:], ALU.subtract, [r_gate], [r_gate])
                TT("dve", ECL[:], CM[:], ptv, ALU.add, [r_gate, r_ps[pi]], [r_gate])
                ACT(ECL[:], ECL[:], AF.Exp, [r_gate], [r_gate])
                P.barrier()

                MS("dve", vaug[:, :, 128:129], 1.0, [r_v])
                MS("dve", zraw, 0.0, [r_zraw])

                def head_qk(h):
                    for qk in range(2):
                        dst, r_dst = (qT, r_q) if qk == 0 else (kT, r_k)
                        ci = qk * 4 + h
                        DMA("pool", wqk_raw, winc[l, ci], [], [r_wqk])
                        DMA("sp", wtapc, wqkp[l, ci], [], [r_wtap])
                        if qk == 1:
                            TS("dve", wtapc, wtapc, HD ** -0.5, None, ALU.mult, None, [r_wtap], [r_wtap])
                        for t in range(3):
                            TS("dve", dg3[:, t, :], ident_f[:], wtapc[:, t:t + 1], None, ALU.mult, None,
                               [r_const, r_wtap], [r_w3])

                        def zpos(a_):
                            return 1 + a_ if a_ < CTX else 3 + a_
                        for (a, b, w) in TL_all:
                            n = b - a
                            pi = next_ps()
                            for kc in range(KC):
                                MM(psb[pi][:, 0:n], wqk_raw[:, kc, :], hx[:, kc, a:b], kc == 0, kc == KC - 1,
                                   [r_wqk] + trs(r_hx, kc, a, b), [r_ps[pi]])
                            ACT(zraw[:, zpos(a):zpos(a) + n], psb[pi][:, 0:n], AF.Copy, [r_ps[pi]], [r_zraw])
                        for (a, b, w) in TL_all:
                            n = b - a
                            pi = next_ps()
                            for t in range(3):
                                MM(psb[pi][:, 0:n], dg3[:, t, :], zraw[:, zpos(a) + t - 1:zpos(a) + t - 1 + n], t == 0, t == 2,
                                   [r_w3, r_zraw], [r_ps[pi]])
                            ACT(dst[:, a:b], psb[pi][:, 0:n], AF.Copy, [r_ps[pi]], [r_dst])
                def head_vo(h):
                    DMA("pool", wvo_s, wvo[l, h], [], [r_wvo])
                    for j in range(NJ):
                        pi = next_ps()
                        for kc in range(KC):
                            MM(psb[pi][:, 0:256], hx[:, kc, j * 128:(j + 1) * 128], wvo_s[:, kc, :], kc == 0, kc == KC - 1,
                               [r_wvo] + trs(r_hx, kc, j * 128, (j + 1) * 128), [r_ps[pi]])
                        CP("dve", vaug[:, j, 0:128], psb[pi][:, 0:128], [r_ps[pi]], [r_v])
                        ACT(sgo[:, j, :], psb[pi][:, 128:256], AF.Sigmoid, [r_ps[pi]], [r_sg])
                        TT("dve", sgo[:, j, :], sgo[:, j, :], gmn_s[:, h * 128:(h + 1) * 128], ALU.mult, [r_sg, r_gate], [r_sg])
                def head_loop(h):
                    MS("dve", hsum, 0.0, [r_hs])
                    for d_ in range(2):
                        MS("dve", Cst[d_], 0.0, [r_C[d_]])
                        MS("dve", Cbf[d_], 0.0, [r_Cb[d_]])
                    order = [list(range(NJ)), [1, 0] + list(range(NJ - 1, 1, -1))]
                    t0f = tmpf[0]
                    t1b = tmpf[1][:, :].bitcast(BF16)
                    dgb2 = [dgb, [dgq[0], dgq[1]]]
                    sT2 = [sTt, [t1b[:, 256:384], t1b[:, 384:512]]]
                    kts2 = [kts, [t1b[:, 512:640], t1b[:, 640:768]]]
                    Et2 = [Et, [t1b[:, 768:896], t1b[:, 896:1024]]]
                    ndi = [t0f[:, 0:130], t0f[:, 130:260]]
                    ndi_den = t0f[:, 0:260].rearrange("p (d e) -> p d e", e=130)[:, :, 128:129]
                    rden_j = t0f[:, 392:394].unsqueeze(2)
                    r_rdj = R()
                    r_ndi = [R(), R()]
                    t0b = t0f[:, 260:390].bitcast(BF16)
                    Cbf2 = [Cbf, [t0b[:, 0:130], t0b[:, 130:260]]]
                    r_Cb2 = [r_Cb, [R(), R()]]
                    rr = lambda: [[R(), R()], [R(), R()]]
                    r_dgb2, r_sT2, r_kts2, r_Et2 = rr(), rr(), rr(), rr()

                    def info(step, d_):
                        j = order[d_][step]
                        return j, step % 2, d_ * 4 + h, (need_ctx or j >= 2), step == NJ - 1

                    ps_hold = {}

                    def a1(step, d_):
                        j, bs, col, need_out, is_last = info(step, d_)
                        if need_out:
                            TS("dve", dgb2[bs][d_], ident_f[:], Bc[:, j, col:col + 1], None, ALU.mult, None,
                               [r_const, r_gate], [r_dgb2[bs][d_]])
                        yield

                    def a2a(step, d_):
                        j, bs, col, need_out, is_last = info(step, d_)
                        c0, c1 = j * 128, (j + 1) * 128
                        if need_out:
                            pd_ = d_
                            MM(psb[pd_][:, 0:128], ones_f[:], dgb2[bs][d_], True, False, [r_const, r_dgb2[bs][d_]], [r_ps[pd_]])
                            MM(psb[pd_][:, 0:128], ident_b[:], (negm_f if d_ == 0 else negm_b)[:], False, True,
                               [r_const], [r_ps[pd_]])
                            yield
                            ACT(Et2[bs][d_], psb[pd_][:, 0:128], AF.Exp, [r_ps[pd_], r_gate], [r_Et2[bs][d_]],
                                bias=CM[:, j, col:col + 1], scale=1.0)
                            yield
                            p_s = 2 + d_
                            MM(psb[p_s][:, 0:128], kT[:, c0:c1], qT[:, c0:c1], True, True, [r_k, r_q], [r_ps[p_s]])
                            yield
                        if not is_last:
                            p_t = next_ps()
                            ptb = psb[p_t][:, 0:64].bitcast(BF16)
                            TR(ptb, kT[:, c0:c1], ident_b[:], [r_k, r_const], [r_ps[p_t]])
                            yield
                            ACT(kts2[bs][d_], ptb, AF.Copy, [r_ps[p_t], r_gate], [r_kts2[bs][d_]], scale=ECL[:, j, col:col + 1])
                            yield

                    def a2b(step, d_):
                        j, bs, col, need_out, is_last = info(step, d_)
                        if need_out:
                            p_s = 2 + d_
                            TT("dve", sT2[bs][d_], psb[p_s][:, 0:128], Et2[bs][d_], ALU.mult,
                               [r_ps[p_s], r_Et2[bs][d_]], [r_sT2[bs][d_]])
                        yield

                    def stage_b(step, d_):
                        j, bs, col, need_out, is_last = info(step, d_)
                        cur, nxt = Cbf2[step % 2][d_], Cbf2[(step + 1) % 2][d_]
                        r_cur, r_nxt = r_Cb2[step % 2][d_], r_Cb2[(step + 1) % 2][d_]
                        if not is_last:
                            p_u = next_ps()
                            MM(psb[p_u][:, 0:129], kts2[bs][d_], vaug[:, j, 0:129], True, True, [r_kts2[bs][d_], r_v], [r_ps[p_u]])
                            yield
                            STT("dve", Cst[d_][:, 0:129], Cst[d_][:, 0:129], EBL[:, j, col:col + 1], psb[p_u][:, 0:129],
                                ALU.mult, ALU.add, [r_C[d_], r_gate, r_ps[p_u]], [r_C[d_]])
                            yield
                            ACT(nxt[:, 0:129], Cst[d_][:, 0:129], AF.Copy, [r_C[d_]], [r_nxt])
                            yield

                    def stage_bo(step, d_):
                        j, bs, col, need_out, is_last = info(step, d_)
                        cur, r_cur = Cbf2[step % 2][d_], r_Cb2[step % 2][d_]
                        if need_out:
                            c0, c1 = j * 128, (j + 1) * 128
                            p_i = next_ps()
                            MM(psb[p_i][:, 0:129], qT[:, c0:c1], cur[:, 0:129], True, True, [r_q, r_cur], [r_ps[p_i]])
                            p_n = next_ps()
                            MM(psb[p_n][:, 0:129], sT2[bs][d_], vaug[:, j, 0:129], True, True, [r_sT2[bs][d_], r_v], [r_ps[p_n]])
                            yield
                            ACT(ndi[d_][:, 0:129], psb[p_i][:, 0:129], AF.Copy, [r_ps[p_i], r_gate], [r_ndi[d_]], scale=EB[:, j, col:col + 1])
                            yield
                            TT("dve", ndi[d_][:, 0:129], ndi[d_][:, 0:129], psb[p_n][:, 0:129], ALU.add, [r_ndi[d_], r_ps[p_n]], [r_ndi[d_]])
                            yield

                    def den_ops(step):
                        j, bs, col, need_out, is_last = info(step, 0)
                        if need_out:
                            STT("dve", rden_j, ndi_den, -1.0, ndi_den, ALU.mult, ALU.max, [r_ndi[0], r_ndi[1]], [r_rdj])
                            TS("dve", rden_j, rden_j, 1.0, None, ALU.max, None, [r_rdj], [r_rdj])
                            RECIP(rden_j, rden_j, [r_rdj], [r_rdj])

                    def stage_b2(step, d_):
                        j, bs, col, need_out, is_last = info(step, d_)
                        if need_out:
                            STT("dve", hsum[:, j, :], ndi[d_][:, 0:128], t0f[:, 392 + d_:393 + d_], hsum[:, j, :], ALU.mult, ALU.add,
                                [r_ndi[d_], r_rdj, r_hs], [r_hs])
                        yield

                    def interleave(*gens):
                        gens = list(gens)
                        while gens:
                            for g in list(gens):
                                try:
                                    next(g)
                                except StopIteration:
                                    gens.remove(g)

                    for bnk in range(4):
                        ps_reserved.add(bnk)
                    interleave(a1(0, 0), a1(0, 1))
                    interleave(a2a(0, 0), a2a(0, 1))
                    interleave(a2b(0, 0), a2b(0, 1))
                    interleave(a1(1, 0), a1(1, 1))
                    for step in range(NJ):
                        if step + 2 < NJ:
                            interleave(a1(step + 2, 0), a1(step + 2, 1))
                        if step + 1 < NJ:
                            interleave(a2a(step + 1, 0), a2a(step + 1, 1))
                        interleave(stage_b(step, 0), stage_b(step, 1))
                        if step + 1 < NJ:
                            interleave(a2b(step + 1, 0), a2b(step + 1, 1))
                        interleave(stage_bo(step, 0), stage_bo(step, 1))
                        den_ops(step)
                        interleave(stage_b2(step, 0), stage_b2(step, 1))
                    for bnk in range(4):
                        ps_reserved.discard(bnk)
                def head_out(h):
                    nj = NJ - j0
                    hv = hsum[:, j0:NJ, :]
                    sqv = ym[:, h, j0 * 128:NT].rearrange("p (j e) -> p j e", e=128)
                    ymh_res = trs(r_ym, h, j0 * 128, NT) + ([r_wvo] if h == 3 else [])
                    P.op("dve", (lambda hv=hv, nj=nj: lambda e: e.tensor_reduce(out=st1[:, 0:nj], in_=hv, axis=AX.X, op=ALU.add))(),
                         [r_hs], [r_st])
                    TT("dve", sqv, hv, hv, ALU.mult, [r_hs], ymh_res)
                    P.op("dve", (lambda sqv=sqv, nj=nj: lambda e: e.tensor_reduce(out=st1[:, NJ:NJ + nj], in_=sqv, axis=AX.X, op=ALU.add))(),
                         ymh_res, [r_st])
                    mean_ = st2[:, 0:nj]
                    var_ = st2[:, NJ:NJ + nj]
                    TS("dve", mean_, st1[:, 0:nj], 1.0 / HD, None, ALU.mult, None, [r_st], [r_st])
                    TT("dve", var_, mean_, mean_, ALU.mult, [r_st], [r_st])
                    STT("dve", var_, st1[:, NJ:NJ + nj], 1.0 / HD, var_, ALU.mult, ALU.subtract, [r_st], [r_st])
                    ACT(var_, var_, AF.Sqrt, [r_st, r_const], [r_st], bias=cst[:, 1:2], scale=1.0)
                    RECIP(var_, var_, [r_st], [r_st])
                    TT("dve", hv, hv, mean_.unsqueeze(2).to_broadcast([128, nj, 128]), ALU.subtract, [r_hs, r_st], [r_hs])
                    TT("dve", hv, hv, var_.unsqueeze(2).to_broadcast([128, nj, 128]), ALU.mult, [r_hs, r_st], [r_hs])
                    TT("dve", sgo[:, j0:NJ, :], hv, sgo[:, j0:NJ, :], ALU.mult, [r_hs, r_sg], [r_sg])
                    for jg in range(j0, NJ, 4):
                        p_t = next_ps()
                        ptb = psb[p_t][:, 0:256].bitcast(BF16)
                        jn = min(4, NJ - jg)
                        for jj in range(jn):
                            TR(ptb[:, jj * 128:(jj + 1) * 128], sgo[:, jg + jj, :], ident_b[:], [r_sg, r_const], [r_ps[p_t]])
                        ACT(ym[:, h, jg * 128:(jg + jn) * 128], ptb[:, 0:jn * 128], AF.Copy, [r_ps[p_t]],
                            trs(r_ym, h, jg * 128, (jg + jn) * 128) + ([r_wvo] if h == 3 else []))

                head_qk(0)
                head_vo(0)
                for h in range(NH):
                    head_loop(h)
                    if h + 1 < NH:
                        head_qk(h + 1)
                    head_out(h)
                    if h + 1 < NH:
                        head_vo(h + 1)
                if l == 0:
                    dump("d_ym", lambda kc: ym[:, kc, :], lambda kc: trs(r_ym, kc, 0, NT))

                P.barrier()
                maybe_stop(10 * l + 4)
                o_wo = 4096
                wo_s = arena(o_wo, KC * KC * 128).rearrange("p (o k c) -> p o k c", o=KC, k=KC)
                r_wo = R()
                obuf = arena(o_wo + 8192, KC * 512, F32).rearrange("p (o t) -> p o t", t=512)
                r_ob = [R() for _ in range(KC)]
                DMA("pool", wo_s.rearrange("p o k c -> p (o k c)"), woutc[l].rearrange("p o k c -> p (o k c)"), [],
                    [r_wo])
                for (a, b, w) in TL:
                    n = b - a
                    for oc in range(KC):
                        pi = next_ps()
                        for kc in range(KC):
                            MM(psb[pi][:, 0:n], wo_s[:, oc, kc, :], ym[:, kc, a:b], kc == 0, kc == KC - 1,
                               [r_wo] + trs(r_ym, kc, a, b), [r_ps[pi]])
                        ACT(obuf[:, oc, 0:n], psb[pi][:, 0:n], AF.Copy, [r_ps[pi]], [r_ob[oc]])
                    resid_update([(a, b, w)], lambda kc, a_, b_: obuf[:, kc, 0:b_ - a_], lambda kc, a_, b_: [r_ob[kc]], G1)
                if l == 0:
                    dump("d_x1", lambda kc: xs[:, kc, :], lambda kc: trs(r_xs, kc, 0, NT))

                P.barrier()
                maybe_stop(10 * l + 5)
                hT = scr[:, 0:24576].rearrange("p (f t) -> p f t", t=768)
                r_hT = [R() for _ in range(32)]
                ob2 = scr[:, 24576:30720].rearrange("p (o t) -> p o t", t=768)
                r_ob2 = [R() for _ in range(KC)]
                w1b = [scr[:, 30720 + i * 2048:30720 + (i + 1) * 2048].rearrange("p (g k c) -> p g k c", g=2, k=KC) for i in range(2)]
                w2b = [scr[:, 34816 + i * 4096:34816 + (i + 1) * 4096].rearrange("p (f c) -> p f c", c=128) for i in range(2)]
                r_w1b = [R(), R()]
                r_w2b = [R(), R()]
                if need_ctx:
                    supers = [[(0, 256, 1), (256, 768, 0)], [(768, 1280, 0), (1280, 1536, 0)], [(1536, 2048, 0), (2048, 2304, 0)]]
                else:
                    supers = [[(256, 768, 0), (768, 1024, 0)], [(1024, 1536, 0), (1536, 1792, 0)], [(1792, 2304, 0)]]
                n1 = 0
                n2 = 0
                prenorm(supers[0], A2, 24, l)
                for isup, sup in enumerate(supers):
                    a0 = sup[0][0]
                    for g in range(16):
                        i = n1 % 2
                        n1 += 1
                        DMA("pool", w1b[i].rearrange("p g k c -> p (g k c)"), w1c[l, g].rearrange("p g k c -> p (g k c)"), [],
                            [r_w1b[i]])
                        for f2 in range(2):
                            f = g * 2 + f2
                            for (a, b, w) in sup:
                                n = b - a
                                pi = next_ps()
                                for kc in range(KC):
                                    MM(psb[pi][:, 0:n], w1b[i][:, f2, kc, :], hx[:, kc, a:b], kc == 0, kc == KC - 1,
                                       [r_w1b[i]] + trs(r_hx, kc, a, b), [r_ps[pi]])
                                ti = next_tf()
                                ACT(tmpf[ti][:, 0:n], psb[pi][:, 0:n], AF.Relu, [r_ps[pi]], [r_tmpf[ti]])
                                ACT(hT[:, f, a - a0:b - a0], tmpf[ti][:, 0:n], AF.Square, [r_tmpf[ti]], [r_hT[f]])
                    if isup + 1 < len(supers):
                        prenorm(supers[isup + 1], A2, 24, l)
                    ssb = []
                    for _ in sup:
                        pss = next_ps()
                        ps_reserved.add(pss)
                        ssb.append(pss)
                    for oc in range(KC):
                        i = n2 % 2
                        n2 += 1
                        DMA("pool", w2b[i].rearrange("p f c -> p (f c)"), w2c[l, oc].rearrange("p f c -> p (f c)"), [],
                            [r_w2b[i]])
                        for si_, (a, b, w) in enumerate(sup):
                            n = b - a
                            pi = next_ps()
                            for f in range(32):
                                MM(psb[pi][:, 0:n], w2b[i][:, f, :], hT[:, f, a - a0:b - a0], f == 0, f == 31,
                                   [r_w2b[i], r_hT[f]], [r_ps[pi]])
                            ACT(ob2[:, oc, a - a0:b - a0], psb[pi][:, 0:n], AF.Copy, [r_ps[pi]], [r_ob2[oc]])
                            sq_i = next_sq()
                            ACT(sqb[sq_i][:, 0:n], psb[pi][:, 0:n], AF.Square, [r_ps[pi]], [r_sqb[sq_i]])
                            MM(psb[ssb[si_]][:, 0:n], ones_b[:], sqb[sq_i][:, 0:n], oc == 0, oc == KC - 1,
                               [r_sqb[sq_i], r_const], [r_ps[ssb[si_]]])
                    for si_, (a, b, w) in enumerate(sup):
                        n = b - a
                        ACT(rs[:, 0:n], psb[ssb[si_]][:, 0:n], AF.Sqrt, [r_ps[ssb[si_]], r_const], [r_rs], bias=cst[:, 0:1], scale=1.0)
                        RECIP(rs[:, 0:n], rs[:, 0:n], [r_rs], [r_rs])
                        for kc in range(KC):
                            ti = next_tf()
                            TT("dve", tmpf[ti][:, 0:n], ob2[:, kc, a - a0:b - a0], rs[:, 0:n], ALU.mult,
                               [r_ob2[kc], r_rs], [r_tmpf[ti]])
                            STT("dve", xs[:, kc, a:b], tmpf[ti][:, 0:n], G2[:, kc, w:w + 1], xs[:, kc, a:b], ALU.mult, ALU.add,
                                [r_tmpf[ti], r_lay] + trs(r_xs, kc, a, b), trs(r_xs, kc, a, b))
                    for pss in ssb:
                        ps_reserved.discard(pss)
                if l == 0:
                    dump("d_x2", lambda kc: xs[:, kc, :], lambda kc: trs(r_xs, kc, 0, NT))
                P.barrier()

        except _Stop:
            pass
        for kc in range(KC):
            DMA("sp", outT[kc], xs[:, kc, CTX:NT], trs(r_xs, kc, CTX, NT), [])
        P.emit()
    return nc


_CACHE = {}


def _consts():
    if "c" in _CACHE:
        return _CACHE["c"]
    c = {}
    c["ident"] = np.eye(128, dtype=np.float32)
    s = np.arange(128)
    c["umask"] = (s[:, None] <= s[None, :]).astype(np.float32)
    c["lmask"] = (s[:, None] >= s[None, :]).astype(np.float32)
    k = np.arange(64)
    ang = 2.0 * np.pi * np.outer(k, k) / 64.0
    cs = np.zeros((128, 256), np.float64)
    for hh in range(2):
        cs[hh * 64:(hh + 1) * 64, hh * 64:(hh + 1) * 64] = np.cos(ang) / 8.0
        cs[hh * 64:(hh + 1) * 64, 128 + hh * 64:128 + (hh + 1) * 64] = np.sin(ang) / 8.0
    c["cs64"] = cs.astype(np.float32)
    t = np.arange(SEQ, dtype=np.int64)
    ph = (np.outer(t, t) % SEQ).astype(np.float64) * (2.0 * np.pi / SEQ)
    dc = (np.cos(ph) / math.sqrt(SEQ)).astype(np.float32)
    ds = (-np.sin(ph) / math.sqrt(SEQ)).astype(np.float32)
    both = np.stack([dc, ds], 0)
    both = both.reshape(2, 4, 4, 128, 4, 512)
    c["dftx"] = np.ascontiguousarray(both.transpose(4, 1, 3, 0, 2, 5)).astype(ml_dtypes.bfloat16)
    t = np.arange(CTX, dtype=np.int64)
    ph = (np.outer(t, t) % CTX).astype(np.float64) * (2.0 * np.pi / CTX)
    both = np.stack([np.cos(ph), -np.sin(ph)], 0) / math.sqrt(CTX)
    both = both.reshape(2, 2, 128, CTX)
    c["dftc"] = np.ascontiguousarray(both.transpose(2, 0, 1, 3)).astype(ml_dtypes.bfloat16)
    rows = SEQ // 64
    quarter = D // 4
    freq = np.exp(-math.log(10000.0) * np.arange(quarter, dtype=np.float32) / quarter).astype(np.float32)
    r = np.broadcast_to(np.arange(rows, dtype=np.float32)[:, None], (rows, 64)).reshape(-1)
    col = np.broadcast_to(np.arange(64, dtype=np.float32)[None, :], (rows, 64)).reshape(-1)
    ar = r[:, None] * freq
    ac = col[:, None] * freq
    pos = np.concatenate([np.sin(ar), np.cos(ar), np.sin(ac), np.cos(ac)], axis=-1).astype(np.float32)
    c["posT"] = np.ascontiguousarray(pos.T).reshape(KC, 128, SEQ)
    _CACHE["c"] = c
    return c


def _chunk_w(w, cols):
    return np.ascontiguousarray(w[:, cols].reshape(KC, 128, -1).transpose(1, 0, 2))


def _prep_shared(inp):
    f = np.float32
    L = DEPTH
    w_in = np.asarray(inp["w_in"], f)
    sh = {}
    wada = np.asarray(inp["w_ada"], f)
    sh["wada"] = np.ascontiguousarray(wada.reshape(L, KC, 128, 12, 512).transpose(0, 3, 2, 1, 4))
    sh["bada"] = np.ascontiguousarray(np.asarray(inp["b_ada"], f).reshape(L, 48, 128).transpose(0, 2, 1))
    gs = np.stack([np.asarray(inp[k], f) for k in ("g_pre_mix", "g_post_mix", "g_pre_mlp", "g_post_mlp")], 1)
    sh["gvec"] = np.ascontiguousarray(gs.reshape(L, 4, KC, 128).transpose(0, 3, 1, 2))
    offs = [Q_OFF + 128 * i for i in range(4)] + [K_OFF + 128 * i for i in range(4)] + \
           [F_OFF, F_OFF + 128, CA_OFF, CA_OFF + 128, CG_OFF, CG_OFF + 128]
    sh["winc"] = np.stack([np.stack([_chunk_w(w_in[l], np.arange(o, o + 128)) for o in offs]) for l in range(L)])
    sh["wvo"] = np.stack([np.stack([_chunk_w(w_in[l], np.concatenate([np.arange(V_OFF + 128 * h, V_OFF + 128 * h + 128),
                                                                      np.arange(O_OFF + 128 * h, O_OFF + 128 * h + 128)]))
                                    for h in range(NH)]) for l in range(L)])
    sh["wgate"] = np.stack([_chunk_w(w_in[l], np.arange(G_OFF, G_OFF + 16)) for l in range(L)])
    sh["bgate"] = np.ascontiguousarray(np.asarray(inp["b_gate"], f).reshape(L, 16))
    wqk = np.asarray(inp["w_qk_conv"], f)
    sh["wqkp"] = np.ascontiguousarray(wqk.reshape(L, 3, 8, 128).transpose(0, 2, 3, 1))
    sh["gmn"] = np.ascontiguousarray(np.asarray(inp["g_mlstm_norm"], f))
    wdw = np.asarray(inp["w_dw"], f)
    sh["wdw"] = np.ascontiguousarray(wdw.reshape(L, CONV_K, 2, 128).transpose(0, 3, 2, 1))
    cv = np.stack([np.asarray(inp[k], f) for k in ("b_dw", "g_conv_ln", "b_conv_ln")], 1)
    sh["cvp"] = np.ascontiguousarray(cv.reshape(L, 3, 2, 128).transpose(0, 3, 1, 2))
    wout = np.asarray(inp["w_out"], f)
    sh["woutc"] = np.ascontiguousarray(wout.reshape(L, KC, 128, KC, 128).transpose(0, 2, 3, 1, 4))
    w1 = np.asarray(inp["w_mlp1"], f)
    sh["w1c"] = np.ascontiguousarray(w1.reshape(L, KC, 128, 16, 2, 128).transpose(0, 3, 2, 4, 1, 5))
    w2 = np.asarray(inp["w_mlp2"], f)
    sh["w2c"] = np.ascontiguousarray(w2.reshape(L, 32, 128, KC, 128).transpose(0, 3, 2, 1, 4))
    c = _consts()
    for k in ("ident", "umask", "lmask", "cs64", "dftx", "dftc", "posT"):
        sh[k] = c[k]
    return sh


def make_in_maps(inp, cores):
    sh = _prep_shared(inp)
    x = np.asarray(inp["x"], np.float32)
    ctx = np.asarray(inp["ctx"], np.float32)
    c = np.asarray(inp["c"], np.float32)
    c_ctx = np.asarray(inp["c_ctx"], np.float32)
    maps = []
    for b in cores:
        m = dict(sh)
        m["xT"] = np.ascontiguousarray(x[b].T).reshape(KC, 128, SEQ)
        m["ctxT"] = np.ascontiguousarray(ctx[b].T).reshape(KC, 128, CTX)
        cv = np.stack([c[b], c_ctx], -1)
        m["cvec"] = np.ascontiguousarray(cv.reshape(KC, 128, 2).transpose(1, 0, 2))
        maps.append(m)
    return maps


def kernel(**inputs):
    if "nc" not in _CACHE:
        _CACHE["nc"] = build_program(dbg=False)
    nc = _CACHE["nc"]
    in_maps = make_in_maps(inputs, list(range(NB)))
    res = run_bass_kernel_spmd(nc, in_maps, core_ids=list(range(NB)))
    out = np.empty((NB, SEQ, D), np.float32)
    for b in range(NB):
        oT = np.asarray(res.results[b]["outT"], np.float32).reshape(D, SEQ)
        out[b] = oT.T
    return out
```

```python
import math
from contextlib import ExitStack

import numpy as np
import ml_dtypes

import concourse.bass as bass
import concourse.mybir as mybir
from concourse.bass_utils import run_bass_kernel_spmd

F32 = mybir.dt.float32
BF16 = mybir.dt.bfloat16
AF = mybir.ActivationFunctionType
ALU = mybir.AluOpType
AX = mybir.AxisListType

ENGS = ("pe", "act", "dve", "pool", "sp")
SEM_LIM = 2000
N_DMA_SEMS = 16


class Res:
    __slots__ = ("name", "w", "r", "excl")

    def __init__(self, name, excl=False):
        self.name = name
        self.w = None
        self.r = []
        self.excl = excl


class Op:
    __slots__ = ("eng", "idx", "fn", "deps", "dma", "signal", "semref", "dma_slot", "dma_val")

    def __init__(self, eng, idx, fn, dma):
        self.eng = eng
        self.idx = idx
        self.fn = fn
        self.deps = []
        self.dma = dma
        self.signal = False
        self.semref = None
        self.dma_slot = None
        self.dma_val = None


class Prog:
    def __init__(self, nc, same_engine_sync=True):
        self.nc = nc
        self.ops = {e: [] for e in ENGS}
        self.n_dma_q = {}
        self.same_engine_sync = same_engine_sync

    def res(self, name="", excl=False):
        return Res(name, excl)

    def op(self, eng, fn, reads=(), writes=(), dma=False):
        o = Op(eng, len(self.ops[eng]), fn, dma)
        if dma:
            half = N_DMA_SEMS // 2
            k = self.n_dma_q.get(eng, 0)
            self.n_dma_q[eng] = k + 1
            o.dma_slot = (k % half) + (half if eng == "pool" else 0)
            o.dma_val = 16 * (k // half + 1)
        deps = []
        for r in reads:
            if r.w is not None:
                deps.append(r.w)
            if r.excl:
                deps.extend(x for x in r.r if x.eng != eng)
        for r in writes:
            if r.w is not None:
                deps.append(r.w)
            deps.extend(r.r)
        for r in reads:
            r.r.append(o)
        for r in writes:
            r.w = o
            r.r = []
        seen = set()
        for d in deps:
            if d is o or id(d) in seen:
                continue
            seen.add(id(d))
            if (not d.dma) and (not dma) and d.eng == eng:
                if eng == "pe" or not self.same_engine_sync:
                    continue
            o.deps.append(d)
        self.ops[eng].append(o)
        return o

    def barrier(self):
        targets = []
        for e in ENGS:
            cs = [o for o in self.ops[e] if not o.dma and o.fn is not None]
            if cs:
                targets.append(cs[-1])
        by_slot = {}
        for e in ENGS:
            for o in self.ops[e]:
                if o.dma and (o.dma_slot not in by_slot or o.dma_val > by_slot[o.dma_slot].dma_val):
                    by_slot[o.dma_slot] = o
        targets += list(by_slot.values())
        for e in ENGS:
            o = Op(e, len(self.ops[e]), None, False)
            o.deps = list(targets) if e != "pe" else [t for t in targets if t.dma or t.eng != e]
            self.ops[e].append(o)

    def emit(self):
        nc = self.nc
        for e in ENGS:
            for o in self.ops[e]:
                for d in o.deps:
                    d.signal = True
        with ExitStack() as st:
            dma_sems = [st.enter_context(nc.semaphore(f"dq{i}")) for i in range(N_DMA_SEMS)]
            for e in ENGS:
                n = sum(1 for o in self.ops[e] if o.signal and not o.dma)
                k = max((n + SEM_LIM - 1) // SEM_LIM, 1)
                sems = [st.enter_context(nc.semaphore(f"s_{e}{i}")) for i in range(k)]
                c = 0
                for o in self.ops[e]:
                    if o.signal and not o.dma:
                        o.semref = (sems[c // SEM_LIM], c % SEM_LIM + 1, c)
                        c += 1
            block = st.enter_context(nc.Block())

            def run(e, eng):
                waited_c = {p: -1 for p in ENGS}
                waited_d = [0] * N_DMA_SEMS
                for o in self.ops[e]:
                    for d in o.deps:
                        if d.dma:
                            if waited_d[d.dma_slot] >= d.dma_val:
                                continue
                            waited_d[d.dma_slot] = d.dma_val
                            eng.wait_ge(dma_sems[d.dma_slot], d.dma_val)
                        else:
                            sem, val, gc = d.semref
                            if waited_c[d.eng] >= gc:
                                continue
                            waited_c[d.eng] = gc
                            eng.wait_ge(sem, val)
                    if o.fn is None:
                        continue
                    if o.dma and o.dma_val > 16 and waited_d[o.dma_slot] < o.dma_val - 16:
                        waited_d[o.dma_slot] = o.dma_val - 16
                        eng.wait_ge(dma_sems[o.dma_slot], o.dma_val - 16)
                    ins = o.fn(eng)
                    if o.dma:
                        ins.then_inc(dma_sems[o.dma_slot], 16)
                    elif o.signal:
                        ins.then_inc(o.semref[0], 1)

            fin_dma = {}
            for e in ENGS:
                for o in self.ops[e]:
                    if o.dma:
                        fin_dma[o.dma_slot] = max(fin_dma.get(o.dma_slot, 0), o.dma_val)

            @block.tensor
            def _(eng):
                run("pe", eng)

            @block.scalar
            def _(eng):
                run("act", eng)

            @block.vector
            def _(eng):
                run("dve", eng)

            @block.gpsimd
            def _(eng):
                run("pool", eng)

            @block.sync
            def _(eng):
                run("sp", eng)
                for slot, val in sorted(fin_dma.items()):
                    eng.wait_ge(dma_sems[slot], val)


D = 1024
NB = 8
SEQ = 2048
CTX = 256
NT = CTX + SEQ
DEPTH = 2
KC = 8
NJ = NT // 128
HD = 128
NH = 4
DFF = 4096
EPS = 1e-6
CONV_K = 31
PADC = 15
Q_OFF, K_OFF, V_OFF, O_OFF, G_OFF = 0, 512, 1024, 1536, 2048
F_OFF = 2064
CA_OFF = 2320
CG_OFF = 2576
UPW = (CTX + 2 * PADC) + (SEQ + 2 * PADC)


def tiles_for(need_ctx):
    t = [(256, 768, 0), (768, 1280, 0), (1280, 1792, 0), (1792, 2304, 0)]
    if need_ctx:
        t = [(0, 256, 1)] + t
    return t


class _Stop(Exception):
    pass


def build_program(dbg=False, stop=None):
    nc = bass.Bass("TRN2", target_bir_lowering=False)
    P = Prog(nc)

    def din(name, shape, dt=F32):
        return nc.dram_tensor(name, list(shape), dt, kind="ExternalInput").ap()

    def dout(name, shape, dt=F32):
        return nc.dram_tensor(name, list(shape), dt, kind="ExternalOutput").ap()

    xT = din("xT", [KC, 128, SEQ])
    ctxT = din("ctxT", [KC, 128, CTX])
    posT = din("posT", [KC, 128, SEQ])
    cvec = din("cvec", [128, KC, 2])
    wada = din("wada", [DEPTH, 12, 128, KC, 512])
    bada = din("bada", [DEPTH, 128, 48])
    gvec = din("gvec", [DEPTH, 128, 4, KC])
    winc = din("winc", [DEPTH, 14, 128, KC, 128])
    wvo = din("wvo", [DEPTH, NH, 128, KC, 256])
    wgate = din("wgate", [DEPTH, 128, KC, 16])
    bgate = din("bgate", [DEPTH, 16])
    wqkp = din("wqkp", [DEPTH, 8, 128, 3])
    gmn = din("gmn", [DEPTH, 512])
    wdw = din("wdw", [DEPTH, 128, 2, CONV_K])
    cvp = din("cvp", [DEPTH, 128, 3, 2])
    woutc = din("woutc", [DEPTH, 128, KC, KC, 128])
    w1c = din("w1c", [DEPTH, 16, 128, 2, KC, 128])
    w2c = din("w2c", [DEPTH, KC, 128, 32, 128])
    ident_d = din("ident", [128, 128])
    umask_d = din("umask", [128, 128])
    lmask_d = din("lmask", [128, 128])
    cs64_d = din("cs64", [128, 256])
    dftx_d = din("dftx", [4, 4, 128, 2, 4, 512], BF16)
    dftc_d = din("dftc", [128, 2, 2, 256], BF16)
    outT = dout("outT", [KC, 128, SEQ])
    dbg_out = {}
    if dbg:
        for nm in ("d_hx", "d_ym", "d_x1", "d_x2"):
            dbg_out[nm] = dout(nm, [KC, 128, NT])

    with ExitStack() as st:
        def sb(name, shape, dt):
            return st.enter_context(nc.sbuf_tensor(name, list(shape), dt))

        xs = sb("xs", [128, KC, NT], F32)
        hx = sb("hx", [128, KC, NT], BF16)
        SCR_ARENA = 25088
        SCR_N = SCR_ARENA + KC * NT
        scr = sb("scr", [128, SCR_N], BF16)
        ym_flat = scr[:, SCR_ARENA:SCR_N]
        ym = ym_flat.rearrange("p (c t) -> p c t", t=NT)
        ident_f = sb("ident_f", [128, 128], F32)
        ident_b = sb("ident_b", [128, 128], BF16)
        umask_f = sb("umask_f", [128, 128], F32)
        lmask_f = sb("lmask_f", [128, 128], F32)
        negm_f = sb("negm_f", [128, 128], BF16)
        negm_b = sb("negm_b", [128, 128], BF16)
        ones_f = sb("ones_f", [128, 128], F32)
        ones_b = sb("ones_b", [128, 128], BF16)
        cs64 = sb("cs64b", [128, 256], BF16)
        cst = sb("cst", [128, 4], F32)
        cv_f = sb("cv_f", [128, KC, 2], F32)
        sc_b = sb("sc_b", [128, KC, 2], BF16)
        mod = sb("mod", [128, DEPTH, 48, 2], F32)
        bada_s = sb("bada_s", [128, DEPTH, 48], F32)
        gv = sb("gv", [128, DEPTH, 4, KC], F32)
        A1 = sb("A1", [128, KC, 2], F32)
        A2 = sb("A2", [128, KC, 2], F32)
        G1 = sb("G1", [128, KC, 2], F32)
        G2 = sb("G2", [128, KC, 2], F32)
        rs = sb("rs", [128, 512], F32)
        sqb = [sb(f"sqb{i}", [128, 512], BF16) for i in range(2)]
        tmpf = [sb(f"tmpf{i}", [128, 512], F32) for i in range(2)]
        psb = [st.enter_context(nc.psum_tensor(f"ps{i}", [128, 512], F32)) for i in range(8)]

        R = P.res
        r_xs = [[R() for _ in range(NJ)] for _ in range(KC)]
        r_hx = [[R() for _ in range(NJ)] for _ in range(KC)]
        r_ym = [[R() for _ in range(NJ)] for _ in range(KC)]
        r_ps = [P.res("ps", excl=True) for _ in range(8)]
        r_const = R()
        r_mod = R()
        r_lay = R()
        r_rs = R()
        r_sqb = [R(), R()]
        r_tmpf = [R(), R()]
        r_arena = R()

        def trs(rl, kc, a, b):
            return [rl[kc][j] for j in range(a // 128, (b + 127) // 128)]

        def trs_all(rl, a, b):
            out = []
            for kc in range(KC):
                out += trs(rl, kc, a, b)
            return out

        cnt = {"ps": 0, "sq": 0, "tf": 0}

        ps_reserved = set()

        def next_ps():
            while True:
                i = cnt["ps"] % 8
                cnt["ps"] += 1
                if i not in ps_reserved:
                    return i

        def next_sq():
            i = cnt["sq"] % 2
            cnt["sq"] += 1
            return i

        def next_tf():
            i = cnt["tf"] % 2
            cnt["tf"] += 1
            return i

        def MM(out, lhsT, rhs, start, stop, reads, writes):
            P.op("pe", lambda e: e.matmul(out, lhsT=lhsT, rhs=rhs, start=start, stop=stop), reads, writes)

        def TR(out, in_, ident, reads, writes):
            P.op("pe", lambda e: e.transpose(out, in_, ident), reads, writes)

        def ACT(out, in_, func, reads, writes, bias=None, scale=None, accum=None):
            kw = {}
            if bias is not None:
                kw["bias"] = bias
            if scale is not None:
                kw["scale"] = scale
            if accum is not None:
                kw["accum_out"] = accum
            P.op("act", lambda e: e.activation(out=out, in_=in_, func=func, **kw), reads, writes)

        def TT(eng, out, in0, in1, op, reads, writes):
            P.op(eng, lambda e: e.tensor_tensor(out=out, in0=in0, in1=in1, op=op), reads, writes)

        def TS(eng, out, in0, s1, s2, op0, op1, reads, writes):
            if s2 is None:
                P.op(eng, lambda e: e.tensor_scalar(out=out, in0=in0, scalar1=s1, scalar2=None, op0=op0), reads, writes)
            else:
                P.op(eng, lambda e: e.tensor_scalar(out=out, in0=in0, scalar1=s1, scalar2=s2, op0=op0, op1=op1), reads, writes)

        def STT(eng, out, in0, scalar, in1, op0, op1, reads, writes):
            P.op(eng, lambda e: e.scalar_tensor_tensor(out=out, in0=in0, scalar=scalar, in1=in1, op0=op0, op1=op1), reads, writes)

        def CP(eng, out, in_, reads, writes):
            P.op(eng, lambda e: e.tensor_copy(out=out, in_=in_), reads, writes)

        def MS(eng, ap, val, writes):
            P.op(eng, lambda e: e.memset(ap, val), (), writes)

        def RECIP(out, in_, reads, writes):
            P.op("dve", lambda e: e.reciprocal(out=out, in_=in_), reads, writes)

        def DMA(q, out, in_, reads, writes):
            P.op(q, lambda e: e.dma_start(out=out, in_=in_), reads, writes, dma=True)

        def arena(off, n, dt=BF16):
            if dt == BF16:
                return scr[:, off:off + n]
            assert off % 2 == 0
            return scr[:, off:off + 2 * n].bitcast(F32)

        DMA("sp", ident_f[:], ident_d, [], [r_const])
        DMA("sp", umask_f[:], umask_d, [], [r_const])
        DMA("sp", lmask_f[:], lmask_d, [], [r_const])
        DMA("pool", cs64[:], cs64_d, [], [r_const])
        DMA("sp", cv_f[:], cvec, [], [r_const])
        DMA("sp", bada_s[:], bada.rearrange("l p n -> p l n"), [], [r_const])
        DMA("sp", gv[:], gvec.rearrange("l p a k -> p l a k"), [], [r_const])
        CP("dve", ident_b[:], ident_f[:], [r_const], [r_const])
        TS("dve", negm_f[:], umask_f[:], -1.0, 30000.0, ALU.add, ALU.mult, [r_const], [r_const])
        TS("dve", negm_b[:], lmask_f[:], -1.0, 30000.0, ALU.add, ALU.mult, [r_const], [r_const])
        MS("dve", ones_f[:], 1.0, [r_const])
        MS("dve", ones_b[:], 1.0, [r_const])
        MS("dve", cst[:, 0:1], 1024.0 * EPS, [r_const])
        MS("dve", cst[:, 1:2], EPS, [r_const])
        MS("dve", cst[:, 2:3], 1.0, [r_const])
        MS("dve", cst[:, 3:4], 0.0, [r_const])

        for kc in range(KC):
            DMA("sp", xs[:, kc, CTX:NT], xT[kc], [], trs(r_xs, kc, CTX, NT))
            DMA("sp", xs[:, kc, 0:CTX], ctxT[kc], [], trs(r_xs, kc, 0, CTX))
        pos_buf = [arena(0, SEQ, F32), arena(2 * SEQ, SEQ, F32)]
        r_pos = [R(), R()]
        for kc in range(KC):
            i = kc % 2
            DMA("sp", pos_buf[i], posT[kc], [], [r_pos[i]])
            TT("dve", xs[:, kc, CTX:NT], xs[:, kc, CTX:NT], pos_buf[i], ALU.add,
               [r_pos[i]] + trs(r_xs, kc, CTX, NT), trs(r_xs, kc, CTX, NT))

        ACT(sc_b[:], cv_f[:], AF.Silu, [r_const], [r_const])
        wa_off = 4 * SEQ
        wa_buf = [arena(wa_off + i * KC * 512, KC * 512).rearrange("p (k c) -> p k c", c=512) for i in range(2)]
        r_wa = [R(), R()]
        def ada_block(l_, blk, buf, r_buf, pv_, r_pv):
            DMA("pool", buf, wada[l_, blk], [], [r_buf])
            for n4 in range(4):
                n = blk * 4 + n4
                for kc in range(KC):
                    MM(pv_[:, n, :], buf[:, kc, n4 * 128:(n4 + 1) * 128], sc_b[:, kc, :], kc == 0, kc == KC - 1,
                       [r_buf, r_const], [r_pv])

        pi = next_ps()
        pv = psb[pi][:, 0:96].rearrange("p (n w) -> p n w", w=2)
        for blk in range(4):
            ada_block(0, blk, wa_buf[blk % 2], r_wa[blk % 2], pv, r_ps[pi])
        TT("dve", mod[:, 0, 0:16], pv[:, 0:16], bada_s[:, 0, 0:16].unsqueeze(2).to_broadcast([128, 16, 2]), ALU.add,
           [r_ps[pi], r_const], [r_mod])
        ada_todo = [(0, blk) for blk in range(4, 12)] + [(1, blk) for blk in range(12)]

        def rstd_of(src_fn, n, reads):
            pi = next_ps()
            for kc in range(KC):
                si = next_sq()
                ACT(sqb[si][:, 0:n], src_fn(kc), AF.Square, reads(kc), [r_sqb[si]])
                MM(psb[pi][:, 0:n], ones_b[:], sqb[si][:, 0:n], kc == 0, kc == KC - 1, [r_sqb[si], r_const], [r_ps[pi]])
            ACT(rs[:, 0:n], psb[pi][:, 0:n], AF.Sqrt, [r_ps[pi], r_const], [r_rs], bias=cst[:, 0:1], scale=1.0)
            RECIP(rs[:, 0:n], rs[:, 0:n], [r_rs], [r_rs])

        def prenorm(tl, Amod, shift_base, l):
            for (a, b, w) in tl:
                n = b - a
                rstd_of(lambda kc: xs[:, kc, a:b], n, lambda kc: trs(r_xs, kc, a, b))
                for kc in range(KC):
                    ti = next_tf()
                    TT("dve", tmpf[ti][:, 0:n], xs[:, kc, a:b], rs[:, 0:n], ALU.mult,
                       trs(r_xs, kc, a, b) + [r_rs], [r_tmpf[ti]])
                    ACT(hx[:, kc, a:b], tmpf[ti][:, 0:n], AF.Identity, [r_tmpf[ti], r_lay, r_mod], trs(r_hx, kc, a, b),
                        bias=mod[:, l, shift_base + kc, w:w + 1], scale=Amod[:, kc, w:w + 1])

        def gen_prenorm(tl, Amod, shift_base, l):
            for (a, b, w) in tl:
                n = b - a
                pi = next_ps()
                ps_reserved.add(pi)
                for kc in range(KC):
                    si = next_sq()
                    ACT(sqb[si][:, 0:n], xs[:, kc, a:b], AF.Square, trs(r_xs, kc, a, b), [r_sqb[si]])
                    MM(psb[pi][:, 0:n], ones_b[:], sqb[si][:, 0:n], kc == 0, kc == KC - 1, [r_sqb[si], r_const], [r_ps[pi]])
                    yield
                ACT(rs[:, 0:n], psb[pi][:, 0:n], AF.Sqrt, [r_ps[pi], r_const], [r_rs], bias=cst[:, 0:1], scale=1.0)
                RECIP(rs[:, 0:n], rs[:, 0:n], [r_rs], [r_rs])
                ps_reserved.discard(pi)
                yield
                for kc in range(KC):
                    ti = next_tf()
                    TT("dve", tmpf[ti][:, 0:n], xs[:, kc, a:b], rs[:, 0:n], ALU.mult,
                       trs(r_xs, kc, a, b) + [r_rs], [r_tmpf[ti]])
                    ACT(hx[:, kc, a:b], tmpf[ti][:, 0:n], AF.Identity, [r_tmpf[ti], r_lay, r_mod], trs(r_hx, kc, a, b),
                        bias=mod[:, l, shift_base + kc, w:w + 1], scale=Amod[:, kc, w:w + 1])
                    yield

        def pull(gen, k):
            if gen is None:
                return
            for _ in range(k):
                try:
                    next(gen)
                except StopIteration:
                    return

        def drain(gen):
            if gen is None:
                return
            for _ in gen:
                pass

        def resid_update(tl, src_fn, src_reads, Gm):
            for (a, b, w) in tl:
                n = b - a
                rstd_of(lambda kc: src_fn(kc, a, b), n, lambda kc: src_reads(kc, a, b))
                for kc in range(KC):
                    ti = next_tf()
                    TT("dve", tmpf[ti][:, 0:n], src_fn(kc, a, b), rs[:, 0:n], ALU.mult,
                       src_reads(kc, a, b) + [r_rs], [r_tmpf[ti]])
                    STT("dve", xs[:, kc, a:b], tmpf[ti][:, 0:n], Gm[:, kc, w:w + 1], xs[:, kc, a:b], ALU.mult, ALU.add,
                        [r_tmpf[ti], r_lay] + trs(r_xs, kc, a, b), trs(r_xs, kc, a, b))

        def dump(name, src_fn, reads_fn):
            if not dbg:
                return
            for kc in range(KC):
                DMA("pool", dbg_out[name][kc], src_fn(kc), reads_fn(kc), [])

        def maybe_stop(k):
            if stop is not None and stop == k:
                raise _Stop()

        try:
            P.barrier()
            for l in range(DEPTH):
                need_ctx = l < DEPTH - 1
                TL_all = tiles_for(True)
                TL = tiles_for(need_ctx)
                j0 = 0 if need_ctx else 2

                def mk(dst, gidx, mbase, plus1):
                    for w in range(2):
                        if plus1:
                            STT("dve", dst[:, :, w], mod[:, l, mbase:mbase + KC, w], 1.0, gv[:, l, gidx, :], ALU.add, ALU.mult,
                                [r_mod, r_const, r_lay], [r_lay])
                        else:
                            TT("dve", dst[:, :, w], mod[:, l, mbase:mbase + KC, w], gv[:, l, gidx, :], ALU.mult,
                               [r_mod, r_const, r_lay], [r_lay])
                    TS("dve", dst[:], dst[:], 32.0, None, ALU.mult, None, [r_lay], [r_lay])

                mk(A1, 0, 8, True)
                if l > 0:
                    mk(G1, 1, 16, False)
                    mk(A2, 2, 32, True)
                    mk(G2, 3, 40, False)

                maybe_stop(10 * l + 1)

                o_w = 0
                wbuf = [arena(o_w + i * 1024, 1024).rearrange("p (k c) -> p k c", c=128) for i in range(4)]
                r_wbuf = [R() for _ in range(4)]
                wcnt = {"n": 0}

                def load_w(src):
                    i = wcnt["n"] % 4
                    wcnt["n"] += 1
                    DMA("pool", wbuf[i], src, [], [r_wbuf[i]])
                    return i

                o_up = 4096
                upad = arena(o_up, 2 * UPW).rearrange("p (c t) -> p c t", t=UPW)
                r_up = R()
                o_vb = o_up + 2 * UPW
                vbuf = arena(o_vb, 2 * NT, F32).rearrange("p (c t) -> p c t", t=NT)
                r_vb = [R(), R()]
                o_cs = o_vb + 4 * NT
                wdw_s = arena(o_cs, 2 * CONV_K, F32).rearrange("p (c k) -> p c k", k=CONV_K)
                cvp_s = arena(o_cs + 4 * CONV_K, 6, F32).rearrange("p (a c) -> p a c", c=2)
                r_cs = R()
                lnt = [ym_flat[:, i * 1024:(i + 1) * 1024].bitcast(F32) for i in range(5)]
                r_lnt = [R() for _ in range(5)]
                dgm = ym_flat[:, 5120:5120 + 2 * CONV_K * 128].rearrange("p (c k m) -> p c k m", k=CONV_K, m=128)
                r_dg = R()

                DMA("sp", wdw_s, wdw[l], [], [r_cs])
                DMA("sp", cvp_s, cvp[l], [], [r_cs])
                MS("dve", upad, 0.0, [r_up])
                for cc in range(2):
                    for k in range(CONV_K):
                        TS("dve", dgm[:, cc, k, :], ident_f[:], wdw_s[:, cc, k:k + 1], None, ALU.mult, None,
                           [r_const, r_cs], [r_dg])

                def upos(a):
                    return a + PADC if a < CTX else (CTX + 2 * PADC) + (a - CTX) + PADC

                iag = [(load_w(winc[l, 10 + cc]), load_w(winc[l, 12 + cc])) for cc in range(2)]
                if l == 0:
                    wa_d = arena(18200, KC * 512).rearrange("p (k c) -> p k c", c=512)
                    r_wad = R()
                    pb0, pb1 = next_ps(), next_ps()
                    ps_reserved.add(pb0)
                    ps_reserved.add(pb1)
                    pvd = [psb[pb0][:, 0:96].rearrange("p (n w) -> p n w", w=2), psb[pb1][:, 0:96].rearrange("p (n w) -> p n w", w=2)]
                    r_pvd = [r_ps[pb0], r_ps[pb1]]

                def ada_some(k):
                    if l != 0:
                        return
                    for _ in range(k):
                        if ada_todo:
                            l_, blk = ada_todo.pop(0)
                            ada_block(l_, blk, wa_d, r_wad, pvd[l_], r_pvd[l_])

                for (a, b, w) in TL:
                    ada_some(2)
                    prenorm([(a, b, w)], A1, 0, l)
                    for cc in range(2):
                        ia, ig = iag[cc]
                        n = b - a
                        pa = next_ps()
                        for kc in range(KC):
                            MM(psb[pa][:, 0:n], wbuf[ia][:, kc, :], hx[:, kc, a:b], kc == 0, kc == KC - 1,
                               [r_wbuf[ia]] + trs(r_hx, kc, a, b), [r_ps[pa]])
                        pg = next_ps()
                        for kc in range(KC):
                            MM(psb[pg][:, 0:n], wbuf[ig][:, kc, :], hx[:, kc, a:b], kc == 0, kc == KC - 1,
                               [r_wbuf[ig]] + trs(r_hx, kc, a, b), [r_ps[pg]])
                        ti = next_tf()
                        ACT(tmpf[ti][:, 0:n], psb[pg][:, 0:n], AF.Sigmoid, [r_ps[pg]], [r_tmpf[ti]])
                        TT("dve", upad[:, cc, upos(a):upos(a) + n], psb[pa][:, 0:n], tmpf[ti][:, 0:n], ALU.mult,
                           [r_ps[pa], r_tmpf[ti]], [r_up])
                if not need_ctx:
                    prenorm([(0, 256, 1)], A1, 0, l)
                if l == 0:
                    dump("d_hx", lambda kc: hx[:, kc, :], lambda kc: trs(r_hx, kc, 0, NT))
                for cc in range(2):
                    for (a, b, w) in TL:
                        ada_some(1)
                        n = b - a
                        pi = next_ps()
                        p0 = upos(a) - PADC
                        for k in range(CONV_K):
                            MM(psb[pi][:, 0:n], dgm[:, cc, k, :], upad[:, cc, p0 + k:p0 + k + n], k == 0, k == CONV_K - 1,
                               [r_dg, r_up], [r_ps[pi]])
                        ACT(vbuf[:, cc, a:b], psb[pi][:, 0:n], AF.Identity, [r_ps[pi], r_cs], [r_vb[cc]],
                            bias=cvp_s[:, 0, cc:cc + 1], scale=1.0)
                if l == 0:
                    ada_some(len(ada_todo))
                    TT("dve", mod[:, 0, 16:48], pvd[0][:, 16:48], bada_s[:, 0, 16:48].unsqueeze(2).to_broadcast([128, 32, 2]), ALU.add,
                       [r_pvd[0], r_const], [r_mod])
                    TT("dve", mod[:, 1], pvd[1], bada_s[:, 1].unsqueeze(2).to_broadcast([128, 48, 2]), ALU.add,
                       [r_pvd[1], r_const], [r_mod])
                    ps_reserved.discard(pb0)
                    ps_reserved.discard(pb1)
                    mk(G1, 1, 16, False)
                    mk(A2, 2, 32, True)
                    mk(G2, 3, 40, False)
                for (a, b, w) in TL:
                    n = b - a
                    p1 = next_ps()
                    p2 = next_ps()
                    for cc in range(2):
                        MM(psb[p1][:, 0:n], ones_f[:], vbuf[:, cc, a:b], cc == 0, cc == 1, [r_const, r_vb[cc]], [r_ps[p1]])
                    for cc in range(2):
                        TT("dve", lnt[0][:, 0:n], vbuf[:, cc, a:b], vbuf[:, cc, a:b], ALU.mult, [r_vb[cc]], [r_lnt[0]])
                        MM(psb[p2][:, 0:n], ones_f[:], lnt[0][:, 0:n], cc == 0, cc == 1, [r_const, r_lnt[0]], [r_ps[p2]])
                    mean = lnt[1]
                    TS("dve", mean[:, 0:n], psb[p1][:, 0:n], 1.0 / 256.0, None, ALU.mult, None, [r_ps[p1]], [r_lnt[1]])
                    TT("dve", lnt[2][:, 0:n], mean[:, 0:n], mean[:, 0:n], ALU.mult, [r_lnt[1]], [r_lnt[2]])
                    STT("dve", lnt[2][:, 0:n], psb[p2][:, 0:n], 1.0 / 256.0, lnt[2][:, 0:n], ALU.mult, ALU.subtract,
                        [r_ps[p2], r_lnt[2]], [r_lnt[2]])
                    ACT(lnt[2][:, 0:n], lnt[2][:, 0:n], AF.Sqrt, [r_lnt[2], r_const], [r_lnt[2]], bias=cst[:, 1:2], scale=1.0)
                    RECIP(lnt[2][:, 0:n], lnt[2][:, 0:n], [r_lnt[2]], [r_lnt[2]])
                    for cc in range(2):
                        TT("dve", lnt[3 + cc][:, 0:n], vbuf[:, cc, a:b], mean[:, 0:n], ALU.subtract,
                           [r_vb[cc], r_lnt[1]], [r_lnt[3 + cc]])
                        TT("dve", lnt[3 + cc][:, 0:n], lnt[3 + cc][:, 0:n], lnt[2][:, 0:n], ALU.mult,
                           [r_lnt[3 + cc], r_lnt[2]], [r_lnt[3 + cc]])
                        ACT(ym[:, 6 + cc, a:b], lnt[3 + cc][:, 0:n], AF.Silu, [r_lnt[3 + cc], r_cs], trs(r_ym, 6 + cc, a, b),
                            bias=cvp_s[:, 2, cc:cc + 1], scale=cvp_s[:, 1, cc:cc + 1])
                P.barrier()

                maybe_stop(10 * l + 2)
                o_uf = 4096
                uF = arena(o_uf, 2 * NT).rearrange("p (c t) -> p c t", t=NT)
                r_uf = [R(), R()]
                o_dp = o_uf + 2 * NT
                dpc = [arena(o_dp + i * 4096, 4096).rearrange("p (s k t) -> p s k t", s=2, k=4) for i in range(3)]
                r_dpc = [R() for _ in range(3)]
                o_dc = o_dp + 3 * 4096
                dcc = arena(o_dc, 1024).rearrange("p (s k t) -> p s k t", s=2, k=2)
                r_dcc = R()
                AB = ym_flat[:, 0:NJ * 512].rearrange("p (j c m) -> p j c m", c=2, m=256)
                r_ab = [R() for _ in range(NJ)]

                for cc in range(2):
                    iw = load_w(winc[l, 8 + cc])
                    for (a, b, w) in TL:
                        n = b - a
                        pi = next_ps()
                        for kc in range(KC):
                            MM(psb[pi][:, 0:n], wbuf[iw][:, kc, :], hx[:, kc, a:b], kc == 0, kc == KC - 1,
                               [r_wbuf[iw]] + trs(r_hx, kc, a, b), [r_ps[pi]])
                        ACT(uF[:, cc, a:b], psb[pi][:, 0:n], AF.Copy, [r_ps[pi]], [r_uf[cc]])
                for j in range(j0, NJ):
                    pi = next_ps()
                    for cc in range(2):
                        MM(psb[pi][:, cc * 256:(cc + 1) * 256], uF[:, cc, j * 128:(j + 1) * 128], cs64[:], True, True,
                           [r_uf[cc], r_const], [r_ps[pi]])
                    CP("dve", AB[:, j].rearrange("p c m -> p (c m)"), psb[pi][:, :], [r_ps[pi]], [r_ab[j]])
                npc = 0
                for tq in range(4):
                    pp = [next_ps(), next_ps()]
                    for kg in range(4):
                        i = npc % 3
                        npc += 1
                        DMA("sp", dpc[i].rearrange("p s k t -> p (s k t)"),
                            dftx_d[tq, kg].rearrange("p s k t -> p (s k t)"), [], [r_dpc[i]])
                        for ki in range(4):
                            j = 2 + kg * 4 + ki
                            for s in range(2):
                                first = (kg == 0 and ki == 0 and s == 0)
                                last = (kg == 3 and ki == 3 and s == 1)
                                for cc in range(2):
                                    MM(psb[pp[cc]][:, :], AB[:, j, cc, s * 128:(s + 1) * 128], dpc[i][:, s, ki, :], first, last,
                                       [r_ab[j], r_dpc[i]], [r_ps[pp[cc]]])
                    for cc in range(2):
                        a = CTX + tq * 512
                        ACT(ym[:, 4 + cc, a:a + 512], psb[pp[cc]][:, :], AF.Copy, [r_ps[pp[cc]]], trs(r_ym, 4 + cc, a, a + 512))
                if need_ctx:
                    DMA("sp", dcc.rearrange("p s k t -> p (s k t)"), dftc_d.rearrange("p s k t -> p (s k t)"), [], [r_dcc])
                    pp = [next_ps(), next_ps()]
                    for ki in range(2):
                        for s in range(2):
                            for cc in range(2):
                                MM(psb[pp[cc]][:, 0:256], AB[:, ki, cc, s * 128:(s + 1) * 128], dcc[:, s, ki, :],
                                   ki == 0 and s == 0, ki == 1 and s == 1, [r_ab[ki], r_dcc], [r_ps[pp[cc]]])
                    for cc in range(2):
                        ACT(ym[:, 4 + cc, 0:CTX], psb[pp[cc]][:, 0:256], AF.Copy, [r_ps[pp[cc]]], trs(r_ym, 4 + cc, 0, CTX))

                P.barrier()
                maybe_stop(10 * l + 3)
                o_s = 0
                dgq = [arena(o_s + i * 256, 128, F32) for i in range(2)]
                qs = [arena(o_s + 512 + i * 128, 128) for i in range(2)]
                sTt = [arena(o_s + 768 + i * 128, 128) for i in range(2)]
                kts = [arena(o_s + 1024 + i * 128, 128) for i in range(2)]
                Et = [arena(o_s + 1280 + i * 128, 128) for i in range(2)]
                Cst = [arena(o_s + 1536 + i * 260, 130, F32) for i in range(2)]
                Cbf = [arena(o_s + 2056 + i * 130, 130) for i in range(2)]
                rden = [arena(o_s + 2316 + i * 4, 2, F32) for i in range(2)]
                st1 = arena(o_s + 2324, 2 * NJ, F32)
                st2 = arena(o_s + 2396, 2 * NJ, F32)
                r_dgq, r_qs, r_sT, r_kts = [R(), R()], [R(), R()], [R(), R()], [R(), R()]
                r_dgb, r_Et = [R(), R()], [R(), R()]
                r_C, r_Cb, r_rden = [R(), R()], [R(), R()], [R(), R()]
                r_st = R()
                wqk_raw = arena(2468, 1024).rearrange("p (k c) -> p k c", c=128)
                r_wqk = R()
                dg3 = arena(3492, 384).rearrange("p (t c) -> p t c", c=128)
                wtapc = arena(3876, 4, F32)[:, 0:3]
                zraw = arena(3884, 2308)
                r_w3, r_wtap, r_zraw = R(), R(), R()
                o_g = 7332
                Gtok = arena(o_g, NJ * 16, F32).rearrange("p (j g) -> p j g", g=16)
                dgb = [arena(o_g + i * 256, 128, F32) for i in range(2)]
                LF = arena(o_g + 576, NJ * 8, F32).rearrange("p (j g) -> p j g", g=8)
                CM = arena(o_g + 864, NJ * 8, F32).rearrange("p (j g) -> p j g", g=8)
                Bc = arena(o_g + 1152, NJ * 8, F32).rearrange("p (j g) -> p j g", g=8)
                EB = arena(o_g + 1440, NJ * 8, F32).rearrange("p (j g) -> p j g", g=8)
                EBL = arena(o_g + 1728, NJ * 8, F32).rearrange("p (j g) -> p j g", g=8)
                ECL = arena(o_g + 2016, NJ * 8, F32).rearrange("p (j g) -> p j g", g=8)
                bg_s = arena(o_g + 2304, 16, F32)
                wg_s = arena(o_g + 2336, KC * 16).rearrange("p (k g) -> p k g", g=16)
                gmn_s = arena(o_g + 2464, 512, F32)
                r_gate = R()
                o_h = o_g + 3488
                qT = arena(o_h, NT)
                kT = arena(o_h + NT, NT)
                vaug = arena(o_h + 2 * NT, NJ * 130).rearrange("p (j e) -> p j e", e=130)
                sgo = arena(o_h + 2 * NT + 2340, NT).rearrange("p (j e) -> p j e", e=128)
                hsum = arena(o_h + 3 * NT + 2340, NT, F32).rearrange("p (j e) -> p j e", e=128)
                assert o_h + 5 * NT + 2340 <= SCR_ARENA, (o_h + 5 * NT + 2340)
                r_q, r_k, r_v, r_sg, r_hs = R(), R(), R(), R(), R()
                wvo_s = ym_flat[:, 3 * NT:3 * NT + KC * 256].rearrange("p (k c) -> p k c", c=256)
                r_wvo = R()

                DMA("pool", wg_s, wgate[l], [], [r_gate])
                DMA("sp", bg_s, bgate[l].partition_broadcast(128), [], [r_gate])
                DMA("sp", gmn_s, gmn[l].partition_broadcast(128), [], [r_gate])
                for jg in range(0, NJ, 6):
                    pi = next_ps()
                    for jj in range(6):
                        j = jg + jj
                        for kc in range(KC):
                            MM(psb[pi][:, jj * 16:(jj + 1) * 16], hx[:, kc, j * 128:(j + 1) * 128], wg_s[:, kc, :], kc == 0, kc == KC - 1,
                               [r_gate] + trs(r_hx, kc, j * 128, (j + 1) * 128), [r_ps[pi]])
                    TT("dve", Gtok[:, jg:jg + 6, :], psb[pi][:, 0:96].rearrange("p (j g) -> p j g", g=16),
                       bg_s.unsqueeze(1).to_broadcast([128, 6, 16]), ALU.add, [r_ps[pi], r_gate], [r_gate])
                CP("dve", CM[:, :, 0:4], Gtok[:, :, 0:4], [r_gate], [r_gate])
                CP("dve", CM[:, :, 4:8], Gtok[:, :, 8:12], [r_gate], [r_gate])
                ACT(LF[:, :, 0:4], Gtok[:, :, 4:8], AF.Abs, [r_gate], [r_gate])
                ACT(LF[:, :, 4:8], Gtok[:, :, 12:16], AF.Abs, [r_gate], [r_gate])
                ACT(LF[:], LF[:], AF.Exp, [r_gate], [r_gate], scale=-1.0)
                ACT(LF[:], LF[:], AF.Ln, [r_gate, r_const], [r_gate], bias=cst[:, 2:3], scale=1.0)
                TS("dve", EB[:, :, 0:4], Gtok[:, :, 4:8], 0.0, None, ALU.min, None, [r_gate], [r_gate])
                TS("dve", EB[:, :, 4:8], Gtok[:, :, 12:16], 0.0, None, ALU.min, None, [r_gate], [r_gate])
                TT("dve", LF[:], EB[:], LF[:], ALU.subtract, [r_gate], [r_gate])
                pi = next_ps()
                pbv = psb[pi][:, 0:NJ * 8].rearrange("p (j g) -> p j g", g=8)
                for j in range(NJ):
                    MM(pbv[:, j, 0:4], umask_f[:], LF[:, j, 0:4], True, True, [r_const, r_gate], [r_ps[pi]])
                    MM(pbv[:, j, 4:8], lmask_f[:], LF[:, j, 4:8], True, True, [r_const, r_gate], [r_ps[pi]])
                CP("dve", Bc[:], pbv, [r_ps[pi]], [r_gate])
                pi = next_ps()
                ptv = psb[pi][:, 0:NJ * 8].rearrange("p (j g) -> p j g", g=8)
                MM(psb[pi][:, 0:NJ * 8], ones_f[:], LF[:].rearrange("p j g -> p (j g)"), True, True, [r_const, r_gate], [r_ps[pi]])
                ACT(EBL[:], ptv, AF.Exp, [r_ps[pi]], [r_gate])
                ACT(EB[:], Bc[:], AF.Exp, [r_gate], [r_gate])
                TT("dve", CM[:], CM[:], Bc[:], ALU.subtract, [r_gate], [r_gate])
                TT("dve", ECL[:], CM[:], ptv, ALU.add, [r_gate, r_ps[pi]], [r_gate])
                ACT(ECL[:], ECL[:], AF.Exp, [r_gate], [r_gate])
                P.barrier()

                MS("dve", vaug[:, :, 128:129], 1.0, [r_v])
                MS("dve", zraw, 0.0, [r_zraw])

                def head_qk(h):
                    for qk in range(2):
                        dst, r_dst = (qT, r_q) if qk == 0 else (kT, r_k)
                        ci = qk * 4 + h
                        DMA("pool", wqk_raw, winc[l, ci], [], [r_wqk])
                        DMA("sp", wtapc, wqkp[l, ci], [], [r_wtap])
                        if qk == 1:
                            TS("dve", wtapc, wtapc, HD ** -0.5, None, ALU.mult, None, [r_wtap], [r_wtap])
                        for t in range(3):
                            TS("dve", dg3[:, t, :], ident_f[:], wtapc[:, t:t + 1], None, ALU.mult, None,
                               [r_const, r_wtap], [r_w3])

                        def zpos(a_):
                            return 1 + a_ if a_ < CTX else 3 + a_
                        for (a, b, w) in TL_all:
                            n = b - a
                            pi = next_ps()
                            for kc in range(KC):
                                MM(psb[pi][:, 0:n], wqk_raw[:, kc, :], hx[:, kc, a:b], kc == 0, kc == KC - 1,
                                   [r_wqk] + trs(r_hx, kc, a, b), [r_ps[pi]])
                            ACT(zraw[:, zpos(a):zpos(a) + n], psb[pi][:, 0:n], AF.Copy, [r_ps[pi]], [r_zraw])
                        for (a, b, w) in TL_all:
                            n = b - a
                            pi = next_ps()
                            for t in range(3):
                                MM(psb[pi][:, 0:n], dg3[:, t, :], zraw[:, zpos(a) + t - 1:zpos(a) + t - 1 + n], t == 0, t == 2,
                                   [r_w3, r_zraw], [r_ps[pi]])
                            ACT(dst[:, a:b], psb[pi][:, 0:n], AF.Copy, [r_ps[pi]], [r_dst])
                def head_vo(h):
                    DMA("pool", wvo_s, wvo[l, h], [], [r_wvo])
                    for j in range(NJ):
                        pi = next_ps()
                        for kc in range(KC):
                            MM(psb[pi][:, 0:256], hx[:, kc, j * 128:(j + 1) * 128], wvo_s[:, kc, :], kc == 0, kc == KC - 1,
                               [r_wvo] + trs(r_hx, kc, j * 128, (j + 1) * 128), [r_ps[pi]])
                        CP("dve", vaug[:, j, 0:128], psb[pi][:, 0:128], [r_ps[pi]], [r_v])
                        ACT(sgo[:, j, :], psb[pi][:, 128:256], AF.Sigmoid, [r_ps[pi]], [r_sg])
                        TT("dve", sgo[:, j, :], sgo[:, j, :], gmn_s[:, h * 128:(h + 1) * 128], ALU.mult, [r_sg, r_gate], [r_sg])
                def head_loop(h):
                    MS("dve", hsum, 0.0, [r_hs])
                    for d_ in range(2):
                        MS("dve", Cst[d_], 0.0, [r_C[d_]])
                        MS("dve", Cbf[d_], 0.0, [r_Cb[d_]])
                    order = [list(range(NJ)), [1, 0] + list(range(NJ - 1, 1, -1))]
                    t0f = tmpf[0]
                    t1b = tmpf[1][:, :].bitcast(BF16)
                    dgb2 = [dgb, [dgq[0], dgq[1]]]
                    sT2 = [sTt, [t1b[:, 256:384], t1b[:, 384:512]]]
                    kts2 = [kts, [t1b[:, 512:640], t1b[:, 640:768]]]
                    Et2 = [Et, [t1b[:, 768:896], t1b[:, 896:1024]]]
                    ndi = [t0f[:, 0:130], t0f[:, 130:260]]
                    ndi_den = t0f[:, 0:260].rearrange("p (d e) -> p d e", e=130)[:, :, 128:129]
                    rden_j = t0f[:, 392:394].unsqueeze(2)
                    r_rdj = R()
                    r_ndi = [R(), R()]
                    t0b = t0f[:, 260:390].bitcast(BF16)
                    Cbf2 = [Cbf, [t0b[:, 0:130], t0b[:, 130:260]]]
                    r_Cb2 = [r_Cb, [R(), R()]]
                    rr = lambda: [[R(), R()], [R(), R()]]
                    r_dgb2, r_sT2, r_kts2, r_Et2 = rr(), rr(), rr(), rr()

                    def info(step, d_):
                        j = order[d_][step]
                        return j, step % 2, d_ * 4 + h, (need_ctx or j >= 2), step == NJ - 1

                    ps_hold = {}

                    def a1(step, d_):
                        j, bs, col, need_out, is_last = info(step, d_)
                        if need_out:
                            TS("dve", dgb2[bs][d_], ident_f[:], Bc[:, j, col:col + 1], None, ALU.mult, None,
                               [r_const, r_gate], [r_dgb2[bs][d_]])
                        yield

                    def a2a(step, d_):
                        j, bs, col, need_out, is_last = info(step, d_)
                        c0, c1 = j * 128, (j + 1) * 128
                        if need_out:
                            pd_ = d_
                            MM(psb[pd_][:, 0:128], ones_f[:], dgb2[bs][d_], True, False, [r_const, r_dgb2[bs][d_]], [r_ps[pd_]])
                            MM(psb[pd_][:, 0:128], ident_b[:], (negm_f if d_ == 0 else negm_b)[:], False, True,
                               [r_const], [r_ps[pd_]])
                            yield
                            ACT(Et2[bs][d_], psb[pd_][:, 0:128], AF.Exp, [r_ps[pd_], r_gate], [r_Et2[bs][d_]],
                                bias=CM[:, j, col:col + 1], scale=1.0)
                            yield
                            p_s = 2 + d_
                            MM(psb[p_s][:, 0:128], kT[:, c0:c1], qT[:, c0:c1], True, True, [r_k, r_q], [r_ps[p_s]])
                            yield
                        if not is_last:
                            p_t = next_ps()
                            ptb = psb[p_t][:, 0:64].bitcast(BF16)
                            TR(ptb, kT[:, c0:c1], ident_b[:], [r_k, r_const], [r_ps[p_t]])
                            yield
                            ACT(kts2[bs][d_], ptb, AF.Copy, [r_ps[p_t], r_gate], [r_kts2[bs][d_]], scale=ECL[:, j, col:col + 1])
                            yield

                    def a2b(step, d_):
                        j, bs, col, need_out, is_last = info(step, d_)
                        if need_out:
                            p_s = 2 + d_
                            TT("dve", sT2[bs][d_], psb[p_s][:, 0:128], Et2[bs][d_], ALU.mult,
                               [r_ps[p_s], r_Et2[bs][d_]], [r_sT2[bs][d_]])
                        yield

                    def stage_b(step, d_):
                        j, bs, col, need_out, is_last = info(step, d_)
                        cur, nxt = Cbf2[step % 2][d_], Cbf2[(step + 1) % 2][d_]
                        r_cur, r_nxt = r_Cb2[step % 2][d_], r_Cb2[(step + 1) % 2][d_]
                        if not is_last:
                            p_u = next_ps()
                            MM(psb[p_u][:, 0:129], kts2[bs][d_], vaug[:, j, 0:129], True, True, [r_kts2[bs][d_], r_v], [r_ps[p_u]])
                            yield
                            STT("dve", Cst[d_][:, 0:129], Cst[d_][:, 0:129], EBL[:, j, col:col + 1], psb[p_u][:, 0:129],
                                ALU.mult, ALU.add, [r_C[d_], r_gate, r_ps[p_u]], [r_C[d_]])
                            yield
                            ACT(nxt[:, 0:129], Cst[d_][:, 0:129], AF.Copy, [r_C[d_]], [r_nxt])
                            yield

                    def stage_bo(step, d_):
                        j, bs, col, need_out, is_last = info(step, d_)
                        cur, r_cur = Cbf2[step % 2][d_], r_Cb2[step % 2][d_]
                        if need_out:
                            c0, c1 = j * 128, (j + 1) * 128
                            p_i = next_ps()
                            MM(psb[p_i][:, 0:129], qT[:, c0:c1], cur[:, 0:129], True, True, [r_q, r_cur], [r_ps[p_i]])
                            p_n = next_ps()
                            MM(psb[p_n][:, 0:129], sT2[bs][d_], vaug[:, j, 0:129], True, True, [r_sT2[bs][d_], r_v], [r_ps[p_n]])
                            yield
                            ACT(ndi[d_][:, 0:129], psb[p_i][:, 0:129], AF.Copy, [r_ps[p_i], r_gate], [r_ndi[d_]], scale=EB[:, j, col:col + 1])
                            yield
                            TT("dve", ndi[d_][:, 0:129], ndi[d_][:, 0:129], psb[p_n][:, 0:129], ALU.add, [r_ndi[d_], r_ps[p_n]], [r_ndi[d_]])
                            yield

                    def den_ops(step):
                        j, bs, col, need_out, is_last = info(step, 0)
                        if need_out:
                            STT("dve", rden_j, ndi_den, -1.0, ndi_den, ALU.mult, ALU.max, [r_ndi[0], r_ndi[1]], [r_rdj])
                            TS("dve", rden_j, rden_j, 1.0, None, ALU.max, None, [r_rdj], [r_rdj])
                            RECIP(rden_j, rden_j, [r_rdj], [r_rdj])

                    def stage_b2(step, d_):
                        j, bs, col, need_out, is_last = info(step, d_)
                        if need_out:
                            STT("dve", hsum[:, j, :], ndi[d_][:, 0:128], t0f[:, 392 + d_:393 + d_], hsum[:, j, :], ALU.mult, ALU.add,
                                [r_ndi[d_], r_rdj, r_hs], [r_hs])
                        yield

                    def interleave(*gens):
                        gens = list(gens)
                        while gens:
                            for g in list(gens):
                                try:
                                    next(g)
                                except StopIteration:
                                    gens.remove(g)

                    for bnk in range(4):
                        ps_reserved.add(bnk)
                    interleave(a1(0, 0), a1(0, 1))
                    interleave(a2a(0, 0), a2a(0, 1))
                    interleave(a2b(0, 0), a2b(0, 1))
                    interleave(a1(1, 0), a1(1, 1))
                    for step in range(NJ):
                        if step + 2 < NJ:
                            interleave(a1(step + 2, 0), a1(step + 2, 1))
                        if step + 1 < NJ:
                            interleave(a2a(step + 1, 0), a2a(step + 1, 1))
                        interleave(stage_b(step, 0), stage_b(step, 1))
                        if step + 1 < NJ:
                            interleave(a2b(step + 1, 0), a2b(step + 1, 1))
                        interleave(stage_bo(step, 0), stage_bo(step, 1))
                        den_ops(step)
                        interleave(stage_b2(step, 0), stage_b2(step, 1))
                    for bnk in range(4):
                        ps_reserved.discard(bnk)
                def head_out(h):
                    nj = NJ - j0
                    hv = hsum[:, j0:NJ, :]
                    sqv = ym[:, h, j0 * 128:NT].rearrange("p (j e) -> p j e", e=128)
                    ymh_res = trs(r_ym, h, j0 * 128, NT) + ([r_wvo] if h == 3 else [])
                    P.op("dve", (lambda hv=hv, nj=nj: lambda e: e.tensor_reduce(out=st1[:, 0:nj], in_=hv, axis=AX.X, op=ALU.add))(),
                         [r_hs], [r_st])
                    TT("dve", sqv, hv, hv, ALU.mult, [r_hs], ymh_res)
                    P.op("dve", (lambda sqv=sqv, nj=nj: lambda e: e.tensor_reduce(out=st1[:, NJ:NJ + nj], in_=sqv, axis=AX.X, op=ALU.add))(),
                         ymh_res, [r_st])
                    mean_ = st2[:, 0:nj]
                    var_ = st2[:, NJ:NJ + nj]
                    TS("dve", mean_, st1[:, 0:nj], 1.0 / HD, None, ALU.mult, None, [r_st], [r_st])
                    TT("dve", var_, mean_, mean_, ALU.mult, [r_st], [r_st])
                    STT("dve", var_, st1[:, NJ:NJ + nj], 1.0 / HD, var_, ALU.mult, ALU.subtract, [r_st], [r_st])
                    ACT(var_, var_, AF.Sqrt, [r_st, r_const], [r_st], bias=cst[:, 1:2], scale=1.0)
                    RECIP(var_, var_, [r_st], [r_st])
                    TT("dve", hv, hv, mean_.unsqueeze(2).to_broadcast([128, nj, 128]), ALU.subtract, [r_hs, r_st], [r_hs])
                    TT("dve", hv, hv, var_.unsqueeze(2).to_broadcast([128, nj, 128]), ALU.mult, [r_hs, r_st], [r_hs])
                    TT("dve", sgo[:, j0:NJ, :], hv, sgo[:, j0:NJ, :], ALU.mult, [r_hs, r_sg], [r_sg])
                    for jg in range(j0, NJ, 4):
                        p_t = next_ps()
                        ptb = psb[p_t][:, 0:256].bitcast(BF16)
                        jn = min(4, NJ - jg)
                        for jj in range(jn):
                            TR(ptb[:, jj * 128:(jj + 1) * 128], sgo[:, jg + jj, :], ident_b[:], [r_sg, r_const], [r_ps[p_t]])
                        ACT(ym[:, h, jg * 128:(jg + jn) * 128], ptb[:, 0:jn * 128], AF.Copy, [r_ps[p_t]],
                            trs(r_ym, h, jg * 128, (jg + jn) * 128) + ([r_wvo] if h == 3 else []))

                head_qk(0)
                head_vo(0)
                for h in range(NH):
                    head_loop(h)
                    if h + 1 < NH:
                        head_qk(h + 1)
                    head_out(h)
                    if h + 1 < NH:
                        head_vo(h + 1)
                if l == 0:
                    dump("d_ym", lambda kc: ym[:, kc, :], lambda kc: trs(r_ym, kc, 0, NT))

                P.barrier()
                maybe_stop(10 * l + 4)
                o_wo = 4096
                wo_s = arena(o_wo, KC * KC * 128).rearrange("p (o k c) -> p o k c", o=KC, k=KC)
                r_wo = R()
                obuf = arena(o_wo + 8192, KC * 512, F32).rearrange("p (o t) -> p o t", t=512)
                r_ob = [R() for _ in range(KC)]
                DMA("pool", wo_s.rearrange("p o k c -> p (o k c)"), woutc[l].rearrange("p o k c -> p (o k c)"), [],
                    [r_wo])
                for (a, b, w) in TL:
                    n = b - a
                    for oc in range(KC):
                        pi = next_ps()
                        for kc in range(KC):
                            MM(psb[pi][:, 0:n], wo_s[:, oc, kc, :], ym[:, kc, a:b], kc == 0, kc == KC - 1,
                               [r_wo] + trs(r_ym, kc, a, b), [r_ps[pi]])
                        ACT(obuf[:, oc, 0:n], psb[pi][:, 0:n], AF.Copy, [r_ps[pi]], [r_ob[oc]])
                    resid_update([(a, b, w)], lambda kc, a_, b_: obuf[:, kc, 0:b_ - a_], lambda kc, a_, b_: [r_ob[kc]], G1)
                if l == 0:
                    dump("d_x1", lambda kc: xs[:, kc, :], lambda kc: trs(r_xs, kc, 0, NT))

                P.barrier()
                maybe_stop(10 * l + 5)
                hT = scr[:, 0:24576].rearrange("p (f t) -> p f t", t=768)
                r_hT = [R() for _ in range(32)]
                ob2 = scr[:, 24576:30720].rearrange("p (o t) -> p o t", t=768)
                r_ob2 = [R() for _ in range(KC)]
                w1b = [scr[:, 30720 + i * 2048:30720 + (i + 1) * 2048].rearrange("p (g k c) -> p g k c", g=2, k=KC) for i in range(2)]
                w2b = [scr[:, 34816 + i * 4096:34816 + (i + 1) * 4096].rearrange("p (f c) -> p f c", c=128) for i in range(2)]
                r_w1b = [R(), R()]
                r_w2b = [R(), R()]
                if need_ctx:
                    supers = [[(0, 256, 1), (256, 768, 0)], [(768, 1280, 0), (1280, 1536, 0)], [(1536, 2048, 0), (2048, 2304, 0)]]
                else:
                    supers = [[(256, 768, 0), (768, 1024, 0)], [(1024, 1536, 0), (1536, 1792, 0)], [(1792, 2304, 0)]]
                n1 = 0
                n2 = 0
                def gen_mlp_resid(sup, ssb, a0):
                    for si_, (a, b, w) in enumerate(sup):
                        n = b - a
                        ACT(rs[:, 0:n], psb[ssb[si_]][:, 0:n], AF.Sqrt, [r_ps[ssb[si_]], r_const], [r_rs], bias=cst[:, 0:1], scale=1.0)
                        RECIP(rs[:, 0:n], rs[:, 0:n], [r_rs], [r_rs])
                        ps_reserved.discard(ssb[si_])
                        yield
                        for kc in range(KC):
                            ti = next_tf()
                            TT("dve", tmpf[ti][:, 0:n], ob2[:, kc, a - a0:b - a0], rs[:, 0:n], ALU.mult,
                               [r_ob2[kc], r_rs], [r_tmpf[ti]])
                            STT("dve", xs[:, kc, a:b], tmpf[ti][:, 0:n], G2[:, kc, w:w + 1], xs[:, kc, a:b], ALU.mult, ALU.add,
                                [r_tmpf[ti], r_lay] + trs(r_xs, kc, a, b), trs(r_xs, kc, a, b))
                            yield

                prenorm(supers[0], A2, 24, l)
                pending = None
                for isup, sup in enumerate(supers):
                    a0 = sup[0][0]
                    for g in range(16):
                        i = n1 % 2
                        n1 += 1
                        DMA("pool", w1b[i].rearrange("p g k c -> p (g k c)"), w1c[l, g].rearrange("p g k c -> p (g k c)"), [],
                            [r_w1b[i]])
                        for f2 in range(2):
                            f = g * 2 + f2
                            for (a, b, w) in sup:
                                n = b - a
                                pi = next_ps()
                                for kc in range(KC):
                                    MM(psb[pi][:, 0:n], w1b[i][:, f2, kc, :], hx[:, kc, a:b], kc == 0, kc == KC - 1,
                                       [r_w1b[i]] + trs(r_hx, kc, a, b), [r_ps[pi]])
                                ti = next_tf()
                                ACT(tmpf[ti][:, 0:n], psb[pi][:, 0:n], AF.Relu, [r_ps[pi]], [r_tmpf[ti]])
                                TT("dve", hT[:, f, a - a0:b - a0], tmpf[ti][:, 0:n], tmpf[ti][:, 0:n], ALU.mult, [r_tmpf[ti]], [r_hT[f]])
                        pull(pending, 2)
                    drain(pending)
                    pending = None
                    gp = gen_prenorm(supers[isup + 1], A2, 24, l) if isup + 1 < len(supers) else None
                    ssb = []
                    for _ in sup:
                        pss = next_ps()
                        ps_reserved.add(pss)
                        ssb.append(pss)
                    for oc in range(KC):
                        i = n2 % 2
                        n2 += 1
                        DMA("pool", w2b[i].rearrange("p f c -> p (f c)"), w2c[l, oc].rearrange("p f c -> p (f c)"), [],
                            [r_w2b[i]])
                        for si_, (a, b, w) in enumerate(sup):
                            n = b - a
                            pi = next_ps()
                            for f in range(32):
                                MM(psb[pi][:, 0:n], w2b[i][:, f, :], hT[:, f, a - a0:b - a0], f == 0, f == 31,
                                   [r_w2b[i], r_hT[f]], [r_ps[pi]])
                            ACT(ob2[:, oc, a - a0:b - a0], psb[pi][:, 0:n], AF.Copy, [r_ps[pi]], [r_ob2[oc]])
                            sq_i = next_sq()
                            ACT(sqb[sq_i][:, 0:n], psb[pi][:, 0:n], AF.Square, [r_ps[pi]], [r_sqb[sq_i]])
                            MM(psb[ssb[si_]][:, 0:n], ones_b[:], sqb[sq_i][:, 0:n], oc == 0, oc == KC - 1,
                               [r_sqb[sq_i], r_const], [r_ps[ssb[si_]]])
                            pull(gp, 3)
                    drain(gp)
                    pending = gen_mlp_resid(sup, ssb, a0)
                drain(pending)
                if l == 0:
                    dump("d_x2", lambda kc: xs[:, kc, :], lambda kc: trs(r_xs, kc, 0, NT))
                P.barrier()

        except _Stop:
            pass
        for kc in range(KC):
            DMA("sp", outT[kc], xs[:, kc, CTX:NT], trs(r_xs, kc, CTX, NT), [])
        P.emit()
    return nc


_CACHE = {}


def _consts():
    if "c" in _CACHE:
        return _CACHE["c"]
    c = {}
    c["ident"] = np.eye(128, dtype=np.float32)
    s = np.arange(128)
    c["umask"] = (s[:, None] <= s[None, :]).astype(np.float32)
    c["lmask"] = (s[:, None] >= s[None, :]).astype(np.float32)
    k = np.arange(64)
    ang = 2.0 * np.pi * np.outer(k, k) / 64.0
    cs = np.zeros((128, 256), np.float64)
    for hh in range(2):
        cs[hh * 64:(hh + 1) * 64, hh * 64:(hh + 1) * 64] = np.cos(ang) / 8.0
        cs[hh * 64:(hh + 1) * 64, 128 + hh * 64:128 + (hh + 1) * 64] = np.sin(ang) / 8.0
    c["cs64"] = cs.astype(np.float32)
    t = np.arange(SEQ, dtype=np.int64)
    ph = (np.outer(t, t) % SEQ).astype(np.float64) * (2.0 * np.pi / SEQ)
    dc = (np.cos(ph) / math.sqrt(SEQ)).astype(np.float32)
    ds = (-np.sin(ph) / math.sqrt(SEQ)).astype(np.float32)
    both = np.stack([dc, ds], 0)
    both = both.reshape(2, 4, 4, 128, 4, 512)
    c["dftx"] = np.ascontiguousarray(both.transpose(4, 1, 3, 0, 2, 5)).astype(ml_dtypes.bfloat16)
    t = np.arange(CTX, dtype=np.int64)
    ph = (np.outer(t, t) % CTX).astype(np.float64) * (2.0 * np.pi / CTX)
    both = np.stack([np.cos(ph), -np.sin(ph)], 0) / math.sqrt(CTX)
    both = both.reshape(2, 2, 128, CTX)
    c["dftc"] = np.ascontiguousarray(both.transpose(2, 0, 1, 3)).astype(ml_dtypes.bfloat16)
    rows = SEQ // 64
    quarter = D // 4
    freq = np.exp(-math.log(10000.0) * np.arange(quarter, dtype=np.float32) / quarter).astype(np.float32)
    r = np.broadcast_to(np.arange(rows, dtype=np.float32)[:, None], (rows, 64)).reshape(-1)
    col = np.broadcast_to(np.arange(64, dtype=np.float32)[None, :], (rows, 64)).reshape(-1)
    ar = r[:, None] * freq
    ac = col[:, None] * freq
    pos = np.concatenate([np.sin(ar), np.cos(ar), np.sin(ac), np.cos(ac)], axis=-1).astype(np.float32)
    c["posT"] = np.ascontiguousarray(pos.T).reshape(KC, 128, SEQ)
    _CACHE["c"] = c
    return c


def _chunk_w(w, cols):
    return np.ascontiguousarray(w[:, cols].reshape(KC, 128, -1).transpose(1, 0, 2))


def _prep_shared(inp):
    f = np.float32
    L = DEPTH
    w_in = np.asarray(inp["w_in"], f)
    sh = {}
    wada = np.asarray(inp["w_ada"], f)
    sh["wada"] = np.ascontiguousarray(wada.reshape(L, KC, 128, 12, 512).transpose(0, 3, 2, 1, 4))
    sh["bada"] = np.ascontiguousarray(np.asarray(inp["b_ada"], f).reshape(L, 48, 128).transpose(0, 2, 1))
    gs = np.stack([np.asarray(inp[k], f) for k in ("g_pre_mix", "g_post_mix", "g_pre_mlp", "g_post_mlp")], 1)
    sh["gvec"] = np.ascontiguousarray(gs.reshape(L, 4, KC, 128).transpose(0, 3, 1, 2))
    offs = [Q_OFF + 128 * i for i in range(4)] + [K_OFF + 128 * i for i in range(4)] + \
           [F_OFF, F_OFF + 128, CA_OFF, CA_OFF + 128, CG_OFF, CG_OFF + 128]
    sh["winc"] = np.stack([np.stack([_chunk_w(w_in[l], np.arange(o, o + 128)) for o in offs]) for l in range(L)])
    sh["wvo"] = np.stack([np.stack([_chunk_w(w_in[l], np.concatenate([np.arange(V_OFF + 128 * h, V_OFF + 128 * h + 128),
                                                                      np.arange(O_OFF + 128 * h, O_OFF + 128 * h + 128)]))
                                    for h in range(NH)]) for l in range(L)])
    sh["wgate"] = np.stack([_chunk_w(w_in[l], np.arange(G_OFF, G_OFF + 16)) for l in range(L)])
    sh["bgate"] = np.ascontiguousarray(np.asarray(inp["b_gate"], f).reshape(L, 16))
    wqk = np.asarray(inp["w_qk_conv"], f)
    sh["wqkp"] = np.ascontiguousarray(wqk.reshape(L, 3, 8, 128).transpose(0, 2, 3, 1))
    sh["gmn"] = np.ascontiguousarray(np.asarray(inp["g_mlstm_norm"], f))
    wdw = np.asarray(inp["w_dw"], f)
    sh["wdw"] = np.ascontiguousarray(wdw.reshape(L, CONV_K, 2, 128).transpose(0, 3, 2, 1))
    cv = np.stack([np.asarray(inp[k], f) for k in ("b_dw", "g_conv_ln", "b_conv_ln")], 1)
    sh["cvp"] = np.ascontiguousarray(cv.reshape(L, 3, 2, 128).transpose(0, 3, 1, 2))
    wout = np.asarray(inp["w_out"], f)
    sh["woutc"] = np.ascontiguousarray(wout.reshape(L, KC, 128, KC, 128).transpose(0, 2, 3, 1, 4))
    w1 = np.asarray(inp["w_mlp1"], f)
    sh["w1c"] = np.ascontiguousarray(w1.reshape(L, KC, 128, 16, 2, 128).transpose(0, 3, 2, 4, 1, 5))
    w2 = np.asarray(inp["w_mlp2"], f)
    sh["w2c"] = np.ascontiguousarray(w2.reshape(L, 32, 128, KC, 128).transpose(0, 3, 2, 1, 4))
    c = _consts()
    for k in ("ident", "umask", "lmask", "cs64", "dftx", "dftc", "posT"):
        sh[k] = c[k]
    return sh


def make_in_maps(inp, cores):
    sh = _prep_shared(inp)
    x = np.asarray(inp["x"], np.float32)
    ctx = np.asarray(inp["ctx"], np.float32)
    c = np.asarray(inp["c"], np.float32)
    c_ctx = np.asarray(inp["c_ctx"], np.float32)
    maps = []
    for b in cores:
        m = dict(sh)
        m["xT"] = np.ascontiguousarray(x[b].T).reshape(KC, 128, SEQ)
        m["ctxT"] = np.ascontiguousarray(ctx[b].T).reshape(KC, 128, CTX)
        cv = np.stack([c[b], c_ctx], -1)
        m["cvec"] = np.ascontiguousarray(cv.reshape(KC, 128, 2).transpose(1, 0, 2))
        maps.append(m)
    return maps


def kernel(**inputs):
    if "nc" not in _CACHE:
        _CACHE["nc"] = build_program(dbg=False)
    nc = _CACHE["nc"]
    in_maps = make_in_maps(inputs, list(range(NB)))
    res = run_bass_kernel_spmd(nc, in_maps, core_ids=list(range(NB)))
    out = np.empty((NB, SEQ, D), np.float32)
    for b in range(NB):
        oT = np.asarray(res.results[b]["outT"], np.float32).reshape(D, SEQ)
        out[b] = oT.T
    return out
```

```python
import math
from contextlib import ExitStack

import numpy as np
import ml_dtypes

import concourse.bass as bass
import concourse.mybir as mybir
from concourse.bass_utils import run_bass_kernel_spmd

F32 = mybir.dt.float32
BF16 = mybir.dt.bfloat16
AF = mybir.ActivationFunctionType
ALU = mybir.AluOpType
AX = mybir.AxisListType

ENGS = ("pe", "act", "dve", "pool", "sp")
SEM_LIM = 2000
N_DMA_SEMS = 16


class Res:
    __slots__ = ("name", "w", "r", "excl")

    def __init__(self, name, excl=False):
        self.name = name
        self.w = None
        self.r = []
        self.excl = excl


class Op:
    __slots__ = ("eng", "idx", "fn", "deps", "dma", "signal", "semref", "dma_slot", "dma_val")

    def __init__(self, eng, idx, fn, dma):
        self.eng = eng
        self.idx = idx
        self.fn = fn
        self.deps = []
        self.dma = dma
        self.signal = False
        self.semref = None
        self.dma_slot = None
        self.dma_val = None


class Prog:
    def __init__(self, nc, same_engine_sync=True):
        self.nc = nc
        self.ops = {e: [] for e in ENGS}
        self.n_dma_q = {}
        self.same_engine_sync = same_engine_sync

    def res(self, name="", excl=False):
        return Res(name, excl)

    def op(self, eng, fn, reads=(), writes=(), dma=False):
        o = Op(eng, len(self.ops[eng]), fn, dma)
        if dma:
            half = N_DMA_SEMS // 2
            k = self.n_dma_q.get(eng, 0)
            self.n_dma_q[eng] = k + 1
            o.dma_slot = (k % half) + (half if eng == "pool" else 0)
            o.dma_val = 16 * (k // half + 1)
        deps = []
        for r in reads:
            if r.w is not None:
                deps.append(r.w)
            if r.excl:
                deps.extend(x for x in r.r if x.eng != eng)
        for r in writes:
            if r.w is not None:
                deps.append(r.w)
            deps.extend(r.r)
        for r in reads:
            r.r.append(o)
        for r in writes:
            r.w = o
            r.r = []
        seen = set()
        for d in deps:
            if d is o or id(d) in seen:
                continue
            seen.add(id(d))
            if (not d.dma) and (not dma) and d.eng == eng:
                if eng == "pe" or not self.same_engine_sync:
                    continue
            o.deps.append(d)
        self.ops[eng].append(o)
        return o

    def barrier(self):
        targets = []
        for e in ENGS:
            cs = [o for o in self.ops[e] if not o.dma and o.fn is not None]
            if cs:
                targets.append(cs[-1])
        by_slot = {}
        for e in ENGS:
            for o in self.ops[e]:
                if o.dma and (o.dma_slot not in by_slot or o.dma_val > by_slot[o.dma_slot].dma_val):
                    by_slot[o.dma_slot] = o
        targets += list(by_slot.values())
        for e in ENGS:
            o = Op(e, len(self.ops[e]), None, False)
            o.deps = list(targets) if e != "pe" else [t for t in targets if t.dma or t.eng != e]
            self.ops[e].append(o)

    def emit(self):
        nc = self.nc
        for e in ENGS:
            for o in self.ops[e]:
                for d in o.deps:
                    d.signal = True
        with ExitStack() as st:
            dma_sems = [st.enter_context(nc.semaphore(f"dq{i}")) for i in range(N_DMA_SEMS)]
            for e in ENGS:
                n = sum(1 for o in self.ops[e] if o.signal and not o.dma)
                k = max((n + SEM_LIM - 1) // SEM_LIM, 1)
                sems = [st.enter_context(nc.semaphore(f"s_{e}{i}")) for i in range(k)]
                c = 0
                for o in self.ops[e]:
                    if o.signal and not o.dma:
                        o.semref = (sems[c // SEM_LIM], c % SEM_LIM + 1, c)
                        c += 1
            block = st.enter_context(nc.Block())

            def run(e, eng):
                waited_c = {p: -1 for p in ENGS}
                waited_d = [0] * N_DMA_SEMS
                for o in self.ops[e]:
                    for d in o.deps:
                        if d.dma:
                            if waited_d[d.dma_slot] >= d.dma_val:
                                continue
                            waited_d[d.dma_slot] = d.dma_val
                            eng.wait_ge(dma_sems[d.dma_slot], d.dma_val)
                        else:
                            sem, val, gc = d.semref
                            if waited_c[d.eng] >= gc:
                                continue
                            waited_c[d.eng] = gc
                            eng.wait_ge(sem, val)
                    if o.fn is None:
                        continue
                    if o.dma and o.dma_val > 16 and waited_d[o.dma_slot] < o.dma_val - 16:
                        waited_d[o.dma_slot] = o.dma_val - 16
                        eng.wait_ge(dma_sems[o.dma_slot], o.dma_val - 16)
                    ins = o.fn(eng)
                    if o.dma:
                        ins.then_inc(dma_sems[o.dma_slot], 16)
                    elif o.signal:
                        ins.then_inc(o.semref[0], 1)

            fin_dma = {}
            for e in ENGS:
                for o in self.ops[e]:
                    if o.dma:
                        fin_dma[o.dma_slot] = max(fin_dma.get(o.dma_slot, 0), o.dma_val)

            @block.tensor
            def _(eng):
                run("pe", eng)

            @block.scalar
            def _(eng):
                run("act", eng)

            @block.vector
            def _(eng):
                run("dve", eng)

            @block.gpsimd
            def _(eng):
                run("pool", eng)

            @block.sync
            def _(eng):
                run("sp", eng)
                for slot, val in sorted(fin_dma.items()):
                    eng.wait_ge(dma_sems[slot], val)


D = 1024
NB = 8
SEQ = 2048
CTX = 256
NT = CTX + SEQ
DEPTH = 2
KC = 8
NJ = NT // 128
HD = 128
NH = 4
DFF = 4096
EPS = 1e-6
CONV_K = 31
PADC = 15
Q_OFF, K_OFF, V_OFF, O_OFF, G_OFF = 0, 512, 1024, 1536, 2048
F_OFF = 2064
CA_OFF = 2320
CG_OFF = 2576
UPW = (CTX + 2 * PADC) + (SEQ + 2 * PADC)


def tiles_for(need_ctx):
    t = [(256, 768, 0), (768, 1280, 0), (1280, 1792, 0), (1792, 2304, 0)]
    if need_ctx:
        t = [(0, 256, 1)] + t
    return t


class _Stop(Exception):
    pass


def build_program(dbg=False, stop=None):
    nc = bass.Bass("TRN2", target_bir_lowering=False)
    P = Prog(nc)

    def din(name, shape, dt=F32):
        return nc.dram_tensor(name, list(shape), dt, kind="ExternalInput").ap()

    def dout(name, shape, dt=F32):
        return nc.dram_tensor(name, list(shape), dt, kind="ExternalOutput").ap()

    xT = din("xT", [KC, 128, SEQ])
    ctxT = din("ctxT", [KC, 128, CTX])
    posT = din("posT", [KC, 128, SEQ])
    cvec = din("cvec", [128, KC, 2])
    wada = din("wada", [DEPTH, 12, 128, KC, 512])
    bada = din("bada", [DEPTH, 128, 48])
    gvec = din("gvec", [DEPTH, 128, 4, KC])
    winc = din("winc", [DEPTH, 14, 128, KC, 128])
    wvo = din("wvo", [DEPTH, NH, 128, KC, 256])
    wgate = din("wgate", [DEPTH, 128, KC, 16])
    bgate = din("bgate", [DEPTH, 16])
    wqkp = din("wqkp", [DEPTH, 8, 128, 3])
    gmn = din("gmn", [DEPTH, 512])
    wdw = din("wdw", [DEPTH, 128, 2, CONV_K])
    cvp = din("cvp", [DEPTH, 128, 3, 2])
    woutc = din("woutc", [DEPTH, 128, KC, KC, 128])
    w1c = din("w1c", [DEPTH, 16, 128, 2, KC, 128])
    w2c = din("w2c", [DEPTH, KC, 128, 32, 128])
    ident_d = din("ident", [128, 128])
    umask_d = din("umask", [128, 128])
    lmask_d = din("lmask", [128, 128])
    cs64_d = din("cs64", [128, 256])
    dftx_d = din("dftx", [4, 4, 128, 2, 4, 512], BF16)
    dftc_d = din("dftc", [128, 2, 2, 256], BF16)
    outT = dout("outT", [KC, 128, SEQ])
    dbg_out = {}
    if dbg:
        for nm in ("d_hx", "d_ym", "d_x1", "d_x2"):
            dbg_out[nm] = dout(nm, [KC, 128, NT])

    with ExitStack() as st:
        def sb(name, shape, dt):
            return st.enter_context(nc.sbuf_tensor(name, list(shape), dt))

        xs = sb("xs", [128, KC, NT], F32)
        hx = sb("hx", [128, KC, NT], BF16)
        SCR_ARENA = 25088
        SCR_N = SCR_ARENA + KC * NT
        scr = sb("scr", [128, SCR_N], BF16)
        ym_flat = scr[:, SCR_ARENA:SCR_N]
        ym = ym_flat.rearrange("p (c t) -> p c t", t=NT)
        ident_f = sb("ident_f", [128, 128], F32)
        ident_b = sb("ident_b", [128, 128], BF16)
        umask_f = sb("umask_f", [128, 128], F32)
        lmask_f = sb("lmask_f", [128, 128], F32)
        negm_f = sb("negm_f", [128, 128], BF16)
        negm_b = sb("negm_b", [128, 128], BF16)
        ones_f = sb("ones_f", [128, 128], F32)
        ones_b = sb("ones_b", [128, 128], BF16)
        cs64 = sb("cs64b", [128, 256], BF16)
        cst = sb("cst", [128, 4], F32)
        cv_f = sb("cv_f", [128, KC, 2], F32)
        sc_b = sb("sc_b", [128, KC, 2], BF16)
        mod = sb("mod", [128, DEPTH, 48, 2], F32)
        bada_s = sb("bada_s", [128, DEPTH, 48], F32)
        gv = sb("gv", [128, DEPTH, 4, KC], F32)
        A1 = sb("A1", [128, KC, 2], F32)
        A2 = sb("A2", [128, KC, 2], F32)
        G1 = sb("G1", [128, KC, 2], F32)
        G2 = sb("G2", [128, KC, 2], F32)
        rs = sb("rs", [128, 512], F32)
        sqb = [sb(f"sqb{i}", [128, 512], BF16) for i in range(2)]
        tmpf = [sb(f"tmpf{i}", [128, 512], F32) for i in range(2)]
        psb = [st.enter_context(nc.psum_tensor(f"ps{i}", [128, 512], F32)) for i in range(8)]

        R = P.res
        r_xs = [[R() for _ in range(NJ)] for _ in range(KC)]
        r_hx = [[R() for _ in range(NJ)] for _ in range(KC)]
        r_ym = [[R() for _ in range(NJ)] for _ in range(KC)]
        r_ps = [P.res("ps", excl=True) for _ in range(8)]
        r_const = R()
        r_mod = R()
        r_lay = R()
        r_rs = R()
        r_sqb = [R(), R()]
        r_tmpf = [R(), R()]
        r_arena = R()

        def trs(rl, kc, a, b):
            return [rl[kc][j] for j in range(a // 128, (b + 127) // 128)]

        def trs_all(rl, a, b):
            out = []
            for kc in range(KC):
                out += trs(rl, kc, a, b)
            return out

        cnt = {"ps": 0, "sq": 0, "tf": 0}

        ps_reserved = set()

        def next_ps():
            while True:
                i = cnt["ps"] % 8
                cnt["ps"] += 1
                if i not in ps_reserved:
                    return i

        def next_sq():
            i = cnt["sq"] % 2
            cnt["sq"] += 1
            return i

        def next_tf():
            i = cnt["tf"] % 2
            cnt["tf"] += 1
            return i

        def MM(out, lhsT, rhs, start, stop, reads, writes):
            P.op("pe", lambda e: e.matmul(out, lhsT=lhsT, rhs=rhs, start=start, stop=stop), reads, writes)

        def TR(out, in_, ident, reads, writes):
            P.op("pe", lambda e: e.transpose(out, in_, ident), reads, writes)

        def ACT(out, in_, func, reads, writes, bias=None, scale=None, accum=None):
            kw = {}
            if bias is not None:
                kw["bias"] = bias
            if scale is not None:
                kw["scale"] = scale
            if accum is not None:
                kw["accum_out"] = accum
            P.op("act", lambda e: e.activation(out=out, in_=in_, func=func, **kw), reads, writes)

        def TT(eng, out, in0, in1, op, reads, writes):
            P.op(eng, lambda e: e.tensor_tensor(out=out, in0=in0, in1=in1, op=op), reads, writes)

        def TS(eng, out, in0, s1, s2, op0, op1, reads, writes):
            if s2 is None:
                P.op(eng, lambda e: e.tensor_scalar(out=out, in0=in0, scalar1=s1, scalar2=None, op0=op0), reads, writes)
            else:
                P.op(eng, lambda e: e.tensor_scalar(out=out, in0=in0, scalar1=s1, scalar2=s2, op0=op0, op1=op1), reads, writes)

        def STT(eng, out, in0, scalar, in1, op0, op1, reads, writes):
            P.op(eng, lambda e: e.scalar_tensor_tensor(out=out, in0=in0, scalar=scalar, in1=in1, op0=op0, op1=op1), reads, writes)

        def CP(eng, out, in_, reads, writes):
            P.op(eng, lambda e: e.tensor_copy(out=out, in_=in_), reads, writes)

        def MS(eng, ap, val, writes):
            P.op(eng, lambda e: e.memset(ap, val), (), writes)

        def RECIP(out, in_, reads, writes):
            P.op("dve", lambda e: e.reciprocal(out=out, in_=in_), reads, writes)

        def DMA(q, out, in_, reads, writes):
            P.op(q, lambda e: e.dma_start(out=out, in_=in_), reads, writes, dma=True)

        def arena(off, n, dt=BF16):
            if dt == BF16:
                return scr[:, off:off + n]
            assert off % 2 == 0
            return scr[:, off:off + 2 * n].bitcast(F32)

        DMA("sp", ident_f[:], ident_d, [], [r_const])
        DMA("sp", umask_f[:], umask_d, [], [r_const])
        DMA("sp", lmask_f[:], lmask_d, [], [r_const])
        DMA("pool", cs64[:], cs64_d, [], [r_const])
        DMA("sp", cv_f[:], cvec, [], [r_const])
        DMA("sp", bada_s[:], bada.rearrange("l p n -> p l n"), [], [r_const])
        DMA("sp", gv[:], gvec.rearrange("l p a k -> p l a k"), [], [r_const])
        CP("dve", ident_b[:], ident_f[:], [r_const], [r_const])
        TS("dve", negm_f[:], umask_f[:], -1.0, 30000.0, ALU.add, ALU.mult, [r_const], [r_const])
        TS("dve", negm_b[:], lmask_f[:], -1.0, 30000.0, ALU.add, ALU.mult, [r_const], [r_const])
        MS("dve", ones_f[:], 1.0, [r_const])
        MS("dve", ones_b[:], 1.0, [r_const])
        MS("dve", cst[:, 0:1], 1024.0 * EPS, [r_const])
        MS("dve", cst[:, 1:2], EPS, [r_const])
        MS("dve", cst[:, 2:3], 1.0, [r_const])
        MS("dve", cst[:, 3:4], 0.0, [r_const])

        for kc in range(KC):
            DMA("sp", xs[:, kc, 0:CTX], ctxT[kc], [], trs(r_xs, kc, 0, CTX))
        pos_buf = [arena(i * 1024, 512, F32) for i in range(8)]
        r_pos = [R() for _ in range(8)]
        npos = 0
        for xa in range(0, SEQ, 512):
            for kc in range(KC):
                a_, b_ = CTX + xa, CTX + xa + 512
                DMA("sp", xs[:, kc, a_:b_], xT[kc][:, xa:xa + 512], [], trs(r_xs, kc, a_, b_))
                i = npos % 8
                npos += 1
                DMA("sp", pos_buf[i], posT[kc][:, xa:xa + 512], [], [r_pos[i]])
                TT("dve", xs[:, kc, a_:b_], xs[:, kc, a_:b_], pos_buf[i], ALU.add,
                   [r_pos[i]] + trs(r_xs, kc, a_, b_), trs(r_xs, kc, a_, b_))

        ACT(sc_b[:], cv_f[:], AF.Silu, [r_const], [r_const])
        wa_off = 8192
        wa_buf = [arena(wa_off + i * KC * 512, KC * 512).rearrange("p (k c) -> p k c", c=512) for i in range(2)]
        r_wa = [R(), R()]
        def ada_block(l_, blk, buf, r_buf, pv_, r_pv):
            DMA("pool", buf, wada[l_, blk], [], [r_buf])
            for n4 in range(4):
                n = blk * 4 + n4
                for kc in range(KC):
                    MM(pv_[:, n, :], buf[:, kc, n4 * 128:(n4 + 1) * 128], sc_b[:, kc, :], kc == 0, kc == KC - 1,
                       [r_buf, r_const], [r_pv])

        pi = next_ps()
        pv = psb[pi][:, 0:96].rearrange("p (n w) -> p n w", w=2)
        for blk in range(4):
            ada_block(0, blk, wa_buf[blk % 2], r_wa[blk % 2], pv, r_ps[pi])
        TT("dve", mod[:, 0, 0:16], pv[:, 0:16], bada_s[:, 0, 0:16].unsqueeze(2).to_broadcast([128, 16, 2]), ALU.add,
           [r_ps[pi], r_const], [r_mod])
        ada_todo = [(0, blk) for blk in range(4, 12)] + [(1, blk) for blk in range(12)]

        def rstd_of(src_fn, n, reads):
            pi = next_ps()
            for kc in range(KC):
                si = next_sq()
                ACT(sqb[si][:, 0:n], src_fn(kc), AF.Square, reads(kc), [r_sqb[si]])
                MM(psb[pi][:, 0:n], ones_b[:], sqb[si][:, 0:n], kc == 0, kc == KC - 1, [r_sqb[si], r_const], [r_ps[pi]])
            ACT(rs[:, 0:n], psb[pi][:, 0:n], AF.Sqrt, [r_ps[pi], r_const], [r_rs], bias=cst[:, 0:1], scale=1.0)
            RECIP(rs[:, 0:n], rs[:, 0:n], [r_rs], [r_rs])

        def prenorm(tl, Amod, shift_base, l):
            for (a, b, w) in tl:
                n = b - a
                rstd_of(lambda kc: xs[:, kc, a:b], n, lambda kc: trs(r_xs, kc, a, b))
                for kc in range(KC):
                    ti = next_tf()
                    TT("dve", tmpf[ti][:, 0:n], xs[:, kc, a:b], rs[:, 0:n], ALU.mult,
                       trs(r_xs, kc, a, b) + [r_rs], [r_tmpf[ti]])
                    ACT(hx[:, kc, a:b], tmpf[ti][:, 0:n], AF.Identity, [r_tmpf[ti], r_lay, r_mod], trs(r_hx, kc, a, b),
                        bias=mod[:, l, shift_base + kc, w:w + 1], scale=Amod[:, kc, w:w + 1])

        def gen_prenorm(tl, Amod, shift_base, l):
            for (a, b, w) in tl:
                n = b - a
                pi = next_ps()
                ps_reserved.add(pi)
                for kc in range(KC):
                    si = next_sq()
                    ACT(sqb[si][:, 0:n], xs[:, kc, a:b], AF.Square, trs(r_xs, kc, a, b), [r_sqb[si]])
                    MM(psb[pi][:, 0:n], ones_b[:], sqb[si][:, 0:n], kc == 0, kc == KC - 1, [r_sqb[si], r_const], [r_ps[pi]])
                    yield
                ACT(rs[:, 0:n], psb[pi][:, 0:n], AF.Sqrt, [r_ps[pi], r_const], [r_rs], bias=cst[:, 0:1], scale=1.0)
                RECIP(rs[:, 0:n], rs[:, 0:n], [r_rs], [r_rs])
                ps_reserved.discard(pi)
                yield
                for kc in range(KC):
                    ti = next_tf()
                    TT("dve", tmpf[ti][:, 0:n], xs[:, kc, a:b], rs[:, 0:n], ALU.mult,
                       trs(r_xs, kc, a, b) + [r_rs], [r_tmpf[ti]])
                    ACT(hx[:, kc, a:b], tmpf[ti][:, 0:n], AF.Identity, [r_tmpf[ti], r_lay, r_mod], trs(r_hx, kc, a, b),
                        bias=mod[:, l, shift_base + kc, w:w + 1], scale=Amod[:, kc, w:w + 1])
                    yield

        def pull(gen, k):
            if gen is None:
                return
            for _ in range(k):
                try:
                    next(gen)
                except StopIteration:
                    return

        def drain(gen):
            if gen is None:
                return
            for _ in gen:
                pass

        def resid_update(tl, src_fn, src_reads, Gm):
            for (a, b, w) in tl:
                n = b - a
                rstd_of(lambda kc: src_fn(kc, a, b), n, lambda kc: src_reads(kc, a, b))
                for kc in range(KC):
                    ti = next_tf()
                    TT("dve", tmpf[ti][:, 0:n], src_fn(kc, a, b), rs[:, 0:n], ALU.mult,
                       src_reads(kc, a, b) + [r_rs], [r_tmpf[ti]])
                    STT("dve", xs[:, kc, a:b], tmpf[ti][:, 0:n], Gm[:, kc, w:w + 1], xs[:, kc, a:b], ALU.mult, ALU.add,
                        [r_tmpf[ti], r_lay] + trs(r_xs, kc, a, b), trs(r_xs, kc, a, b))

        def dump(name, src_fn, reads_fn):
            if not dbg:
                return
            for kc in range(KC):
                DMA("pool", dbg_out[name][kc], src_fn(kc), reads_fn(kc), [])

        def maybe_stop(k):
            if stop is not None and stop == k:
                raise _Stop()

        try:
            P.barrier()
            for l in range(DEPTH):
                need_ctx = l < DEPTH - 1
                TL_all = tiles_for(True)
                TL = tiles_for(need_ctx)
                j0 = 0 if need_ctx else 2

                def mk(dst, gidx, mbase, plus1):
                    for w in range(2):
                        if plus1:
                            STT("dve", dst[:, :, w], mod[:, l, mbase:mbase + KC, w], 1.0, gv[:, l, gidx, :], ALU.add, ALU.mult,
                                [r_mod, r_const, r_lay], [r_lay])
                        else:
                            TT("dve", dst[:, :, w], mod[:, l, mbase:mbase + KC, w], gv[:, l, gidx, :], ALU.mult,
                               [r_mod, r_const, r_lay], [r_lay])
                    TS("dve", dst[:], dst[:], 32.0, None, ALU.mult, None, [r_lay], [r_lay])

                mk(A1, 0, 8, True)
                if l > 0:
                    mk(G1, 1, 16, False)
                    mk(A2, 2, 32, True)
                    mk(G2, 3, 40, False)

                maybe_stop(10 * l + 1)

                o_w = 0
                wbuf = [arena(o_w + i * 1024, 1024).rearrange("p (k c) -> p k c", c=128) for i in range(4)]
                r_wbuf = [R() for _ in range(4)]
                wcnt = {"n": 0}

                def load_w(src):
                    i = wcnt["n"] % 4
                    wcnt["n"] += 1
                    DMA("pool", wbuf[i], src, [], [r_wbuf[i]])
                    return i

                o_up = 4096
                upad = arena(o_up, 2 * UPW).rearrange("p (c t) -> p c t", t=UPW)
                r_up = R()
                o_vb = o_up + 2 * UPW
                vbuf = arena(o_vb, 2 * NT, F32).rearrange("p (c t) -> p c t", t=NT)
                r_vb = [R(), R()]
                o_cs = o_vb + 4 * NT
                wdw_s = arena(o_cs, 2 * CONV_K, F32).rearrange("p (c k) -> p c k", k=CONV_K)
                cvp_s = arena(o_cs + 4 * CONV_K, 6, F32).rearrange("p (a c) -> p a c", c=2)
                r_cs = R()
                lnt = [ym_flat[:, i * 1024:(i + 1) * 1024].bitcast(F32) for i in range(5)]
                r_lnt = [R() for _ in range(5)]
                dgm = ym_flat[:, 5120:5120 + 2 * CONV_K * 128].rearrange("p (c k m) -> p c k m", k=CONV_K, m=128)
                r_dg = R()

                DMA("sp", wdw_s, wdw[l], [], [r_cs])
                DMA("sp", cvp_s, cvp[l], [], [r_cs])
                MS("dve", upad, 0.0, [r_up])
                for cc in range(2):
                    for k in range(CONV_K):
                        TS("dve", dgm[:, cc, k, :], ident_f[:], wdw_s[:, cc, k:k + 1], None, ALU.mult, None,
                           [r_const, r_cs], [r_dg])

                def upos(a):
                    return a + PADC if a < CTX else (CTX + 2 * PADC) + (a - CTX) + PADC

                iag = [(load_w(winc[l, 10 + cc]), load_w(winc[l, 12 + cc])) for cc in range(2)]
                if l == 0:
                    wa_d = arena(18200, KC * 512).rearrange("p (k c) -> p k c", c=512)
                    r_wad = R()
                    pb0, pb1 = next_ps(), next_ps()
                    ps_reserved.add(pb0)
                    ps_reserved.add(pb1)
                    pvd = [psb[pb0][:, 0:96].rearrange("p (n w) -> p n w", w=2), psb[pb1][:, 0:96].rearrange("p (n w) -> p n w", w=2)]
                    r_pvd = [r_ps[pb0], r_ps[pb1]]

                def ada_some(k):
                    if l != 0:
                        return
                    for _ in range(k):
                        if ada_todo:
                            l_, blk = ada_todo.pop(0)
                            ada_block(l_, blk, wa_d, r_wad, pvd[l_], r_pvd[l_])

                for (a, b, w) in TL:
                    ada_some(2)
                    prenorm([(a, b, w)], A1, 0, l)
                    for cc in range(2):
                        ia, ig = iag[cc]
                        n = b - a
                        pa = next_ps()
                        for kc in range(KC):
                            MM(psb[pa][:, 0:n], wbuf[ia][:, kc, :], hx[:, kc, a:b], kc == 0, kc == KC - 1,
                               [r_wbuf[ia]] + trs(r_hx, kc, a, b), [r_ps[pa]])
                        pg = next_ps()
                        for kc in range(KC):
                            MM(psb[pg][:, 0:n], wbuf[ig][:, kc, :], hx[:, kc, a:b], kc == 0, kc == KC - 1,
                               [r_wbuf[ig]] + trs(r_hx, kc, a, b), [r_ps[pg]])
                        ti = next_tf()
                        ACT(tmpf[ti][:, 0:n], psb[pg][:, 0:n], AF.Sigmoid, [r_ps[pg]], [r_tmpf[ti]])
                        TT("dve", upad[:, cc, upos(a):upos(a) + n], psb[pa][:, 0:n], tmpf[ti][:, 0:n], ALU.mult,
                           [r_ps[pa], r_tmpf[ti]], [r_up])
                if not need_ctx:
                    prenorm([(0, 256, 1)], A1, 0, l)
                if l == 0:
                    dump("d_hx", lambda kc: hx[:, kc, :], lambda kc: trs(r_hx, kc, 0, NT))
                for cc in range(2):
                    for (a, b, w) in TL:
                        ada_some(1)
                        n = b - a
                        pi = next_ps()
                        p0 = upos(a) - PADC
                        for k in range(CONV_K):
                            MM(psb[pi][:, 0:n], dgm[:, cc, k, :], upad[:, cc, p0 + k:p0 + k + n], k == 0, k == CONV_K - 1,
                               [r_dg, r_up], [r_ps[pi]])
                        ACT(vbuf[:, cc, a:b], psb[pi][:, 0:n], AF.Identity, [r_ps[pi], r_cs], [r_vb[cc]],
                            bias=cvp_s[:, 0, cc:cc + 1], scale=1.0)
                if l == 0:
                    ada_some(len(ada_todo))
                    TT("dve", mod[:, 0, 16:48], pvd[0][:, 16:48], bada_s[:, 0, 16:48].unsqueeze(2).to_broadcast([128, 32, 2]), ALU.add,
                       [r_pvd[0], r_const], [r_mod])
                    TT("dve", mod[:, 1], pvd[1], bada_s[:, 1].unsqueeze(2).to_broadcast([128, 48, 2]), ALU.add,
                       [r_pvd[1], r_const], [r_mod])
                    ps_reserved.discard(pb0)
                    ps_reserved.discard(pb1)
                    mk(G1, 1, 16, False)
                    mk(A2, 2, 32, True)
                    mk(G2, 3, 40, False)
                for (a, b, w) in TL:
                    n = b - a
                    p1 = next_ps()
                    p2 = next_ps()
                    for cc in range(2):
                        MM(psb[p1][:, 0:n], ones_f[:], vbuf[:, cc, a:b], cc == 0, cc == 1, [r_const, r_vb[cc]], [r_ps[p1]])
                    for cc in range(2):
                        TT("dve", lnt[0][:, 0:n], vbuf[:, cc, a:b], vbuf[:, cc, a:b], ALU.mult, [r_vb[cc]], [r_lnt[0]])
                        MM(psb[p2][:, 0:n], ones_f[:], lnt[0][:, 0:n], cc == 0, cc == 1, [r_const, r_lnt[0]], [r_ps[p2]])
                    mean = lnt[1]
                    TS("dve", mean[:, 0:n], psb[p1][:, 0:n], 1.0 / 256.0, None, ALU.mult, None, [r_ps[p1]], [r_lnt[1]])
                    TT("dve", lnt[2][:, 0:n], mean[:, 0:n], mean[:, 0:n], ALU.mult, [r_lnt[1]], [r_lnt[2]])
                    STT("dve", lnt[2][:, 0:n], psb[p2][:, 0:n], 1.0 / 256.0, lnt[2][:, 0:n], ALU.mult, ALU.subtract,
                        [r_ps[p2], r_lnt[2]], [r_lnt[2]])
                    ACT(lnt[2][:, 0:n], lnt[2][:, 0:n], AF.Sqrt, [r_lnt[2], r_const], [r_lnt[2]], bias=cst[:, 1:2], scale=1.0)
                    RECIP(lnt[2][:, 0:n], lnt[2][:, 0:n], [r_lnt[2]], [r_lnt[2]])
                    for cc in range(2):
                        TT("dve", lnt[3 + cc][:, 0:n], vbuf[:, cc, a:b], mean[:, 0:n], ALU.subtract,
                           [r_vb[cc], r_lnt[1]], [r_lnt[3 + cc]])
                        TT("dve", lnt[3 + cc][:, 0:n], lnt[3 + cc][:, 0:n], lnt[2][:, 0:n], ALU.mult,
                           [r_lnt[3 + cc], r_lnt[2]], [r_lnt[3 + cc]])
                        ACT(ym[:, 6 + cc, a:b], lnt[3 + cc][:, 0:n], AF.Silu, [r_lnt[3 + cc], r_cs], trs(r_ym, 6 + cc, a, b),
                            bias=cvp_s[:, 2, cc:cc + 1], scale=cvp_s[:, 1, cc:cc + 1])
                P.barrier()

                maybe_stop(10 * l + 2)
                o_uf = 4096
                uF = arena(o_uf, 2 * NT).rearrange("p (c t) -> p c t", t=NT)
                r_uf = [R(), R()]
                o_dp = o_uf + 2 * NT
                dpc = [arena(o_dp + i * 4096, 4096).rearrange("p (s k t) -> p s k t", s=2, k=4) for i in range(3)]
                r_dpc = [R() for _ in range(3)]
                o_dc = o_dp + 3 * 4096
                dcc = arena(o_dc, 1024).rearrange("p (s k t) -> p s k t", s=2, k=2)
                r_dcc = R()
                AB = ym_flat[:, 0:NJ * 512].rearrange("p (j c m) -> p j c m", c=2, m=256)
                r_ab = [R() for _ in range(NJ)]

                for cc in range(2):
                    iw = load_w(winc[l, 8 + cc])
                    for (a, b, w) in TL:
                        n = b - a
                        pi = next_ps()
                        for kc in range(KC):
                            MM(psb[pi][:, 0:n], wbuf[iw][:, kc, :], hx[:, kc, a:b], kc == 0, kc == KC - 1,
                               [r_wbuf[iw]] + trs(r_hx, kc, a, b), [r_ps[pi]])
                        ACT(uF[:, cc, a:b], psb[pi][:, 0:n], AF.Copy, [r_ps[pi]], [r_uf[cc]])
                for j in range(j0, NJ):
                    pi = next_ps()
                    for cc in range(2):
                        MM(psb[pi][:, cc * 256:(cc + 1) * 256], uF[:, cc, j * 128:(j + 1) * 128], cs64[:], True, True,
                           [r_uf[cc], r_const], [r_ps[pi]])
                    CP("dve", AB[:, j].rearrange("p c m -> p (c m)"), psb[pi][:, :], [r_ps[pi]], [r_ab[j]])
                npc = 0
                for tq in range(4):
                    pp = [next_ps(), next_ps()]
                    for kg in range(4):
                        i = npc % 3
                        npc += 1
                        DMA("sp", dpc[i].rearrange("p s k t -> p (s k t)"),
                            dftx_d[tq, kg].rearrange("p s k t -> p (s k t)"), [], [r_dpc[i]])
                        for ki in range(4):
                            j = 2 + kg * 4 + ki
                            for s in range(2):
                                first = (kg == 0 and ki == 0 and s == 0)
                                last = (kg == 3 and ki == 3 and s == 1)
                                for cc in range(2):
                                    MM(psb[pp[cc]][:, :], AB[:, j, cc, s * 128:(s + 1) * 128], dpc[i][:, s, ki, :], first, last,
                                       [r_ab[j], r_dpc[i]], [r_ps[pp[cc]]])
                    for cc in range(2):
                        a = CTX + tq * 512
                        ACT(ym[:, 4 + cc, a:a + 512], psb[pp[cc]][:, :], AF.Copy, [r_ps[pp[cc]]], trs(r_ym, 4 + cc, a, a + 512))
                if need_ctx:
                    DMA("sp", dcc.rearrange("p s k t -> p (s k t)"), dftc_d.rearrange("p s k t -> p (s k t)"), [], [r_dcc])
                    pp = [next_ps(), next_ps()]
                    for ki in range(2):
                        for s in range(2):
                            for cc in range(2):
                                MM(psb[pp[cc]][:, 0:256], AB[:, ki, cc, s * 128:(s + 1) * 128], dcc[:, s, ki, :],
                                   ki == 0 and s == 0, ki == 1 and s == 1, [r_ab[ki], r_dcc], [r_ps[pp[cc]]])
                    for cc in range(2):
                        ACT(ym[:, 4 + cc, 0:CTX], psb[pp[cc]][:, 0:256], AF.Copy, [r_ps[pp[cc]]], trs(r_ym, 4 + cc, 0, CTX))

                P.barrier()
                maybe_stop(10 * l + 3)
                o_s = 0
                dgq = [arena(o_s + i * 256, 128, F32) for i in range(2)]
                qs = [arena(o_s + 512 + i * 128, 128) for i in range(2)]
                sTt = [arena(o_s + 768 + i * 128, 128) for i in range(2)]
                kts = [arena(o_s + 1024 + i * 128, 128) for i in range(2)]
                Et = [arena(o_s + 1280 + i * 128, 128) for i in range(2)]
                Cst = [arena(o_s + 1536 + i * 260, 130, F32) for i in range(2)]
                Cbf = [arena(o_s + 2056 + i * 130, 130) for i in range(2)]
                rden = [arena(o_s + 2316 + i * 4, 2, F32) for i in range(2)]
                st1 = arena(o_s + 2324, 2 * NJ, F32)
                st2 = arena(o_s + 2396, 2 * NJ, F32)
                r_dgq, r_qs, r_sT, r_kts = [R(), R()], [R(), R()], [R(), R()], [R(), R()]
                r_dgb, r_Et = [R(), R()], [R(), R()]
                r_C, r_Cb, r_rden = [R(), R()], [R(), R()], [R(), R()]
                r_st = R()
                wqk_raw = arena(2468, 1024).rearrange("p (k c) -> p k c", c=128)
                r_wqk = R()
                dg3 = arena(3492, 384).rearrange("p (t c) -> p t c", c=128)
                wtapc = arena(3876, 4, F32)[:, 0:3]
                zraw = arena(3884, 2308)
                r_w3, r_wtap, r_zraw = R(), R(), R()
                o_g = 7332
                Gtok = arena(o_g, NJ * 16, F32).rearrange("p (j g) -> p j g", g=16)
                dgb = [arena(o_g + i * 256, 128, F32) for i in range(2)]
                LF = arena(o_g + 576, NJ * 8, F32).rearrange("p (j g) -> p j g", g=8)
                CM = arena(o_g + 864, NJ * 8, F32).rearrange("p (j g) -> p j g", g=8)
                Bc = arena(o_g + 1152, NJ * 8, F32).rearrange("p (j g) -> p j g", g=8)
                EB = arena(o_g + 1440, NJ * 8, F32).rearrange("p (j g) -> p j g", g=8)
                EBL = arena(o_g + 1728, NJ * 8, F32).rearrange("p (j g) -> p j g", g=8)
                ECL = arena(o_g + 2016, NJ * 8, F32).rearrange("p (j g) -> p j g", g=8)
                bg_s = arena(o_g + 2304, 16, F32)
                wg_s = arena(o_g + 2336, KC * 16).rearrange("p (k g) -> p k g", g=16)
                gmn_s = arena(o_g + 2464, 512, F32)
                r_gate = R()
                o_h = o_g + 3488
                qT = arena(o_h, NT)
                kT = arena(o_h + NT, NT)
                vaug = arena(o_h + 2 * NT, NJ * 130).rearrange("p (j e) -> p j e", e=130)
                sgo = arena(o_h + 2 * NT + 2340, NT).rearrange("p (j e) -> p j e", e=128)
                hsum = arena(o_h + 3 * NT + 2340, NT, F32).rearrange("p (j e) -> p j e", e=128)
                assert o_h + 5 * NT + 2340 <= SCR_ARENA, (o_h + 5 * NT + 2340)
                r_q, r_k, r_v, r_sg, r_hs = R(), R(), R(), R(), R()
                wvo_s = ym_flat[:, 3 * NT:3 * NT + KC * 256].rearrange("p (k c) -> p k c", c=256)
                r_wvo = R()

                DMA("pool", wg_s, wgate[l], [], [r_gate])
                DMA("sp", bg_s, bgate[l].partition_broadcast(128), [], [r_gate])
                DMA("sp", gmn_s, gmn[l].partition_broadcast(128), [], [r_gate])
                for jg in range(0, NJ, 6):
                    pi = next_ps()
                    for jj in range(6):
                        j = jg + jj
                        for kc in range(KC):
                            MM(psb[pi][:, jj * 16:(jj + 1) * 16], hx[:, kc, j * 128:(j + 1) * 128], wg_s[:, kc, :], kc == 0, kc == KC - 1,
                               [r_gate] + trs(r_hx, kc, j * 128, (j + 1) * 128), [r_ps[pi]])
                    TT("dve", Gtok[:, jg:jg + 6, :], psb[pi][:, 0:96].rearrange("p (j g) -> p j g", g=16),
                       bg_s.unsqueeze(1).to_broadcast([128, 6, 16]), ALU.add, [r_ps[pi], r_gate], [r_gate])
                CP("dve", CM[:, :, 0:4], Gtok[:, :, 0:4], [r_gate], [r_gate])
                CP("dve", CM[:, :, 4:8], Gtok[:, :, 8:12], [r_gate], [r_gate])
                ACT(LF[:, :, 0:4], Gtok[:, :, 4:8], AF.Abs, [r_gate], [r_gate])
                ACT(LF[:, :, 4:8], Gtok[:, :, 12:16], AF.Abs, [r_gate], [r_gate])
                ACT(LF[:], LF[:], AF.Exp, [r_gate], [r_gate], scale=-1.0)
                ACT(LF[:], LF[:], AF.Ln, [r_gate, r_const], [r_gate], bias=cst[:, 2:3], scale=1.0)
                TS("dve", EB[:, :, 0:4], Gtok[:, :, 4:8], 0.0, None, ALU.min, None, [r_gate], [r_gate])
                TS("dve", EB[:, :, 4:8], Gtok[:, :, 12:16], 0.0, None, ALU.min, None, [r_gate], [r_gate])
                TT("dve", LF[:], EB[:], LF[:], ALU.subtract, [r_gate], [r_gate])
                pi = next_ps()
                pbv = psb[pi][:, 0:NJ * 8].rearrange("p (j g) -> p j g", g=8)
                for j in range(NJ):
                    MM(pbv[:, j, 0:4], umask_f[:], LF[:, j, 0:4], True, True, [r_const, r_gate], [r_ps[pi]])
                    MM(pbv[:, j, 4:8], lmask_f[:], LF[:, j, 4:8], True, True, [r_const, r_gate], [r_ps[pi]])
                CP("dve", Bc[:], pbv, [r_ps[pi]], [r_gate])
                pi = next_ps()
                ptv = psb[pi][:, 0:NJ * 8].rearrange("p (j g) -> p j g", g=8)
                MM(psb[pi][:, 0:NJ * 8], ones_f[:], LF[:].rearrange("p j g -> p (j g)"), True, True, [r_const, r_gate], [r_ps[pi]])
                ACT(EBL[:], ptv, AF.Exp, [r_ps[pi]], [r_gate])
                ACT(EB[:], Bc[:], AF.Exp, [r_gate], [r_gate])
                TT("dve", CM[:], CM[:], Bc[:], ALU.subtract, [r_gate], [r_gate])
                TT("dve", ECL[:], CM[:], ptv, ALU.add, [r_gate, r_ps[pi]], [r_gate])
                ACT(ECL[:], ECL[:], AF.Exp, [r_gate], [r_gate])
                P.barrier()

                MS("dve", vaug[:, :, 128:129], 1.0, [r_v])
                MS("dve", zraw, 0.0, [r_zraw])

                def head_qk(h):
                    for qk in range(2):
                        dst, r_dst = (qT, r_q) if qk == 0 else (kT, r_k)
                        ci = qk * 4 + h
                        DMA("pool", wqk_raw, winc[l, ci], [], [r_wqk])
                        DMA("sp", wtapc, wqkp[l, ci], [], [r_wtap])
                        if qk == 1:
                            TS("dve", wtapc, wtapc, HD ** -0.5, None, ALU.mult, None, [r_wtap], [r_wtap])
                        for t in range(3):
                            TS("dve", dg3[:, t, :], ident_f[:], wtapc[:, t:t + 1], None, ALU.mult, None,
                               [r_const, r_wtap], [r_w3])

                        def zpos(a_):
                            return 1 + a_ if a_ < CTX else 3 + a_
                        for (a, b, w) in TL_all:
                            n = b - a
                            pi = next_ps()
                            for kc in range(KC):
                                MM(psb[pi][:, 0:n], wqk_raw[:, kc, :], hx[:, kc, a:b], kc == 0, kc == KC - 1,
                                   [r_wqk] + trs(r_hx, kc, a, b), [r_ps[pi]])
                            ACT(zraw[:, zpos(a):zpos(a) + n], psb[pi][:, 0:n], AF.Copy, [r_ps[pi]], [r_zraw])
                        for (a, b, w) in TL_all:
                            n = b - a
                            pi = next_ps()
                            for t in range(3):
                                MM(psb[pi][:, 0:n], dg3[:, t, :], zraw[:, zpos(a) + t - 1:zpos(a) + t - 1 + n], t == 0, t == 2,
                                   [r_w3, r_zraw], [r_ps[pi]])
                            ACT(dst[:, a:b], psb[pi][:, 0:n], AF.Copy, [r_ps[pi]], [r_dst])
                def head_vo(h):
                    DMA("pool", wvo_s, wvo[l, h], [], [r_wvo])
                    for j in range(NJ):
                        pi = next_ps()
                        for kc in range(KC):
                            MM(psb[pi][:, 0:256], hx[:, kc, j * 128:(j + 1) * 128], wvo_s[:, kc, :], kc == 0, kc == KC - 1,
                               [r_wvo] + trs(r_hx, kc, j * 128, (j + 1) * 128), [r_ps[pi]])
                        CP("dve", vaug[:, j, 0:128], psb[pi][:, 0:128], [r_ps[pi]], [r_v])
                        ACT(sgo[:, j, :], psb[pi][:, 128:256], AF.Sigmoid, [r_ps[pi]], [r_sg])
                        TT("dve", sgo[:, j, :], sgo[:, j, :], gmn_s[:, h * 128:(h + 1) * 128], ALU.mult, [r_sg, r_gate], [r_sg])
                def head_loop(h):
                    MS("dve", hsum, 0.0, [r_hs])
                    for d_ in range(2):
                        MS("dve", Cst[d_], 0.0, [r_C[d_]])
                        MS("dve", Cbf[d_], 0.0, [r_Cb[d_]])
                    order = [list(range(NJ)), [1, 0] + list(range(NJ - 1, 1, -1))]
                    t0f = tmpf[0]
                    t1b = tmpf[1][:, :].bitcast(BF16)
                    dgb2 = [dgb, [dgq[0], dgq[1]]]
                    sT2 = [sTt, [t1b[:, 256:384], t1b[:, 384:512]]]
                    kts2 = [kts, [t1b[:, 512:640], t1b[:, 640:768]]]
                    Et2 = [Et, [t1b[:, 768:896], t1b[:, 896:1024]]]
                    ndi = [t0f[:, 0:130], t0f[:, 130:260]]
                    ndi_den = t0f[:, 0:260].rearrange("p (d e) -> p d e", e=130)[:, :, 128:129]
                    rden_j = t0f[:, 392:394].unsqueeze(2)
                    r_rdj = R()
                    r_ndi = [R(), R()]
                    t0b = t0f[:, 260:390].bitcast(BF16)
                    Cbf2 = [Cbf, [t0b[:, 0:130], t0b[:, 130:260]]]
                    r_Cb2 = [r_Cb, [R(), R()]]
                    rr = lambda: [[R(), R()], [R(), R()]]
                    r_dgb2, r_sT2, r_kts2, r_Et2 = rr(), rr(), rr(), rr()

                    def info(step, d_):
                        j = order[d_][step]
                        return j, step % 2, d_ * 4 + h, (need_ctx or j >= 2), step == NJ - 1

                    ps_hold = {}

                    def a1(step, d_):
                        j, bs, col, need_out, is_last = info(step, d_)
                        if need_out:
                            TS("dve", dgb2[bs][d_], ident_f[:], Bc[:, j, col:col + 1], None, ALU.mult, None,
                               [r_const, r_gate], [r_dgb2[bs][d_]])
                        yield

                    def a2a(step, d_):
                        j, bs, col, need_out, is_last = info(step, d_)
                        c0, c1 = j * 128, (j + 1) * 128
                        if need_out:
                            pd_ = d_
                            MM(psb[pd_][:, 0:128], ones_f[:], dgb2[bs][d_], True, False, [r_const, r_dgb2[bs][d_]], [r_ps[pd_]])
                            MM(psb[pd_][:, 0:128], ident_b[:], (negm_f if d_ == 0 else negm_b)[:], False, True,
                               [r_const], [r_ps[pd_]])
                            yield
                            ACT(Et2[bs][d_], psb[pd_][:, 0:128], AF.Exp, [r_ps[pd_], r_gate], [r_Et2[bs][d_]],
                                bias=CM[:, j, col:col + 1], scale=1.0)
                            yield
                            p_s = 2 + d_
                            MM(psb[p_s][:, 0:128], kT[:, c0:c1], qT[:, c0:c1], True, True, [r_k, r_q], [r_ps[p_s]])
                            yield
                        if not is_last:
                            p_t = next_ps()
                            ptb = psb[p_t][:, 0:64].bitcast(BF16)
                            TR(ptb, kT[:, c0:c1], ident_b[:], [r_k, r_const], [r_ps[p_t]])
                            yield
                            ACT(kts2[bs][d_], ptb, AF.Copy, [r_ps[p_t], r_gate], [r_kts2[bs][d_]], scale=ECL[:, j, col:col + 1])
                            yield

                    def a2b(step, d_):
                        j, bs, col, need_out, is_last = info(step, d_)
                        if need_out:
                            p_s = 2 + d_
                            TT("dve", sT2[bs][d_], psb[p_s][:, 0:128], Et2[bs][d_], ALU.mult,
                               [r_ps[p_s], r_Et2[bs][d_]], [r_sT2[bs][d_]])
                        yield

                    def stage_b(step, d_):
                        j, bs, col, need_out, is_last = info(step, d_)
                        cur, nxt = Cbf2[step % 2][d_], Cbf2[(step + 1) % 2][d_]
                        r_cur, r_nxt = r_Cb2[step % 2][d_], r_Cb2[(step + 1) % 2][d_]
                        if not is_last:
                            p_u = next_ps()
                            MM(psb[p_u][:, 0:129], kts2[bs][d_], vaug[:, j, 0:129], True, True, [r_kts2[bs][d_], r_v], [r_ps[p_u]])
                            yield
                            STT("dve", Cst[d_][:, 0:129], Cst[d_][:, 0:129], EBL[:, j, col:col + 1], psb[p_u][:, 0:129],
                                ALU.mult, ALU.add, [r_C[d_], r_gate, r_ps[p_u]], [r_C[d_]])
                            yield
                            ACT(nxt[:, 0:129], Cst[d_][:, 0:129], AF.Copy, [r_C[d_]], [r_nxt])
                            yield

                    def stage_bo(step, d_):
                        j, bs, col, need_out, is_last = info(step, d_)
                        cur, r_cur = Cbf2[step % 2][d_], r_Cb2[step % 2][d_]
                        if need_out:
                            c0, c1 = j * 128, (j + 1) * 128
                            p_i = next_ps()
                            MM(psb[p_i][:, 0:129], qT[:, c0:c1], cur[:, 0:129], True, True, [r_q, r_cur], [r_ps[p_i]])
                            p_n = next_ps()
                            MM(psb[p_n][:, 0:129], sT2[bs][d_], vaug[:, j, 0:129], True, True, [r_sT2[bs][d_], r_v], [r_ps[p_n]])
                            yield
                            ACT(ndi[d_][:, 0:129], psb[p_i][:, 0:129], AF.Copy, [r_ps[p_i], r_gate], [r_ndi[d_]], scale=EB[:, j, col:col + 1])
                            yield
                            TT("dve", ndi[d_][:, 0:129], ndi[d_][:, 0:129], psb[p_n][:, 0:129], ALU.add, [r_ndi[d_], r_ps[p_n]], [r_ndi[d_]])
                            yield

                    def den_ops(step):
                        j, bs, col, need_out, is_last = info(step, 0)
                        if need_out:
                            STT("dve", rden_j, ndi_den, -1.0, ndi_den, ALU.mult, ALU.max, [r_ndi[0], r_ndi[1]], [r_rdj])
                            TS("dve", rden_j, rden_j, 1.0, None, ALU.max, None, [r_rdj], [r_rdj])
                            RECIP(rden_j, rden_j, [r_rdj], [r_rdj])

                    def stage_b2(step, d_):
                        j, bs, col, need_out, is_last = info(step, d_)
                        if need_out:
                            STT("dve", hsum[:, j, :], ndi[d_][:, 0:128], t0f[:, 392 + d_:393 + d_], hsum[:, j, :], ALU.mult, ALU.add,
                                [r_ndi[d_], r_rdj, r_hs], [r_hs])
                        yield

                    def interleave(*gens):
                        gens = list(gens)
                        while gens:
                            for g in list(gens):
                                try:
                                    next(g)
                                except StopIteration:
                                    gens.remove(g)

                    for bnk in range(4):
                        ps_reserved.add(bnk)
                    interleave(a1(0, 0), a1(0, 1))
                    interleave(a2a(0, 0), a2a(0, 1))
                    interleave(a2b(0, 0), a2b(0, 1))
                    interleave(a1(1, 0), a1(1, 1))
                    for step in range(NJ):
                        if step + 2 < NJ:
                            interleave(a1(step + 2, 0), a1(step + 2, 1))
                        if step + 1 < NJ:
                            interleave(a2a(step + 1, 0), a2a(step + 1, 1))
                        interleave(stage_b(step, 0), stage_b(step, 1))
                        if step + 1 < NJ:
                            interleave(a2b(step + 1, 0), a2b(step + 1, 1))
                        interleave(stage_bo(step, 0), stage_bo(step, 1))
                        den_ops(step)
                        interleave(stage_b2(step, 0), stage_b2(step, 1))
                    for bnk in range(4):
                        ps_reserved.discard(bnk)
                def head_out(h):
                    nj = NJ - j0
                    hv = hsum[:, j0:NJ, :]
                    sqv = ym[:, h, j0 * 128:NT].rearrange("p (j e) -> p j e", e=128)
                    ymh_res = trs(r_ym, h, j0 * 128, NT) + ([r_wvo] if h == 3 else [])
                    P.op("dve", (lambda hv=hv, nj=nj: lambda e: e.tensor_reduce(out=st1[:, 0:nj], in_=hv, axis=AX.X, op=ALU.add))(),
                         [r_hs], [r_st])
                    TT("dve", sqv, hv, hv, ALU.mult, [r_hs], ymh_res)
                    P.op("dve", (lambda sqv=sqv, nj=nj: lambda e: e.tensor_reduce(out=st1[:, NJ:NJ + nj], in_=sqv, axis=AX.X, op=ALU.add))(),
                         ymh_res, [r_st])
                    mean_ = st2[:, 0:nj]
                    var_ = st2[:, NJ:NJ + nj]
                    TS("dve", mean_, st1[:, 0:nj], 1.0 / HD, None, ALU.mult, None, [r_st], [r_st])
                    TT("dve", var_, mean_, mean_, ALU.mult, [r_st], [r_st])
                    STT("dve", var_, st1[:, NJ:NJ + nj], 1.0 / HD, var_, ALU.mult, ALU.subtract, [r_st], [r_st])
                    ACT(var_, var_, AF.Sqrt, [r_st, r_const], [r_st], bias=cst[:, 1:2], scale=1.0)
                    RECIP(var_, var_, [r_st], [r_st])
                    TT("dve", hv, hv, mean_.unsqueeze(2).to_broadcast([128, nj, 128]), ALU.subtract, [r_hs, r_st], [r_hs])
                    TT("dve", hv, hv, var_.unsqueeze(2).to_broadcast([128, nj, 128]), ALU.mult, [r_hs, r_st], [r_hs])
                    TT("dve", sgo[:, j0:NJ, :], hv, sgo[:, j0:NJ, :], ALU.mult, [r_hs, r_sg], [r_sg])
                    for jg in range(j0, NJ, 4):
                        p_t = next_ps()
                        ptb = psb[p_t][:, 0:256].bitcast(BF16)
                        jn = min(4, NJ - jg)
                        for jj in range(jn):
                            TR(ptb[:, jj * 128:(jj + 1) * 128], sgo[:, jg + jj, :], ident_b[:], [r_sg, r_const], [r_ps[p_t]])
                        ACT(ym[:, h, jg * 128:(jg + jn) * 128], ptb[:, 0:jn * 128], AF.Copy, [r_ps[p_t]],
                            trs(r_ym, h, jg * 128, (jg + jn) * 128) + ([r_wvo] if h == 3 else []))

                head_qk(0)
                head_vo(0)
                for h in range(NH):
                    head_loop(h)
                    if h + 1 < NH:
                        head_qk(h + 1)
                    head_out(h)
                    if h + 1 < NH:
                        head_vo(h + 1)
                if l == 0:
                    dump("d_ym", lambda kc: ym[:, kc, :], lambda kc: trs(r_ym, kc, 0, NT))

                P.barrier()
                maybe_stop(10 * l + 4)
                o_wo = 4096
                wo_s = arena(o_wo, KC * KC * 128).rearrange("p (o k c) -> p o k c", o=KC, k=KC)
                r_wo = R()
                obuf = arena(o_wo + 8192, KC * 512, F32).rearrange("p (o t) -> p o t", t=512)
                r_ob = [R() for _ in range(KC)]
                DMA("pool", wo_s.rearrange("p o k c -> p (o k c)"), woutc[l].rearrange("p o k c -> p (o k c)"), [],
                    [r_wo])
                if need_ctx:
                    supers = [[(0, 256, 1), (256, 768, 0)], [(768, 1280, 0), (1280, 1536, 0)], [(1536, 2048, 0), (2048, 2304, 0)]]
                else:
                    supers = [[(256, 768, 0), (768, 1024, 0)], [(1024, 1536, 0), (1536, 1792, 0)], [(1792, 2304, 0)]]
                sup0_todo = list(supers[0])
                for (a, b, w) in TL:
                    n = b - a
                    for oc in range(KC):
                        pi = next_ps()
                        for kc in range(KC):
                            MM(psb[pi][:, 0:n], wo_s[:, oc, kc, :], ym[:, kc, a:b], kc == 0, kc == KC - 1,
                               [r_wo] + trs(r_ym, kc, a, b), [r_ps[pi]])
                        ACT(obuf[:, oc, 0:n], psb[pi][:, 0:n], AF.Copy, [r_ps[pi]], [r_ob[oc]])
                    resid_update([(a, b, w)], lambda kc, a_, b_: obuf[:, kc, 0:b_ - a_], lambda kc, a_, b_: [r_ob[kc]], G1)
                    while sup0_todo and sup0_todo[0][1] <= b:
                        prenorm([sup0_todo.pop(0)], A2, 24, l)
                if l == 0:
                    dump("d_x1", lambda kc: xs[:, kc, :], lambda kc: trs(r_xs, kc, 0, NT))

                P.barrier()
                maybe_stop(10 * l + 5)
                hT = scr[:, 0:24576].rearrange("p (f t) -> p f t", t=768)
                r_hT = [R() for _ in range(32)]
                ob2 = scr[:, 24576:30720].rearrange("p (o t) -> p o t", t=768)
                r_ob2 = [R() for _ in range(KC)]
                w1b = [scr[:, 30720 + i * 2048:30720 + (i + 1) * 2048].rearrange("p (g k c) -> p g k c", g=2, k=KC) for i in range(2)]
                w2b = [scr[:, 34816 + i * 4096:34816 + (i + 1) * 4096].rearrange("p (f c) -> p f c", c=128) for i in range(2)]
                r_w1b = [R(), R()]
                r_w2b = [R(), R()]
                n1 = 0
                n2 = 0
                def gen_mlp_resid(sup, ssb, a0):
                    for si_, (a, b, w) in enumerate(sup):
                        n = b - a
                        ACT(rs[:, 0:n], psb[ssb[si_]][:, 0:n], AF.Sqrt, [r_ps[ssb[si_]], r_const], [r_rs], bias=cst[:, 0:1], scale=1.0)
                        RECIP(rs[:, 0:n], rs[:, 0:n], [r_rs], [r_rs])
                        ps_reserved.discard(ssb[si_])
                        yield
                        for kc in range(KC):
                            ti = next_tf()
                            TT("dve", tmpf[ti][:, 0:n], ob2[:, kc, a - a0:b - a0], rs[:, 0:n], ALU.mult,
                               [r_ob2[kc], r_rs], [r_tmpf[ti]])
                            STT("dve", xs[:, kc, a:b], tmpf[ti][:, 0:n], G2[:, kc, w:w + 1], xs[:, kc, a:b], ALU.mult, ALU.add,
                                [r_tmpf[ti], r_lay] + trs(r_xs, kc, a, b), trs(r_xs, kc, a, b))
                            if l == DEPTH - 1:
                                DMA("sp", outT[kc][:, a - CTX:b - CTX], xs[:, kc, a:b], trs(r_xs, kc, a, b), [])
                            yield

                assert not sup0_todo
                pending = None
                for isup, sup in enumerate(supers):
                    a0 = sup[0][0]
                    for g in range(16):
                        i = n1 % 2
                        n1 += 1
                        DMA("pool", w1b[i].rearrange("p g k c -> p (g k c)"), w1c[l, g].rearrange("p g k c -> p (g k c)"), [],
                            [r_w1b[i]])
                        for f2 in range(2):
                            f = g * 2 + f2
                            for (a, b, w) in sup:
                                n = b - a
                                pi = next_ps()
                                for kc in range(KC):
                                    MM(psb[pi][:, 0:n], w1b[i][:, f2, kc, :], hx[:, kc, a:b], kc == 0, kc == KC - 1,
                                       [r_w1b[i]] + trs(r_hx, kc, a, b), [r_ps[pi]])
                                ti = next_tf()
                                ACT(tmpf[ti][:, 0:n], psb[pi][:, 0:n], AF.Relu, [r_ps[pi]], [r_tmpf[ti]])
                                TT("dve", hT[:, f, a - a0:b - a0], tmpf[ti][:, 0:n], tmpf[ti][:, 0:n], ALU.mult, [r_tmpf[ti]], [r_hT[f]])
                        pull(pending, 2)
                    drain(pending)
                    pending = None
                    gp = gen_prenorm(supers[isup + 1], A2, 24, l) if isup + 1 < len(supers) else None
                    ssb = []
                    for _ in sup:
                        pss = next_ps()
                        ps_reserved.add(pss)
                        ssb.append(pss)
                    for oc in range(KC):
                        i = n2 % 2
                        n2 += 1
                        DMA("pool", w2b[i].rearrange("p f c -> p (f c)"), w2c[l, oc].rearrange("p f c -> p (f c)"), [],
                            [r_w2b[i]])
                        for si_, (a, b, w) in enumerate(sup):
                            n = b - a
                            pi = next_ps()
                            for f in range(32):
                                MM(psb[pi][:, 0:n], w2b[i][:, f, :], hT[:, f, a - a0:b - a0], f == 0, f == 31,
                                   [r_w2b[i], r_hT[f]], [r_ps[pi]])
                            ACT(ob2[:, oc, a - a0:b - a0], psb[pi][:, 0:n], AF.Copy, [r_ps[pi]], [r_ob2[oc]])
                            sq_i = next_sq()
                            ACT(sqb[sq_i][:, 0:n], psb[pi][:, 0:n], AF.Square, [r_ps[pi]], [r_sqb[sq_i]])
                            MM(psb[ssb[si_]][:, 0:n], ones_b[:], sqb[sq_i][:, 0:n], oc == 0, oc == KC - 1,
                               [r_sqb[sq_i], r_const], [r_ps[ssb[si_]]])
                            pull(gp, 3)
                    drain(gp)
                    pending = gen_mlp_resid(sup, ssb, a0)
                drain(pending)
                if l == 0:
                    dump("d_x2", lambda kc: xs[:, kc, :], lambda kc: trs(r_xs, kc, 0, NT))
                P.barrier()

        except _Stop:
            pass
        P.emit()
    return nc


_CACHE = {}


def _consts():
    if "c" in _CACHE:
        return _CACHE["c"]
    c = {}
    c["ident"] = np.eye(128, dtype=np.float32)
    s = np.arange(128)
    c["umask"] = (s[:, None] <= s[None, :]).astype(np.float32)
    c["lmask"] = (s[:, None] >= s[None, :]).astype(np.float32)
    k = np.arange(64)
    ang = 2.0 * np.pi * np.outer(k, k) / 64.0
    cs = np.zeros((128, 256), np.float64)
    for hh in range(2):
        cs[hh * 64:(hh + 1) * 64, hh * 64:(hh + 1) * 64] = np.cos(ang) / 8.0
        cs[hh * 64:(hh + 1) * 64, 128 + hh * 64:128 + (hh + 1) * 64] = np.sin(ang) / 8.0
    c["cs64"] = cs.astype(np.float32)
    t = np.arange(SEQ, dtype=np.int64)
    ph = (np.outer(t, t) % SEQ).astype(np.float64) * (2.0 * np.pi / SEQ)
    dc = (np.cos(ph) / math.sqrt(SEQ)).astype(np.float32)
    ds = (-np.sin(ph) / math.sqrt(SEQ)).astype(np.float32)
    both = np.stack([dc, ds], 0)
    both = both.reshape(2, 4, 4, 128, 4, 512)
    c["dftx"] = np.ascontiguousarray(both.transpose(4, 1, 3, 0, 2, 5)).astype(ml_dtypes.bfloat16)
    t = np.arange(CTX, dtype=np.int64)
    ph = (np.outer(t, t) % CTX).astype(np.float64) * (2.0 * np.pi / CTX)
    both = np.stack([np.cos(ph), -np.sin(ph)], 0) / math.sqrt(CTX)
    both = both.reshape(2, 2, 128, CTX)
    c["dftc"] = np.ascontiguousarray(both.transpose(2, 0, 1, 3)).astype(ml_dtypes.bfloat16)
    rows = SEQ // 64
    quarter = D // 4
    freq = np.exp(-math.log(10000.0) * np.arange(quarter, dtype=np.float32) / quarter).astype(np.float32)
    r = np.broadcast_to(np.arange(rows, dtype=np.float32)[:, None], (rows, 64)).reshape(-1)
    col = np.broadcast_to(np.arange(64, dtype=np.float32)[None, :], (rows, 64)).reshape(-1)
    ar = r[:, None] * freq
    ac = col[:, None] * freq
    pos = np.concatenate([np.sin(ar), np.cos(ar), np.sin(ac), np.cos(ac)], axis=-1).astype(np.float32)
    c["posT"] = np.ascontiguousarray(pos.T).reshape(KC, 128, SEQ)
    _CACHE["c"] = c
    return c


def _chunk_w(w, cols):
    return np.ascontiguousarray(w[:, cols].reshape(KC, 128, -1).transpose(1, 0, 2))


def _prep_shared(inp):
    f = np.float32
    L = DEPTH
    w_in = np.asarray(inp["w_in"], f)
    sh = {}
    wada = np.asarray(inp["w_ada"], f)
    sh["wada"] = np.ascontiguousarray(wada.reshape(L, KC, 128, 12, 512).transpose(0, 3, 2, 1, 4))
    sh["bada"] = np.ascontiguousarray(np.asarray(inp["b_ada"], f).reshape(L, 48, 128).transpose(0, 2, 1))
    gs = np.stack([np.asarray(inp[k], f) for k in ("g_pre_mix", "g_post_mix", "g_pre_mlp", "g_post_mlp")], 1)
    sh["gvec"] = np.ascontiguousarray(gs.reshape(L, 4, KC, 128).transpose(0, 3, 1, 2))
    offs = [Q_OFF + 128 * i for i in range(4)] + [K_OFF + 128 * i for i in range(4)] + \
           [F_OFF, F_OFF + 128, CA_OFF, CA_OFF + 128, CG_OFF, CG_OFF + 128]
    sh["winc"] = np.stack([np.stack([_chunk_w(w_in[l], np.arange(o, o + 128)) for o in offs]) for l in range(L)])
    sh["wvo"] = np.stack([np.stack([_chunk_w(w_in[l], np.concatenate([np.arange(V_OFF + 128 * h, V_OFF + 128 * h + 128),
                                                                      np.arange(O_OFF + 128 * h, O_OFF + 128 * h + 128)]))
                                    for h in range(NH)]) for l in range(L)])
    sh["wgate"] = np.stack([_chunk_w(w_in[l], np.arange(G_OFF, G_OFF + 16)) for l in range(L)])
    sh["bgate"] = np.ascontiguousarray(np.asarray(inp["b_gate"], f).reshape(L, 16))
    wqk = np.asarray(inp["w_qk_conv"], f)
    sh["wqkp"] = np.ascontiguousarray(wqk.reshape(L, 3, 8, 128).transpose(0, 2, 3, 1))
    sh["gmn"] = np.ascontiguousarray(np.asarray(inp["g_mlstm_norm"], f))
    wdw = np.asarray(inp["w_dw"], f)
    sh["wdw"] = np.ascontiguousarray(wdw.reshape(L, CONV_K, 2, 128).transpose(0, 3, 2, 1))
    cv = np.stack([np.asarray(inp[k], f) for k in ("b_dw", "g_conv_ln", "b_conv_ln")], 1)
    sh["cvp"] = np.ascontiguousarray(cv.reshape(L, 3, 2, 128).transpose(0, 3, 1, 2))
    wout = np.asarray(inp["w_out"], f)
    sh["woutc"] = np.ascontiguousarray(wout.reshape(L, KC, 128, KC, 128).transpose(0, 2, 3, 1, 4))
    w1 = np.asarray(inp["w_mlp1"], f)
    sh["w1c"] = np.ascontiguousarray(w1.reshape(L, KC, 128, 16, 2, 128).transpose(0, 3, 2, 4, 1, 5))
    w2 = np.asarray(inp["w_mlp2"], f)
    sh["w2c"] = np.ascontiguousarray(w2.reshape(L, 32, 128, KC, 128).transpose(0, 3, 2, 1, 4))
    c = _consts()
    for k in ("ident", "umask", "lmask", "cs64", "dftx", "dftc", "posT"):
        sh[k] = c[k]
    return sh


def make_in_maps(inp, cores):
    sh = _prep_shared(inp)
    x = np.asarray(inp["x"], np.float32)
    ctx = np.asarray(inp["ctx"], np.float32)
    c = np.asarray(inp["c"], np.float32)
    c_ctx = np.asarray(inp["c_ctx"], np.float32)
    maps = []
    for b in cores:
        m = dict(sh)
        m["xT"] = np.ascontiguousarray(x[b].T).reshape(KC, 128, SEQ)
        m["ctxT"] = np.ascontiguousarray(ctx[b].T).reshape(KC, 128, CTX)
        cv = np.stack([c[b], c_ctx], -1)
        m["cvec"] = np.ascontiguousarray(cv.reshape(KC, 128, 2).transpose(1, 0, 2))
        maps.append(m)
    return maps


def kernel(**inputs):
    if "nc" not in _CACHE:
        _CACHE["nc"] = build_program(dbg=False)
    nc = _CACHE["nc"]
    in_maps = make_in_maps(inputs, list(range(NB)))
    res = run_bass_kernel_spmd(nc, in_maps, core_ids=list(range(NB)))
    out = np.empty((NB, SEQ, D), np.float32)
    for b in range(NB):
        oT = np.asarray(res.results[b]["outT"], np.float32).reshape(D, SEQ)
        out[b] = oT.T
    return out
```
